# Optimizing a Trainium2 kernel written in Bass

```python
import jax, jax.numpy as jnp
from jax import lax
import numpy as np

D_MODEL = 1024
BATCH = 8
SEQ = 2048
DEPTH = 1

GLA_HEADS = 4
GLA_DK = 64
GLA_DV = 128
GLA_GATE_RANK = 16
GLA_GATE_NORMALIZER = 16.0
GLA_CHUNK = 64
MLA_HEADS = 8
MLA_NOPE = 64
MLA_ROPE = 32
MLA_QK = MLA_NOPE + MLA_ROPE
MLA_V = 64
MLA_Q_RANK = 256
MLA_KV_RANK = 128
ROPE_BASE = 10000.0
Q_BLOCK = 128
GLA_WIDTH = GLA_HEADS * GLA_DV
MLA_WIDTH = MLA_HEADS * MLA_V
MIX_WIDTH = GLA_WIDTH + MLA_WIDTH
IN_SPLITS = (GLA_HEADS * GLA_DK, GLA_HEADS * GLA_DK, GLA_WIDTH, GLA_WIDTH,
             GLA_GATE_RANK, GLA_GATE_RANK, MLA_Q_RANK, MLA_KV_RANK, MLA_ROPE)
IN_WIDTH = sum(IN_SPLITS)
N_GROUPS = 4
EXPERTS_PER_GROUP = 8
TOP_K_IN_GROUP = 2
D_EXPERT = 256
EPS = 1e-6

kernel_name = 'hybrid_gla_mla_hmoe_encoder_block'


def rms_norm(x, gain):
    xf = x.astype(jnp.float32)
    y = xf * lax.rsqrt(jnp.mean(xf * xf, axis=-1, keepdims=True) + EPS)
    return (y * gain.astype(jnp.float32)).astype(x.dtype)


def gla_chunked(q, k, v, g, strict):
    B, H, S, DK = q.shape
    DV = v.shape[-1]
    C = GLA_CHUNK
    n = S // C
    qc = q.reshape(B, H, n, C, DK)
    kc = k.reshape(B, H, n, C, DK)
    vc = v.reshape(B, H, n, C, DV)
    b = jnp.cumsum(g.reshape(B, H, n, C, DK), axis=3)
    b_ref = b[:, :, :, C // 2 - 1:C // 2, :]
    q_rel = qc * jnp.exp(b - b_ref)
    k_rel = kc * jnp.exp(b_ref - b)
    scores = jnp.einsum('bhnid,bhnjd->bhnij', q_rel, k_rel)
    mask = jnp.tril(jnp.ones((C, C), dtype=bool), k=-1 if strict else 0)
    scores = jnp.where(mask, scores, 0.0)
    o_intra = jnp.einsum('bhnij,bhnjv->bhniv', scores, vc)
    b_last = b[:, :, :, -1:, :]
    kv_chunk = jnp.einsum('bhncd,bhncv->nbhdv', kc * jnp.exp(b_last - b), vc)
    decay_chunk = jnp.moveaxis(jnp.exp(b_last[:, :, :, 0, :]), 2, 0)

    def step(state, inp):
        dec, kv = inp
        return dec[..., None] * state + kv, state

    state0 = jnp.zeros((B, H, DK, DV), dtype=q.dtype)
    _, states = lax.scan(step, state0, (decay_chunk, kv_chunk))
    o_inter = jnp.einsum('bhnid,nbhdv->bhniv', qc * jnp.exp(b), states)
    return (o_intra + o_inter).reshape(B, H, S, DV)


def rope_tables(positions):
    inv_freq = ROPE_BASE ** (-jnp.arange(0, MLA_ROPE, 2, dtype=jnp.float32) / MLA_ROPE)
    ang = positions.astype(jnp.float32)[..., None] * inv_freq
    return jnp.cos(ang)[:, :, None, :], jnp.sin(ang)[:, :, None, :]


def apply_rope(t, cos, sin):
    t1, t2 = jnp.split(t, 2, axis=-1)
    return jnp.concatenate([t1 * cos - t2 * sin, t1 * sin + t2 * cos], axis=-1)


def dense_block_attention(q, k, v):
    B, S, H, Dq = q.shape
    Dv = v.shape[-1]
    n = S // Q_BLOCK
    qb = q.reshape(B, n, Q_BLOCK, H, Dq).transpose(1, 0, 3, 2, 4)
    kh = k.transpose(0, 2, 1, 3).astype(jnp.float32)
    vh = v.transpose(0, 2, 1, 3).astype(jnp.float32)
    scale = Dq ** -0.5

    def one_block(q_blk):
        s = jnp.einsum('bhqd,bhkd->bhqk', q_blk.astype(jnp.float32), kh) * scale
        p = jax.nn.softmax(s, axis=-1)
        return jnp.einsum('bhqk,bhkv->bhqv', p, vh)

    o = lax.map(one_block, qb)
    return o.transpose(1, 0, 3, 2, 4).reshape(B, S, H * Dv)


def hybrid_mixer(h, positions, w_in, gla_gk_fwd_w, gla_gk_fwd_b, gla_gk_bwd_w, gla_gk_bwd_b,
                 gla_out_gain, mla_q_gain, mla_w_qb, mla_kv_gain, mla_w_kvb,
                 q_norm_gain, k_norm_gain, w_out):
    B, S, _ = h.shape
    f32 = jnp.float32
    proj = h @ w_in
    split_idx = [int(i) for i in np.cumsum(IN_SPLITS)[:-1]]
    g_q, g_k, g_v, g_gate, g_lr_f, g_lr_b, m_q_a, m_kv_a, m_k_rope = jnp.split(proj, split_idx, axis=-1)

    def heads(t, d):
        return t.reshape(B, S, GLA_HEADS, d).transpose(0, 2, 1, 3).astype(f32)

    q = heads(g_q, GLA_DK) * GLA_DK ** -0.5
    k = heads(g_k, GLA_DK)
    v = heads(g_v, GLA_DV)
    lg_f = jax.nn.log_sigmoid((g_lr_f @ gla_gk_fwd_w + gla_gk_fwd_b).astype(f32)) / GLA_GATE_NORMALIZER
    lg_b = jax.nn.log_sigmoid((g_lr_b @ gla_gk_bwd_w + gla_gk_bwd_b).astype(f32)) / GLA_GATE_NORMALIZER
    g_f = heads(lg_f, GLA_DK)
    g_b = heads(lg_b, GLA_DK)
    flip = lambda t: t[:, :, ::-1]
    o_fwd = gla_chunked(q, k, v, g_f, False)
    o_bwd = flip(gla_chunked(flip(q), flip(k), flip(v), flip(g_b), True))
    o_gla = (o_fwd + o_bwd).transpose(0, 2, 1, 3)
    o_gla = rms_norm(o_gla, gla_out_gain) * jax.nn.silu(g_gate.reshape(B, S, GLA_HEADS, GLA_DV).astype(f32))
    gla_out = o_gla.reshape(B, S, GLA_WIDTH).astype(h.dtype)

    mq = (rms_norm(m_q_a, mla_q_gain) @ mla_w_qb).reshape(B, S, MLA_HEADS, MLA_QK)
    mkv = (rms_norm(m_kv_a, mla_kv_gain) @ mla_w_kvb).reshape(B, S, MLA_HEADS, MLA_NOPE + MLA_V)
    mk_nope, mv = mkv[..., :MLA_NOPE], mkv[..., MLA_NOPE:]
    mk = jnp.concatenate([mk_nope, jnp.broadcast_to(m_k_rope[:, :, None, :], (B, S, MLA_HEADS, MLA_ROPE))], axis=-1)
    mq = rms_norm(mq, q_norm_gain).astype(f32)
    mk = rms_norm(mk, k_norm_gain).astype(f32)
    cos, sin = rope_tables(positions)
    mq = jnp.concatenate([mq[..., :MLA_NOPE], apply_rope(mq[..., MLA_NOPE:], cos, sin)], axis=-1)
    mk = jnp.concatenate([mk[..., :MLA_NOPE], apply_rope(mk[..., MLA_NOPE:], cos, sin)], axis=-1)
    mla_out = dense_block_attention(mq, mk, mv).astype(h.dtype)

    return jnp.concatenate([gla_out, mla_out], axis=-1) @ w_out


def hierarchical_moe(h, w_router_group, b_router_group, w_router_expert, b_router_expert,
                     w_expert_gate, w_expert_up, w_expert_down):
    B, S, D = h.shape
    f32 = jnp.float32
    t = h.reshape(B * S, D)
    p_group = jax.nn.softmax((t @ w_router_group).astype(f32) + b_router_group.astype(f32), axis=-1)
    g_idx = jnp.argmax(p_group, axis=-1)
    g_onehot = jax.nn.one_hot(g_idx, N_GROUPS, dtype=f32)
    p_top_group = jnp.sum(p_group * g_onehot, axis=-1)
    logits_e = ((t @ w_router_expert).astype(f32) + b_router_expert.astype(f32)).reshape(-1, N_GROUPS, EXPERTS_PER_GROUP)
    logits_sel = jnp.take_along_axis(logits_e, g_idx[:, None, None], axis=1)[:, 0]
    p_e = jax.nn.softmax(logits_sel, axis=-1)
    top_v, top_i = lax.top_k(p_e, TOP_K_IN_GROUP)
    top_v = top_v / jnp.sum(top_v, axis=-1, keepdims=True)
    w_in_group = jnp.sum(jax.nn.one_hot(top_i, EXPERTS_PER_GROUP, dtype=f32) * top_v[..., None], axis=1)
    gate = g_onehot[:, :, None] * (p_top_group[:, None] * w_in_group)[:, None, :]
    out = jnp.zeros_like(t)
    for gi in range(N_GROUPS):
        hid = jax.nn.silu(jnp.einsum('td,edf->tef', t, w_expert_gate[gi])) * jnp.einsum('td,edf->tef', t, w_expert_up[gi])
        hid = hid * gate[:, gi, :, None].astype(t.dtype)
        out = out + jnp.einsum('tef,efd->td', hid, w_expert_down[gi])
    return out.reshape(B, S, D)


def setup_inputs(seed: int = 0) -> dict:
    key = jax.random.key(seed)
    ks = jax.random.split(key, 24)
    L = DEPTH
    f32 = jnp.float32

    def nrm(k, shape, fan_in):
        return jax.random.normal(k, shape, f32) * fan_in ** -0.5

    def gain(k, shape):
        return 1.0 + 0.05 * jax.random.normal(k, shape, f32)

    def bias(k, shape):
        return 0.01 * jax.random.normal(k, shape, f32)

    return {
        'x': jax.random.normal(ks[0], (BATCH, SEQ, D_MODEL), f32),
        'positions': jnp.broadcast_to(jnp.arange(SEQ, dtype=jnp.int32)[None, :], (BATCH, SEQ)),
        'norm1_gain': gain(ks[1], (L, D_MODEL)),
        'w_in': nrm(ks[2], (L, D_MODEL, IN_WIDTH), D_MODEL),
        'gla_gk_fwd_w': nrm(ks[3], (L, GLA_GATE_RANK, GLA_HEADS * GLA_DK), GLA_GATE_RANK),
        'gla_gk_fwd_b': bias(ks[4], (L, GLA_HEADS * GLA_DK)),
        'gla_gk_bwd_w': nrm(ks[5], (L, GLA_GATE_RANK, GLA_HEADS * GLA_DK), GLA_GATE_RANK),
        'gla_gk_bwd_b': bias(ks[6], (L, GLA_HEADS * GLA_DK)),
        'gla_out_gain': gain(ks[7], (L, GLA_DV)),
        'mla_q_gain': gain(ks[8], (L, MLA_Q_RANK)),
        'mla_w_qb': nrm(ks[9], (L, MLA_Q_RANK, MLA_HEADS * MLA_QK), MLA_Q_RANK),
        'mla_kv_gain': gain(ks[10], (L, MLA_KV_RANK)),
        'mla_w_kvb': nrm(ks[11], (L, MLA_KV_RANK, MLA_HEADS * (MLA_NOPE + MLA_V)), MLA_KV_RANK),
        'q_norm_gain': gain(ks[12], (L, MLA_QK)),
        'k_norm_gain': gain(ks[13], (L, MLA_QK)),
        'w_out': nrm(ks[14], (L, MIX_WIDTH, D_MODEL), MIX_WIDTH),
        'norm2_gain': gain(ks[15], (L, D_MODEL)),
        'w_router_group': nrm(ks[16], (L, D_MODEL, N_GROUPS), D_MODEL),
        'b_router_group': bias(ks[17], (L, N_GROUPS)),
        'w_router_expert': nrm(ks[18], (L, D_MODEL, N_GROUPS * EXPERTS_PER_GROUP), D_MODEL),
        'b_router_expert': bias(ks[19], (L, N_GROUPS * EXPERTS_PER_GROUP)),
        'w_expert_gate': nrm(ks[20], (L, N_GROUPS, EXPERTS_PER_GROUP, D_MODEL, D_EXPERT), D_MODEL),
        'w_expert_up': nrm(ks[21], (L, N_GROUPS, EXPERTS_PER_GROUP, D_MODEL, D_EXPERT), D_MODEL),
        'w_expert_down': nrm(ks[22], (L, N_GROUPS, EXPERTS_PER_GROUP, D_EXPERT, D_MODEL), D_EXPERT),
    }


def reference(x, positions, norm1_gain, w_in, gla_gk_fwd_w, gla_gk_fwd_b, gla_gk_bwd_w, gla_gk_bwd_b,
              gla_out_gain, mla_q_gain, mla_w_qb, mla_kv_gain, mla_w_kvb, q_norm_gain, k_norm_gain,
              w_out, norm2_gain, w_router_group, b_router_group, w_router_expert, b_router_expert,
              w_expert_gate, w_expert_up, w_expert_down):
    for l in range(DEPTH):
        h = rms_norm(x, norm1_gain[l])
        x = x + hybrid_mixer(h, positions, w_in[l], gla_gk_fwd_w[l], gla_gk_fwd_b[l], gla_gk_bwd_w[l],
                             gla_gk_bwd_b[l], gla_out_gain[l], mla_q_gain[l], mla_w_qb[l], mla_kv_gain[l],
                             mla_w_kvb[l], q_norm_gain[l], k_norm_gain[l], w_out[l])
        h = rms_norm(x, norm2_gain[l])
        x = x + hierarchical_moe(h, w_router_group[l], b_router_group[l], w_router_expert[l],
                                 b_router_expert[l], w_expert_gate[l], w_expert_up[l], w_expert_down[l])
    return x
```

```python
import contextlib
import math
import numpy as np
import ml_dtypes
import concourse.bass as bass
import concourse.mybir as mybir
from concourse.bass_utils import run_bass_kernel_spmd

F32 = mybir.dt.float32
BF16 = mybir.dt.bfloat16
I32 = mybir.dt.int32
ALU = mybir.AluOpType
AF = mybir.ActivationFunctionType
AX = mybir.AxisListType

S_LEN = 2048
D = 1024
NT = 16
EPS = 1e-6
PI = math.pi


class T:
    __slots__ = ("name", "w", "r")

    def __init__(self, name):
        self.name = name
        self.w = None
        self.r = []


class Op:
    __slots__ = ("eng", "fn", "deps", "signal", "sig", "dma", "dsem", "dval")


class Sched:
    ENGS = ("pe", "act", "dve", "pool", "sp")

    def __init__(self, nc, n_dma_sems=12):
        self.nc = nc
        self.ops = {e: [] for e in self.ENGS}
        self.n_dma_sems = n_dma_sems
        self.dma_count = {e: 0 for e in self.ENGS}
        self.tiles = {}
        self.pending = {e: [] for e in self.ENGS}
        self.dma_since_barrier = []
        self.stopped = False

    def t(self, name):
        if name not in self.tiles:
            self.tiles[name] = T(name)
        return self.tiles[name]

    def _tl(self, lst):
        out = []
        for x in lst:
            if isinstance(x, str):
                out.append(self.t(x))
            elif isinstance(x, (list, tuple)):
                out.extend(self._tl(x))
            elif x is not None:
                out.append(x)
        return out

    def barrier(self):
        if self.stopped:
            return
        lasts = []
        for e in self.ENGS:
            for o in reversed(self.ops[e]):
                if not o.dma:
                    lasts.append(o)
                    break
        lasts.extend(self.dma_since_barrier)
        self.dma_since_barrier = []
        for e in self.ENGS:
            self.pending[e] = list(lasts)

    def op(self, eng, fn, reads=(), writes=(), dma=False):
        if self.stopped:
            return None
        o = Op()
        o.eng = eng
        o.fn = fn
        o.dma = dma
        o.signal = False
        o.sig = 0
        deps = {}
        reads = self._tl(reads)
        writes = self._tl(writes)
        for t in reads:
            if t.w is not None:
                deps[id(t.w)] = (t.w, "raw")
            if t.name[0] == "P" and t.name[1:].isdigit():
                for r in t.r:
                    if id(r) not in deps and r.eng != eng:
                        deps[id(r)] = (r, "war")
        for t in writes:
            if t.w is not None and id(t.w) not in deps:
                deps[id(t.w)] = (t.w, "waw")
            for r in t.r:
                if id(r) not in deps:
                    deps[id(r)] = (r, "war")
        if self.pending[eng]:
            for p in self.pending[eng]:
                deps[id(p)] = (p, "raw")
            self.pending[eng] = []
        dl = []
        for p, kind in deps.values():
            if p.eng == eng and not p.dma:
                if eng == "pe":
                    continue
                if kind != "raw" and not dma:
                    continue
            dl.append(p)
        o.deps = dl
        for p in dl:
            p.signal = True
        for t in reads:
            if not dma:
                t.r = [r for r in t.r if r.dma or r.eng != eng]
            t.r.append(o)
        for t in writes:
            t.w = o
            t.r = []
        if dma:
            i = self.dma_count[eng]
            self.dma_count[eng] += 1
            o.dsem = i % self.n_dma_sems
            o.dval = 16 * (i // self.n_dma_sems + 1)
            self.dma_since_barrier.append(o)
        self.ops[eng].append(o)
        return o

    def emit(self, es, final_wait_ops=()):
        nc = self.nc
        sems = {e: es.enter_context(nc.semaphore("s_" + e)) for e in self.ENGS}
        dsems = {e: [es.enter_context(nc.semaphore("d_%s_%d" % (e, i)))
                     for i in range(self.n_dma_sems)]
                 for e in self.ENGS if self.dma_count[e] > 0}
        for e in self.ENGS:
            c = 0
            for o in self.ops[e]:
                if o.signal and not o.dma:
                    c += 1
                    o.sig = c
        block = es.enter_context(nc.Block())
        eng_obj = {"pe": block.tensor, "act": block.scalar, "dve": block.vector,
                   "pool": block.gpsimd, "sp": block.sync}
        for e in self.ENGS:
            ops = self.ops[e]
            if not ops:
                continue

            def body(engine, e=e, ops=ops):
                waited = {}

                def wait(sem, key, val):
                    if waited.get(key, 0) >= val:
                        return
                    waited[key] = val
                    engine.wait_ge(sem, val)

                for o in ops:
                    for p in o.deps:
                        if p.dma:
                            wait(dsems[p.eng][p.dsem], ("d", p.eng, p.dsem), p.dval)
                        else:
                            wait(sems[p.eng], ("c", p.eng), p.sig)
                    if o.dma and o.dval > 16:
                        wait(dsems[e][o.dsem], ("d", e, o.dsem), o.dval - 16)
                    ins = o.fn(engine)
                    if o.dma:
                        ins.then_inc(dsems[e][o.dsem], 16)
                    elif o.signal:
                        ins.then_inc(sems[e], 1)
                if e == "sp":
                    for o in final_wait_ops:
                        if o is None:
                            continue
                        wait(dsems[o.eng][o.dsem], ("d", o.eng, o.dsem), o.dval)

            eng_obj[e](body)


SB_BASE = 16640
SB_END = 229376


class Alloc:
    def __init__(self, nc):
        self.nc = nc
        self.off = SB_BASE
        self.n = 0

    def mark(self):
        return self.off

    def reset(self, m):
        self.off = m

    def __call__(self, name, shape, dt, at=None):
        esz = 2 if dt == BF16 else 4
        nb = int(np.prod(shape[1:])) * esz
        nb = (nb + 63) // 64 * 64
        self.n += 1
        if at is None:
            at = self.off
            self.off += nb
        assert at + nb <= SB_END, ("SBUF overflow", name, at, nb)
        return self.nc.alloc_sbuf_tensor_at("%s_%d" % (name, self.n), list(shape), dt, offset=at)


def bcast_ap(ap, pattern):
    return bass.AP(ap.tensor, ap.offset, [list(ap.ap[0])] + [list(p) for p in pattern])


def build(debug=False, stop_after=None):
    nc = bass.Bass("TRN2", target_bir_lowering=False)
    dr = lambda n, s, dt=F32: nc.dram_tensor(n, list(s), dt, kind="ExternalInput")
    x_d = dr("x", [S_LEN, D])
    pos_d = dr("pos", [128, NT], I32)
    g1_d = dr("norm1_gain", [1, D])
    win_d = dr("w_in", [D, 1984])
    gkf_w = dr("gla_gk_fwd_w", [16, 256])
    gkf_b = dr("gla_gk_fwd_b", [1, 256])
    gkb_w = dr("gla_gk_bwd_w", [16, 256])
    gkb_b = dr("gla_gk_bwd_b", [1, 256])
    go_d = dr("gla_out_gain", [128, 1])
    gqa_d = dr("mla_q_gain", [1, 256])
    wqb_d = dr("mla_w_qb", [256, 768])
    gkva_d = dr("mla_kv_gain", [1, 128])
    wkvb_d = dr("mla_w_kvb", [128, 1024])
    gqn_d = dr("q_norm_gain", [1, 96])
    gkn_d = dr("k_norm_gain", [1, 96])
    wout_d = dr("w_out", [D, D])
    g2_d = dr("norm2_gain", [1, D])
    wrg_d = dr("w_router_group", [D, 4])
    brg_d = dr("b_router_group", [1, 4])
    wre_d = dr("w_router_expert", [D, 32])
    bre_d = dr("b_router_expert", [1, 32])
    weg_d = dr("w_expert_gate", [32, D, 256])
    weu_d = dr("w_expert_up", [32, D, 256])
    wed_d = dr("w_expert_down", [32, 256, D])
    ident_d = dr("c_ident", [128, 128], BF16)
    masks_d = dr("c_masks", [128, 256], BF16)
    invf_d = dr("c_invf", [128, 16])
    sel_d = dr("c_sel", [32, 32 * 128], BF16)
    lrb_d = dr("c_lrbias", [64, 1])
    out_d = nc.dram_tensor("out", [S_LEN, D], F32, kind="ExternalOutput")
    dbg = {}
    if debug:
        dbg["mix"] = nc.dram_tensor("d_mix", [128, 8 * S_LEN], BF16, kind="ExternalOutput")
        dbg["x1"] = nc.dram_tensor("d_x1", [S_LEN, D], F32, kind="ExternalOutput")
        dbg["gate"] = nc.dram_tensor("d_gate", [S_LEN, 32], F32, kind="ExternalOutput")

    if debug:
        dbg["gen"] = nc.dram_tensor("d_gen", [128, 8 * S_LEN], BF16, kind="ExternalOutput")
    S = Sched(nc)
    A = Alloc(nc)
    op = S.op
    final_ops = []

    with contextlib.ExitStack() as es:
        P = [es.enter_context(nc.psum_tensor("pb%d" % i, [128, 512], F32)) for i in range(8)]
        Pb = [p.bitcast(BF16) for p in P]
        PN = ["P%d" % i for i in range(8)]

        def dma(q, out, in_, reads=(), writes=(), **kw):
            return op(q, lambda e: e.dma_start(out=out, in_=in_, **kw), reads=reads, writes=writes, dma=True)

        def act(out, in_, func, reads, writes, **kw):
            return op("act", lambda e: e.activation(out=out, in_=in_, func=func, **kw), reads=reads, writes=writes)

        def rsqrt_act(out, in_, n, reads, writes):
            act(out, in_, AF.Ln, reads, writes, scale=1.0 / n, bias=EPS)
            act(out, out, AF.Exp, writes, writes, scale=-0.5)

        def tt(eng, out, in0, in1, o, reads, writes):
            return op(eng, lambda e: e.tensor_tensor(out=out, in0=in0, in1=in1, op=o), reads=reads, writes=writes)

        def ts(eng, out, in0, s1, s2, o0, o1, reads, writes):
            if o1 is None:
                return op(eng, lambda e: e.tensor_scalar(out=out, in0=in0, scalar1=s1, scalar2=None, op0=o0),
                          reads=reads, writes=writes)
            return op(eng, lambda e: e.tensor_scalar(out=out, in0=in0, scalar1=s1, scalar2=s2, op0=o0, op1=o1),
                      reads=reads, writes=writes)

        def stt(out, in0, sc, in1, o0, o1, reads, writes):
            return op("dve", lambda e: e.scalar_tensor_tensor(out=out, in0=in0, scalar=sc, in1=in1, op0=o0, op1=o1),
                      reads=reads, writes=writes)

        def mm(out, lhsT, rhs, start, stop, reads, writes):
            return op("pe", lambda e: e.matmul(out, lhsT=lhsT, rhs=rhs, start=start, stop=stop),
                      reads=reads, writes=writes)

        def tr(out, in_, ident, reads, writes):
            return op("pe", lambda e: e.transpose(out=out, in_=in_, identity=ident), reads=reads, writes=writes)

        def cp(eng, out, in_, reads, writes):
            if eng == "act":
                return act(out, in_, AF.Copy, reads, writes)
            return op(eng, lambda e: e.tensor_copy(out=out, in_=in_), reads=reads, writes=writes)

        def memset(eng, ap, val, writes):
            return op(eng, lambda e: e.memset(ap, val), writes=writes)

        def bcast_row(dram, n):
            return bass.AP(dram, 0, [[0, 128], [1, n]])

        ident = A("ident", [128, 128], BF16)
        mixT = A("mixT", [128, 8, S_LEN], BF16)
        dma("sp", ident[:, :], ident_d[:, :], writes=["ident"])
        L0 = A.mark()

        hT = A("hT", [128, 8, S_LEN], BF16)
        E1 = A.mark()
        g1 = A("g1", [128, D], F32)
        xt = [A("xt%d" % i, [128, D], F32) for i in range(2)]
        hb = [A("hb%d" % i, [128, D], BF16) for i in range(2)]
        sqj = A("sqj", [128, D], F32)
        st1 = A("st1", [128, 4], F32)
        dma("sp", g1[:, :], bcast_row(g1_d, D), writes=["g1"])

        def norm_to_T(src_ap_fn, src_tiles, gain, gname, dstT, dname, pfx, t, pbank):
            i = t % 2
            ssq = st1[:, 0:1]
            rs = st1[:, 1:2]
            act(sqj[:, :], src_ap_fn(t), AF.Square, src_tiles, [pfx + "sqj", pfx + "ssq"], accum_out=ssq)
            rsqrt_act(rs, ssq, D, [pfx + "ssq"], [pfx + "rs"])
            stt(hb[i][:, :], src_ap_fn(t), rs, gain[:, :], ALU.mult, ALU.mult,
                src_tiles + [pfx + "rs", gname], [pfx + "hb%d" % i])
            pbv = Pb[pbank][:, :].rearrange("p (c n) -> p c n", c=8)
            for kc in range(8):
                tr(pbv[:, kc, :], hb[i][:, kc * 128:(kc + 1) * 128], ident[:, :],
                   [pfx + "hb%d" % i, "ident"], [PN[pbank]])
            cp("dve" if t % 2 else "act", dstT[:, :, t * 128:(t + 1) * 128], pbv, [PN[pbank]], [dname + "_%d" % (t // 4)])

        for t in range(NT):
            i = t % 2
            dma("sp", xt[i][:, :], x_d[t * 128:(t + 1) * 128, :], writes=["xt%d" % i])
            norm_to_T(lambda t, i=i: xt[i][:, :], ["xt%d" % i], g1, "g1", hT, "hT", "A", t, t % 2)
        hT_tiles = ["hT_%d" % k for k in range(4)]
        S.barrier()
        A.reset(E1)
        def checkpoint(name, dump=None, reads=()):
            if stop_after == name:
                if dump is not None and debug:
                    S.barrier()
                    final_ops.append(dma("sp", dbg["gen"][:, :], dump, reads=list(reads)))
                S.stopped = True

        checkpoint("A", hT[:, :, :].rearrange("p c n -> p (c n)"))

        wm = A("w_in_mla", [128, 8, 416], BF16)
        wqb = A("wqb", [128, 2, 768], BF16)
        wkvb = A("wkvb", [128, 1024], BF16)
        cs = A("cs", [128, NT, 32], F32)
        qhT = A("qhT", [128, 8, S_LEN], BF16)
        khT = A("khT", [128, 8, S_LEN], BF16)
        vA = A("vA", [128, NT, 8, 128], BF16)
        gqa = A("gqa", [128, 384], F32)
        gqn = A("gqn", [128, 96], F32)
        gkn = A("gkn", [128, 96], F32)
        B1m = A.mark()
        dma("pool", wm[:, :, :], win_d.ap()[:, 1568:1984].rearrange("(c p) n -> p c n", p=128), writes=["wm"])
        dma("pool", wqb[:, :, :], wqb_d.ap().rearrange("(c p) n -> p c n", p=128), writes=["wqb"])
        dma("pool", wkvb[:, :], wkvb_d[:, :], writes=["wkvb"])
        dma("sp", gqa[:, 0:256], bcast_row(gqa_d, 256), writes=["gqa"])
        dma("sp", gqa[:, 256:384], bcast_row(gkva_d, 128), writes=["gqa"])
        dma("sp", gqn[:, :], bcast_row(gqn_d, 96), writes=["gqn"])
        dma("sp", gkn[:, :], bcast_row(gkn_d, 96), writes=["gkn"])
        posi = A("posi", [128, NT], I32)
        posf = A("posf", [128, NT], F32)
        invf = A("invf", [128, 16], F32)
        ang = A("ang", [128, NT, 16], F32)
        kk = A("kk", [128, NT, 16], F32)
        ki = A("ki", [128, NT, 16], I32)
        rr = A("rr", [128, NT, 16], F32)
        yy = A("yy", [128, NT, 16], F32)
        m_ = A("m_", [128, NT, 16], F32)
        dma("sp", posi[:, :], pos_d[:, :], writes=["posi"])
        dma("sp", invf[:, :], invf_d[:, :], writes=["invf"])
        cp("dve", posf[:, :], posi[:, :], ["posi"], ["posf"])
        for t in range(NT):
            ts("dve", ang[:, t, :], invf[:, :], posf[:, t:t + 1], None, ALU.mult, None, ["invf", "posf"], ["ang"])
        ts("dve", kk[:, :, :], ang[:, :, :], 1.0 / (2 * PI), None, ALU.mult, None, ["ang"], ["kk"])
        cp("dve", ki[:, :, :], kk[:, :, :], ["kk"], ["ki"])
        cp("dve", kk[:, :, :], ki[:, :, :], ["ki"], ["kk"])
        stt(rr[:, :, :], kk[:, :, :], -2 * PI, ang[:, :, :], ALU.mult, ALU.add, ["kk", "ang"], ["rr"])
        for which, shift in ((1, 0.0), (0, PI / 2)):
            ts("dve", yy[:, :, :], rr[:, :, :], shift, None, ALU.add, None, ["rr"], ["yy"])
            ts("dve", m_[:, :, :], yy[:, :, :], PI, None, ALU.is_gt, None, ["yy"], ["m_"])
            stt(yy[:, :, :], m_[:, :, :], -2 * PI, yy[:, :, :], ALU.mult, ALU.add, ["m_", "yy"], ["yy"])
            ts("dve", m_[:, :, :], yy[:, :, :], -PI, None, ALU.is_lt, None, ["yy"], ["m_"])
            stt(yy[:, :, :], m_[:, :, :], 2 * PI, yy[:, :, :], ALU.mult, ALU.add, ["m_", "yy"], ["yy"])
            ts("dve", yy[:, :, :], yy[:, :, :], PI, -PI, ALU.min, ALU.max, ["yy"], ["yy"])
            act(cs[:, :, which * 16:(which + 1) * 16], yy[:, :, :], AF.Sin, ["yy"], ["cs"])
        checkpoint("B1a")
        memset("pool", vA[:, :, :, :], 1.0, ["vA"])
        checkpoint("B1b")

        sq2 = A("sq2", [128, 1024], F32)
        stq = A("stq", [128, 32], F32)
        ab = A("ab", [128, 384], BF16)
        abT = A("abT", [128, 3, 128], BF16)
        krw = A("krw", [128, 32], F32)
        qn = A("qn", [128, 8, 96], F32)
        kn = A("kn", [128, 8, 96], F32)
        qf = A("qf", [128, 8, 96], BF16)
        kf = A("kf", [128, 8, 96], BF16)
        ra = A("ra", [128, 8, 16], F32)
        rb = A("rb", [128, 8, 16], F32)

        def rope(src, dst, sname, dname, t):
            cosb = bcast_ap(cs[:, t, 0:16], [[0, 8], [1, 16]])
            sinb = bcast_ap(cs[:, t, 16:32], [[0, 8], [1, 16]])
            t1 = src[:, :, 64:80]
            t2 = src[:, :, 80:96]
            tt("pool", ra[:, :, :], t1, cosb, ALU.mult, [sname, "cs"], ["ra"])
            tt("pool", rb[:, :, :], t2, sinb, ALU.mult, [sname, "cs"], ["rb"])
            tt("pool", dst[:, :, 64:80], ra[:, :, :], rb[:, :, :], ALU.subtract, ["ra", "rb"], [dname])
            tt("pool", ra[:, :, :], t1, sinb, ALU.mult, [sname, "cs"], ["ra"])
            tt("pool", rb[:, :, :], t2, cosb, ALU.mult, [sname, "cs"], ["rb"])
            tt("pool", dst[:, :, 80:96], ra[:, :, :], rb[:, :, :], ALU.add, ["ra", "rb"], [dname])
            cp("pool", dst[:, :, 0:64], src[:, :, 0:64], [sname], [dname])

        for t in range(NT):
            tsl = slice(t * 128, (t + 1) * 128)
            hTt = "hT_%d" % (t // 4)
            for kc in range(8):
                mm(P[0][:, 0:416], hT[:, kc, tsl], wm[:, kc, :], kc == 0, kc == 7, [hTt, "wm"], ["P0"])
            checkpoint("B1c0")
            act(sq2[:, 0:256], P[0][:, 0:256], AF.Square, ["P0"], ["sq2", "stq0"], accum_out=stq[:, 0:1])
            act(sq2[:, 256:384], P[0][:, 256:384], AF.Square, ["P0"], ["sq2", "stq1"], accum_out=stq[:, 1:2])
            act(sq2[:, 384:416], P[0][:, 384:416], AF.Square, ["P0"], ["sq2", "stq2"], accum_out=stq[:, 2:3])
            checkpoint("B1c1")
            rsqrt_act(stq[:, 3:4], stq[:, 0:1], 256, ["stq0"], ["stq3"])
            rsqrt_act(stq[:, 4:5], stq[:, 1:2], 128, ["stq1"], ["stq4"])
            checkpoint("B1c2")
            stt(ab[:, 0:256], P[0][:, 0:256], stq[:, 3:4], gqa[:, 0:256], ALU.mult, ALU.mult, ["P0", "stq3", "gqa"], ["ab"])
            checkpoint("B1c3")
            stt(ab[:, 256:384], P[0][:, 256:384], stq[:, 4:5], gqa[:, 256:384], ALU.mult, ALU.mult, ["P0", "stq4", "gqa"], ["ab"])
            checkpoint("B1c4")
            cp("act", krw[:, :], P[0][:, 384:416], ["P0"], ["krw"])
            checkpoint("B1c")
            p1v = Pb[1][:, 0:384].rearrange("p (c n) -> p c n", c=3)
            for c in range(3):
                tr(p1v[:, c, :], ab[:, c * 128:(c + 1) * 128], ident[:, :], ["ab", "ident"], ["P1"])
            cp("act", abT[:, :, :], p1v, ["P1"], ["abT"])
            for nb in range(2):
                for kc in range(2):
                    mm(P[2 + nb][:, 0:384], abT[:, kc, :], wqb[:, kc, nb * 384:(nb + 1) * 384], kc == 0, kc == 1,
                       ["abT", "wqb"], [PN[2 + nb]])
                mm(P[4 + nb][:, :], abT[:, 2, :], wkvb[:, nb * 512:(nb + 1) * 512], True, True, ["abT", "wkvb"], [PN[4 + nb]])
            checkpoint("B1d")
            for nb in range(2):
                act(sq2[:, nb * 384:(nb + 1) * 384], P[2 + nb][:, 0:384], AF.Square, [PN[2 + nb]], ["sq2"])
            op("dve", lambda e: e.tensor_reduce(out=stq[:, 8:16], in_=sq2[:, 0:768].rearrange("p (h d) -> p h d", h=8),
                                                axis=AX.X, op=ALU.add), reads=["sq2"], writes=["stq8"])
            rsqrt_act(stq[:, 8:16], stq[:, 8:16], 96, ["stq8"], ["stq8"])
            for h in range(8):
                nb, hh = divmod(h, 4)
                stt(qn[:, h, :], P[2 + nb][:, hh * 96:(hh + 1) * 96], stq[:, 8 + h:9 + h], gqn[:, :], ALU.mult, ALU.mult,
                    [PN[2 + nb], "stq8", "gqn"], ["qn"])
            checkpoint("B1e")
            for nb in range(2):
                src = P[4 + nb][:, :].rearrange("p (h d) -> p h d", h=4)[:, :, 0:64]
                dst = sq2[:, nb * 256:(nb + 1) * 256].rearrange("p (h d) -> p h d", h=4)
                act(dst, src, AF.Square, [PN[4 + nb]], ["sq2"])
            op("dve", lambda e: e.tensor_reduce(out=stq[:, 16:24], in_=sq2[:, 0:512].rearrange("p (h d) -> p h d", h=8),
                                                axis=AX.X, op=ALU.add), reads=["sq2"], writes=["stq16"])
            ts("dve", stq[:, 16:24], stq[:, 16:24], stq[:, 2:3], None, ALU.add, None, ["stq16", "stq2"], ["stq16"])
            rsqrt_act(stq[:, 16:24], stq[:, 16:24], 96, ["stq16"], ["stq16"])
            for h in range(8):
                nb, hh = divmod(h, 4)
                stt(kn[:, h, 0:64], P[4 + nb][:, hh * 128:hh * 128 + 64], stq[:, 16 + h:17 + h], gkn[:, 0:64],
                    ALU.mult, ALU.mult, [PN[4 + nb], "stq16", "gkn"], ["kn"])
                stt(kn[:, h, 64:96], krw[:, :], stq[:, 16 + h:17 + h], gkn[:, 64:96],
                    ALU.mult, ALU.mult, ["krw", "stq16", "gkn"], ["kn"])
            for nb in range(2):
                srcv = P[4 + nb][:, :].rearrange("p (a b d) -> p a b d", a=2, b=2)
                dstv = vA[:, t, nb * 4:nb * 4 + 4, :].rearrange("p (a b) d -> p a b d", b=2)
                cp("act", dstv[:, :, 0, 0:64], srcv[:, :, 0, 64:128], [PN[4 + nb]], ["vA"])
                cp("act", dstv[:, :, 1, 64:128], srcv[:, :, 1, 64:128], [PN[4 + nb]], ["vA"])
            checkpoint("B1f")
            rope(qn, qf, "qn", "qf", t)
            rope(kn, kf, "kn", "kf", t)
            checkpoint("B1g")
            p6v = Pb[6][:, :].rearrange("p (h n) -> p h n", h=8)
            p7v = Pb[7][:, :].rearrange("p (h n) -> p h n", h=8)
            for h in range(8):
                tr(p6v[0:96, h, :], qf[:, h, :], ident[:, :], ["qf", "ident"], ["P6"])
            for h in range(8):
                tr(p7v[0:96, h, :], kf[:, h, :], ident[:, :], ["kf", "ident"], ["P7"])
            cp("dve", qhT[0:96, :, tsl], p6v[0:96, :, :], ["P6"], ["qhT_%d" % (t // 4)])
            cp("act", khT[0:96, :, tsl], p7v[0:96, :, :], ["P7"], ["khT"])
        S.barrier()
        A.reset(B1m)

        checkpoint("B1", qhT[:, :, :].rearrange("p c n -> p (c n)"))
        pbuf = [A("pbuf%d" % i, [128, 512], BF16) for i in range(4)]
        lnb = A("lnb", [128, 512], F32)
        rcb = A("rcb", [128, 512], F32)
        scale = 96 ** -0.5
        it = 0
        for h in range(8):
            even = (h % 2 == 0)
            vrows = slice(0, 64) if even else slice(64, 128)
            srows = slice(64, 128) if even else slice(0, 64)
            for qg in range(4):
                qsl = slice(qg * 512, (qg + 1) * 512)
                ob = 4 + (it % 2)
                seq = []
                for kt in range(16):
                    seq.append(("s", kt))
                    if kt >= 2:
                        seq.append(("pv", kt - 2))
                seq += [("pv", 14), ("pv", 15)]
                for kind, kt in seq:
                    sb_ = kt % 3
                    pi = kt % 4
                    if kind == "s":
                        mm(P[sb_][:, :], khT[0:96, h, kt * 128:(kt + 1) * 128], qhT[0:96, h, qsl], True, True,
                           ["khT", "qhT_%d" % qg], [PN[sb_]])
                        act(pbuf[pi][:, :], P[sb_][:, :], AF.Exp, [PN[sb_]], ["pbuf%d" % pi], scale=scale)
                    else:
                        lhsT = vA[:, kt, h, :]
                        mm(P[ob][:, :], lhsT, pbuf[pi][:, :], kt == 0, kt == 15, ["vA", "pbuf%d" % pi], [PN[ob]])
                act(lnb[vrows, :], P[ob][srows, :], AF.Ln, [PN[ob]], ["lnb"])
                act(rcb[vrows, :], lnb[vrows, :], AF.Exp, ["lnb"], ["rcb"], scale=-1.0)
                tt("dve", mixT[vrows, 4 + h // 2, qsl], P[ob][vrows, :], rcb[vrows, :], ALU.mult, [PN[ob], "rcb"], ["mixT_m"])
                it += 1
        S.barrier()
        A.reset(E1)

        checkpoint("C", mixT[:, :, :].rearrange("p c n -> p (c n)"))
        wg = A("w_in_gla", [128, 8, 1568], BF16)
        R2 = A.mark()
        qkT = A("qkT", [128, 4, S_LEN], F32)
        vtok = A("vtok", [128, NT, 512], BF16)
        sgT = A("sgT", [128, 4, S_LEN], BF16)
        lrT = A("lrT", [64, S_LEN], F32)
        wlr = A("wlr", [128, 8, 64], BF16)
        lrb = A("lrb", [64, 1], F32)
        waug = A("waug", [64, 512], F32)
        masks = A("masks", [128, 256], BF16)
        gout = A("gout", [128, 1], F32)
        onesf = A("onesf", [128, 128], F32)
        R4 = A.mark()
        dma("pool", wg[:, :, :], win_d.ap()[:, 0:1568].rearrange("(c p) n -> p c n", p=128), writes=["wg"])
        memset("pool", wlr[:, :, :], 0.0, ["wlr"])
        dma("pool", wlr[:, :, 0:16], win_d.ap()[:, 1536:1552].rearrange("(c p) n -> p c n", p=128), reads=["wlr"], writes=["wlr"])
        dma("pool", wlr[:, :, 32:48], win_d.ap()[:, 1552:1568].rearrange("(c p) n -> p c n", p=128), reads=["wlr"], writes=["wlr"])
        dma("sp", lrb[:, :], lrb_d[:, :], writes=["lrb"])
        memset("pool", waug[:, :], 0.0, ["waug"])
        dma("sp", waug[0:16, 0:256], gkf_w[:, :], reads=["waug"], writes=["waug"])
        dma("sp", waug[16:17, 0:256], gkf_b[:, :], reads=["waug"], writes=["waug"])
        dma("sp", waug[16:17, 256:512], gkb_b[:, :], reads=["waug"], writes=["waug"])
        dma("sp", waug[32:48, 256:512], gkb_w[:, :], reads=["waug"], writes=["waug"])
        dma("sp", masks[:, :], masks_d[:, :], writes=["masks"])
        dma("sp", gout[:, :], go_d[:, :], writes=["gout"])
        memset("pool", onesf[:, :], 1.0, ["onesf"])
        blk = 0
        for kind, idx in [("q", 0), ("q", 1), ("k", 0), ("k", 1), ("g", 0), ("g", 1), ("g", 2), ("g", 3), ("lr", 0)]:
            for tg in range(4):
                pbk = blk % 4
                blk += 1
                tgs = slice(tg * 512, (tg + 1) * 512)
                for kc in range(8):
                    if kind == "q":
                        lhsT = wg[:, kc, idx * 128:(idx + 1) * 128]
                    elif kind == "k":
                        lhsT = wg[:, kc, 256 + idx * 128:256 + (idx + 1) * 128]
                    elif kind == "g":
                        lhsT = wg[:, kc, 1024 + idx * 128:1024 + (idx + 1) * 128]
                    else:
                        lhsT = wlr[:, kc, :]
                    mrows = 64 if kind == "lr" else 128
                    mm(P[pbk][0:mrows, :], lhsT, hT[:, kc, tgs], kc == 0, kc == 7, ["hT_%d" % tg, "wg", "wlr"], [PN[pbk]])
                if kind == "q":
                    act(qkT[:, idx, tgs], P[pbk][:, :], AF.Copy, [PN[pbk]], ["qT"], scale=0.125)
                elif kind == "k":
                    cp("dve", qkT[:, 2 + idx, tgs], P[pbk][:, :], [PN[pbk]], ["kT"])
                elif kind == "g":
                    act(sgT[:, idx, tgs], P[pbk][:, :], AF.Silu, [PN[pbk]], ["sgT"])
                else:
                    act(lrT[:, tgs], P[pbk][0:64, :], AF.Identity, [PN[pbk], "lrb"], ["lrT"], bias=lrb[:, :])
        for t in range(NT):
            pbk = 4 + t % 2
            tsl = slice(t * 128, (t + 1) * 128)
            for kc in range(8):
                mm(P[pbk][:, :], hT[:, kc, tsl], wg[:, kc, 512:1024], kc == 0, kc == 7, ["hT_%d" % (t // 4), "wg"], [PN[pbk]])
            cp("dve" if t % 2 else "act", vtok[:, t, :], P[pbk][:, :], [PN[pbk]], ["vtok"])
        S.barrier()

        checkpoint("B2", sgT[:, :, :].rearrange("p c n -> p (c n)"))
        HTB = L0
        tmp = [A("gt%d" % i, [128, 1024], F32, at=HTB + i * 4096) for i in range(4)]
        prod = {}
        names = [(d_, hp, k_) for d_ in (0, 1) for hp in (0, 1) for k_ in ("qr", "kr", "qb")]
        slots = [HTB + 16384 + i * 4096 for i in range(4)] + [E1 + 16384 + i * 4096 for i in range(2)]
        for i, nm in enumerate(names):
            if i < 6:
                prod[nm] = A("pr", [128, S_LEN], BF16, at=slots[i])
            else:
                prod[nm] = A("pr", [128, S_LEN], BF16)
        kdT = A("kdT", [128, 1024], BF16)
        dec = A("dec", [128, 4, 32], F32)
        smask = A("smask", [128, 1024], F32)
        kd = A("kd", [128, NT, 512], BF16, at=E1)
        memset("pool", smask[:, :], 1.0, ["smask"])
        memset("pool", smask[:, :].rearrange("p (c j) -> p c j", j=64)[:, :, 0:1], 0.0, ["smask"])
        G, Fc, Dt, Eb = tmp
        for d_ in (0, 1):
            for hp in (0, 1):
                dh = d_ * 2 + hp
                qT = qkT[:, hp, :]
                kT = qkT[:, 2 + hp, :]
                for half in range(2):
                    hs = slice(half * 1024, (half + 1) * 1024)
                    for j in range(2):
                        pbk = j
                        cols = slice(half * 1024 + j * 512, half * 1024 + (j + 1) * 512)
                        mm(P[pbk][:, :], waug[0:64, dh * 128:(dh + 1) * 128], lrT[0:64, cols], True, True,
                           ["waug", "lrT"], [PN[pbk]])
                        act(Eb[:, j * 512:(j + 1) * 512], P[pbk][:, :], AF.Exp, [PN[pbk]], ["Eb"], scale=-1.0)
                    act(G[:, :], Eb[:, :], AF.Ln, ["Eb"], ["G"], bias=1.0)
                    op("dve", lambda e: e.tensor_tensor_scan(out=Fc[:, :], data0=smask[:, :], data1=G[:, :], initial=0.0,
                                                             op0=ALU.mult, op1=ALU.add), reads=["smask", "G"], writes=["Fc"])
                    Fv = Fc[:, :].rearrange("p (c j) -> p c j", j=64)
                    Dv = Dt[:, :].rearrange("p (c j) -> p c j", j=64)
                    T63 = bcast_ap(Fc[:, 63:64], [[64, 16], [0, 64]])
                    act(dec[:, dh, half * 16:(half + 1) * 16], Fv[:, :, 63], AF.Exp, ["Fc"], ["dec"], scale=-1.0 / 16)
                    if d_ == 0:
                        ref = bcast_ap(Fc[:, 31:32], [[64, 16], [0, 64]])
                        tt("dve", Dv, Fv, ref, ALU.subtract, ["Fc"], ["Dt"])
                        act(Eb[:, :], Dt[:, :], AF.Exp, ["Dt"], ["Eb"], scale=-1.0 / 16)
                        tt("pool", prod[(0, hp, "qr")][:, hs], qT[:, hs], Eb[:, :], ALU.mult, ["qT", "kT", "Eb"], ["prod"])
                        act(Eb[:, :], Dt[:, :], AF.Exp, ["Dt"], ["Eb"], scale=1.0 / 16)
                        tt("pool", prod[(0, hp, "kr")][:, hs], kT[:, hs], Eb[:, :], ALU.mult, ["qT", "kT", "Eb"], ["prod"])
                        act(Eb[:, :], Fc[:, :], AF.Exp, ["Fc"], ["Eb"], scale=-1.0 / 16)
                        tt("pool", prod[(0, hp, "qb")][:, hs], qT[:, hs], Eb[:, :], ALU.mult, ["qT", "kT", "Eb"], ["prod"])
                        tt("dve", Dv, Fv, T63, ALU.subtract, ["Fc"], ["Dt"])
                        act(Eb[:, :], Dt[:, :], AF.Exp, ["Dt"], ["Eb"], scale=1.0 / 16)
                        tt("pool", kdT[:, :], kT[:, hs], Eb[:, :], ALU.mult, ["qT", "kT", "Eb"], ["kdT"])
                    else:
                        tt("dve", G[:, :], Fc[:, :], G[:, :], ALU.subtract, ["Fc", "G"], ["G"])
                        Gv = G[:, :].rearrange("p (c j) -> p c j", j=64)
                        ref = bcast_ap(G[:, 32:33], [[64, 16], [0, 64]])
                        tt("dve", Dv, Gv, ref, ALU.subtract, ["G"], ["Dt"])
                        act(Eb[:, :], Dt[:, :], AF.Exp, ["Dt"], ["Eb"], scale=1.0 / 16)
                        tt("pool", prod[(1, hp, "qr")][:, hs], qT[:, hs], Eb[:, :], ALU.mult, ["qT", "kT", "Eb"], ["prod"])
                        act(Eb[:, :], Dt[:, :], AF.Exp, ["Dt"], ["Eb"], scale=-1.0 / 16)
                        tt("pool", prod[(1, hp, "kr")][:, hs], kT[:, hs], Eb[:, :], ALU.mult, ["qT", "kT", "Eb"], ["prod"])
                        tt("dve", Dv, Gv, T63, ALU.subtract, ["G", "Fc"], ["Dt"])
                        act(Eb[:, :], Dt[:, :], AF.Exp, ["Dt"], ["Eb"], scale=1.0 / 16)
                        tt("pool", prod[(1, hp, "qb")][:, hs], qT[:, hs], Eb[:, :], ALU.mult, ["qT", "kT", "Eb"], ["prod"])
                        act(Eb[:, :], G[:, :], AF.Exp, ["G"], ["Eb"], scale=-1.0 / 16)
                        tt("pool", kdT[:, :], kT[:, hs], Eb[:, :], ALU.mult, ["qT", "kT", "Eb"], ["kdT"])
                    for g4 in range(2):
                        pbk = 2 + g4
                        pv = Pb[pbk][:, 0:512].rearrange("p (t n) -> p t n", t=4)
                        for tq in range(4):
                            c0 = (g4 * 4 + tq) * 128
                            tr(pv[:, tq, :], kdT[:, c0:c0 + 128], ident[:, :], ["kdT", "ident"], [PN[pbk]])
                        t0 = half * 8 + g4 * 4
                        cp("dve", kd[:, t0:t0 + 4, dh * 128:(dh + 1) * 128], pv, [PN[pbk]], ["kd"])
        S.barrier()

        checkpoint("D1", kd[:, :, :].rearrange("p c n -> p (c n)"))
        qk_off = R2
        Sst = [A("Sst%d" % i, [128, 32, 128], BF16, at=qk_off + i * 8192) for i in range(4)]
        Sf = [A("Sf%d" % i, [128, 256], F32) for i in range(4)]
        for dh in range(4):
            memset("pool", Sf[dh][:, :], 0.0, ["Sf%d" % dh])
        for step in range(32):
            for dh in range(4):
                d_, hp = divmod(dh, 2)
                n = step if d_ == 0 else 31 - step
                t, c = divmod(n, 2)
                rows = slice(c * 64, (c + 1) * 64)
                cp("pool", Sst[dh][0:64, n, :], Sf[dh][0:64, 0:128], ["Sf%d" % dh], ["SstA%d" % dh])
                cp("act", Sst[dh][64:128, n, :], Sf[dh][64:128, 128:256], ["Sf%d" % dh], ["SstB%d" % dh])
                if step == 31:
                    continue
                pbk = 4 * c + dh
                mm(P[pbk][:, 0:256], kd[rows, t, dh * 128:(dh + 1) * 128], vtok[rows, t, hp * 256:(hp + 1) * 256],
                   True, True, ["kd", "vtok"], [PN[pbk]])
                stt(Sf[dh][:, :], Sf[dh][:, :], dec[:, dh, n:n + 1], P[pbk][:, 0:256], ALU.mult, ALU.add,
                    ["Sf%d" % dh, "dec", PN[pbk]], ["Sf%d" % dh])
        S.barrier()

        checkpoint("D2", Sst[0][:, :, :].rearrange("p c n -> p (c n)"))
        smb = [A("smb%d" % i, [128, 2, 2, 128], BF16) for i in range(2)]
        sqo = A("sqo", [128, 256], F32)
        rso = A("rso", [128, 256], F32)
        t1o = A("t1o", [128, 256], F32)
        for t in range(NT):
            tsl = slice(t * 128, (t + 1) * 128)
            for par in range(2):
                rows = slice(par * 64, (par + 1) * 64)
                sbk = par
                obk = 2 + par
                scv = P[sbk][:, :].rearrange("p (a b n) -> p a b n", a=2, b=2)
                for hp in range(2):
                    for d_ in range(2):
                        mm(scv[:, hp, d_, :], prod[(d_, hp, "kr")][rows, tsl], prod[(d_, hp, "qr")][rows, tsl], True, True,
                           ["prod"], [PN[sbk]])
                mk = bcast_ap(masks[:, 0:256], [[0, 2], [1, 256]])
                tt("dve", smb[par][:, :, :, :].rearrange("p a b n -> p a (b n)"),
                   P[sbk][:, :].rearrange("p (a m) -> p a m", a=2), mk, ALU.mult, [PN[sbk], "masks"], ["smb%d" % par])
                ov = P[obk][:, 0:256].rearrange("p (a n) -> p a n", a=2)
                for hp in range(2):
                    h = hp * 2 + par
                    mm(ov[:, hp, :], vtok[:, t, h * 128:(h + 1) * 128], smb[par][:, hp, 0, :], True, False,
                       ["vtok", "smb%d" % par], [PN[obk]])
                    mm(ov[:, hp, :], vtok[:, t, h * 128:(h + 1) * 128], smb[par][:, hp, 1, :], False, False,
                       ["vtok", "smb%d" % par], [PN[obk]])
                    for d_ in range(2):
                        dh = d_ * 2 + hp
                        for c in range(2):
                            n = t * 2 + c
                            csl = slice(t * 128 + c * 64, t * 128 + (c + 1) * 64)
                            last = (d_ == 1 and c == 1)
                            mm(ov[:, hp, c * 64:(c + 1) * 64], Sst[dh][rows, n, :], prod[(d_, hp, "qb")][rows, csl],
                               False, last, ["SstA%d" % dh, "SstB%d" % dh, "prod"], [PN[obk]])
                act(sqo[:, :], P[obk][:, 0:256], AF.Square, [PN[obk]], ["sqo"])
                ebk = 4 + par
                mm(P[ebk][:, 0:256], onesf[:, :], sqo[:, :], True, True, ["onesf", "sqo"], [PN[ebk]])
                rsqrt_act(rso[:, :], P[ebk][:, 0:256], 128, [PN[ebk]], ["rso"])
                stt(t1o[:, :], P[obk][:, 0:256], gout[:, 0:1], rso[:, :], ALU.mult, ALU.mult, [PN[obk], "gout", "rso"], ["t1o"])
                for hp in range(2):
                    h = hp * 2 + par
                    tt("pool", mixT[:, h, tsl], t1o[:, hp * 128:(hp + 1) * 128], sgT[:, h, tsl], ALU.mult,
                       ["t1o", "sgT"], ["mixT_g"])
        S.barrier()
        A.reset(L0)
        if debug:
            final_ops.append(dma("sp", dbg["mix"][:, :], mixT[:, :, :].rearrange("p c n -> p (c n)"), reads=["mixT_g", "mixT_m"]))

        X = A("X", [128, NT, D], F32)
        h2T = A("h2T", [128, 8, S_LEN], BF16)
        E2 = A.mark()
        wo = A("wo", [128, 8, D], BF16)
        dma("pool", wo[:, :, :], wout_d.ap().rearrange("(c p) n -> p c n", p=128), writes=["wo"])
        for t in range(NT):
            tsl = slice(t * 128, (t + 1) * 128)
            dma("sp", X[:, t, :], x_d[tsl, :], writes=["X%d" % t])
            for ch in range(2):
                pbk = (t % 2) * 2 + ch
                for kc in range(8):
                    mm(P[pbk][:, :], mixT[:, kc, tsl], wo[:, kc, ch * 512:(ch + 1) * 512], kc == 0, kc == 7,
                       ["mixT_g", "mixT_m", "wo"], [PN[pbk]])
                tt("dve", X[:, t, ch * 512:(ch + 1) * 512], X[:, t, ch * 512:(ch + 1) * 512], P[pbk][:, :], ALU.add,
                   ["X%d" % t, PN[pbk]], ["X%d" % t])
        if debug:
            for t in range(NT):
                final_ops.append(dma("sp", dbg["x1"][t * 128:(t + 1) * 128, :], X[:, t, :], reads=["X%d" % t]))
        S.barrier()
        A.reset(E2)

        g2 = A("g2", [128, D], F32)
        wr = A("wr", [128, 8, 36], BF16)
        rbias = A("rbias", [128, 36], F32)
        gT = A("gT", [32, 2, S_LEN], BF16)
        sel = A("sel", [32, 32, 128], BF16)
        Fm = A.mark()
        hb = [A("hb2_%d" % i, [128, D], BF16) for i in range(2)]
        sqj = A("sqj2", [128, D], F32)
        st1 = A("st1_2", [128, 4], F32)
        lg = A("lg", [128, 36], F32)
        rt = A("rt", [128, 64], F32)
        gate = A("gate", [128, 32], F32)
        ghl = A("ghl", [128, 2, 32], BF16)
        gtmp = A("gtmp", [128, 32], F32)
        dma("sp", g2[:, :], bcast_row(g2_d, D), writes=["g2"])
        dma("pool", wr[:, :, 0:4], wrg_d.ap().rearrange("(c p) n -> p c n", p=128), writes=["wr"])
        dma("pool", wr[:, :, 4:36], wre_d.ap().rearrange("(c p) n -> p c n", p=128), writes=["wr"])
        dma("sp", rbias[:, 0:4], bcast_row(brg_d, 4), writes=["rbias"])
        dma("sp", rbias[:, 4:36], bcast_row(bre_d, 32), writes=["rbias"])
        dma("sp", sel[:, :, :], sel_d.ap().rearrange("p (e n) -> p e n", e=32), writes=["sel"])
        for t in range(NT):
            tsl = slice(t * 128, (t + 1) * 128)
            norm_to_T(lambda t: X[:, t, :], ["X%d" % t], g2, "g2", h2T, "h2T", "F", t, t % 2)
            for kc in range(8):
                mm(P[2][:, 0:36], h2T[:, kc, tsl], wr[:, kc, :], kc == 0, kc == 7, ["h2T_%d" % (t // 4), "wr"], ["P2"])
            tt("dve", lg[:, :], P[2][:, 0:36], rbias[:, :], ALU.add, ["P2", "rbias"], ["lg"])
            mg = rt[:, 0:1]
            op("dve", lambda e: e.tensor_reduce(out=rt[:, 0:1], in_=lg[:, 0:4], axis=AX.X, op=ALU.max), reads=["lg"], writes=["rt0"])
            ts("dve", rt[:, 1:2], mg, -1.0, None, ALU.mult, None, ["rt0"], ["rt1"])
            act(rt[:, 4:8], lg[:, 0:4], AF.Exp, ["lg", "rt1"], ["rt4", "rt2"], bias=rt[:, 1:2], accum_out=rt[:, 2:3])
            op("dve", lambda e: e.reciprocal(out=rt[:, 3:4], in_=rt[:, 2:3]), reads=["rt2"], writes=["rt3"])
            ts("dve", rt[:, 8:12], lg[:, 0:4], mg, rt[:, 3:4], ALU.is_equal, ALU.mult, ["lg", "rt0", "rt3"], ["rt8"])
            ts("dve", rt[:, 12:16], lg[:, 0:4], mg, None, ALU.is_equal, None, ["lg", "rt0"], ["rt12"])
            ts("dve", rt[:, 16:24], lg[:, 4:12], rt[:, 12:13], None, ALU.mult, None, ["lg", "rt12"], ["rt16"])
            for g_ in range(1, 4):
                stt(rt[:, 16:24], lg[:, 4 + 8 * g_:12 + 8 * g_], rt[:, 12 + g_:13 + g_], rt[:, 16:24], ALU.mult, ALU.add,
                    ["lg", "rt12", "rt16"], ["rt16"])
            op("dve", lambda e: e.max(out=rt[:, 24:32], in_=rt[:, 16:24]), reads=["rt16"], writes=["rt24"])
            tt("dve", rt[:, 32:33], rt[:, 25:26], rt[:, 24:25], ALU.subtract, ["rt24"], ["rt32"])
            act(rt[:, 33:34], rt[:, 32:33], AF.Exp, ["rt32"], ["rt33"])
            ts("dve", rt[:, 34:35], rt[:, 33:34], 1.0, None, ALU.add, None, ["rt33"], ["rt34"])
            op("dve", lambda e: e.reciprocal(out=rt[:, 35:36], in_=rt[:, 34:35]), reads=["rt34"], writes=["rt35"])
            tt("dve", rt[:, 36:37], rt[:, 33:34], rt[:, 35:36], ALU.mult, ["rt33", "rt35"], ["rt36"])
            ts("dve", rt[:, 40:48], rt[:, 16:24], rt[:, 24:25], rt[:, 35:36], ALU.is_equal, ALU.mult, ["rt16", "rt24", "rt35"], ["rt40"])
            ts("dve", rt[:, 48:56], rt[:, 16:24], rt[:, 25:26], rt[:, 36:37], ALU.is_equal, ALU.mult, ["rt16", "rt24", "rt36"], ["rt48"])
            tt("dve", rt[:, 40:48], rt[:, 40:48], rt[:, 48:56], ALU.add, ["rt40", "rt48"], ["rt40"])
            for g_ in range(4):
                ts("dve", gate[:, g_ * 8:(g_ + 1) * 8], rt[:, 40:48], rt[:, 8 + g_:9 + g_], None, ALU.mult, None,
                   ["rt40", "rt8"], ["gate"])
            if debug:
                final_ops.append(dma("sp", dbg["gate"][tsl, :], gate[:, :], reads=["gate"]))
            cp("dve", ghl[:, 0, :], gate[:, :], ["gate"], ["ghl"])
            tt("dve", gtmp[:, :], gate[:, :], ghl[:, 0, :], ALU.subtract, ["gate", "ghl"], ["gtmp"])
            cp("dve", ghl[:, 1, :], gtmp[:, :], ["gtmp", "ghl"], ["ghl"])
            p3v = Pb[3][:, 0:256].rearrange("p (a n) -> p a n", a=2)
            for a_ in range(2):
                tr(p3v[0:32, a_, :], ghl[:, a_, :], ident[:, :], ["ghl", "ident"], ["P3"])
            cp("act", gT[0:32, :, tsl], p3v[0:32, :, :], ["P3"], ["gT"])
        S.barrier()
        A.reset(Fm)

        EG = 2
        NEG = 32 // EG
        wgt = [[A("wg%d_%d" % (b, j), [128, 8, 256], BF16) for j in range(EG)] for b in range(2)]
        wup = [[A("wu%d_%d" % (b, j), [128, 8, 256], BF16) for j in range(EG)] for b in range(2)]
        wdn = [[A("wd%d_%d" % (b, j), [128, 2, D], BF16) for j in range(EG)] for b in range(2)]
        MX = SB_BASE + 256
        hid = [A("hid%d" % b, [128, EG, 2, 512], BF16, at=MX + b * 4096) for b in range(2)]
        sil = [A("sil%d" % b, [128, 512], F32, at=MX + 8192 + b * 2048) for b in range(2)]
        t1m = [A("t1m%d" % b, [128, 512], F32, at=MX + 12288 + b * 2048) for b in range(2)]
        itc = 0
        for eg in range(NEG):
            b = eg % 2
            for j in range(EG):
                e_ = eg * EG + j
                dma("pool", wgt[b][j][:, :, :], weg_d.ap()[e_].rearrange("(c p) f -> p c f", p=128), writes=["wgt%d" % b])
                dma("pool", wup[b][j][:, :, :], weu_d.ap()[e_].rearrange("(c p) f -> p c f", p=128), writes=["wup%d" % b])
                dma("pool", wdn[b][j][:, :, :], wed_d.ap()[e_].rearrange("(c p) d -> p c d", p=128), writes=["wdn%d" % b])
            for tg in range(4):
                hbi = itc % 2
                itc += 1
                tgs = slice(tg * 512, (tg + 1) * 512)
                for j in range(EG):
                    e_ = eg * EG + j
                    gbk = 6 + j
                    mm(P[gbk][:, :], sel[0:32, e_, :], gT[0:32, 0, tgs], True, False, ["sel", "gT"], [PN[gbk]])
                    mm(P[gbk][:, :], sel[0:32, e_, :], gT[0:32, 1, tgs], False, True, ["sel", "gT"], [PN[gbk]])
                    for fh in range(2):
                        k2 = (j * 2 + fh) % 2
                        gb_, ub_ = 0 + k2, 2 + k2
                        for kc in range(8):
                            mm(P[gb_][:, :], wgt[b][j][:, kc, fh * 128:(fh + 1) * 128], h2T[:, kc, tgs], kc == 0, kc == 7,
                               ["wgt%d" % b, "h2T_%d" % tg], [PN[gb_]])
                        for kc in range(8):
                            mm(P[ub_][:, :], wup[b][j][:, kc, fh * 128:(fh + 1) * 128], h2T[:, kc, tgs], kc == 0, kc == 7,
                               ["wup%d" % b, "h2T_%d" % tg], [PN[ub_]])
                        act(sil[k2][:, :], P[gb_][:, :], AF.Silu, [PN[gb_]], ["sil%d" % k2])
                        tt("dve", t1m[k2][:, :], sil[k2][:, :], P[ub_][:, :], ALU.mult, ["sil%d" % k2, PN[ub_]], ["t1m%d" % k2])
                        tt("dve", hid[hbi][:, j, fh, :], t1m[k2][:, :], P[gbk][:, :], ALU.mult, ["t1m%d" % k2, PN[gbk]],
                           ["hid%d" % hbi])
                for tt_ in range(4):
                    t = tg * 4 + tt_
                    for ch in range(2):
                        abk = 4 + (tt_ * 2 + ch) % 2
                        n_acc = EG * 2
                        a_i = 0
                        for j in range(EG):
                            for fh in range(2):
                                mm(P[abk][:, :], hid[hbi][:, j, fh, tt_ * 128:(tt_ + 1) * 128],
                                   wdn[b][j][:, fh, ch * 512:(ch + 1) * 512], a_i == 0, a_i == n_acc - 1,
                                   ["hid%d" % hbi, "wdn%d" % b], [PN[abk]])
                                a_i += 1
                        tt("dve", X[:, t, ch * 512:(ch + 1) * 512], X[:, t, ch * 512:(ch + 1) * 512], P[abk][:, :], ALU.add,
                           ["X%d" % t, PN[abk]], ["X%d" % t])
        for t in range(NT):
            final_ops.append(dma("sp", out_d[t * 128:(t + 1) * 128, :], X[:, t, :], reads=["X%d" % t]))

        S.emit(es, final_wait_ops=final_ops)
    return nc


def make_consts():
    ident = np.eye(128, dtype=np.float32).astype(ml_dtypes.bfloat16)
    j = np.arange(128)[:, None]
    i = np.arange(128)[None, :]
    same = (j // 64) == (i // 64)
    mf = (same & (j <= i)).astype(np.float32)
    mb = (same & (j > i)).astype(np.float32)
    masks = np.concatenate([mf, mb], axis=1).astype(ml_dtypes.bfloat16)
    invf = (10000.0 ** (-np.arange(0, 32, 2, dtype=np.float32) / 32)).astype(np.float32)
    invf = np.broadcast_to(invf[None, :], (128, 16)).copy()
    sel = np.zeros((32, 32, 128), np.float32)
    for e in range(32):
        sel[e, e, :] = 1.0
    sel = sel.reshape(32, 32 * 128).astype(ml_dtypes.bfloat16)
    lrb = np.zeros((64, 1), np.float32)
    lrb[16, 0] = 1.0
    return {"c_ident": ident, "c_masks": masks, "c_invf": invf, "c_sel": sel, "c_lrbias": lrb}


_NC_CACHE = {}


def make_in_maps(inputs, n_cores=8):
    c = make_consts()
    f = lambda k: np.ascontiguousarray(np.asarray(inputs[k], dtype=np.float32)[0])
    shared = {
        "norm1_gain": f("norm1_gain").reshape(1, D),
        "w_in": f("w_in"),
        "gla_gk_fwd_w": f("gla_gk_fwd_w"), "gla_gk_fwd_b": f("gla_gk_fwd_b").reshape(1, 256),
        "gla_gk_bwd_w": f("gla_gk_bwd_w"), "gla_gk_bwd_b": f("gla_gk_bwd_b").reshape(1, 256),
        "gla_out_gain": f("gla_out_gain").reshape(128, 1),
        "mla_q_gain": f("mla_q_gain").reshape(1, 256), "mla_w_qb": f("mla_w_qb"),
        "mla_kv_gain": f("mla_kv_gain").reshape(1, 128), "mla_w_kvb": f("mla_w_kvb"),
        "q_norm_gain": f("q_norm_gain").reshape(1, 96), "k_norm_gain": f("k_norm_gain").reshape(1, 96),
        "w_out": f("w_out"), "norm2_gain": f("norm2_gain").reshape(1, D),
        "w_router_group": f("w_router_group"), "b_router_group": f("b_router_group").reshape(1, 4),
        "w_router_expert": f("w_router_expert"), "b_router_expert": f("b_router_expert").reshape(1, 32),
        "w_expert_gate": f("w_expert_gate").reshape(32, D, 256),
        "w_expert_up": f("w_expert_up").reshape(32, D, 256),
        "w_expert_down": f("w_expert_down").reshape(32, 256, D),
    }
    shared.update(c)
    x = np.asarray(inputs["x"], dtype=np.float32)
    pos = np.asarray(inputs["positions"]).astype(np.int32)
    maps = []
    for b in range(n_cores):
        m = dict(shared)
        m["x"] = np.ascontiguousarray(x[b])
        m["pos"] = np.ascontiguousarray(pos[b].reshape(NT, 128).T)
        maps.append(m)
    return maps


def kernel(**inputs):
    if "nc" not in _NC_CACHE:
        _NC_CACHE["nc"] = build()
    nc = _NC_CACHE["nc"]
    maps = make_in_maps(inputs, 8)
    res = run_bass_kernel_spmd(nc, maps, core_ids=list(range(8)))
    out = np.stack([np.asarray(r["out"], dtype=np.float32) for r in res.results], axis=0)
    return out
```

```python
import contextlib
import math
import numpy as np
import ml_dtypes
import concourse.bass as bass
import concourse.mybir as mybir
from concourse.bass_utils import run_bass_kernel_spmd

F32 = mybir.dt.float32
BF16 = mybir.dt.bfloat16
I32 = mybir.dt.int32
ALU = mybir.AluOpType
AF = mybir.ActivationFunctionType
AX = mybir.AxisListType

S_LEN = 2048
D = 1024
NT = 16
EPS = 1e-6
PI = math.pi


class T:
    __slots__ = ("name", "w", "r")

    def __init__(self, name):
        self.name = name
        self.w = None
        self.r = []


class Op:
    __slots__ = ("eng", "fn", "deps", "signal", "sig", "dma", "dsem", "dval", "alld", "n", "seg", "idx", "nbytes")


class Sched:
    ENGS = ("pe", "act", "dve", "pool", "sp")

    def __init__(self, nc, n_dma_sems=12):
        self.nc = nc
        self.ops = {e: [] for e in self.ENGS}
        self.n_dma_sems = n_dma_sems
        self.dma_count = {e: 0 for e in self.ENGS}
        self.tiles = {}
        self.pending = {e: [] for e in self.ENGS}
        self.dma_since_barrier = []
        self.stopped = False
        self.seg = 0
        self.nops = 0

    def t(self, name):
        if name not in self.tiles:
            self.tiles[name] = T(name)
        return self.tiles[name]

    def _tl(self, lst):
        out = []
        for x in lst:
            if isinstance(x, str):
                out.append(self.t(x))
            elif isinstance(x, (list, tuple)):
                out.extend(self._tl(x))
            elif x is not None:
                out.append(x)
        return out

    def barrier(self):
        if self.stopped:
            return
        lasts = []
        for e in self.ENGS:
            for o in reversed(self.ops[e]):
                if not o.dma:
                    lasts.append(o)
                    break
        lasts.extend(self.dma_since_barrier)
        self.dma_since_barrier = []
        for e in self.ENGS:
            self.pending[e] = list(lasts)
        self.seg += 1

    def op(self, eng, fn, reads=(), writes=(), dma=False, n=64, nbytes=0):
        if self.stopped:
            return None
        o = Op()
        o.n = n
        o.nbytes = nbytes
        o.seg = self.seg
        o.idx = self.nops
        self.nops += 1
        o.eng = eng
        o.fn = fn
        o.dma = dma
        o.signal = False
        o.sig = 0
        deps = {}
        reads = self._tl(reads)
        writes = self._tl(writes)
        for t in reads:
            if t.w is not None:
                deps[id(t.w)] = (t.w, "raw")
            if t.name[0] == "P" and t.name[1:].isdigit():
                for r in t.r:
                    if id(r) not in deps and r.eng != eng:
                        deps[id(r)] = (r, "war")
        for t in writes:
            if t.w is not None and id(t.w) not in deps:
                deps[id(t.w)] = (t.w, "waw")
            for r in t.r:
                if id(r) not in deps:
                    deps[id(r)] = (r, "war")
        if self.pending[eng]:
            for p in self.pending[eng]:
                deps[id(p)] = (p, "raw")
            self.pending[eng] = []
        o.alld = [p for p, _k in deps.values()]
        dl = []
        for p, kind in deps.values():
            if p.eng == eng and not p.dma:
                if eng == "pe":
                    continue
                if kind != "raw" and not dma and eng != "pool":
                    continue
            dl.append(p)
        o.deps = dl
        for p in dl:
            p.signal = True
        for t in reads:
            if not dma:
                for r in t.r:
                    if not r.dma and r.eng == eng:
                        o.alld.append(r)
                t.r = [r for r in t.r if r.dma or r.eng != eng]
            t.r.append(o)
        for t in writes:
            t.w = o
            t.r = []
        if dma:
            self.dma_count[eng] += 1
            self.dma_since_barrier.append(o)
        self.ops[eng].append(o)
        return o

    @staticmethod
    def _dur(o):
        n = o.n
        if o.dma:
            return 60.0 if o.eng == "sp" else 900.0
        if o.eng == "pe":
            return 30.0 + max(n, 64) / 2.0
        if o.eng == "act":
            return 220.0 + n / 1.4
        if o.eng == "dve":
            return 120.0 + n * 1.3
        return 550.0 + n * 0.75

    def reschedule(self):
        allops = []
        for e in self.ENGS:
            allops.extend(self.ops[e])
        allops.sort(key=lambda o: o.idx)
        import heapq
        new = {e: [] for e in self.ENGS}
        segs = {}
        for o in allops:
            segs.setdefault(o.seg, []).append(o)
        LAT = 250.0
        for sg in sorted(segs):
            ops = segs[sg]
            inseg = set(id(o) for o in ops)
            done = {}
            users = {}
            indeg = {}
            first = {}
            for o in ops:
                if o.eng not in first:
                    first[o.eng] = o
                elif first[o.eng] not in o.alld:
                    o.alld.append(first[o.eng])
            for o in ops:
                k = 0
                for p in o.alld:
                    if id(p) in inseg:
                        k += 1
                        users.setdefault(id(p), []).append(o)
                indeg[id(o)] = k
            ready = {e: [] for e in self.ENGS}
            efree = {e: 0.0 for e in self.ENGS}
            rtime = {}
            for o in ops:
                if indeg[id(o)] == 0:
                    rtime[id(o)] = 0.0
                    heapq.heappush(ready[o.eng], (0.0, o.idx, o))
            left = len(ops)
            while left:
                best = None
                for e in self.ENGS:
                    if ready[e]:
                        rt, ix, o = ready[e][0]
                        st = max(rt, efree[e])
                        if best is None or (st, ix) < (best[0], best[1]):
                            best = (st, ix, e)
                st, ix, e = best
                rt, ix, o = heapq.heappop(ready[e])
                d = self._dur(o)
                efree[e] = st + d
                fin = st + d
                if o.dma:
                    fin = st + 2000.0 + o.nbytes / 150.0
                done[id(o)] = fin
                new[e].append(o)
                left -= 1
                for u in users.get(id(o), ()):
                    indeg[id(u)] -= 1
                    lat = 0.0 if (u.eng == o.eng and not o.dma) else LAT
                    rtime[id(u)] = max(rtime.get(id(u), 0.0), fin + lat)
                    if indeg[id(u)] == 0:
                        heapq.heappush(ready[u.eng], (rtime[id(u)], u.idx, u))
        self.ops = new

    def emit(self, es, final_wait_ops=()):
        nc = self.nc
        if RESCHEDULE:
            self.reschedule()
        sems = {e: es.enter_context(nc.semaphore("s_" + e)) for e in self.ENGS}
        dsems = {e: [es.enter_context(nc.semaphore("d_%s_%d" % (e, i)))
                     for i in range(self.n_dma_sems)]
                 for e in self.ENGS if self.dma_count[e] > 0}
        for e in self.ENGS:
            c = 0
            i = 0
            for o in self.ops[e]:
                if o.dma:
                    o.dsem = i % self.n_dma_sems
                    o.dval = 16 * (i // self.n_dma_sems + 1)
                    i += 1
                elif o.signal:
                    c += 1
                    o.sig = c
        block = es.enter_context(nc.Block())
        eng_obj = {"pe": block.tensor, "act": block.scalar, "dve": block.vector,
                   "pool": block.gpsimd, "sp": block.sync}
        for e in self.ENGS:
            ops = self.ops[e]
            if not ops:
                continue

            def body(engine, e=e, ops=ops):
                waited = {}

                def wait(sem, key, val):
                    if waited.get(key, 0) >= val:
                        return
                    waited[key] = val
                    engine.wait_ge(sem, val)

                for o in ops:
                    for p in o.deps:
                        if p.dma:
                            wait(dsems[p.eng][p.dsem], ("d", p.eng, p.dsem), p.dval)
                        else:
                            wait(sems[p.eng], ("c", p.eng), p.sig)
                    if o.dma and o.dval > 16:
                        wait(dsems[e][o.dsem], ("d", e, o.dsem), o.dval - 16)
                    ins = o.fn(engine)
                    if o.dma:
                        ins.then_inc(dsems[e][o.dsem], 16)
                    elif o.signal:
                        ins.then_inc(sems[e], 1)
                if e == "sp":
                    for o in final_wait_ops:
                        if o is None:
                            continue
                        wait(dsems[o.eng][o.dsem], ("d", o.eng, o.dsem), o.dval)

            eng_obj[e](body)


RESCHEDULE = True
SB_BASE = 16640
SB_END = 229376


class Alloc:
    def __init__(self, nc):
        self.nc = nc
        self.off = SB_BASE
        self.n = 0

    def mark(self):
        return self.off

    def reset(self, m):
        self.off = m

    def __call__(self, name, shape, dt, at=None):
        esz = 2 if dt == BF16 else 4
        nb = int(np.prod(shape[1:])) * esz
        nb = (nb + 63) // 64 * 64
        self.n += 1
        if at is None:
            at = self.off
            self.off += nb
        assert at + nb <= SB_END, ("SBUF overflow", name, at, nb)
        return self.nc.alloc_sbuf_tensor_at("%s_%d" % (name, self.n), list(shape), dt, offset=at)


def bcast_ap(ap, pattern):
    return bass.AP(ap.tensor, ap.offset, [list(ap.ap[0])] + [list(p) for p in pattern])


def build(debug=False, stop_after=None):
    nc = bass.Bass("TRN2", target_bir_lowering=False)
    dr = lambda n, s, dt=F32: nc.dram_tensor(n, list(s), dt, kind="ExternalInput")
    x_d = dr("x", [S_LEN, D])
    pos_d = dr("pos", [128, NT], I32)
    g1_d = dr("norm1_gain", [1, D])
    win_d = dr("w_in", [D, 1984])
    gkf_w = dr("gla_gk_fwd_w", [16, 256])
    gkf_b = dr("gla_gk_fwd_b", [1, 256])
    gkb_w = dr("gla_gk_bwd_w", [16, 256])
    gkb_b = dr("gla_gk_bwd_b", [1, 256])
    go_d = dr("gla_out_gain", [128, 1])
    gqa_d = dr("mla_q_gain", [1, 256])
    wqb_d = dr("mla_w_qb", [256, 768])
    gkva_d = dr("mla_kv_gain", [1, 128])
    wkvb_d = dr("mla_w_kvb", [128, 1024])
    gqn_d = dr("q_norm_gain", [1, 96])
    gkn_d = dr("k_norm_gain", [1, 96])
    wout_d = dr("w_out", [D, D])
    g2_d = dr("norm2_gain", [1, D])
    wrg_d = dr("w_router_group", [D, 4])
    brg_d = dr("b_router_group", [1, 4])
    wre_d = dr("w_router_expert", [D, 32])
    bre_d = dr("b_router_expert", [1, 32])
    weg_d = dr("w_expert_gate", [32, D, 256])
    weu_d = dr("w_expert_up", [32, D, 256])
    wed_d = dr("w_expert_down", [32, 256, D])
    ident_d = dr("c_ident", [128, 128], BF16)
    masks_d = dr("c_masks", [128, 256], BF16)
    invf_d = dr("c_invf", [128, 16])
    sel_d = dr("c_sel", [32, 32 * 128], BF16)
    lrb_d = dr("c_lrbias", [64, 1])
    out_d = nc.dram_tensor("out", [S_LEN, D], F32, kind="ExternalOutput")
    dbg = {}
    if debug:
        dbg["mix"] = nc.dram_tensor("d_mix", [128, 8 * S_LEN], BF16, kind="ExternalOutput")
        dbg["x1"] = nc.dram_tensor("d_x1", [S_LEN, D], F32, kind="ExternalOutput")
        dbg["gate"] = nc.dram_tensor("d_gate", [S_LEN, 32], F32, kind="ExternalOutput")

    if debug:
        dbg["gen"] = nc.dram_tensor("d_gen", [128, 8 * S_LEN], BF16, kind="ExternalOutput")
    S = Sched(nc)
    A = Alloc(nc)
    op = S.op
    final_ops = []

    with contextlib.ExitStack() as es:
        P = [es.enter_context(nc.psum_tensor("pb%d" % i, [128, 512], F32)) for i in range(8)]
        Pb = [p.bitcast(BF16) for p in P]
        PN = ["P%d" % i for i in range(8)]

        def fsz(ap):
            r = 1
            for d_ in list(ap.shape)[1:]:
                r *= int(d_)
            return r

        def dma(q, out, in_, reads=(), writes=(), **kw):
            return op(q, lambda e: e.dma_start(out=out, in_=in_, **kw), reads=reads, writes=writes, dma=True,
                      nbytes=fsz(out) * 4 * 128)

        def act(out, in_, func, reads, writes, **kw):
            return op("act", lambda e: e.activation(out=out, in_=in_, func=func, **kw), reads=reads, writes=writes,
                      n=fsz(out))

        def rsqrt_act(out, in_, n, reads, writes):
            act(out, in_, AF.Ln, reads, writes, scale=1.0 / n, bias=EPS)
            act(out, out, AF.Exp, writes, writes, scale=-0.5)

        def tt(eng, out, in0, in1, o, reads, writes):
            return op(eng, lambda e: e.tensor_tensor(out=out, in0=in0, in1=in1, op=o), reads=reads, writes=writes,
                      n=fsz(out))

        def ts(eng, out, in0, s1, s2, o0, o1, reads, writes):
            if o1 is None:
                return op(eng, lambda e: e.tensor_scalar(out=out, in0=in0, scalar1=s1, scalar2=None, op0=o0),
                          reads=reads, writes=writes, n=fsz(out))
            return op(eng, lambda e: e.tensor_scalar(out=out, in0=in0, scalar1=s1, scalar2=s2, op0=o0, op1=o1),
                      reads=reads, writes=writes, n=fsz(out))

        def stt(out, in0, sc, in1, o0, o1, reads, writes):
            return op("dve", lambda e: e.scalar_tensor_tensor(out=out, in0=in0, scalar=sc, in1=in1, op0=o0, op1=o1),
                      reads=reads, writes=writes, n=fsz(out))

        def mm(out, lhsT, rhs, start, stop, reads, writes):
            return op("pe", lambda e: e.matmul(out, lhsT=lhsT, rhs=rhs, start=start, stop=stop),
                      reads=reads, writes=writes, n=fsz(rhs) * (4 if rhs.dtype == F32 else 1))

        def tr(out, in_, ident, reads, writes):
            return op("pe", lambda e: e.transpose(out=out, in_=in_, identity=ident), reads=reads, writes=writes, n=128)

        def cp(eng, out, in_, reads, writes):
            if eng == "act":
                return act(out, in_, AF.Copy, reads, writes)
            return op(eng, lambda e: e.tensor_copy(out=out, in_=in_), reads=reads, writes=writes, n=fsz(out))

        def memset(eng, ap, val, writes):
            return op(eng, lambda e: e.memset(ap, val), writes=writes, n=fsz(ap))

        def bcast_row(dram, n):
            return bass.AP(dram, 0, [[0, 128], [1, n]])

        ident = A("ident", [128, 128], BF16)
        mixT = A("mixT", [128, 8, S_LEN], BF16)
        dma("sp", ident[:, :], ident_d[:, :], writes=["ident"])
        L0 = A.mark()

        hT = A("hT", [128, 8, S_LEN], BF16)
        E1 = A.mark()
        g1 = A("g1", [128, D], F32)
        xt = [A("xt%d" % i, [128, D], F32) for i in range(2)]
        hb = [A("hb%d" % i, [128, D], BF16) for i in range(2)]
        sqj = A("sqj", [128, D], F32)
        st1 = A("st1", [128, 4], F32)
        dma("sp", g1[:, :], bcast_row(g1_d, D), writes=["g1"])

        def norm_to_T(src_ap_fn, src_tiles, gain, gname, dstT, dname, pfx, t, pbank):
            i = t % 2
            ssq = st1[:, 0:1]
            rs = st1[:, 1:2]
            act(sqj[:, :], src_ap_fn(t), AF.Square, src_tiles, [pfx + "sqj", pfx + "ssq"], accum_out=ssq)
            rsqrt_act(rs, ssq, D, [pfx + "ssq"], [pfx + "rs"])
            stt(hb[i][:, :], src_ap_fn(t), rs, gain[:, :], ALU.mult, ALU.mult,
                src_tiles + [pfx + "rs", gname], [pfx + "hb%d" % i])
            pbv = Pb[pbank][:, :].rearrange("p (c n) -> p c n", c=8)
            for kc in range(8):
                tr(pbv[:, kc, :], hb[i][:, kc * 128:(kc + 1) * 128], ident[:, :],
                   [pfx + "hb%d" % i, "ident"], [PN[pbank]])
            cp("dve" if t % 2 else "act", dstT[:, :, t * 128:(t + 1) * 128], pbv, [PN[pbank]], [dname + "_%d" % (t // 4)])

        for t in range(NT):
            i = t % 2
            dma("sp", xt[i][:, :], x_d[t * 128:(t + 1) * 128, :], writes=["xt%d" % i])
            norm_to_T(lambda t, i=i: xt[i][:, :], ["xt%d" % i], g1, "g1", hT, "hT", "A", t, t % 2)
        hT_tiles = ["hT_%d" % k for k in range(4)]
        S.barrier()
        A.reset(E1)
        def checkpoint(name, dump=None, reads=()):
            if stop_after == name:
                if dump is not None and debug:
                    S.barrier()
                    final_ops.append(dma("sp", dbg["gen"][:, :], dump, reads=list(reads)))
                S.stopped = True

        checkpoint("A")

        wm = A("w_in_mla", [128, 8, 416], BF16)
        wqb = A("wqb", [128, 2, 768], BF16)
        wkvb = A("wkvb", [128, 1024], BF16)
        cs = A("cs", [128, NT, 32], F32)
        qhT = A("qhT", [128, 8, S_LEN], BF16)
        khT = A("khT", [128, 8, S_LEN], BF16)
        vA = A("vA", [128, NT, 8, 128], BF16)
        gqa = A("gqa", [128, 384], F32)
        gqk = A("gqk", [128, 16, 96], F32)
        B1m = A.mark()
        dma("pool", wm[:, :, :], win_d.ap()[:, 1568:1984].rearrange("(c p) n -> p c n", p=128), writes=["wm"])
        dma("pool", wqb[:, :, :], wqb_d.ap().rearrange("(c p) n -> p c n", p=128), writes=["wqb"])
        dma("pool", wkvb[:, :], wkvb_d[:, :], writes=["wkvb"])
        dma("sp", gqa[:, 0:256], bcast_row(gqa_d, 256), writes=["gqa"])
        dma("sp", gqa[:, 256:384], bcast_row(gkva_d, 128), writes=["gqa"])
        dma("sp", gqk[:, 0:8, :], bass.AP(gqn_d, 0, [[0, 128], [0, 8], [1, 96]]), writes=["gqk"])
        dma("sp", gqk[:, 8:16, :], bass.AP(gkn_d, 0, [[0, 128], [0, 8], [1, 96]]), writes=["gqk"])
        posi = A("posi", [128, NT], I32)
        posf = A("posf", [128, NT], F32)
        invf = A("invf", [128, 16], F32)
        ang = A("ang", [128, NT, 16], F32)
        kk = A("kk", [128, NT, 16], F32)
        ki = A("ki", [128, NT, 16], I32)
        rr = A("rr", [128, NT, 16], F32)
        yy = A("yy", [128, NT, 16], F32)
        m_ = A("m_", [128, NT, 16], F32)
        dma("sp", posi[:, :], pos_d[:, :], writes=["posi"])
        dma("sp", invf[:, :], invf_d[:, :], writes=["invf"])
        cp("dve", posf[:, :], posi[:, :], ["posi"], ["posf"])
        for t in range(NT):
            ts("dve", ang[:, t, :], invf[:, :], posf[:, t:t + 1], None, ALU.mult, None, ["invf", "posf"], ["ang"])
        ts("dve", kk[:, :, :], ang[:, :, :], 1.0 / (2 * PI), None, ALU.mult, None, ["ang"], ["kk"])
        cp("dve", ki[:, :, :], kk[:, :, :], ["kk"], ["ki"])
        cp("dve", kk[:, :, :], ki[:, :, :], ["ki"], ["kk"])
        stt(rr[:, :, :], kk[:, :, :], -2 * PI, ang[:, :, :], ALU.mult, ALU.add, ["kk", "ang"], ["rr"])
        for which, shift in ((1, 0.0), (0, PI / 2)):
            ts("dve", yy[:, :, :], rr[:, :, :], shift, None, ALU.add, None, ["rr"], ["yy"])
            ts("dve", m_[:, :, :], yy[:, :, :], PI, None, ALU.is_gt, None, ["yy"], ["m_"])
            stt(yy[:, :, :], m_[:, :, :], -2 * PI, yy[:, :, :], ALU.mult, ALU.add, ["m_", "yy"], ["yy"])
            ts("dve", m_[:, :, :], yy[:, :, :], -PI, None, ALU.is_lt, None, ["yy"], ["m_"])
            stt(yy[:, :, :], m_[:, :, :], 2 * PI, yy[:, :, :], ALU.mult, ALU.add, ["m_", "yy"], ["yy"])
            ts("dve", yy[:, :, :], yy[:, :, :], PI, -PI, ALU.min, ALU.max, ["yy"], ["yy"])
            act(cs[:, :, which * 16:(which + 1) * 16], yy[:, :, :], AF.Sin, ["yy"], ["cs"])
        memset("pool", vA[:, :, :, :], 1.0, ["vA"])
        S.barrier()
        A.reset(B1m)

        sqjb = [A("sqjb%d" % i, [128, 416], BF16) for i in range(2)]
        stq = [A("stq%d" % i, [128, 32], F32) for i in range(2)]
        ab = [A("ab%d" % i, [128, 384], BF16) for i in range(2)]
        abT = [A("abT%d" % i, [128, 3, 128], BF16) for i in range(2)]
        kraw = [A("kraw%d" % i, [128, 8, 96], F32) for i in range(2)]
        sqn = A("sqn", [128, 16, 96], F32)
        rg = A("rg", [128, 16, 32], F32)
        ra = A("ra", [128, 16, 16], F32)
        rb = A("rb", [128, 16, 16], F32)
        qkf = A("qkf", [128, 16, 96], BF16)
        SQ2 = math.sqrt(2.0)

        for t in range(NT):
            i = t % 2
            sI = "_%d" % i
            tsl = slice(t * 128, (t + 1) * 128)
            hTt = "hT_%d" % (t // 4)
            for kc in range(8):
                mm(P[0][:, 0:416], hT[:, kc, tsl], wm[:, kc, :], kc == 0, kc == 7, [hTt, "wm"], ["P0"])
            act(sqjb[i][:, 0:256], P[0][:, 0:256], AF.Square, ["P0"], ["sqjb" + sI, "stqA" + sI], accum_out=stq[i][:, 0:1])
            act(sqjb[i][:, 256:384], P[0][:, 256:384], AF.Square, ["P0"], ["sqjb" + sI, "stqA" + sI],
                accum_out=stq[i][:, 1:2], scale=SQ2)
            rsqrt_act(stq[i][:, 2:4], stq[i][:, 0:2], 256, ["stqA" + sI], ["stqB" + sI])
            stt(ab[i][:, 0:256], P[0][:, 0:256], stq[i][:, 2:3], gqa[:, 0:256], ALU.mult, ALU.mult,
                ["P0", "stqB" + sI, "gqa"], ["ab" + sI])
            stt(ab[i][:, 256:384], P[0][:, 256:384], stq[i][:, 3:4], gqa[:, 256:384], ALU.mult, ALU.mult,
                ["P0", "stqB" + sI, "gqa"], ["ab" + sI])
            act(kraw[i][:, :, 64:96], bcast_ap(P[0][:, 384:416], [[0, 8], [1, 32]]), AF.Copy, ["P0"], ["krawR" + sI])
            p1v = Pb[1][:, 0:384].rearrange("p (c n) -> p c n", c=3)
            for c in range(3):
                tr(p1v[:, c, :], ab[i][:, c * 128:(c + 1) * 128], ident[:, :], ["ab" + sI, "ident"], ["P1"])
            cp("act", abT[i][:, :, :], p1v, ["P1"], ["abT" + sI])
            for nb in range(2):
                for kc in range(2):
                    mm(P[2 + nb][:, 0:384], abT[i][:, kc, :], wqb[:, kc, nb * 384:(nb + 1) * 384], kc == 0, kc == 1,
                       ["abT" + sI, "wqb"], [PN[2 + nb]])
                mm(P[4 + nb][:, :], abT[i][:, 2, :], wkvb[:, nb * 512:(nb + 1) * 512], True, True,
                   ["abT" + sI, "wkvb"], [PN[4 + nb]])
            for nb in range(2):
                srck = P[4 + nb][:, :].rearrange("p (h d) -> p h d", h=4)[:, :, 0:64]
                cp("dve", kraw[i][:, nb * 4:nb * 4 + 4, 0:64], srck, [PN[4 + nb]], ["krawN" + sI])
                srcv = P[4 + nb][:, :].rearrange("p (a b d) -> p a b d", a=2, b=2)
                dstv = vA[:, t, nb * 4:nb * 4 + 4, :].rearrange("p (a b) d -> p a b d", b=2)
                cp("act", dstv[:, :, 0, 0:64], srcv[:, :, 0, 64:128], [PN[4 + nb]], ["vA"])
                cp("act", dstv[:, :, 1, 64:128], srcv[:, :, 1, 64:128], [PN[4 + nb]], ["vA"])
            for nb in range(2):
                act(sqn[:, nb * 4:nb * 4 + 4, :], P[2 + nb][:, 0:384].rearrange("p (h d) -> p h d", h=4), AF.Square,
                    [PN[2 + nb]], ["sqn"])
            act(sqn[:, 8:16, :], kraw[i][:, :, :], AF.Square, ["krawN" + sI, "krawR" + sI], ["sqn"])
            op("dve", lambda e, i=i: e.tensor_reduce(out=stq[i][:, 8:24], in_=sqn[:, :, :], axis=AX.X, op=ALU.add),
               reads=["sqn"], writes=["stqC" + sI])
            rsqrt_act(stq[i][:, 8:24], stq[i][:, 8:24], 96, ["stqC" + sI], ["stqC" + sI])
            for nb in range(2):
                tt("dve", sqn[:, nb * 4:nb * 4 + 4, :], P[2 + nb][:, 0:384].rearrange("p (h d) -> p h d", h=4),
                   bcast_ap(stq[i][:, 8 + nb * 4:9 + nb * 4], [[1, 4], [0, 96]]), ALU.mult,
                   [PN[2 + nb], "stqC" + sI], ["sqn"])
            tt("dve", sqn[:, 8:16, :], kraw[i][:, :, :], bcast_ap(stq[i][:, 16:17], [[1, 8], [0, 96]]), ALU.mult,
               ["krawN" + sI, "krawR" + sI, "stqC" + sI], ["sqn"])
            tt("pool", qkf[:, :, 0:64], sqn[:, :, 0:64], gqk[:, :, 0:64], ALU.mult, ["sqn", "gqk"], ["qkf"])
            tt("pool", rg[:, :, :], sqn[:, :, 64:96], gqk[:, :, 64:96], ALU.mult, ["sqn", "gqk"], ["rg"])
            cosb = bcast_ap(cs[:, t, 0:16], [[0, 16], [1, 16]])
            sinb = bcast_ap(cs[:, t, 16:32], [[0, 16], [1, 16]])
            t1 = rg[:, :, 0:16]
            t2 = rg[:, :, 16:32]
            tt("pool", ra[:, :, :], t1, cosb, ALU.mult, ["rg", "cs"], ["ra"])
            tt("pool", rb[:, :, :], t2, sinb, ALU.mult, ["rg", "cs"], ["rb"])
            tt("pool", qkf[:, :, 64:80], ra[:, :, :], rb[:, :, :], ALU.subtract, ["ra", "rb"], ["qkf"])
            tt("pool", ra[:, :, :], t1, sinb, ALU.mult, ["rg", "cs"], ["ra"])
            tt("pool", rb[:, :, :], t2, cosb, ALU.mult, ["rg", "cs"], ["rb"])
            tt("pool", qkf[:, :, 80:96], ra[:, :, :], rb[:, :, :], ALU.add, ["ra", "rb"], ["qkf"])
            p6v = Pb[6][:, :].rearrange("p (h n) -> p h n", h=8)
            p7v = Pb[7][:, :].rearrange("p (h n) -> p h n", h=8)
            for h in range(8):
                tr(p6v[0:96, h, :], qkf[:, h, :], ident[:, :], ["qkf", "ident"], ["P6"])
            for h in range(8):
                tr(p7v[0:96, h, :], qkf[:, 8 + h, :], ident[:, :], ["qkf", "ident"], ["P7"])
            cp("dve", qhT[0:96, :, tsl], p6v[0:96, :, :], ["P6"], ["qhT_%d" % (t // 4)])
            cp("act", khT[0:96, :, tsl], p7v[0:96, :, :], ["P7"], ["khT"])
        S.barrier()
        A.reset(B1m)
        checkpoint("B1")
        pbuf = [A("pbuf%d" % i, [128, 512], BF16) for i in range(4)]
        lnb = A("lnb", [128, 512], F32)
        rcb = A("rcb", [128, 512], F32)
        scale = 96 ** -0.5
        it = 0
        for h in range(8):
            even = (h % 2 == 0)
            vrows = slice(0, 64) if even else slice(64, 128)
            srows = slice(64, 128) if even else slice(0, 64)
            for qg in range(4):
                qsl = slice(qg * 512, (qg + 1) * 512)
                ob = 4 + (it % 2)
                seq = []
                for kt in range(16):
                    seq.append(("s", kt))
                    if kt >= 2:
                        seq.append(("pv", kt - 2))
                seq += [("pv", 14), ("pv", 15)]
                for kind, kt in seq:
                    sb_ = kt % 3
                    pi = kt % 4
                    if kind == "s":
                        mm(P[sb_][:, :], khT[0:96, h, kt * 128:(kt + 1) * 128], qhT[0:96, h, qsl], True, True,
                           ["khT", "qhT_%d" % qg], [PN[sb_]])
                        act(pbuf[pi][:, :], P[sb_][:, :], AF.Exp, [PN[sb_]], ["pbuf%d" % pi], scale=scale)
                    else:
                        lhsT = vA[:, kt, h, :]
                        mm(P[ob][:, :], lhsT, pbuf[pi][:, :], kt == 0, kt == 15, ["vA", "pbuf%d" % pi], [PN[ob]])
                act(lnb[vrows, :], P[ob][srows, :], AF.Ln, [PN[ob]], ["lnb"])
                act(rcb[vrows, :], lnb[vrows, :], AF.Exp, ["lnb"], ["rcb"], scale=-1.0)
                tt("dve", mixT[vrows, 4 + h // 2, qsl], P[ob][vrows, :], rcb[vrows, :], ALU.mult, [PN[ob], "rcb"], ["mixT_m"])
                it += 1
        S.barrier()
        A.reset(E1)

        checkpoint("C")
        wg = A("w_in_gla", [128, 8, 1568], BF16)
        R2 = A.mark()
        qkT = A("qkT", [128, 4, S_LEN], F32)
        vtok = A("vtok", [128, NT, 512], BF16)
        sgT = A("sgT", [128, 4, S_LEN], BF16)
        lrT = A("lrT", [64, S_LEN], F32)
        wlr = A("wlr", [128, 8, 64], BF16)
        lrb = A("lrb", [64, 1], F32)
        waug = A("waug", [64, 512], F32)
        masks = A("masks", [128, 256], BF16)
        gout = A("gout", [128, 1], F32)
        onesf = A("onesf", [128, 128], F32)
        R4 = A.mark()
        dma("pool", wg[:, :, :], win_d.ap()[:, 0:1568].rearrange("(c p) n -> p c n", p=128), writes=["wg"])
        memset("pool", wlr[:, :, :], 0.0, ["wlr"])
        dma("pool", wlr[:, :, 0:16], win_d.ap()[:, 1536:1552].rearrange("(c p) n -> p c n", p=128), reads=["wlr"], writes=["wlr"])
        dma("pool", wlr[:, :, 32:48], win_d.ap()[:, 1552:1568].rearrange("(c p) n -> p c n", p=128), reads=["wlr"], writes=["wlr"])
        dma("sp", lrb[:, :], lrb_d[:, :], writes=["lrb"])
        memset("pool", waug[:, :], 0.0, ["waug"])
        dma("sp", waug[0:16, 0:256], gkf_w[:, :], reads=["waug"], writes=["waug"])
        dma("sp", waug[16:17, 0:256], gkf_b[:, :], reads=["waug"], writes=["waug"])
        dma("sp", waug[16:17, 256:512], gkb_b[:, :], reads=["waug"], writes=["waug"])
        dma("sp", waug[32:48, 256:512], gkb_w[:, :], reads=["waug"], writes=["waug"])
        dma("sp", masks[:, :], masks_d[:, :], writes=["masks"])
        dma("sp", gout[:, :], go_d[:, :], writes=["gout"])
        memset("pool", onesf[:, :], 1.0, ["onesf"])
        blk = 0
        for kind, idx in [("q", 0), ("q", 1), ("k", 0), ("k", 1), ("g", 0), ("g", 1), ("g", 2), ("g", 3), ("lr", 0)]:
            for tg in range(4):
                pbk = blk % 4
                blk += 1
                tgs = slice(tg * 512, (tg + 1) * 512)
                for kc in range(8):
                    if kind == "q":
                        lhsT = wg[:, kc, idx * 128:(idx + 1) * 128]
                    elif kind == "k":
                        lhsT = wg[:, kc, 256 + idx * 128:256 + (idx + 1) * 128]
                    elif kind == "g":
                        lhsT = wg[:, kc, 1024 + idx * 128:1024 + (idx + 1) * 128]
                    else:
                        lhsT = wlr[:, kc, :]
                    mrows = 64 if kind == "lr" else 128
                    mm(P[pbk][0:mrows, :], lhsT, hT[:, kc, tgs], kc == 0, kc == 7, ["hT_%d" % tg, "wg", "wlr"], [PN[pbk]])
                if kind == "q":
                    act(qkT[:, idx, tgs], P[pbk][:, :], AF.Copy, [PN[pbk]], ["qT"], scale=0.125)
                elif kind == "k":
                    cp("dve", qkT[:, 2 + idx, tgs], P[pbk][:, :], [PN[pbk]], ["kT"])
                elif kind == "g":
                    act(sgT[:, idx, tgs], P[pbk][:, :], AF.Silu, [PN[pbk]], ["sgT"])
                else:
                    act(lrT[:, tgs], P[pbk][0:64, :], AF.Identity, [PN[pbk], "lrb"], ["lrT"], bias=lrb[:, :])
        for t in range(NT):
            pbk = 4 + t % 2
            tsl = slice(t * 128, (t + 1) * 128)
            for kc in range(8):
                mm(P[pbk][:, :], hT[:, kc, tsl], wg[:, kc, 512:1024], kc == 0, kc == 7, ["hT_%d" % (t // 4), "wg"], [PN[pbk]])
            cp("dve" if t % 2 else "act", vtok[:, t, :], P[pbk][:, :], [PN[pbk]], ["vtok"])
        S.barrier()

        checkpoint("B2")
        HTB = L0
        tmp = [A("gt%d" % i, [128, 1024], F32, at=HTB + i * 4096) for i in range(4)]
        prod = {}
        names = [(d_, hp, k_) for d_ in (0, 1) for hp in (0, 1) for k_ in ("qr", "kr", "qb")]
        slots = [HTB + 16384 + i * 4096 for i in range(4)] + [E1 + 16384 + i * 4096 for i in range(2)]
        for i, nm in enumerate(names):
            if i < 6:
                prod[nm] = A("pr", [128, S_LEN], BF16, at=slots[i])
            else:
                prod[nm] = A("pr", [128, S_LEN], BF16)
        kdT = A("kdT", [128, 1024], BF16)
        dec = A("dec", [128, 4, 32], F32)
        smask = A("smask", [128, 1024], F32)
        kd = A("kd", [128, NT, 512], BF16, at=E1)
        memset("pool", smask[:, :], 1.0, ["smask"])
        memset("pool", smask[:, :].rearrange("p (c j) -> p c j", j=64)[:, :, 0:1], 0.0, ["smask"])
        G, Fc, Dt, Eb = tmp
        for d_ in (0, 1):
            for hp in (0, 1):
                dh = d_ * 2 + hp
                qT = qkT[:, hp, :]
                kT = qkT[:, 2 + hp, :]
                for half in range(2):
                    hs = slice(half * 1024, (half + 1) * 1024)
                    for j in range(2):
                        pbk = j
                        cols = slice(half * 1024 + j * 512, half * 1024 + (j + 1) * 512)
                        mm(P[pbk][:, :], waug[0:64, dh * 128:(dh + 1) * 128], lrT[0:64, cols], True, True,
                           ["waug", "lrT"], [PN[pbk]])
                        act(Eb[:, j * 512:(j + 1) * 512], P[pbk][:, :], AF.Exp, [PN[pbk]], ["Eb"], scale=-1.0)
                    act(G[:, :], Eb[:, :], AF.Ln, ["Eb"], ["G"], bias=1.0)
                    op("dve", lambda e: e.tensor_tensor_scan(out=Fc[:, :], data0=smask[:, :], data1=G[:, :], initial=0.0,
                                                             op0=ALU.mult, op1=ALU.add), reads=["smask", "G"], writes=["Fc"])
                    Fv = Fc[:, :].rearrange("p (c j) -> p c j", j=64)
                    Dv = Dt[:, :].rearrange("p (c j) -> p c j", j=64)
                    T63 = bcast_ap(Fc[:, 63:64], [[64, 16], [0, 64]])
                    act(dec[:, dh, half * 16:(half + 1) * 16], Fv[:, :, 63], AF.Exp, ["Fc"], ["dec"], scale=-1.0 / 16)
                    if d_ == 0:
                        ref = bcast_ap(Fc[:, 31:32], [[64, 16], [0, 64]])
                        tt("dve", Dv, Fv, ref, ALU.subtract, ["Fc"], ["Dt"])
                        act(Eb[:, :], Dt[:, :], AF.Exp, ["Dt"], ["Eb"], scale=-1.0 / 16)
                        tt("pool", prod[(0, hp, "qr")][:, hs], qT[:, hs], Eb[:, :], ALU.mult, ["qT", "kT", "Eb"], ["prod"])
                        act(Eb[:, :], Dt[:, :], AF.Exp, ["Dt"], ["Eb"], scale=1.0 / 16)
                        tt("pool", prod[(0, hp, "kr")][:, hs], kT[:, hs], Eb[:, :], ALU.mult, ["qT", "kT", "Eb"], ["prod"])
                        act(Eb[:, :], Fc[:, :], AF.Exp, ["Fc"], ["Eb"], scale=-1.0 / 16)
                        tt("pool", prod[(0, hp, "qb")][:, hs], qT[:, hs], Eb[:, :], ALU.mult, ["qT", "kT", "Eb"], ["prod"])
                        tt("dve", Dv, Fv, T63, ALU.subtract, ["Fc"], ["Dt"])
                        act(Eb[:, :], Dt[:, :], AF.Exp, ["Dt"], ["Eb"], scale=1.0 / 16)
                        tt("pool", kdT[:, :], kT[:, hs], Eb[:, :], ALU.mult, ["qT", "kT", "Eb"], ["kdT"])
                    else:
                        tt("dve", G[:, :], Fc[:, :], G[:, :], ALU.subtract, ["Fc", "G"], ["G"])
                        Gv = G[:, :].rearrange("p (c j) -> p c j", j=64)
                        ref = bcast_ap(G[:, 32:33], [[64, 16], [0, 64]])
                        tt("dve", Dv, Gv, ref, ALU.subtract, ["G"], ["Dt"])
                        act(Eb[:, :], Dt[:, :], AF.Exp, ["Dt"], ["Eb"], scale=1.0 / 16)
                        tt("pool", prod[(1, hp, "qr")][:, hs], qT[:, hs], Eb[:, :], ALU.mult, ["qT", "kT", "Eb"], ["prod"])
                        act(Eb[:, :], Dt[:, :], AF.Exp, ["Dt"], ["Eb"], scale=-1.0 / 16)
                        tt("pool", prod[(1, hp, "kr")][:, hs], kT[:, hs], Eb[:, :], ALU.mult, ["qT", "kT", "Eb"], ["prod"])
                        tt("dve", Dv, Gv, T63, ALU.subtract, ["G", "Fc"], ["Dt"])
                        act(Eb[:, :], Dt[:, :], AF.Exp, ["Dt"], ["Eb"], scale=1.0 / 16)
                        tt("pool", prod[(1, hp, "qb")][:, hs], qT[:, hs], Eb[:, :], ALU.mult, ["qT", "kT", "Eb"], ["prod"])
                        act(Eb[:, :], G[:, :], AF.Exp, ["G"], ["Eb"], scale=-1.0 / 16)
                        tt("pool", kdT[:, :], kT[:, hs], Eb[:, :], ALU.mult, ["qT", "kT", "Eb"], ["kdT"])
                    for g4 in range(2):
                        pbk = 2 + g4
                        pv = Pb[pbk][:, 0:512].rearrange("p (t n) -> p t n", t=4)
                        for tq in range(4):
                            c0 = (g4 * 4 + tq) * 128
                            tr(pv[:, tq, :], kdT[:, c0:c0 + 128], ident[:, :], ["kdT", "ident"], [PN[pbk]])
                        t0 = half * 8 + g4 * 4
                        cp("dve", kd[:, t0:t0 + 4, dh * 128:(dh + 1) * 128], pv, [PN[pbk]], ["kd"])
        S.barrier()

        checkpoint("D1")
        qk_off = R2
        Sst = [A("Sst%d" % i, [128, 32, 128], BF16, at=qk_off + i * 8192) for i in range(4)]
        Sf = [A("Sf%d" % i, [128, 256], F32) for i in range(4)]
        for dh in range(4):
            memset("pool", Sf[dh][:, :], 0.0, ["Sf%d" % dh])
        for step in range(32):
            for dh in range(4):
                d_, hp = divmod(dh, 2)
                n = step if d_ == 0 else 31 - step
                t, c = divmod(n, 2)
                rows = slice(c * 64, (c + 1) * 64)
                cp("pool", Sst[dh][0:64, n, :], Sf[dh][0:64, 0:128], ["Sf%d" % dh], ["SstA%d" % dh])
                cp("act", Sst[dh][64:128, n, :], Sf[dh][64:128, 128:256], ["Sf%d" % dh], ["SstB%d" % dh])
                if step == 31:
                    continue
                pbk = 4 * c + dh
                mm(P[pbk][:, 0:256], kd[rows, t, dh * 128:(dh + 1) * 128], vtok[rows, t, hp * 256:(hp + 1) * 256],
                   True, True, ["kd", "vtok"], [PN[pbk]])
                stt(Sf[dh][:, :], Sf[dh][:, :], dec[:, dh, n:n + 1], P[pbk][:, 0:256], ALU.mult, ALU.add,
                    ["Sf%d" % dh, "dec", PN[pbk]], ["Sf%d" % dh])
        S.barrier()

        checkpoint("D2")
        smb = [A("smb%d" % i, [128, 2, 2, 128], BF16) for i in range(2)]
        sqo = A("sqo", [128, 256], F32)
        rso = A("rso", [128, 256], F32)
        t1o = A("t1o", [128, 256], F32)
        for t in range(NT):
            tsl = slice(t * 128, (t + 1) * 128)
            for par in range(2):
                rows = slice(par * 64, (par + 1) * 64)
                sbk = par
                obk = 2 + par
                scv = P[sbk][:, :].rearrange("p (a b n) -> p a b n", a=2, b=2)
                for hp in range(2):
                    for d_ in range(2):
                        mm(scv[:, hp, d_, :], prod[(d_, hp, "kr")][rows, tsl], prod[(d_, hp, "qr")][rows, tsl], True, True,
                           ["prod"], [PN[sbk]])
                mk = bcast_ap(masks[:, 0:256], [[0, 2], [1, 256]])
                tt("dve", smb[par][:, :, :, :].rearrange("p a b n -> p a (b n)"),
                   P[sbk][:, :].rearrange("p (a m) -> p a m", a=2), mk, ALU.mult, [PN[sbk], "masks"], ["smb%d" % par])
                ov = P[obk][:, 0:256].rearrange("p (a n) -> p a n", a=2)
                for hp in range(2):
                    h = hp * 2 + par
                    mm(ov[:, hp, :], vtok[:, t, h * 128:(h + 1) * 128], smb[par][:, hp, 0, :], True, False,
                       ["vtok", "smb%d" % par], [PN[obk]])
                    mm(ov[:, hp, :], vtok[:, t, h * 128:(h + 1) * 128], smb[par][:, hp, 1, :], False, False,
                       ["vtok", "smb%d" % par], [PN[obk]])
                    for d_ in range(2):
                        dh = d_ * 2 + hp
                        for c in range(2):
                            n = t * 2 + c
                            csl = slice(t * 128 + c * 64, t * 128 + (c + 1) * 64)
                            last = (d_ == 1 and c == 1)
                            mm(ov[:, hp, c * 64:(c + 1) * 64], Sst[dh][rows, n, :], prod[(d_, hp, "qb")][rows, csl],
                               False, last, ["SstA%d" % dh, "SstB%d" % dh, "prod"], [PN[obk]])
                act(sqo[:, :], P[obk][:, 0:256], AF.Square, [PN[obk]], ["sqo"])
                ebk = 4 + par
                mm(P[ebk][:, 0:256], onesf[:, :], sqo[:, :], True, True, ["onesf", "sqo"], [PN[ebk]])
                rsqrt_act(rso[:, :], P[ebk][:, 0:256], 128, [PN[ebk]], ["rso"])
                stt(t1o[:, :], P[obk][:, 0:256], gout[:, 0:1], rso[:, :], ALU.mult, ALU.mult, [PN[obk], "gout", "rso"], ["t1o"])
                for hp in range(2):
                    h = hp * 2 + par
                    tt("pool", mixT[:, h, tsl], t1o[:, hp * 128:(hp + 1) * 128], sgT[:, h, tsl], ALU.mult,
                       ["t1o", "sgT"], ["mixT_g"])
        S.barrier()
        A.reset(L0)
        if debug:
            final_ops.append(dma("sp", dbg["mix"][:, :], mixT[:, :, :].rearrange("p c n -> p (c n)"), reads=["mixT_g", "mixT_m"]))

        checkpoint("D3")
        X = A("X", [128, NT, D], F32)
        h2T = A("h2T", [128, 8, S_LEN], BF16)
        E2 = A.mark()
        wo = A("wo", [128, 8, D], BF16)
        dma("pool", wo[:, :, :], wout_d.ap().rearrange("(c p) n -> p c n", p=128), writes=["wo"])
        for t in range(NT):
            tsl = slice(t * 128, (t + 1) * 128)
            dma("sp", X[:, t, :], x_d[tsl, :], writes=["X%d" % t])
            for ch in range(2):
                pbk = (t % 2) * 2 + ch
                for kc in range(8):
                    mm(P[pbk][:, :], mixT[:, kc, tsl], wo[:, kc, ch * 512:(ch + 1) * 512], kc == 0, kc == 7,
                       ["mixT_g", "mixT_m", "wo"], [PN[pbk]])
                tt("dve", X[:, t, ch * 512:(ch + 1) * 512], X[:, t, ch * 512:(ch + 1) * 512], P[pbk][:, :], ALU.add,
                   ["X%d" % t, PN[pbk]], ["X%d" % t])
        if debug:
            for t in range(NT):
                final_ops.append(dma("sp", dbg["x1"][t * 128:(t + 1) * 128, :], X[:, t, :], reads=["X%d" % t]))
        S.barrier()
        A.reset(E2)

        checkpoint("E")
        g2 = A("g2", [128, D], F32)
        wr = A("wr", [128, 8, 36], BF16)
        rbias = A("rbias", [128, 36], F32)
        gT = A("gT", [32, 2, S_LEN], BF16)
        sel = A("sel", [32, 32, 128], BF16)
        Fm = A.mark()
        hb = [A("hb2_%d" % i, [128, D], BF16) for i in range(2)]
        sqj = A("sqj2", [128, D], F32)
        st1 = A("st1_2", [128, 4], F32)
        lg = A("lg", [128, 36], F32)
        rt = A("rt", [128, 64], F32)
        gate = A("gate", [128, 32], F32)
        ghl = A("ghl", [128, 2, 32], BF16)
        gtmp = A("gtmp", [128, 32], F32)
        dma("sp", g2[:, :], bcast_row(g2_d, D), writes=["g2"])
        dma("pool", wr[:, :, 0:4], wrg_d.ap().rearrange("(c p) n -> p c n", p=128), writes=["wr"])
        dma("pool", wr[:, :, 4:36], wre_d.ap().rearrange("(c p) n -> p c n", p=128), writes=["wr"])
        dma("sp", rbias[:, 0:4], bcast_row(brg_d, 4), writes=["rbias"])
        dma("sp", rbias[:, 4:36], bcast_row(bre_d, 32), writes=["rbias"])
        dma("sp", sel[:, :, :], sel_d.ap().rearrange("p (e n) -> p e n", e=32), writes=["sel"])
        for t in range(NT):
            tsl = slice(t * 128, (t + 1) * 128)
            norm_to_T(lambda t: X[:, t, :], ["X%d" % t], g2, "g2", h2T, "h2T", "F", t, t % 2)
            for kc in range(8):
                mm(P[2][:, 0:36], h2T[:, kc, tsl], wr[:, kc, :], kc == 0, kc == 7, ["h2T_%d" % (t // 4), "wr"], ["P2"])
            tt("dve", lg[:, :], P[2][:, 0:36], rbias[:, :], ALU.add, ["P2", "rbias"], ["lg"])
            mg = rt[:, 0:1]
            op("dve", lambda e: e.tensor_reduce(out=rt[:, 0:1], in_=lg[:, 0:4], axis=AX.X, op=ALU.max), reads=["lg"], writes=["rt0"])
            ts("dve", rt[:, 1:2], mg, -1.0, None, ALU.mult, None, ["rt0"], ["rt1"])
            act(rt[:, 4:8], lg[:, 0:4], AF.Exp, ["lg", "rt1"], ["rt4", "rt2"], bias=rt[:, 1:2], accum_out=rt[:, 2:3])
            op("dve", lambda e: e.reciprocal(out=rt[:, 3:4], in_=rt[:, 2:3]), reads=["rt2"], writes=["rt3"])
            ts("dve", rt[:, 8:12], lg[:, 0:4], mg, rt[:, 3:4], ALU.is_equal, ALU.mult, ["lg", "rt0", "rt3"], ["rt8"])
            ts("dve", rt[:, 12:16], lg[:, 0:4], mg, None, ALU.is_equal, None, ["lg", "rt0"], ["rt12"])
            ts("dve", rt[:, 16:24], lg[:, 4:12], rt[:, 12:13], None, ALU.mult, None, ["lg", "rt12"], ["rt16"])
            for g_ in range(1, 4):
                stt(rt[:, 16:24], lg[:, 4 + 8 * g_:12 + 8 * g_], rt[:, 12 + g_:13 + g_], rt[:, 16:24], ALU.mult, ALU.add,
                    ["lg", "rt12", "rt16"], ["rt16"])
            op("dve", lambda e: e.max(out=rt[:, 24:32], in_=rt[:, 16:24]), reads=["rt16"], writes=["rt24"])
            tt("dve", rt[:, 32:33], rt[:, 25:26], rt[:, 24:25], ALU.subtract, ["rt24"], ["rt32"])
            act(rt[:, 33:34], rt[:, 32:33], AF.Exp, ["rt32"], ["rt33"])
            ts("dve", rt[:, 34:35], rt[:, 33:34], 1.0, None, ALU.add, None, ["rt33"], ["rt34"])
            op("dve", lambda e: e.reciprocal(out=rt[:, 35:36], in_=rt[:, 34:35]), reads=["rt34"], writes=["rt35"])
            tt("dve", rt[:, 36:37], rt[:, 33:34], rt[:, 35:36], ALU.mult, ["rt33", "rt35"], ["rt36"])
            ts("dve", rt[:, 40:48], rt[:, 16:24], rt[:, 24:25], rt[:, 35:36], ALU.is_equal, ALU.mult, ["rt16", "rt24", "rt35"], ["rt40"])
            ts("dve", rt[:, 48:56], rt[:, 16:24], rt[:, 25:26], rt[:, 36:37], ALU.is_equal, ALU.mult, ["rt16", "rt24", "rt36"], ["rt48"])
            tt("dve", rt[:, 40:48], rt[:, 40:48], rt[:, 48:56], ALU.add, ["rt40", "rt48"], ["rt40"])
            for g_ in range(4):
                ts("dve", gate[:, g_ * 8:(g_ + 1) * 8], rt[:, 40:48], rt[:, 8 + g_:9 + g_], None, ALU.mult, None,
                   ["rt40", "rt8"], ["gate"])
            if debug:
                final_ops.append(dma("sp", dbg["gate"][tsl, :], gate[:, :], reads=["gate"]))
            cp("dve", ghl[:, 0, :], gate[:, :], ["gate"], ["ghl"])
            tt("dve", gtmp[:, :], gate[:, :], ghl[:, 0, :], ALU.subtract, ["gate", "ghl"], ["gtmp"])
            cp("dve", ghl[:, 1, :], gtmp[:, :], ["gtmp", "ghl"], ["ghl"])
            p3v = Pb[3][:, 0:256].rearrange("p (a n) -> p a n", a=2)
            for a_ in range(2):
                tr(p3v[0:32, a_, :], ghl[:, a_, :], ident[:, :], ["ghl", "ident"], ["P3"])
            cp("act", gT[0:32, :, tsl], p3v[0:32, :, :], ["P3"], ["gT"])
        S.barrier()
        A.reset(Fm)

        checkpoint("F")
        EG = 2
        NEG = 32 // EG
        wgt = [[A("wg%d_%d" % (b, j), [128, 8, 256], BF16) for j in range(EG)] for b in range(2)]
        wup = [[A("wu%d_%d" % (b, j), [128, 8, 256], BF16) for j in range(EG)] for b in range(2)]
        wdn = [[A("wd%d_%d" % (b, j), [128, 2, D], BF16) for j in range(EG)] for b in range(2)]
        MX = SB_BASE + 256
        hid = [A("hid%d" % b, [128, EG, 2, 512], BF16, at=MX + b * 4096) for b in range(2)]
        sil = [A("sil%d" % b, [128, 512], F32, at=MX + 8192 + b * 2048) for b in range(2)]
        t1m = [A("t1m%d" % b, [128, 512], F32, at=MX + 12288 + b * 2048) for b in range(2)]
        itc = 0
        for eg in range(NEG):
            b = eg % 2
            for j in range(EG):
                e_ = eg * EG + j
                dma("pool", wgt[b][j][:, :, :], weg_d.ap()[e_].rearrange("(c p) f -> p c f", p=128), writes=["wgt%d" % b])
                dma("pool", wup[b][j][:, :, :], weu_d.ap()[e_].rearrange("(c p) f -> p c f", p=128), writes=["wup%d" % b])
                dma("pool", wdn[b][j][:, :, :], wed_d.ap()[e_].rearrange("(c p) d -> p c d", p=128), writes=["wdn%d" % b])
            for tg in range(4):
                hbi = itc % 2
                itc += 1
                tgs = slice(tg * 512, (tg + 1) * 512)
                for j in range(EG):
                    e_ = eg * EG + j
                    gbk = 6 + j
                    mm(P[gbk][:, :], sel[0:32, e_, :], gT[0:32, 0, tgs], True, False, ["sel", "gT"], [PN[gbk]])
                    mm(P[gbk][:, :], sel[0:32, e_, :], gT[0:32, 1, tgs], False, True, ["sel", "gT"], [PN[gbk]])
                    for fh in range(2):
                        k2 = (j * 2 + fh) % 2
                        gb_, ub_ = 0 + k2, 2 + k2
                        for kc in range(8):
                            mm(P[gb_][:, :], wgt[b][j][:, kc, fh * 128:(fh + 1) * 128], h2T[:, kc, tgs], kc == 0, kc == 7,
                               ["wgt%d" % b, "h2T_%d" % tg], [PN[gb_]])
                        for kc in range(8):
                            mm(P[ub_][:, :], wup[b][j][:, kc, fh * 128:(fh + 1) * 128], h2T[:, kc, tgs], kc == 0, kc == 7,
                               ["wup%d" % b, "h2T_%d" % tg], [PN[ub_]])
                        act(sil[k2][:, :], P[gb_][:, :], AF.Silu, [PN[gb_]], ["sil%d" % k2])
                        tt("dve", t1m[k2][:, :], sil[k2][:, :], P[ub_][:, :], ALU.mult, ["sil%d" % k2, PN[ub_]], ["t1m%d" % k2])
                        tt("dve", hid[hbi][:, j, fh, :], t1m[k2][:, :], P[gbk][:, :], ALU.mult, ["t1m%d" % k2, PN[gbk]],
                           ["hid%d" % hbi])
                for tt_ in range(4):
                    t = tg * 4 + tt_
                    for ch in range(2):
                        abk = 4 + (tt_ * 2 + ch) % 2
                        n_acc = EG * 2
                        a_i = 0
                        for j in range(EG):
                            for fh in range(2):
                                mm(P[abk][:, :], hid[hbi][:, j, fh, tt_ * 128:(tt_ + 1) * 128],
                                   wdn[b][j][:, fh, ch * 512:(ch + 1) * 512], a_i == 0, a_i == n_acc - 1,
                                   ["hid%d" % hbi, "wdn%d" % b], [PN[abk]])
                                a_i += 1
                        tt("dve", X[:, t, ch * 512:(ch + 1) * 512], X[:, t, ch * 512:(ch + 1) * 512], P[abk][:, :], ALU.add,
                           ["X%d" % t, PN[abk]], ["X%d" % t])
        for t in range(NT):
            final_ops.append(dma("sp", out_d[t * 128:(t + 1) * 128, :], X[:, t, :], reads=["X%d" % t]))

        S.emit(es, final_wait_ops=final_ops)
    return nc


def make_consts():
    ident = np.eye(128, dtype=np.float32).astype(ml_dtypes.bfloat16)
    j = np.arange(128)[:, None]
    i = np.arange(128)[None, :]
    same = (j // 64) == (i // 64)
    mf = (same & (j <= i)).astype(np.float32)
    mb = (same & (j > i)).astype(np.float32)
    masks = np.concatenate([mf, mb], axis=1).astype(ml_dtypes.bfloat16)
    invf = (10000.0 ** (-np.arange(0, 32, 2, dtype=np.float32) / 32)).astype(np.float32)
    invf = np.broadcast_to(invf[None, :], (128, 16)).copy()
    sel = np.zeros((32, 32, 128), np.float32)
    for e in range(32):
        sel[e, e, :] = 1.0
    sel = sel.reshape(32, 32 * 128).astype(ml_dtypes.bfloat16)
    lrb = np.zeros((64, 1), np.float32)
    lrb[16, 0] = 1.0
    return {"c_ident": ident, "c_masks": masks, "c_invf": invf, "c_sel": sel, "c_lrbias": lrb}


_NC_CACHE = {}


def make_in_maps(inputs, n_cores=8):
    c = make_consts()
    f = lambda k: np.ascontiguousarray(np.asarray(inputs[k], dtype=np.float32)[0])
    shared = {
        "norm1_gain": f("norm1_gain").reshape(1, D),
        "w_in": f("w_in"),
        "gla_gk_fwd_w": f("gla_gk_fwd_w"), "gla_gk_fwd_b": f("gla_gk_fwd_b").reshape(1, 256),
        "gla_gk_bwd_w": f("gla_gk_bwd_w"), "gla_gk_bwd_b": f("gla_gk_bwd_b").reshape(1, 256),
        "gla_out_gain": f("gla_out_gain").reshape(128, 1),
        "mla_q_gain": f("mla_q_gain").reshape(1, 256), "mla_w_qb": f("mla_w_qb"),
        "mla_kv_gain": f("mla_kv_gain").reshape(1, 128), "mla_w_kvb": f("mla_w_kvb"),
        "q_norm_gain": f("q_norm_gain").reshape(1, 96), "k_norm_gain": f("k_norm_gain").reshape(1, 96),
        "w_out": f("w_out"), "norm2_gain": f("norm2_gain").reshape(1, D),
        "w_router_group": f("w_router_group"), "b_router_group": f("b_router_group").reshape(1, 4),
        "w_router_expert": f("w_router_expert"), "b_router_expert": f("b_router_expert").reshape(1, 32),
        "w_expert_gate": f("w_expert_gate").reshape(32, D, 256),
        "w_expert_up": f("w_expert_up").reshape(32, D, 256),
        "w_expert_down": f("w_expert_down").reshape(32, 256, D),
    }
    shared.update(c)
    x = np.asarray(inputs["x"], dtype=np.float32)
    pos = np.asarray(inputs["positions"]).astype(np.int32)
    maps = []
    for b in range(n_cores):
        m = dict(shared)
        m["x"] = np.ascontiguousarray(x[b])
        m["pos"] = np.ascontiguousarray(pos[b].reshape(NT, 128).T)
        maps.append(m)
    return maps


def kernel(**inputs):
    if "nc" not in _NC_CACHE:
        _NC_CACHE["nc"] = build()
    nc = _NC_CACHE["nc"]
    maps = make_in_maps(inputs, 8)
    res = run_bass_kernel_spmd(nc, maps, core_ids=list(range(8)))
    out = np.stack([np.asarray(r["out"], dtype=np.float32) for r in res.results], axis=0)
    return out
```

```python
import contextlib
import math
import numpy as np
import ml_dtypes
import concourse.bass as bass
import concourse.mybir as mybir
from concourse.bass_utils import run_bass_kernel_spmd

F32 = mybir.dt.float32
BF16 = mybir.dt.bfloat16
I32 = mybir.dt.int32
ALU = mybir.AluOpType
AF = mybir.ActivationFunctionType
AX = mybir.AxisListType

S_LEN = 2048
D = 1024
NT = 16
EPS = 1e-6
PI = math.pi


class T:
    __slots__ = ("name", "w", "r")

    def __init__(self, name):
        self.name = name
        self.w = None
        self.r = []


class Op:
    __slots__ = ("eng", "fn", "deps", "signal", "sig", "dma", "dsem", "dval", "alld", "n", "seg", "idx", "nbytes")


class Sched:
    ENGS = ("pe", "act", "dve", "pool", "sp")

    def __init__(self, nc, n_dma_sems=12):
        self.nc = nc
        self.ops = {e: [] for e in self.ENGS}
        self.n_dma_sems = n_dma_sems
        self.dma_count = {e: 0 for e in self.ENGS}
        self.tiles = {}
        self.pending = {e: [] for e in self.ENGS}
        self.dma_since_barrier = []
        self.stopped = False
        self.seg = 0
        self.nops = 0

    def t(self, name):
        if name not in self.tiles:
            self.tiles[name] = T(name)
        return self.tiles[name]

    def _tl(self, lst):
        out = []
        for x in lst:
            if isinstance(x, str):
                out.append(self.t(x))
            elif isinstance(x, (list, tuple)):
                out.extend(self._tl(x))
            elif x is not None:
                out.append(x)
        return out

    def alias(self, new_names, old_names):
        if self.stopped:
            return
        for nn in new_names:
            tn = self.t(nn)
            for on in old_names:
                to = self.t(on)
                if to.w is not None:
                    tn.r.append(to.w)
                tn.r.extend(to.r)

    def barrier(self):
        if self.stopped:
            return
        lasts = []
        for e in self.ENGS:
            for o in reversed(self.ops[e]):
                if not o.dma:
                    lasts.append(o)
                    break
        lasts.extend(self.dma_since_barrier)
        self.dma_since_barrier = []
        for e in self.ENGS:
            self.pending[e] = list(lasts)
        self.seg += 1

    def op(self, eng, fn, reads=(), writes=(), dma=False, n=64, nbytes=0):
        if self.stopped:
            return None
        o = Op()
        o.n = n
        o.nbytes = nbytes
        o.seg = self.seg
        o.idx = self.nops
        self.nops += 1
        o.eng = eng
        o.fn = fn
        o.dma = dma
        o.signal = False
        o.sig = 0
        deps = {}
        reads = self._tl(reads)
        writes = self._tl(writes)
        for t in reads:
            if t.w is not None:
                deps[id(t.w)] = (t.w, "raw")
            if t.name[0] == "P" and t.name[1:].isdigit():
                for r in t.r:
                    if id(r) not in deps and r.eng != eng:
                        deps[id(r)] = (r, "war")
        for t in writes:
            if t.w is not None and id(t.w) not in deps:
                deps[id(t.w)] = (t.w, "waw")
            for r in t.r:
                if id(r) not in deps:
                    deps[id(r)] = (r, "war")
        if self.pending[eng]:
            for p in self.pending[eng]:
                deps[id(p)] = (p, "raw")
            self.pending[eng] = []
        o.alld = [p for p, _k in deps.values()]
        dl = []
        for p, kind in deps.values():
            if p.eng == eng and not p.dma:
                if eng == "pe":
                    continue
                if kind != "raw" and not dma and eng != "pool":
                    continue
            dl.append(p)
        o.deps = dl
        for p in dl:
            p.signal = True
        for t in reads:
            if not dma:
                for r in t.r:
                    if not r.dma and r.eng == eng:
                        o.alld.append(r)
                t.r = [r for r in t.r if r.dma or r.eng != eng]
            t.r.append(o)
        for t in writes:
            t.w = o
            t.r = []
        if dma:
            self.dma_count[eng] += 1
            self.dma_since_barrier.append(o)
        self.ops[eng].append(o)
        return o

    @staticmethod
    def _dur(o):
        n = o.n
        if o.dma:
            return 60.0 if o.eng == "sp" else 900.0
        if o.eng == "pe":
            return 30.0 + max(n, 64) / 2.0
        if o.eng == "act":
            return 220.0 + n / 1.4
        if o.eng == "dve":
            return 120.0 + n * 1.3
        return 550.0 + n * 0.75

    def reschedule(self):
        allops = []
        for e in self.ENGS:
            allops.extend(self.ops[e])
        allops.sort(key=lambda o: o.idx)
        import heapq
        new = {e: [] for e in self.ENGS}
        segs = {}
        for o in allops:
            segs.setdefault(o.seg, []).append(o)
        LAT = 250.0
        for sg in sorted(segs):
            ops = segs[sg]
            inseg = set(id(o) for o in ops)
            done = {}
            users = {}
            indeg = {}
            first = {}
            for o in ops:
                if o.eng not in first:
                    first[o.eng] = o
                elif first[o.eng] not in o.alld:
                    o.alld.append(first[o.eng])
            for o in ops:
                k = 0
                for p in o.alld:
                    if id(p) in inseg:
                        k += 1
                        users.setdefault(id(p), []).append(o)
                indeg[id(o)] = k
            ready = {e: [] for e in self.ENGS}
            efree = {e: 0.0 for e in self.ENGS}
            rtime = {}
            for o in ops:
                if indeg[id(o)] == 0:
                    rtime[id(o)] = 0.0
                    heapq.heappush(ready[o.eng], (0.0, o.idx, o))
            left = len(ops)
            while left:
                best = None
                for e in self.ENGS:
                    if ready[e]:
                        rt, ix, o = ready[e][0]
                        st = max(rt, efree[e])
                        if best is None or (st, ix) < (best[0], best[1]):
                            best = (st, ix, e)
                st, ix, e = best
                rt, ix, o = heapq.heappop(ready[e])
                d = self._dur(o)
                efree[e] = st + d
                fin = st + d
                if o.dma:
                    fin = st + 2000.0 + o.nbytes / 150.0
                done[id(o)] = fin
                new[e].append(o)
                left -= 1
                for u in users.get(id(o), ()):
                    indeg[id(u)] -= 1
                    lat = 0.0 if (u.eng == o.eng and not o.dma) else LAT
                    rtime[id(u)] = max(rtime.get(id(u), 0.0), fin + lat)
                    if indeg[id(u)] == 0:
                        heapq.heappush(ready[u.eng], (rtime[id(u)], u.idx, u))
        self.ops = new

    def emit(self, es, final_wait_ops=()):
        nc = self.nc
        if RESCHEDULE:
            self.reschedule()
        sems = {e: es.enter_context(nc.semaphore("s_" + e)) for e in self.ENGS}
        dsems = {e: [es.enter_context(nc.semaphore("d_%s_%d" % (e, i)))
                     for i in range(self.n_dma_sems)]
                 for e in self.ENGS if self.dma_count[e] > 0}
        for e in self.ENGS:
            c = 0
            i = 0
            for o in self.ops[e]:
                if o.dma:
                    o.dsem = i % self.n_dma_sems
                    o.dval = 16 * (i // self.n_dma_sems + 1)
                    i += 1
                elif o.signal:
                    c += 1
                    o.sig = c
        block = es.enter_context(nc.Block())
        eng_obj = {"pe": block.tensor, "act": block.scalar, "dve": block.vector,
                   "pool": block.gpsimd, "sp": block.sync}
        for e in self.ENGS:
            ops = self.ops[e]
            if not ops:
                continue

            def body(engine, e=e, ops=ops):
                waited = {}

                def wait(sem, key, val):
                    if waited.get(key, 0) >= val:
                        return
                    waited[key] = val
                    engine.wait_ge(sem, val)

                for o in ops:
                    for p in o.deps:
                        if p.dma:
                            wait(dsems[p.eng][p.dsem], ("d", p.eng, p.dsem), p.dval)
                        else:
                            wait(sems[p.eng], ("c", p.eng), p.sig)
                    if o.dma and o.dval > 16:
                        wait(dsems[e][o.dsem], ("d", e, o.dsem), o.dval - 16)
                    ins = o.fn(engine)
                    if o.dma:
                        ins.then_inc(dsems[e][o.dsem], 16)
                    elif o.signal:
                        ins.then_inc(sems[e], 1)
                if e == "sp":
                    for o in final_wait_ops:
                        if o is None:
                            continue
                        wait(dsems[o.eng][o.dsem], ("d", o.eng, o.dsem), o.dval)

            eng_obj[e](body)


RESCHEDULE = True
SB_BASE = 16640
SB_END = 229376


class Alloc:
    def __init__(self, nc):
        self.nc = nc
        self.off = SB_BASE
        self.n = 0

    def mark(self):
        return self.off

    def reset(self, m):
        self.off = m

    def __call__(self, name, shape, dt, at=None):
        esz = 2 if dt == BF16 else 4
        nb = int(np.prod(shape[1:])) * esz
        nb = (nb + 63) // 64 * 64
        self.n += 1
        if at is None:
            at = self.off
            self.off += nb
        assert at + nb <= SB_END, ("SBUF overflow", name, at, nb)
        return self.nc.alloc_sbuf_tensor_at("%s_%d" % (name, self.n), list(shape), dt, offset=at)


def bcast_ap(ap, pattern):
    return bass.AP(ap.tensor, ap.offset, [list(ap.ap[0])] + [list(p) for p in pattern])


def build(debug=False, stop_after=None):
    nc = bass.Bass("TRN2", target_bir_lowering=False)
    dr = lambda n, s, dt=F32: nc.dram_tensor(n, list(s), dt, kind="ExternalInput")
    x_d = dr("x", [S_LEN, D])
    pos_d = dr("pos", [128, NT], I32)
    g1_d = dr("norm1_gain", [1, D])
    win_d = dr("w_in", [D, 1984])
    gkf_w = dr("gla_gk_fwd_w", [16, 256])
    gkf_b = dr("gla_gk_fwd_b", [1, 256])
    gkb_w = dr("gla_gk_bwd_w", [16, 256])
    gkb_b = dr("gla_gk_bwd_b", [1, 256])
    go_d = dr("gla_out_gain", [128, 1])
    gqa_d = dr("mla_q_gain", [1, 256])
    wqb_d = dr("mla_w_qb", [256, 768])
    gkva_d = dr("mla_kv_gain", [1, 128])
    wkvb_d = dr("mla_w_kvb", [128, 1024])
    gqn_d = dr("q_norm_gain", [1, 96])
    gkn_d = dr("k_norm_gain", [1, 96])
    wout_d = dr("w_out", [D, D])
    g2_d = dr("norm2_gain", [1, D])
    wrg_d = dr("w_router_group", [D, 4])
    brg_d = dr("b_router_group", [1, 4])
    wre_d = dr("w_router_expert", [D, 32])
    bre_d = dr("b_router_expert", [1, 32])
    weg_d = dr("w_expert_gate", [32, D, 256])
    weu_d = dr("w_expert_up", [32, D, 256])
    wed_d = dr("w_expert_down", [32, 256, D])
    ident_d = dr("c_ident", [128, 128], BF16)
    masks_d = dr("c_masks", [128, 256], BF16)
    invf_d = dr("c_invf", [128, 16])
    sel_d = dr("c_sel", [32, 32 * 128], BF16)
    lrb_d = dr("c_lrbias", [64, 1])
    out_d = nc.dram_tensor("out", [S_LEN, D], F32, kind="ExternalOutput")
    dbg = {}
    if debug:
        dbg["mix"] = nc.dram_tensor("d_mix", [128, 8 * S_LEN], BF16, kind="ExternalOutput")
        dbg["x1"] = nc.dram_tensor("d_x1", [S_LEN, D], F32, kind="ExternalOutput")
        dbg["gate"] = nc.dram_tensor("d_gate", [S_LEN, 32], F32, kind="ExternalOutput")

    if debug:
        dbg["gen"] = nc.dram_tensor("d_gen", [128, 8 * S_LEN], BF16, kind="ExternalOutput")
    S = Sched(nc)
    A = Alloc(nc)
    op = S.op
    final_ops = []

    with contextlib.ExitStack() as es:
        P = [es.enter_context(nc.psum_tensor("pb%d" % i, [128, 512], F32)) for i in range(8)]
        Pb = [p.bitcast(BF16) for p in P]
        PN = ["P%d" % i for i in range(8)]

        def fsz(ap):
            r = 1
            for d_ in list(ap.shape)[1:]:
                r *= int(d_)
            return r

        def dma(q, out, in_, reads=(), writes=(), **kw):
            return op(q, lambda e: e.dma_start(out=out, in_=in_, **kw), reads=reads, writes=writes, dma=True,
                      nbytes=fsz(out) * 4 * 128)

        def act(out, in_, func, reads, writes, **kw):
            return op("act", lambda e: e.activation(out=out, in_=in_, func=func, **kw), reads=reads, writes=writes,
                      n=fsz(out))

        def rsqrt_act(out, in_, n, reads, writes):
            act(out, in_, AF.Ln, reads, writes, scale=1.0 / n, bias=EPS)
            act(out, out, AF.Exp, writes, writes, scale=-0.5)

        def tt(eng, out, in0, in1, o, reads, writes):
            return op(eng, lambda e: e.tensor_tensor(out=out, in0=in0, in1=in1, op=o), reads=reads, writes=writes,
                      n=fsz(out))

        def ts(eng, out, in0, s1, s2, o0, o1, reads, writes):
            if o1 is None:
                return op(eng, lambda e: e.tensor_scalar(out=out, in0=in0, scalar1=s1, scalar2=None, op0=o0),
                          reads=reads, writes=writes, n=fsz(out))
            return op(eng, lambda e: e.tensor_scalar(out=out, in0=in0, scalar1=s1, scalar2=s2, op0=o0, op1=o1),
                      reads=reads, writes=writes, n=fsz(out))

        def stt(out, in0, sc, in1, o0, o1, reads, writes):
            return op("dve", lambda e: e.scalar_tensor_tensor(out=out, in0=in0, scalar=sc, in1=in1, op0=o0, op1=o1),
                      reads=reads, writes=writes, n=fsz(out))

        def mm(out, lhsT, rhs, start, stop, reads, writes):
            return op("pe", lambda e: e.matmul(out, lhsT=lhsT, rhs=rhs, start=start, stop=stop),
                      reads=reads, writes=writes, n=fsz(rhs) * (4 if rhs.dtype == F32 else 1))

        def tr(out, in_, ident, reads, writes):
            return op("pe", lambda e: e.transpose(out=out, in_=in_, identity=ident), reads=reads, writes=writes, n=128)

        def cp(eng, out, in_, reads, writes):
            if eng == "act":
                return act(out, in_, AF.Copy, reads, writes)
            return op(eng, lambda e: e.tensor_copy(out=out, in_=in_), reads=reads, writes=writes, n=fsz(out))

        def memset(eng, ap, val, writes):
            return op(eng, lambda e: e.memset(ap, val), writes=writes, n=fsz(ap))

        def bcast_row(dram, n):
            return bass.AP(dram, 0, [[0, 128], [1, n]])

        ident = A("ident", [128, 128], BF16)
        mixT = A("mixT", [128, 8, S_LEN], BF16)
        dma("sp", ident[:, :], ident_d[:, :], writes=["ident"])
        L0 = A.mark()

        hT = A("hT", [128, 8, S_LEN], BF16)
        E1 = A.mark()
        g1 = A("g1", [128, D], F32)
        xt = [A("xt%d" % i, [128, D], F32) for i in range(2)]
        hb = [A("hb%d" % i, [128, D], BF16) for i in range(2)]
        sqj = A("sqj", [128, D], F32)
        st1 = A("st1", [128, 4], F32)
        dma("sp", g1[:, :], bcast_row(g1_d, D), writes=["g1"])

        def norm_to_T(src_ap_fn, src_tiles, gain, gname, dstT, dname, pfx, t, pbank):
            i = t % 2
            ssq = st1[:, 0:1]
            rs = st1[:, 1:2]
            act(sqj[:, :], src_ap_fn(t), AF.Square, src_tiles, [pfx + "sqj", pfx + "ssq"], accum_out=ssq)
            rsqrt_act(rs, ssq, D, [pfx + "ssq"], [pfx + "rs"])
            stt(hb[i][:, :], src_ap_fn(t), rs, gain[:, :], ALU.mult, ALU.mult,
                src_tiles + [pfx + "rs", gname], [pfx + "hb%d" % i])
            pbv = Pb[pbank][:, :].rearrange("p (c n) -> p c n", c=8)
            for kc in range(8):
                tr(pbv[:, kc, :], hb[i][:, kc * 128:(kc + 1) * 128], ident[:, :],
                   [pfx + "hb%d" % i, "ident"], [PN[pbank]])
            cp("dve" if t % 2 else "act", dstT[:, :, t * 128:(t + 1) * 128], pbv, [PN[pbank]], [dname + "_%d" % (t // 4)])

        for t in range(NT):
            i = t % 2
            dma("sp", xt[i][:, :], x_d[t * 128:(t + 1) * 128, :], writes=["xt%d" % i])
            norm_to_T(lambda t, i=i: xt[i][:, :], ["xt%d" % i], g1, "g1", hT, "hT", "A", t, t % 2)
        hT_tiles = ["hT_%d" % k for k in range(4)]
        S.barrier()
        A.reset(E1)
        def checkpoint(name, dump=None, reads=()):
            if stop_after == name:
                if dump is not None and debug:
                    S.barrier()
                    final_ops.append(dma("sp", dbg["gen"][:, :], dump, reads=list(reads)))
                S.stopped = True

        checkpoint("A")

        wm = A("w_in_mla", [128, 8, 416], BF16)
        wqb = A("wqb", [128, 2, 768], BF16)
        wkvb = A("wkvb", [128, 1024], BF16)
        cs = A("cs", [128, NT, 32], F32)
        qhT = A("qhT", [128, 8, S_LEN], BF16)
        khT = A("khT", [128, 8, S_LEN], BF16)
        vA = A("vA", [128, NT, 8, 128], BF16)
        gqa = A("gqa", [128, 384], F32)
        gqk = A("gqk", [128, 16, 96], F32)
        B1m = A.mark()
        dma("pool", wm[:, :, :], win_d.ap()[:, 1568:1984].rearrange("(c p) n -> p c n", p=128), writes=["wm"])
        dma("pool", wqb[:, :, :], wqb_d.ap().rearrange("(c p) n -> p c n", p=128), writes=["wqb"])
        dma("pool", wkvb[:, :], wkvb_d[:, :], writes=["wkvb"])
        dma("sp", gqa[:, 0:256], bcast_row(gqa_d, 256), writes=["gqa"])
        dma("sp", gqa[:, 256:384], bcast_row(gkva_d, 128), writes=["gqa"])
        dma("sp", gqk[:, 0:8, :], bass.AP(gqn_d, 0, [[0, 128], [0, 8], [1, 96]]), writes=["gqk"])
        dma("sp", gqk[:, 8:16, :], bass.AP(gkn_d, 0, [[0, 128], [0, 8], [1, 96]]), writes=["gqk"])
        posi = A("posi", [128, NT], I32)
        posf = A("posf", [128, NT], F32)
        invf = A("invf", [128, 16], F32)
        ang = A("ang", [128, NT, 16], F32)
        kk = A("kk", [128, NT, 16], F32)
        ki = A("ki", [128, NT, 16], I32)
        rr = A("rr", [128, NT, 16], F32)
        yy = A("yy", [128, NT, 16], F32)
        m_ = A("m_", [128, NT, 16], F32)
        dma("sp", posi[:, :], pos_d[:, :], writes=["posi"])
        dma("sp", invf[:, :], invf_d[:, :], writes=["invf"])
        cp("dve", posf[:, :], posi[:, :], ["posi"], ["posf"])
        for t in range(NT):
            ts("dve", ang[:, t, :], invf[:, :], posf[:, t:t + 1], None, ALU.mult, None, ["invf", "posf"], ["ang"])
        ts("dve", kk[:, :, :], ang[:, :, :], 1.0 / (2 * PI), None, ALU.mult, None, ["ang"], ["kk"])
        cp("dve", ki[:, :, :], kk[:, :, :], ["kk"], ["ki"])
        cp("dve", kk[:, :, :], ki[:, :, :], ["ki"], ["kk"])
        stt(rr[:, :, :], kk[:, :, :], -2 * PI, ang[:, :, :], ALU.mult, ALU.add, ["kk", "ang"], ["rr"])
        for which, shift in ((1, 0.0), (0, PI / 2)):
            ts("dve", yy[:, :, :], rr[:, :, :], shift, None, ALU.add, None, ["rr"], ["yy"])
            ts("dve", m_[:, :, :], yy[:, :, :], PI, None, ALU.is_gt, None, ["yy"], ["m_"])
            stt(yy[:, :, :], m_[:, :, :], -2 * PI, yy[:, :, :], ALU.mult, ALU.add, ["m_", "yy"], ["yy"])
            ts("dve", m_[:, :, :], yy[:, :, :], -PI, None, ALU.is_lt, None, ["yy"], ["m_"])
            stt(yy[:, :, :], m_[:, :, :], 2 * PI, yy[:, :, :], ALU.mult, ALU.add, ["m_", "yy"], ["yy"])
            ts("dve", yy[:, :, :], yy[:, :, :], PI, -PI, ALU.min, ALU.max, ["yy"], ["yy"])
            act(cs[:, :, which * 16:(which + 1) * 16], yy[:, :, :], AF.Sin, ["yy"], ["cs"])
        memset("pool", vA[:, :, :, :], 1.0, ["vA"])
        S.barrier()
        A.reset(B1m)

        sqjb = [A("sqjb%d" % i, [128, 416], BF16) for i in range(2)]
        stq = [A("stq%d" % i, [128, 32], F32) for i in range(2)]
        ab = [A("ab%d" % i, [128, 384], BF16) for i in range(2)]
        abT = [A("abT%d" % i, [128, 3, 128], BF16) for i in range(2)]
        kraw = [A("kraw%d" % i, [128, 8, 96], F32) for i in range(2)]
        sqn = A("sqn", [128, 16, 96], F32)
        rg = A("rg", [128, 16, 32], F32)
        ra = A("ra", [128, 16, 16], F32)
        rb = A("rb", [128, 16, 16], F32)
        qkf = A("qkf", [128, 16, 96], BF16)
        SQ2 = math.sqrt(2.0)

        for t in range(NT):
            i = t % 2
            sI = "_%d" % i
            tsl = slice(t * 128, (t + 1) * 128)
            hTt = "hT_%d" % (t // 4)
            for kc in range(8):
                mm(P[0][:, 0:416], hT[:, kc, tsl], wm[:, kc, :], kc == 0, kc == 7, [hTt, "wm"], ["P0"])
            act(sqjb[i][:, 0:256], P[0][:, 0:256], AF.Square, ["P0"], ["sqjb" + sI, "stqA" + sI], accum_out=stq[i][:, 0:1])
            act(sqjb[i][:, 256:384], P[0][:, 256:384], AF.Square, ["P0"], ["sqjb" + sI, "stqA" + sI],
                accum_out=stq[i][:, 1:2], scale=SQ2)
            rsqrt_act(stq[i][:, 2:4], stq[i][:, 0:2], 256, ["stqA" + sI], ["stqB" + sI])
            stt(ab[i][:, 0:256], P[0][:, 0:256], stq[i][:, 2:3], gqa[:, 0:256], ALU.mult, ALU.mult,
                ["P0", "stqB" + sI, "gqa"], ["ab" + sI])
            stt(ab[i][:, 256:384], P[0][:, 256:384], stq[i][:, 3:4], gqa[:, 256:384], ALU.mult, ALU.mult,
                ["P0", "stqB" + sI, "gqa"], ["ab" + sI])
            act(kraw[i][:, :, 64:96], bcast_ap(P[0][:, 384:416], [[0, 8], [1, 32]]), AF.Copy, ["P0"], ["krawR" + sI])
            p1v = Pb[1][:, 0:384].rearrange("p (c n) -> p c n", c=3)
            for c in range(3):
                tr(p1v[:, c, :], ab[i][:, c * 128:(c + 1) * 128], ident[:, :], ["ab" + sI, "ident"], ["P1"])
            cp("act", abT[i][:, :, :], p1v, ["P1"], ["abT" + sI])
            for nb in range(2):
                for kc in range(2):
                    mm(P[2 + nb][:, 0:384], abT[i][:, kc, :], wqb[:, kc, nb * 384:(nb + 1) * 384], kc == 0, kc == 1,
                       ["abT" + sI, "wqb"], [PN[2 + nb]])
                mm(P[4 + nb][:, :], abT[i][:, 2, :], wkvb[:, nb * 512:(nb + 1) * 512], True, True,
                   ["abT" + sI, "wkvb"], [PN[4 + nb]])
            for nb in range(2):
                srck = P[4 + nb][:, :].rearrange("p (h d) -> p h d", h=4)[:, :, 0:64]
                cp("dve", kraw[i][:, nb * 4:nb * 4 + 4, 0:64], srck, [PN[4 + nb]], ["krawN" + sI])
                srcv = P[4 + nb][:, :].rearrange("p (a b d) -> p a b d", a=2, b=2)
                dstv = vA[:, t, nb * 4:nb * 4 + 4, :].rearrange("p (a b) d -> p a b d", b=2)
                cp("act", dstv[:, :, 0, 0:64], srcv[:, :, 0, 64:128], [PN[4 + nb]], ["vA"])
                cp("act", dstv[:, :, 1, 64:128], srcv[:, :, 1, 64:128], [PN[4 + nb]], ["vA"])
            for nb in range(2):
                act(sqn[:, nb * 4:nb * 4 + 4, :], P[2 + nb][:, 0:384].rearrange("p (h d) -> p h d", h=4), AF.Square,
                    [PN[2 + nb]], ["sqn"])
            act(sqn[:, 8:16, :], kraw[i][:, :, :], AF.Square, ["krawN" + sI, "krawR" + sI], ["sqn"])
            op("dve", lambda e, i=i: e.tensor_reduce(out=stq[i][:, 8:24], in_=sqn[:, :, :], axis=AX.X, op=ALU.add),
               reads=["sqn"], writes=["stqC" + sI])
            rsqrt_act(stq[i][:, 8:24], stq[i][:, 8:24], 96, ["stqC" + sI], ["stqC" + sI])
            for nb in range(2):
                tt("dve", sqn[:, nb * 4:nb * 4 + 4, :], P[2 + nb][:, 0:384].rearrange("p (h d) -> p h d", h=4),
                   bcast_ap(stq[i][:, 8 + nb * 4:9 + nb * 4], [[1, 4], [0, 96]]), ALU.mult,
                   [PN[2 + nb], "stqC" + sI], ["sqn"])
            tt("dve", sqn[:, 8:16, :], kraw[i][:, :, :], bcast_ap(stq[i][:, 16:17], [[1, 8], [0, 96]]), ALU.mult,
               ["krawN" + sI, "krawR" + sI, "stqC" + sI], ["sqn"])
            tt("pool", qkf[:, :, 0:64], sqn[:, :, 0:64], gqk[:, :, 0:64], ALU.mult, ["sqn", "gqk"], ["qkf"])
            tt("pool", rg[:, :, :], sqn[:, :, 64:96], gqk[:, :, 64:96], ALU.mult, ["sqn", "gqk"], ["rg"])
            cosb = bcast_ap(cs[:, t, 0:16], [[0, 16], [1, 16]])
            sinb = bcast_ap(cs[:, t, 16:32], [[0, 16], [1, 16]])
            t1 = rg[:, :, 0:16]
            t2 = rg[:, :, 16:32]
            tt("pool", ra[:, :, :], t1, cosb, ALU.mult, ["rg", "cs"], ["ra"])
            tt("pool", rb[:, :, :], t2, sinb, ALU.mult, ["rg", "cs"], ["rb"])
            tt("pool", qkf[:, :, 64:80], ra[:, :, :], rb[:, :, :], ALU.subtract, ["ra", "rb"], ["qkf"])
            tt("pool", ra[:, :, :], t1, sinb, ALU.mult, ["rg", "cs"], ["ra"])
            tt("pool", rb[:, :, :], t2, cosb, ALU.mult, ["rg", "cs"], ["rb"])
            tt("pool", qkf[:, :, 80:96], ra[:, :, :], rb[:, :, :], ALU.add, ["ra", "rb"], ["qkf"])
            p6v = Pb[6][:, :].rearrange("p (h n) -> p h n", h=8)
            p7v = Pb[7][:, :].rearrange("p (h n) -> p h n", h=8)
            for h in range(8):
                tr(p6v[0:96, h, :], qkf[:, h, :], ident[:, :], ["qkf", "ident"], ["P6"])
            for h in range(8):
                tr(p7v[0:96, h, :], qkf[:, 8 + h, :], ident[:, :], ["qkf", "ident"], ["P7"])
            cp("dve", qhT[0:96, :, tsl], p6v[0:96, :, :], ["P6"], ["qhT_%d" % (t // 4)])
            cp("act", khT[0:96, :, tsl], p7v[0:96, :, :], ["P7"], ["khT"])
        S.barrier()
        A.reset(B1m)
        checkpoint("B1")
        pbuf = [A("pbuf%d" % i, [128, 512], BF16) for i in range(4)]
        lnb = A("lnb", [128, 512], F32)
        rcb = A("rcb", [128, 512], F32)
        scale = 96 ** -0.5
        it = 0
        for h in range(8):
            even = (h % 2 == 0)
            vrows = slice(0, 64) if even else slice(64, 128)
            srows = slice(64, 128) if even else slice(0, 64)
            for qg in range(4):
                qsl = slice(qg * 512, (qg + 1) * 512)
                ob = 4 + (it % 2)
                seq = []
                for kt in range(16):
                    seq.append(("s", kt))
                    if kt >= 2:
                        seq.append(("pv", kt - 2))
                seq += [("pv", 14), ("pv", 15)]
                for kind, kt in seq:
                    sb_ = kt % 3
                    pi = kt % 4
                    if kind == "s":
                        mm(P[sb_][:, :], khT[0:96, h, kt * 128:(kt + 1) * 128], qhT[0:96, h, qsl], True, True,
                           ["khT", "qhT_%d" % qg], [PN[sb_]])
                        act(pbuf[pi][:, :], P[sb_][:, :], AF.Exp, [PN[sb_]], ["pbuf%d" % pi], scale=scale)
                    else:
                        lhsT = vA[:, kt, h, :]
                        mm(P[ob][:, :], lhsT, pbuf[pi][:, :], kt == 0, kt == 15, ["vA", "pbuf%d" % pi], [PN[ob]])
                act(lnb[vrows, :], P[ob][srows, :], AF.Ln, [PN[ob]], ["lnb"])
                act(rcb[vrows, :], lnb[vrows, :], AF.Exp, ["lnb"], ["rcb"], scale=-1.0)
                tt("dve", mixT[vrows, 4 + h // 2, qsl], P[ob][vrows, :], rcb[vrows, :], ALU.mult, [PN[ob], "rcb"], ["mixT_m"])
                it += 1
        S.barrier()
        A.reset(E1)

        checkpoint("C")
        wg = A("w_in_gla", [128, 8, 1568], BF16)
        R2 = A.mark()
        qkT = A("qkT", [128, 4, S_LEN], F32)
        vtok = A("vtok", [128, NT, 512], BF16)
        sgT = A("sgT", [128, 4, S_LEN], BF16)
        lrT = A("lrT", [64, S_LEN], F32)
        wlr = A("wlr", [128, 8, 64], BF16)
        lrb = A("lrb", [64, 1], F32)
        waug = A("waug", [64, 512], F32)
        masks = A("masks", [128, 256], BF16)
        gout = A("gout", [128, 1], F32)
        onesf = A("onesf", [128, 128], F32)
        R4 = A.mark()
        dma("pool", wg[:, :, :], win_d.ap()[:, 0:1568].rearrange("(c p) n -> p c n", p=128), writes=["wg"])
        memset("pool", wlr[:, :, :], 0.0, ["wlr"])
        dma("pool", wlr[:, :, 0:16], win_d.ap()[:, 1536:1552].rearrange("(c p) n -> p c n", p=128), reads=["wlr"], writes=["wlr"])
        dma("pool", wlr[:, :, 32:48], win_d.ap()[:, 1552:1568].rearrange("(c p) n -> p c n", p=128), reads=["wlr"], writes=["wlr"])
        dma("sp", lrb[:, :], lrb_d[:, :], writes=["lrb"])
        memset("pool", waug[:, :], 0.0, ["waug"])
        dma("sp", waug[0:16, 0:256], gkf_w[:, :], reads=["waug"], writes=["waug"])
        dma("sp", waug[16:17, 0:256], gkf_b[:, :], reads=["waug"], writes=["waug"])
        dma("sp", waug[16:17, 256:512], gkb_b[:, :], reads=["waug"], writes=["waug"])
        dma("sp", waug[32:48, 256:512], gkb_w[:, :], reads=["waug"], writes=["waug"])
        dma("sp", masks[:, :], masks_d[:, :], writes=["masks"])
        dma("sp", gout[:, :], go_d[:, :], writes=["gout"])
        memset("pool", onesf[:, :], 1.0, ["onesf"])
        blk = 0
        for kind, idx in [("q", 0), ("q", 1), ("k", 0), ("k", 1), ("g", 0), ("g", 1), ("g", 2), ("g", 3), ("lr", 0)]:
            for tg in range(4):
                pbk = blk % 4
                blk += 1
                tgs = slice(tg * 512, (tg + 1) * 512)
                for kc in range(8):
                    if kind == "q":
                        lhsT = wg[:, kc, idx * 128:(idx + 1) * 128]
                    elif kind == "k":
                        lhsT = wg[:, kc, 256 + idx * 128:256 + (idx + 1) * 128]
                    elif kind == "g":
                        lhsT = wg[:, kc, 1024 + idx * 128:1024 + (idx + 1) * 128]
                    else:
                        lhsT = wlr[:, kc, :]
                    mrows = 64 if kind == "lr" else 128
                    mm(P[pbk][0:mrows, :], lhsT, hT[:, kc, tgs], kc == 0, kc == 7, ["hT_%d" % tg, "wg", "wlr"], [PN[pbk]])
                if kind == "q":
                    act(qkT[:, idx, tgs], P[pbk][:, :], AF.Copy, [PN[pbk]], ["qT"], scale=0.125)
                elif kind == "k":
                    cp("dve", qkT[:, 2 + idx, tgs], P[pbk][:, :], [PN[pbk]], ["kT"])
                elif kind == "g":
                    act(sgT[:, idx, tgs], P[pbk][:, :], AF.Silu, [PN[pbk]], ["sgT"])
                else:
                    act(lrT[:, tgs], P[pbk][0:64, :], AF.Identity, [PN[pbk], "lrb"], ["lrT"], bias=lrb[:, :])
        for t in range(NT):
            pbk = 4 + t % 2
            tsl = slice(t * 128, (t + 1) * 128)
            for kc in range(8):
                mm(P[pbk][:, :], hT[:, kc, tsl], wg[:, kc, 512:1024], kc == 0, kc == 7, ["hT_%d" % (t // 4), "wg"], [PN[pbk]])
            cp("dve" if t % 2 else "act", vtok[:, t, :], P[pbk][:, :], [PN[pbk]], ["vtok"])
        S.barrier()

        checkpoint("B2")
        HTB = L0
        tmp = [A("gt%d" % i, [128, 1024], F32, at=HTB + i * 4096) for i in range(4)]
        prod = {}
        names = [(d_, hp, k_) for d_ in (0, 1) for hp in (0, 1) for k_ in ("qr", "kr", "qb")]
        slots = [HTB + 16384 + i * 4096 for i in range(4)] + [E1 + 16384 + i * 4096 for i in range(2)]
        for i, nm in enumerate(names):
            if i < 6:
                prod[nm] = A("pr", [128, S_LEN], BF16, at=slots[i])
            else:
                prod[nm] = A("pr", [128, S_LEN], BF16)
        kdT = A("kdT", [128, 1024], BF16)
        dec = A("dec", [128, 4, 32], F32)
        smask = A("smask", [128, 1024], F32)
        kd = A("kd", [128, NT, 512], BF16, at=E1)
        memset("pool", smask[:, :], 1.0, ["smask"])
        memset("pool", smask[:, :].rearrange("p (c j) -> p c j", j=64)[:, :, 0:1], 0.0, ["smask"])
        G, Fc, Dt, Eb = tmp
        Dt2 = A("Dt2", [128, 1024], F32)
        Eb2 = A("Eb2", [128, 1024], F32)
        DtL = [(Dt, "Dt"), (Dt2, "Dt2")]
        EbL = [(Eb, "Eb"), (Eb2, "Eb2")]
        cnt = {"d": 0, "e": 0}

        def nextD():
            cnt["d"] += 1
            return DtL[cnt["d"] % 2]

        def nextE():
            cnt["e"] += 1
            return EbL[cnt["e"] % 2]

        def exp_prod(src, sname, scl, dst, base, bname, dname="prod"):
            E_, en = nextE()
            act(E_[:, :], src, AF.Exp, [sname], [en], scale=scl)
            tt("pool", dst, base, E_[:, :], ALU.mult, [bname, en], [dname])
        for d_ in (0, 1):
            for hp in (0, 1):
                dh = d_ * 2 + hp
                qT = qkT[:, hp, :]
                kT = qkT[:, 2 + hp, :]
                for half in range(2):
                    hs = slice(half * 1024, (half + 1) * 1024)
                    for j in range(2):
                        pbk = j
                        cols = slice(half * 1024 + j * 512, half * 1024 + (j + 1) * 512)
                        mm(P[pbk][:, :], waug[0:64, dh * 128:(dh + 1) * 128], lrT[0:64, cols], True, True,
                           ["waug", "lrT"], [PN[pbk]])
                        act(Eb[:, j * 512:(j + 1) * 512], P[pbk][:, :], AF.Exp, [PN[pbk]], ["Eb"], scale=-1.0)
                    act(G[:, :], Eb[:, :], AF.Ln, ["Eb"], ["G"], bias=1.0)
                    op("dve", lambda e: e.tensor_tensor_scan(out=Fc[:, :], data0=smask[:, :], data1=G[:, :], initial=0.0,
                                                             op0=ALU.mult, op1=ALU.add), reads=["smask", "G"], writes=["Fc"])
                    Fv = Fc[:, :].rearrange("p (c j) -> p c j", j=64)
                    Dv = Dt[:, :].rearrange("p (c j) -> p c j", j=64)
                    T63 = bcast_ap(Fc[:, 63:64], [[64, 16], [0, 64]])
                    act(dec[:, dh, half * 16:(half + 1) * 16], Fv[:, :, 63], AF.Exp, ["Fc"], ["dec"], scale=-1.0 / 16)
                    if d_ == 0:
                        ref = bcast_ap(Fc[:, 31:32], [[64, 16], [0, 64]])
                        D_, dn = nextD()
                        tt("dve", D_[:, :].rearrange("p (c j) -> p c j", j=64), Fv, ref, ALU.subtract, ["Fc"], [dn])
                        exp_prod(D_[:, :], dn, -1.0 / 16, prod[(0, hp, "qr")][:, hs], qT[:, hs], "qT")
                        exp_prod(D_[:, :], dn, 1.0 / 16, prod[(0, hp, "kr")][:, hs], kT[:, hs], "kT")
                        exp_prod(Fc[:, :], "Fc", -1.0 / 16, prod[(0, hp, "qb")][:, hs], qT[:, hs], "qT")
                        D_, dn = nextD()
                        tt("dve", D_[:, :].rearrange("p (c j) -> p c j", j=64), Fv, T63, ALU.subtract, ["Fc"], [dn])
                        exp_prod(D_[:, :], dn, 1.0 / 16, kdT[:, :], kT[:, hs], "kT", "kdT")
                    else:
                        tt("dve", G[:, :], Fc[:, :], G[:, :], ALU.subtract, ["Fc", "G"], ["G"])
                        Gv = G[:, :].rearrange("p (c j) -> p c j", j=64)
                        ref = bcast_ap(G[:, 32:33], [[64, 16], [0, 64]])
                        D_, dn = nextD()
                        tt("dve", D_[:, :].rearrange("p (c j) -> p c j", j=64), Gv, ref, ALU.subtract, ["G"], [dn])
                        exp_prod(D_[:, :], dn, 1.0 / 16, prod[(1, hp, "qr")][:, hs], qT[:, hs], "qT")
                        exp_prod(D_[:, :], dn, -1.0 / 16, prod[(1, hp, "kr")][:, hs], kT[:, hs], "kT")
                        D_, dn = nextD()
                        tt("dve", D_[:, :].rearrange("p (c j) -> p c j", j=64), Gv, T63, ALU.subtract, ["G", "Fc"], [dn])
                        exp_prod(D_[:, :], dn, 1.0 / 16, prod[(1, hp, "qb")][:, hs], qT[:, hs], "qT")
                        exp_prod(G[:, :], "G", -1.0 / 16, kdT[:, :], kT[:, hs], "kT", "kdT")
                    for g4 in range(2):
                        pbk = 2 + g4
                        pv = Pb[pbk][:, 0:512].rearrange("p (t n) -> p t n", t=4)
                        for tq in range(4):
                            c0 = (g4 * 4 + tq) * 128
                            tr(pv[:, tq, :], kdT[:, c0:c0 + 128], ident[:, :], ["kdT", "ident"], [PN[pbk]])
                        t0 = half * 8 + g4 * 4
                        cp("dve", kd[:, t0:t0 + 4, dh * 128:(dh + 1) * 128], pv, [PN[pbk]], ["kd"])
        S.barrier()

        checkpoint("D1")
        qk_off = R2
        Sst = [A("Sst%d" % i, [128, 32, 128], BF16, at=qk_off + i * 8192) for i in range(4)]
        Sf = [A("Sf%d" % i, [128, 256], F32, at=HTB + i * 1024) for i in range(4)]
        for dh in range(4):
            memset("pool", Sf[dh][:, :], 0.0, ["Sf%d" % dh])
        for step in range(32):
            for dh in range(4):
                d_, hp = divmod(dh, 2)
                n = step if d_ == 0 else 31 - step
                t, c = divmod(n, 2)
                rows = slice(c * 64, (c + 1) * 64)
                cp("pool", Sst[dh][0:64, n, :], Sf[dh][0:64, 0:128], ["Sf%d" % dh], ["SstA%d" % dh])
                cp("act", Sst[dh][64:128, n, :], Sf[dh][64:128, 128:256], ["Sf%d" % dh], ["SstB%d" % dh])
                if step == 31:
                    continue
                pbk = 4 * c + dh
                mm(P[pbk][:, 0:256], kd[rows, t, dh * 128:(dh + 1) * 128], vtok[rows, t, hp * 256:(hp + 1) * 256],
                   True, True, ["kd", "vtok"], [PN[pbk]])
                stt(Sf[dh][:, :], Sf[dh][:, :], dec[:, dh, n:n + 1], P[pbk][:, 0:256], ALU.mult, ALU.add,
                    ["Sf%d" % dh, "dec", PN[pbk]], ["Sf%d" % dh])
        S.barrier()

        checkpoint("D2")
        smb = [A("smb%d" % i, [128, 2, 2, 128], BF16, at=HTB + 4096 + i * 1024) for i in range(2)]
        sqoL = [A("sqo%d" % i, [128, 256], F32, at=HTB + 6144 + i * 1024) for i in range(2)]
        rsoL = [A("rso%d" % i, [128, 256], F32, at=HTB + 8192 + i * 1024) for i in range(2)]
        t1oL = [A("t1o%d" % i, [128, 256], F32, at=HTB + 10240 + i * 1024) for i in range(2)]
        for t in range(NT):
            tsl = slice(t * 128, (t + 1) * 128)
            for par in range(2):
                rows = slice(par * 64, (par + 1) * 64)
                sbk = par
                obk = 2 + par
                scv = P[sbk][:, :].rearrange("p (a b n) -> p a b n", a=2, b=2)
                for hp in range(2):
                    for d_ in range(2):
                        mm(scv[:, hp, d_, :], prod[(d_, hp, "kr")][rows, tsl], prod[(d_, hp, "qr")][rows, tsl], True, True,
                           ["prod"], [PN[sbk]])
                mk = bcast_ap(masks[:, 0:256], [[0, 2], [1, 256]])
                tt("dve", smb[par][:, :, :, :].rearrange("p a b n -> p a (b n)"),
                   P[sbk][:, :].rearrange("p (a m) -> p a m", a=2), mk, ALU.mult, [PN[sbk], "masks"], ["smb%d" % par])
                ov = P[obk][:, 0:256].rearrange("p (a n) -> p a n", a=2)
                for hp in range(2):
                    h = hp * 2 + par
                    mm(ov[:, hp, :], vtok[:, t, h * 128:(h + 1) * 128], smb[par][:, hp, 0, :], True, False,
                       ["vtok", "smb%d" % par], [PN[obk]])
                    mm(ov[:, hp, :], vtok[:, t, h * 128:(h + 1) * 128], smb[par][:, hp, 1, :], False, False,
                       ["vtok", "smb%d" % par], [PN[obk]])
                    for d_ in range(2):
                        dh = d_ * 2 + hp
                        for c in range(2):
                            n = t * 2 + c
                            csl = slice(t * 128 + c * 64, t * 128 + (c + 1) * 64)
                            last = (d_ == 1 and c == 1)
                            mm(ov[:, hp, c * 64:(c + 1) * 64], Sst[dh][rows, n, :], prod[(d_, hp, "qb")][rows, csl],
                               False, last, ["SstA%d" % dh, "SstB%d" % dh, "prod"], [PN[obk]])
                sqo, rso, t1o = sqoL[par], rsoL[par], t1oL[par]
                sP = "%d" % par
                act(sqo[:, :], P[obk][:, 0:256], AF.Square, [PN[obk]], ["sqo" + sP])
                ebk = 4 + par
                mm(P[ebk][:, 0:256], onesf[:, :], sqo[:, :], True, True, ["onesf", "sqo" + sP], [PN[ebk]])
                rsqrt_act(rso[:, :], P[ebk][:, 0:256], 128, [PN[ebk]], ["rso" + sP])
                stt(t1o[:, :], P[obk][:, 0:256], gout[:, 0:1], rso[:, :], ALU.mult, ALU.mult, [PN[obk], "gout", "rso" + sP], ["t1o" + sP])
                for hp in range(2):
                    h = hp * 2 + par
                    tt("pool", mixT[:, h, tsl], t1o[:, hp * 128:(hp + 1) * 128], sgT[:, h, tsl], ALU.mult,
                       ["t1o" + sP, "sgT"], ["mixT_g"])
        S.barrier()
        A.reset(L0)
        if debug:
            final_ops.append(dma("sp", dbg["mix"][:, :], mixT[:, :, :].rearrange("p c n -> p (c n)"), reads=["mixT_g", "mixT_m"]))

        checkpoint("D3")
        X = A("X", [128, NT, D], F32)
        h2T = A("h2T", [128, 8, S_LEN], BF16)
        wo = A("wo", [128, 8, D], BF16)
        WO_OFF = A.off - 16384
        g2 = A("g2", [128, D], F32)
        wr = A("wr", [128, 8, 36], BF16)
        rbias = A("rbias", [128, 36], F32)
        gT = A("gT", [32, 2, S_LEN], BF16)
        sel = A("sel", [32, 32, 128], BF16)
        lgA = A("lgA", [128, NT, 36], F32)
        hb = [A("hb2_%d" % i, [128, D], BF16) for i in range(2)]
        sqj = A("sqj2", [128, D], BF16)
        st1 = A("st1_2", [128, 4], F32)
        dma("pool", wo[:, :, :], wout_d.ap().rearrange("(c p) n -> p c n", p=128), writes=["wo"])
        dma("sp", g2[:, :], bcast_row(g2_d, D), writes=["g2"])
        dma("pool", wr[:, :, 0:4], wrg_d.ap().rearrange("(c p) n -> p c n", p=128), writes=["wr"])
        dma("pool", wr[:, :, 4:36], wre_d.ap().rearrange("(c p) n -> p c n", p=128), writes=["wr"])
        dma("sp", rbias[:, 0:4], bcast_row(brg_d, 4), writes=["rbias"])
        dma("sp", rbias[:, 4:36], bcast_row(bre_d, 32), writes=["rbias"])
        dma("sp", sel[:, :, :], sel_d.ap().rearrange("p (e n) -> p e n", e=32), writes=["sel"])
        for t in range(NT):
            tsl = slice(t * 128, (t + 1) * 128)
            dma("sp", X[:, t, :], x_d[tsl, :], writes=["X%d" % t])
            for ch in range(2):
                pbk = (t % 2) * 2 + ch
                for kc in range(8):
                    mm(P[pbk][:, :], mixT[:, kc, tsl], wo[:, kc, ch * 512:(ch + 1) * 512], kc == 0, kc == 7,
                       ["mixT_g", "mixT_m", "wo"], [PN[pbk]])
                tt("dve", X[:, t, ch * 512:(ch + 1) * 512], X[:, t, ch * 512:(ch + 1) * 512], P[pbk][:, :], ALU.add,
                   ["X%d" % t, PN[pbk]], ["X%d" % t])
        if debug:
            for t in range(NT):
                final_ops.append(dma("sp", dbg["x1"][t * 128:(t + 1) * 128, :], X[:, t, :], reads=["X%d" % t]))
        checkpoint("E")
        for t in range(NT):
            tsl = slice(t * 128, (t + 1) * 128)
            norm_to_T(lambda t: X[:, t, :], ["X%d" % t], g2, "g2", h2T, "h2T", "F", t, 4 + t % 2)
            rbk = 6 + t % 2
            for kc in range(8):
                mm(P[rbk][:, 0:36], h2T[:, kc, tsl], wr[:, kc, :], kc == 0, kc == 7, ["h2T_%d" % (t // 4), "wr"], [PN[rbk]])
            tt("dve", lgA[:, t, :], P[rbk][:, 0:36], rbias[:, :], ALU.add, [PN[rbk], "rbias"], ["lgA"])

        def bl(ap2, k):
            return bcast_ap(ap2, [list(ap2.ap[1]), [0, k]])

        def red(out, in_, o, reads, writes):
            return op("dve", lambda e: e.tensor_reduce(out=out, in_=in_, axis=AX.X, op=o), reads=reads, writes=writes,
                      n=fsz(in_))

        r16 = lambda nm: A(nm, [128, NT], F32)
        r4 = lambda nm: A(nm, [128, NT, 4], F32)
        r8 = lambda nm: A(nm, [128, NT, 8], F32)
        mg, s4, ptop, m1, m2, dm, e2, den, w1, w2 = [r16("r16_%d" % i) for i in range(10)]
        d4, e4, oh, ohp = [r4("r4_%d" % i) for i in range(4)]
        ls, tmp8, eq1, ls2, eq2, wg8 = [r8("r8_%d" % i) for i in range(6)]
        gate = A("gate", [128, NT, 32], F32)
        gtmp = A("gtmp", [128, NT, 32], F32)
        ghl = A("ghl", [128, 2, NT, 32], BF16)
        red(mg[:, :], lgA[:, :, 0:4], ALU.max, ["lgA"], ["mg"])
        tt("dve", d4[:, :, :], lgA[:, :, 0:4], bl(mg[:, :], 4), ALU.subtract, ["lgA", "mg"], ["d4"])
        act(e4[:, :, :], d4[:, :, :], AF.Exp, ["d4"], ["e4"])
        red(s4[:, :], e4[:, :, :], ALU.add, ["e4"], ["s4"])
        op("dve", lambda e: e.reciprocal(out=ptop[:, :], in_=s4[:, :]), reads=["s4"], writes=["ptop"], n=128)
        ts("dve", oh[:, :, :], d4[:, :, :], 0.0, None, ALU.is_equal, None, ["d4"], ["oh"])
        tt("dve", ohp[:, :, :], oh[:, :, :], bl(ptop[:, :], 4), ALU.mult, ["oh", "ptop"], ["ohp"])
        tt("dve", ls[:, :, :], lgA[:, :, 4:12], bl(oh[:, :, 0], 8), ALU.mult, ["lgA", "oh"], ["ls"])
        for g_ in range(1, 4):
            tt("dve", tmp8[:, :, :], lgA[:, :, 4 + 8 * g_:12 + 8 * g_], bl(oh[:, :, g_], 8), ALU.mult, ["lgA", "oh"], ["tmp8"])
            tt("dve", ls[:, :, :], ls[:, :, :], tmp8[:, :, :], ALU.add, ["ls", "tmp8"], ["ls"])
        red(m1[:, :], ls[:, :, :], ALU.max, ["ls"], ["m1"])
        tt("dve", eq1[:, :, :], ls[:, :, :], bl(m1[:, :], 8), ALU.is_equal, ["ls", "m1"], ["eq1"])
        stt(ls2[:, :, :], eq1[:, :, :], -1e30, ls[:, :, :], ALU.mult, ALU.add, ["eq1", "ls"], ["ls2"])
        red(m2[:, :], ls2[:, :, :], ALU.max, ["ls2"], ["m2"])
        tt("dve", eq2[:, :, :], ls2[:, :, :], bl(m2[:, :], 8), ALU.is_equal, ["ls2", "m2"], ["eq2"])
        tt("dve", dm[:, :], m2[:, :], m1[:, :], ALU.subtract, ["m1", "m2"], ["dm"])
        act(e2[:, :], dm[:, :], AF.Exp, ["dm"], ["e2"])
        ts("dve", den[:, :], e2[:, :], 1.0, None, ALU.add, None, ["e2"], ["den"])
        op("dve", lambda e: e.reciprocal(out=w1[:, :], in_=den[:, :]), reads=["den"], writes=["w1"], n=128)
        tt("dve", w2[:, :], e2[:, :], w1[:, :], ALU.mult, ["e2", "w1"], ["w2"])
        tt("dve", wg8[:, :, :], eq1[:, :, :], bl(w1[:, :], 8), ALU.mult, ["eq1", "w1"], ["wg8"])
        tt("dve", tmp8[:, :, :], eq2[:, :, :], bl(w2[:, :], 8), ALU.mult, ["eq2", "w2"], ["tmp8"])
        tt("dve", wg8[:, :, :], wg8[:, :, :], tmp8[:, :, :], ALU.add, ["wg8", "tmp8"], ["wg8"])
        for g_ in range(4):
            tt("dve", gate[:, :, g_ * 8:(g_ + 1) * 8], wg8[:, :, :], bl(ohp[:, :, g_], 8), ALU.mult, ["wg8", "ohp"], ["gate"])
        if debug:
            for t in range(NT):
                final_ops.append(dma("sp", dbg["gate"][t * 128:(t + 1) * 128, :], gate[:, t, :], reads=["gate"]))
        cp("dve", ghl[:, 0, :, :], gate[:, :, :], ["gate"], ["ghl0"])
        tt("dve", gtmp[:, :, :], gate[:, :, :], ghl[:, 0, :, :], ALU.subtract, ["gate", "ghl0"], ["gtmp"])
        cp("dve", ghl[:, 1, :, :], gtmp[:, :, :], ["gtmp"], ["ghl1"])
        for a_ in range(2):
            for half in range(2):
                bk = 4 + a_ * 2 + half
                pv = Pb[bk][:, :].rearrange("p (t n) -> p t n", t=8)
                for tq in range(8):
                    tr(pv[0:32, tq, :], ghl[:, a_, half * 8 + tq, :], ident[:, :], ["ghl%d" % a_, "ident"], [PN[bk]])
                cp("act" if half else "dve", gT[0:32, a_, half * 1024:(half + 1) * 1024], Pb[bk][0:32, :], [PN[bk]], ["gT"])
        checkpoint("F")

        EG = 2
        NEG = 32 // EG
        MX = SB_BASE + 256
        S.alias(["hid0", "hid1", "sil0", "sil1", "t1m0", "t1m1", "wdn0", "wdn1"], ["mixT_g", "mixT_m"])
        hid = [A("hid%d" % b, [128, EG, 2, 512], BF16, at=MX + b * 4096) for b in range(2)]
        sil = [A("sil%d" % b, [128, 512], F32, at=MX + 8192 + b * 2048) for b in range(2)]
        t1m = [A("t1m%d" % b, [128, 512], F32, at=MX + 12288 + b * 2048) for b in range(2)]
        wdn = [[A("wd%d_%d" % (b, j), [128, 2, D], BF16, at=MX + 16384 + (b * EG + j) * 4096) for j in range(EG)] for b in range(2)]
        wgt = [None, None]
        wup = [None, None]
        wgt[0] = [A("wg0_%d" % j, [128, 8, 256], BF16) for j in range(EG)]
        wup[0] = [A("wu0_%d" % j, [128, 8, 256], BF16) for j in range(EG)]
        wgt[1] = [A("wg1_%d" % j, [128, 8, 256], BF16, at=WO_OFF + j * 4096) for j in range(EG)]
        wup[1] = [A("wu1_%d" % j, [128, 8, 256], BF16, at=WO_OFF + 8192 + j * 4096) for j in range(EG)]
        itc = 0
        for eg in range(NEG):
            b = eg % 2
            extra = ["wo"] if b == 1 else []
            for j in range(EG):
                e_ = eg * EG + j
                dma("pool", wgt[b][j][:, :, :], weg_d.ap()[e_].rearrange("(c p) f -> p c f", p=128), writes=["wgt%d" % b] + extra)
                dma("pool", wup[b][j][:, :, :], weu_d.ap()[e_].rearrange("(c p) f -> p c f", p=128), writes=["wup%d" % b] + extra)
                dma("pool", wdn[b][j][:, :, :], wed_d.ap()[e_].rearrange("(c p) d -> p c d", p=128), writes=["wdn%d" % b])
            for tg in range(4):
                hbi = itc % 2
                itc += 1
                tgs = slice(tg * 512, (tg + 1) * 512)
                for j in range(EG):
                    e_ = eg * EG + j
                    gbk = 6 + j
                    for fh in range(2):
                        k2 = fh
                        gb_, ub_ = 0 + k2, 2 + k2
                        for kc in range(8):
                            mm(P[gb_][:, :], wgt[b][j][:, kc, fh * 128:(fh + 1) * 128], h2T[:, kc, tgs], kc == 0, kc == 7,
                               ["wgt%d" % b, "h2T_%d" % tg], [PN[gb_]])
                        for kc in range(8):
                            mm(P[ub_][:, :], wup[b][j][:, kc, fh * 128:(fh + 1) * 128], h2T[:, kc, tgs], kc == 0, kc == 7,
                               ["wup%d" % b, "h2T_%d" % tg], [PN[ub_]])
                        if fh == 0:
                            mm(P[gbk][:, :], sel[0:32, e_, :], gT[0:32, 0, tgs], True, False, ["sel", "gT"], [PN[gbk]])
                            mm(P[gbk][:, :], sel[0:32, e_, :], gT[0:32, 1, tgs], False, True, ["sel", "gT"], [PN[gbk]])
                        act(sil[k2][:, :], P[gb_][:, :], AF.Silu, [PN[gb_]], ["sil%d" % k2])
                        tt("dve", t1m[k2][:, :], sil[k2][:, :], P[ub_][:, :], ALU.mult, ["sil%d" % k2, PN[ub_]], ["t1m%d" % k2])
                        tt("dve", hid[hbi][:, j, fh, :], t1m[k2][:, :], P[gbk][:, :], ALU.mult, ["t1m%d" % k2, PN[gbk]],
                           ["hid%d" % hbi])
                for tt_ in range(4):
                    t = tg * 4 + tt_
                    for ch in range(2):
                        abk = 4 + (tt_ * 2 + ch) % 2
                        n_acc = EG * 2
                        a_i = 0
                        for j in range(EG):
                            for fh in range(2):
                                mm(P[abk][:, :], hid[hbi][:, j, fh, tt_ * 128:(tt_ + 1) * 128],
                                   wdn[b][j][:, fh, ch * 512:(ch + 1) * 512], a_i == 0, a_i == n_acc - 1,
                                   ["hid%d" % hbi, "wdn%d" % b], [PN[abk]])
                                a_i += 1
                        tt("dve", X[:, t, ch * 512:(ch + 1) * 512], X[:, t, ch * 512:(ch + 1) * 512], P[abk][:, :], ALU.add,
                           ["X%d" % t, PN[abk]], ["X%d" % t])
        for t in range(NT):
            final_ops.append(dma("sp", out_d[t * 128:(t + 1) * 128, :], X[:, t, :], reads=["X%d" % t]))

        S.emit(es, final_wait_ops=final_ops)
    return nc


def make_consts():
    ident = np.eye(128, dtype=np.float32).astype(ml_dtypes.bfloat16)
    j = np.arange(128)[:, None]
    i = np.arange(128)[None, :]
    same = (j // 64) == (i // 64)
    mf = (same & (j <= i)).astype(np.float32)
    mb = (same & (j > i)).astype(np.float32)
    masks = np.concatenate([mf, mb], axis=1).astype(ml_dtypes.bfloat16)
    invf = (10000.0 ** (-np.arange(0, 32, 2, dtype=np.float32) / 32)).astype(np.float32)
    invf = np.broadcast_to(invf[None, :], (128, 16)).copy()
    sel = np.zeros((32, 32, 128), np.float32)
    for e in range(32):
        sel[e, e, :] = 1.0
    sel = sel.reshape(32, 32 * 128).astype(ml_dtypes.bfloat16)
    lrb = np.zeros((64, 1), np.float32)
    lrb[16, 0] = 1.0
    return {"c_ident": ident, "c_masks": masks, "c_invf": invf, "c_sel": sel, "c_lrbias": lrb}


_NC_CACHE = {}


def make_in_maps(inputs, n_cores=8):
    c = make_consts()
    f = lambda k: np.ascontiguousarray(np.asarray(inputs[k], dtype=np.float32)[0])
    shared = {
        "norm1_gain": f("norm1_gain").reshape(1, D),
        "w_in": f("w_in"),
        "gla_gk_fwd_w": f("gla_gk_fwd_w"), "gla_gk_fwd_b": f("gla_gk_fwd_b").reshape(1, 256),
        "gla_gk_bwd_w": f("gla_gk_bwd_w"), "gla_gk_bwd_b": f("gla_gk_bwd_b").reshape(1, 256),
        "gla_out_gain": f("gla_out_gain").reshape(128, 1),
        "mla_q_gain": f("mla_q_gain").reshape(1, 256), "mla_w_qb": f("mla_w_qb"),
        "mla_kv_gain": f("mla_kv_gain").reshape(1, 128), "mla_w_kvb": f("mla_w_kvb"),
        "q_norm_gain": f("q_norm_gain").reshape(1, 96), "k_norm_gain": f("k_norm_gain").reshape(1, 96),
        "w_out": f("w_out"), "norm2_gain": f("norm2_gain").reshape(1, D),
        "w_router_group": f("w_router_group"), "b_router_group": f("b_router_group").reshape(1, 4),
        "w_router_expert": f("w_router_expert"), "b_router_expert": f("b_router_expert").reshape(1, 32),
        "w_expert_gate": f("w_expert_gate").reshape(32, D, 256),
        "w_expert_up": f("w_expert_up").reshape(32, D, 256),
        "w_expert_down": f("w_expert_down").reshape(32, 256, D),
    }
    shared.update(c)
    x = np.asarray(inputs["x"], dtype=np.float32)
    pos = np.asarray(inputs["positions"]).astype(np.int32)
    maps = []
    for b in range(n_cores):
        m = dict(shared)
        m["x"] = np.ascontiguousarray(x[b])
        m["pos"] = np.ascontiguousarray(pos[b].reshape(NT, 128).T)
        maps.append(m)
    return maps


def kernel(**inputs):
    if "nc" not in _NC_CACHE:
        _NC_CACHE["nc"] = build()
    nc = _NC_CACHE["nc"]
    maps = make_in_maps(inputs, 8)
    res = run_bass_kernel_spmd(nc, maps, core_ids=list(range(8)))
    out = np.stack([np.asarray(r["out"], dtype=np.float32) for r in res.results], axis=0)
    return out
```

```python
import contextlib
import math
import numpy as np
import ml_dtypes
import concourse.bass as bass
import concourse.mybir as mybir
from concourse.bass_utils import run_bass_kernel_spmd

F32 = mybir.dt.float32
BF16 = mybir.dt.bfloat16
I32 = mybir.dt.int32
ALU = mybir.AluOpType
AF = mybir.ActivationFunctionType
AX = mybir.AxisListType

S_LEN = 2048
D = 1024
NT = 16
EPS = 1e-6
PI = math.pi


class T:
    __slots__ = ("name", "w", "r")

    def __init__(self, name):
        self.name = name
        self.w = None
        self.r = []


class Op:
    __slots__ = ("eng", "fn", "deps", "signal", "sig", "dma", "dsem", "dval", "alld", "n", "seg", "idx", "nbytes", "tag")


class Sched:
    ENGS = ("pe", "act", "dve", "pool", "sp")

    def __init__(self, nc, n_dma_sems=12):
        self.nc = nc
        self.ops = {e: [] for e in self.ENGS}
        self.n_dma_sems = n_dma_sems
        self.dma_count = {e: 0 for e in self.ENGS}
        self.tiles = {}
        self.pending = {e: [] for e in self.ENGS}
        self.dma_since_barrier = []
        self.stopped = False
        self.seg = 0
        self.nops = 0

    def t(self, name):
        if name not in self.tiles:
            self.tiles[name] = T(name)
        return self.tiles[name]

    def _tl(self, lst):
        out = []
        for x in lst:
            if isinstance(x, str):
                out.append(self.t(x))
            elif isinstance(x, (list, tuple)):
                out.extend(self._tl(x))
            elif x is not None:
                out.append(x)
        return out

    def alias(self, new_names, old_names):
        if self.stopped:
            return
        for nn in new_names:
            tn = self.t(nn)
            for on in old_names:
                to = self.t(on)
                if to.w is not None:
                    tn.r.append(to.w)
                tn.r.extend(to.r)

    def barrier(self):
        if self.stopped:
            return
        lasts = []
        for e in self.ENGS:
            for o in reversed(self.ops[e]):
                if not o.dma:
                    lasts.append(o)
                    break
        lasts.extend(self.dma_since_barrier)
        self.dma_since_barrier = []
        for e in self.ENGS:
            self.pending[e] = list(lasts)
        self.seg += 1

    def op(self, eng, fn, reads=(), writes=(), dma=False, n=64, nbytes=0):
        if self.stopped:
            return None
        o = Op()
        o.n = n
        o.nbytes = nbytes
        o.seg = self.seg
        o.idx = self.nops
        self.nops += 1
        o.eng = eng
        o.fn = fn
        o.dma = dma
        o.signal = False
        o.sig = 0
        deps = {}
        reads = self._tl(reads)
        writes = self._tl(writes)
        for t in reads:
            if t.w is not None:
                deps[id(t.w)] = (t.w, "raw")
            if t.name[0] == "P" and t.name[1:].isdigit():
                for r in t.r:
                    if id(r) not in deps and r.eng != eng:
                        deps[id(r)] = (r, "war")
        for t in writes:
            if t.w is not None and id(t.w) not in deps:
                deps[id(t.w)] = (t.w, "waw")
            for r in t.r:
                if id(r) not in deps:
                    deps[id(r)] = (r, "war")
        if self.pending[eng]:
            for p in self.pending[eng]:
                deps[id(p)] = (p, "raw")
            self.pending[eng] = []
        o.alld = [p for p, _k in deps.values()]
        o.tag = ("R:" + ",".join(t.name for t in reads) + " W:" + ",".join(t.name for t in writes))
        dl = []
        for p, kind in deps.values():
            if p.eng == eng and not p.dma:
                if eng == "pe":
                    continue
                if kind != "raw" and not dma and eng != "pool":
                    continue
            dl.append(p)
        o.deps = dl
        for p in dl:
            p.signal = True
        for t in reads:
            if not dma:
                for r in t.r:
                    if not r.dma and r.eng == eng:
                        o.alld.append(r)
                t.r = [r for r in t.r if r.dma or r.eng != eng]
            t.r.append(o)
        for t in writes:
            t.w = o
            t.r = []
        if dma:
            self.dma_count[eng] += 1
            self.dma_since_barrier.append(o)
        self.ops[eng].append(o)
        return o

    @staticmethod
    def _dur(o):
        n = o.n
        if o.dma:
            return 60.0 if o.eng == "sp" else 900.0
        if o.eng == "pe":
            return 30.0 + max(n, 64) / 2.0
        if o.eng == "act":
            return 220.0 + n / 1.4
        if o.eng == "dve":
            return 120.0 + n * 1.3
        return 550.0 + n * 0.75

    def reschedule(self):
        allops = []
        for e in self.ENGS:
            allops.extend(self.ops[e])
        allops.sort(key=lambda o: o.idx)
        import heapq
        new = {e: [] for e in self.ENGS}
        segs = {}
        for o in allops:
            segs.setdefault(o.seg, []).append(o)
        LAT = 250.0
        for sg in sorted(segs):
            ops = segs[sg]
            inseg = set(id(o) for o in ops)
            done = {}
            users = {}
            indeg = {}
            first = {}
            for o in ops:
                if o.eng not in first:
                    first[o.eng] = o
                elif first[o.eng] not in o.alld:
                    o.alld.append(first[o.eng])
            for o in ops:
                k = 0
                for p in o.alld:
                    if id(p) in inseg:
                        k += 1
                        users.setdefault(id(p), []).append(o)
                indeg[id(o)] = k
            ready = {e: [] for e in self.ENGS}
            efree = {e: 0.0 for e in self.ENGS}
            rtime = {}
            for o in ops:
                if indeg[id(o)] == 0:
                    rtime[id(o)] = 0.0
                    ready[o.eng].append(o)
            left = len(ops)
            SLACK = 0.0
            while left:
                best = None
                for e in self.ENGS:
                    rl = ready[e]
                    if not rl:
                        continue
                    ef = efree[e]
                    oldest = None
                    fill = None
                    for o in rl:
                        st = max(rtime[id(o)], ef)
                        if oldest is None or o.idx < oldest[1].idx:
                            oldest = (st, o)
                        if fill is None or (st, o.idx) < (fill[0], fill[1].idx):
                            fill = (st, o)
                    pick = oldest if oldest[0] <= fill[0] + SLACK else fill
                    if best is None or (pick[0], pick[1].idx) < (best[0], best[1].idx):
                        best = pick
                st, o = best
                e = o.eng
                ready[e].remove(o)
                d = self._dur(o)
                efree[e] = st + d
                fin = st + d
                if o.dma:
                    fin = st + 2000.0 + o.nbytes / 150.0
                done[id(o)] = fin
                new[e].append(o)
                left -= 1
                for u in users.get(id(o), ()):
                    indeg[id(u)] -= 1
                    lat = 0.0 if (u.eng == o.eng and not o.dma) else LAT
                    rtime[id(u)] = max(rtime.get(id(u), 0.0), fin + lat)
                    if indeg[id(u)] == 0:
                        ready[u.eng].append(u)
        self.ops = new

    def emit(self, es, final_wait_ops=()):
        nc = self.nc
        if RESCHEDULE:
            self.reschedule()
        sems = {e: es.enter_context(nc.semaphore("s_" + e)) for e in self.ENGS}
        dsems = {e: [es.enter_context(nc.semaphore("d_%s_%d" % (e, i)))
                     for i in range(self.n_dma_sems)]
                 for e in self.ENGS if self.dma_count[e] > 0}
        for e in self.ENGS:
            c = 0
            i = 0
            for o in self.ops[e]:
                if o.dma:
                    o.dsem = i % self.n_dma_sems
                    o.dval = 16 * (i // self.n_dma_sems + 1)
                    i += 1
                elif o.signal:
                    c += 1
                    o.sig = c
        block = es.enter_context(nc.Block())
        eng_obj = {"pe": block.tensor, "act": block.scalar, "dve": block.vector,
                   "pool": block.gpsimd, "sp": block.sync}
        for e in self.ENGS:
            ops = self.ops[e]
            if not ops:
                continue

            def body(engine, e=e, ops=ops):
                waited = {}

                def wait(sem, key, val):
                    if waited.get(key, 0) >= val:
                        return
                    waited[key] = val
                    engine.wait_ge(sem, val)

                for o in ops:
                    for p in o.deps:
                        if p.dma:
                            wait(dsems[p.eng][p.dsem], ("d", p.eng, p.dsem), p.dval)
                        else:
                            wait(sems[p.eng], ("c", p.eng), p.sig)
                    if o.dma and o.dval > 16:
                        wait(dsems[e][o.dsem], ("d", e, o.dsem), o.dval - 16)
                    ins = o.fn(engine)
                    if o.dma:
                        ins.then_inc(dsems[e][o.dsem], 16)
                    elif o.signal:
                        ins.then_inc(sems[e], 1)
                if e == "sp":
                    for o in final_wait_ops:
                        if o is None:
                            continue
                        wait(dsems[o.eng][o.dsem], ("d", o.eng, o.dsem), o.dval)

            eng_obj[e](body)


RESCHEDULE = True
SB_BASE = 16640
SB_END = 229376


class Alloc:
    def __init__(self, nc):
        self.nc = nc
        self.off = SB_BASE
        self.n = 0

    def mark(self):
        return self.off

    def reset(self, m):
        self.off = m

    def __call__(self, name, shape, dt, at=None):
        esz = 2 if dt == BF16 else 4
        nb = int(np.prod(shape[1:])) * esz
        nb = (nb + 63) // 64 * 64
        self.n += 1
        if at is None:
            at = self.off
            self.off += nb
        assert at + nb <= SB_END, ("SBUF overflow", name, at, nb)
        return self.nc.alloc_sbuf_tensor_at("%s_%d" % (name, self.n), list(shape), dt, offset=at)


def bcast_ap(ap, pattern):
    return bass.AP(ap.tensor, ap.offset, [list(ap.ap[0])] + [list(p) for p in pattern])


def build(debug=False, stop_after=None):
    nc = bass.Bass("TRN2", target_bir_lowering=False)
    dr = lambda n, s, dt=F32: nc.dram_tensor(n, list(s), dt, kind="ExternalInput")
    x_d = dr("x", [S_LEN, D])
    pos_d = dr("pos", [128, NT], I32)
    g1_d = dr("norm1_gain", [1, D])
    win_d = dr("w_in", [D, 1984])
    gkf_w = dr("gla_gk_fwd_w", [16, 256])
    gkf_b = dr("gla_gk_fwd_b", [1, 256])
    gkb_w = dr("gla_gk_bwd_w", [16, 256])
    gkb_b = dr("gla_gk_bwd_b", [1, 256])
    go_d = dr("gla_out_gain", [128, 1])
    gqa_d = dr("mla_q_gain", [1, 256])
    wqb_d = dr("mla_w_qb", [256, 768])
    gkva_d = dr("mla_kv_gain", [1, 128])
    wkvb_d = dr("mla_w_kvb", [128, 1024])
    gqn_d = dr("q_norm_gain", [1, 96])
    gkn_d = dr("k_norm_gain", [1, 96])
    wout_d = dr("w_out", [D, D])
    g2_d = dr("norm2_gain", [1, D])
    wrg_d = dr("w_router_group", [D, 4])
    brg_d = dr("b_router_group", [1, 4])
    wre_d = dr("w_router_expert", [D, 32])
    bre_d = dr("b_router_expert", [1, 32])
    weg_d = dr("w_expert_gate", [32, D, 256])
    weu_d = dr("w_expert_up", [32, D, 256])
    wed_d = dr("w_expert_down", [32, 256, D])
    ident_d = dr("c_ident", [128, 128], BF16)
    masks_d = dr("c_masks", [128, 256], BF16)
    invf_d = dr("c_invf", [128, 16])
    sel_d = dr("c_sel", [32, 32 * 128], BF16)
    lrb_d = dr("c_lrbias", [64, 1])
    out_d = nc.dram_tensor("out", [S_LEN, D], F32, kind="ExternalOutput")
    dbg = {}
    if debug:
        dbg["mix"] = nc.dram_tensor("d_mix", [128, 8 * S_LEN], BF16, kind="ExternalOutput")
        dbg["x1"] = nc.dram_tensor("d_x1", [S_LEN, D], F32, kind="ExternalOutput")
        dbg["gate"] = nc.dram_tensor("d_gate", [S_LEN, 32], F32, kind="ExternalOutput")

    if debug:
        dbg["gen"] = nc.dram_tensor("d_gen", [128, 8 * S_LEN], BF16, kind="ExternalOutput")
    S = Sched(nc)
    A = Alloc(nc)
    op = S.op
    final_ops = []

    with contextlib.ExitStack() as es:
        P = [es.enter_context(nc.psum_tensor("pb%d" % i, [128, 512], F32)) for i in range(8)]
        Pb = [p.bitcast(BF16) for p in P]
        PN = ["P%d" % i for i in range(8)]

        def fsz(ap):
            r = 1
            for d_ in list(ap.shape)[1:]:
                r *= int(d_)
            return r

        def dma(q, out, in_, reads=(), writes=(), **kw):
            return op(q, lambda e: e.dma_start(out=out, in_=in_, **kw), reads=reads, writes=writes, dma=True,
                      nbytes=fsz(out) * 4 * 128)

        def act(out, in_, func, reads, writes, **kw):
            return op("act", lambda e: e.activation(out=out, in_=in_, func=func, **kw), reads=reads, writes=writes,
                      n=fsz(out))

        def rsqrt_act(out, in_, n, reads, writes):
            act(out, in_, AF.Ln, reads, writes, scale=1.0 / n, bias=EPS)
            act(out, out, AF.Exp, writes, writes, scale=-0.5)

        def tt(eng, out, in0, in1, o, reads, writes):
            return op(eng, lambda e: e.tensor_tensor(out=out, in0=in0, in1=in1, op=o), reads=reads, writes=writes,
                      n=fsz(out))

        def ts(eng, out, in0, s1, s2, o0, o1, reads, writes):
            if o1 is None:
                return op(eng, lambda e: e.tensor_scalar(out=out, in0=in0, scalar1=s1, scalar2=None, op0=o0),
                          reads=reads, writes=writes, n=fsz(out))
            return op(eng, lambda e: e.tensor_scalar(out=out, in0=in0, scalar1=s1, scalar2=s2, op0=o0, op1=o1),
                      reads=reads, writes=writes, n=fsz(out))

        def stt(out, in0, sc, in1, o0, o1, reads, writes):
            return op("dve", lambda e: e.scalar_tensor_tensor(out=out, in0=in0, scalar=sc, in1=in1, op0=o0, op1=o1),
                      reads=reads, writes=writes, n=fsz(out))

        def mm(out, lhsT, rhs, start, stop, reads, writes):
            return op("pe", lambda e: e.matmul(out, lhsT=lhsT, rhs=rhs, start=start, stop=stop),
                      reads=reads, writes=writes, n=fsz(rhs) * (4 if rhs.dtype == F32 else 1))

        def tr(out, in_, ident, reads, writes):
            return op("pe", lambda e: e.transpose(out=out, in_=in_, identity=ident), reads=reads, writes=writes, n=128)

        def cp(eng, out, in_, reads, writes):
            if eng == "act":
                return act(out, in_, AF.Copy, reads, writes)
            return op(eng, lambda e: e.tensor_copy(out=out, in_=in_), reads=reads, writes=writes, n=fsz(out))

        def memset(eng, ap, val, writes):
            return op(eng, lambda e: e.memset(ap, val), writes=writes, n=fsz(ap))

        def bcast_row(dram, n):
            return bass.AP(dram, 0, [[0, 128], [1, n]])

        ident = A("ident", [128, 128], BF16)
        mixT = A("mixT", [128, 8, S_LEN], BF16)
        dma("sp", ident[:, :], ident_d[:, :], writes=["ident"])
        L0 = A.mark()

        hT = A("hT", [128, 8, S_LEN], BF16)
        E1 = A.mark()
        g1 = A("g1", [128, D], F32)
        xt = [A("xt%d" % i, [128, D], F32) for i in range(2)]
        hb = [A("hb%d" % i, [128, D], BF16) for i in range(2)]
        sqj = A("sqj", [128, D], F32)
        st1 = A("st1", [128, 4], F32)
        dma("sp", g1[:, :], bcast_row(g1_d, D), writes=["g1"])

        def norm_to_T(src_ap_fn, src_tiles, gain, gname, dstT, dname, pfx, t, pbank):
            i = t % 2
            ssq = st1[:, 0:1]
            rs = st1[:, 1:2]
            act(sqj[:, :], src_ap_fn(t), AF.Square, src_tiles, [pfx + "sqj", pfx + "ssq"], accum_out=ssq)
            rsqrt_act(rs, ssq, D, [pfx + "ssq"], [pfx + "rs"])
            stt(hb[i][:, :], src_ap_fn(t), rs, gain[:, :], ALU.mult, ALU.mult,
                src_tiles + [pfx + "rs", gname], [pfx + "hb%d" % i])
            pbv = Pb[pbank][:, :].rearrange("p (c n) -> p c n", c=8)
            for kc in range(8):
                tr(pbv[:, kc, :], hb[i][:, kc * 128:(kc + 1) * 128], ident[:, :],
                   [pfx + "hb%d" % i, "ident"], [PN[pbank]])
            cp("dve" if t % 2 else "act", dstT[:, :, t * 128:(t + 1) * 128], pbv, [PN[pbank]], [dname + "_%d" % (t // 4)])

        for t in range(NT):
            i = t % 2
            dma("sp", xt[i][:, :], x_d[t * 128:(t + 1) * 128, :], writes=["xt%d" % i])
            norm_to_T(lambda t, i=i: xt[i][:, :], ["xt%d" % i], g1, "g1", hT, "hT", "A", t, t % 2)
        hT_tiles = ["hT_%d" % k for k in range(4)]
        S.barrier()
        A.reset(E1)
        def checkpoint(name, dump=None, reads=()):
            if stop_after == name:
                if dump is not None and debug:
                    S.barrier()
                    final_ops.append(dma("sp", dbg["gen"][:, :], dump, reads=list(reads)))
                S.stopped = True

        checkpoint("A")

        wm = A("w_in_mla", [128, 8, 416], BF16)
        wqb = A("wqb", [128, 2, 768], BF16)
        wkvb = A("wkvb", [128, 1024], BF16)
        cs = A("cs", [128, NT, 64], F32)
        qhT = A("qhT", [128, 8, S_LEN], BF16)
        khT = A("khT", [128, 8, S_LEN], BF16)
        vA = A("vA", [128, NT, 8, 128], BF16)
        gqa = A("gqa", [128, 384], F32)
        gqk = A("gqkr", [128, 16, 32], F32)
        gcol = A("gcol", [128, 2], F32)
        B1m = A.mark()
        dma("pool", wm[:, :, :], win_d.ap()[:, 1568:1984].rearrange("(c p) n -> p c n", p=128), writes=["wm"])
        dma("pool", wqb[:, :, :], wqb_d.ap().rearrange("(c p) n -> p c n", p=128), writes=["wqb"])
        dma("pool", wkvb[:, :], wkvb_d[:, :], writes=["wkvb"])
        dma("sp", gqa[:, 0:256], bcast_row(gqa_d, 256), writes=["gqa"])
        dma("sp", gqa[:, 256:384], bcast_row(gkva_d, 128), writes=["gqa"])
        dma("sp", gqk[:, 0:8, :], bass.AP(gqn_d, 64, [[0, 128], [0, 8], [1, 32]]), writes=["gqk"])
        dma("sp", gqk[:, 8:16, :], bass.AP(gkn_d, 64, [[0, 128], [0, 8], [1, 32]]), writes=["gqk"])
        memset("pool", gcol[:, :], 1.0, ["gcol"])
        dma("sp", gcol[0:64, 0:1], bass.AP(gqn_d, 0, [[1, 64], [1, 1]]), reads=["gcol"], writes=["gcol"])
        dma("sp", gcol[0:64, 1:2], bass.AP(gkn_d, 0, [[1, 64], [1, 1]]), reads=["gcol"], writes=["gcol"])
        posi = A("posi", [128, NT], I32)
        posf = A("posf", [128, NT], F32)
        invf = A("invf", [128, 16], F32)
        ang = A("ang", [128, NT, 16], F32)
        kk = A("kk", [128, NT, 16], F32)
        ki = A("ki", [128, NT, 16], I32)
        rr = A("rr", [128, NT, 16], F32)
        yy = A("yy", [128, NT, 16], F32)
        m_ = A("m_", [128, NT, 16], F32)
        dma("sp", posi[:, :], pos_d[:, :], writes=["posi"])
        dma("sp", invf[:, :], invf_d[:, :], writes=["invf"])
        cp("dve", posf[:, :], posi[:, :], ["posi"], ["posf"])
        for t in range(NT):
            ts("dve", ang[:, t, :], invf[:, :], posf[:, t:t + 1], None, ALU.mult, None, ["invf", "posf"], ["ang"])
        ts("dve", kk[:, :, :], ang[:, :, :], 1.0 / (2 * PI), None, ALU.mult, None, ["ang"], ["kk"])
        cp("dve", ki[:, :, :], kk[:, :, :], ["kk"], ["ki"])
        cp("dve", kk[:, :, :], ki[:, :, :], ["ki"], ["kk"])
        stt(rr[:, :, :], kk[:, :, :], -2 * PI, ang[:, :, :], ALU.mult, ALU.add, ["kk", "ang"], ["rr"])
        for which, shift in ((1, 0.0), (0, PI / 2)):
            ts("dve", yy[:, :, :], rr[:, :, :], shift, None, ALU.add, None, ["rr"], ["yy"])
            ts("dve", m_[:, :, :], yy[:, :, :], PI, None, ALU.is_gt, None, ["yy"], ["m_"])
            stt(yy[:, :, :], m_[:, :, :], -2 * PI, yy[:, :, :], ALU.mult, ALU.add, ["m_", "yy"], ["yy"])
            ts("dve", m_[:, :, :], yy[:, :, :], -PI, None, ALU.is_lt, None, ["yy"], ["m_"])
            stt(yy[:, :, :], m_[:, :, :], 2 * PI, yy[:, :, :], ALU.mult, ALU.add, ["m_", "yy"], ["yy"])
            ts("dve", yy[:, :, :], yy[:, :, :], PI, -PI, ALU.min, ALU.max, ["yy"], ["yy"])
            if which == 0:
                act(cs[:, :, 0:16], yy[:, :, :], AF.Sin, ["yy"], ["cs"])
                act(cs[:, :, 16:32], yy[:, :, :], AF.Sin, ["yy"], ["cs"])
            else:
                act(cs[:, :, 48:64], yy[:, :, :], AF.Sin, ["yy"], ["cs"])
                act(cs[:, :, 32:48], cs[:, :, 48:64], AF.Copy, ["cs"], ["cs"], scale=-1.0)
        memset("pool", vA[:, :, :, :], 1.0, ["vA"])
        S.barrier()
        A.reset(B1m)

        sqjb1 = A("sqjb", [128, 416], BF16)
        sqjb = [sqjb1, sqjb1]
        stq = [A("stq%d" % i, [128, 32], F32) for i in range(2)]
        ab = [A("ab%d" % i, [128, 384], BF16) for i in range(2)]
        abT = [A("abT%d" % i, [128, 3, 128], BF16) for i in range(2)]
        kraw = [A("kraw%d" % i, [128, 8, 96], F32) for i in range(2)]
        sqn = A("sqn", [128, 16, 96], BF16)
        rg = [A("rg%d" % i, [128, 16, 32], F32) for i in range(2)]
        rg2 = A("rg2", [128, 16, 48], F32)
        rb = A("rb", [128, 16, 32], F32)
        qkf = [A("qkf%d" % i, [128, 16, 96], BF16) for i in range(2)]
        SQ2 = math.sqrt(2.0)

        for t in range(NT):
            i = t % 2
            sI = "_%d" % i
            tsl = slice(t * 128, (t + 1) * 128)
            hTt = "hT_%d" % (t // 4)
            for kc in range(8):
                mm(P[0][:, 0:416], hT[:, kc, tsl], wm[:, kc, :], kc == 0, kc == 7, [hTt, "wm"], ["P0"])
            act(sqjb[i][:, 0:256], P[0][:, 0:256], AF.Square, ["P0"], ["stqA" + sI], accum_out=stq[i][:, 0:1])
            act(sqjb[i][:, 256:384], P[0][:, 256:384], AF.Square, ["P0"], ["stqA" + sI],
                accum_out=stq[i][:, 1:2], scale=SQ2)
            rsqrt_act(stq[i][:, 2:4], stq[i][:, 0:2], 256, ["stqA" + sI], ["stqB" + sI])
            stt(ab[i][:, 0:256], P[0][:, 0:256], stq[i][:, 2:3], gqa[:, 0:256], ALU.mult, ALU.mult,
                ["P0", "stqB" + sI, "gqa"], ["ab" + sI])
            stt(ab[i][:, 256:384], P[0][:, 256:384], stq[i][:, 3:4], gqa[:, 256:384], ALU.mult, ALU.mult,
                ["P0", "stqB" + sI, "gqa"], ["ab" + sI])
            act(kraw[i][:, :, 64:96], bcast_ap(P[0][:, 384:416], [[0, 8], [1, 32]]), AF.Copy, ["P0"], ["krawR" + sI])
            p1v = Pb[1][:, 0:384].rearrange("p (c n) -> p c n", c=3)
            for c in range(3):
                tr(p1v[:, c, :], ab[i][:, c * 128:(c + 1) * 128], ident[:, :], ["ab" + sI, "ident"], ["P1"])
            cp("act", abT[i][:, :, :], p1v, ["P1"], ["abT" + sI])
            for nb in range(2):
                for kc in range(2):
                    mm(P[2 + nb][:, 0:384], abT[i][:, kc, :], wqb[:, kc, nb * 384:(nb + 1) * 384], kc == 0, kc == 1,
                       ["abT" + sI, "wqb"], [PN[2 + nb]])
                mm(P[4 + nb][:, :], abT[i][:, 2, :], wkvb[:, nb * 512:(nb + 1) * 512], True, True,
                   ["abT" + sI, "wkvb"], [PN[4 + nb]])
            for nb in range(2):
                srck = P[4 + nb][:, :].rearrange("p (h d) -> p h d", h=4)[:, :, 0:64]
                cp("dve", kraw[i][:, nb * 4:nb * 4 + 4, 0:64], srck, [PN[4 + nb]], ["krawN" + sI])
                srcv = P[4 + nb][:, :].rearrange("p (a b d) -> p a b d", a=2, b=2)
                dstv = vA[:, t, nb * 4:nb * 4 + 4, :].rearrange("p (a b) d -> p a b d", b=2)
                cp("act", dstv[:, :, 0, 0:64], srcv[:, :, 0, 64:128], [PN[4 + nb]], ["vA"])
                cp("act", dstv[:, :, 1, 64:128], srcv[:, :, 1, 64:128], [PN[4 + nb]], ["vA"])
            for nb in range(2):
                act(sqn[:, nb * 4:nb * 4 + 4, :], P[2 + nb][:, 0:384].rearrange("p (h d) -> p h d", h=4), AF.Square,
                    [PN[2 + nb]], ["sqn"])
            act(sqn[:, 8:16, :], kraw[i][:, :, :], AF.Square, ["krawN" + sI, "krawR" + sI], ["sqn"])
            op("dve", lambda e, i=i: e.tensor_reduce(out=stq[i][:, 8:24], in_=sqn[:, :, :], axis=AX.X, op=ALU.add),
               reads=["sqn"], writes=["stqC" + sI])
            rsqrt_act(stq[i][:, 8:24], stq[i][:, 8:24], 96, ["stqC" + sI], ["stqC" + sI])
            for nb in range(2):
                pv = P[2 + nb][:, 0:384].rearrange("p (h d) -> p h d", h=4)
                rq = stq[i][:, 8 + nb * 4:9 + nb * 4]
                tt("dve", qkf[i][:, nb * 4:nb * 4 + 4, 0:64], pv[:, :, 0:64], bcast_ap(rq, [[1, 4], [0, 64]]), ALU.mult,
                   [PN[2 + nb], "stqC" + sI], ["qkf" + sI])
                tt("dve", rg[i][:, nb * 4:nb * 4 + 4, :], pv[:, :, 64:96], bcast_ap(rq, [[1, 4], [0, 32]]), ALU.mult,
                   [PN[2 + nb], "stqC" + sI], ["rg" + sI])
            rk = stq[i][:, 16:17]
            tt("dve", qkf[i][:, 8:16, 0:64], kraw[i][:, :, 0:64], bcast_ap(rk, [[1, 8], [0, 64]]), ALU.mult,
               ["krawN" + sI, "stqC" + sI], ["qkf" + sI])
            tt("dve", rg[i][:, 8:16, :], kraw[i][:, :, 64:96], bcast_ap(rk, [[1, 8], [0, 32]]), ALU.mult,
               ["krawR" + sI, "stqC" + sI], ["rg" + sI])
            tt("pool", rg2[:, :, 0:32], rg[i][:, :, :], gqk[:, :, :], ALU.mult, ["rg" + sI, "gqk"], ["rg2"])
            tt("pool", rg2[:, :, 32:48], rg[i][:, :, 0:16], gqk[:, :, 0:16], ALU.mult, ["rg" + sI, "gqk"], ["rg2"])
            c1 = bcast_ap(cs[:, t, 0:32], [[0, 16], [1, 32]])
            c2 = bcast_ap(cs[:, t, 32:64], [[0, 16], [1, 32]])
            tt("pool", rg[i][:, :, :], rg2[:, :, 0:32], c1, ALU.mult, ["rg2", "cs"], ["rg" + sI])
            tt("pool", rb[:, :, :], rg2[:, :, 16:48], c2, ALU.mult, ["rg2", "cs"], ["rb"])
            tt("pool", qkf[i][:, :, 64:96], rg[i][:, :, :], rb[:, :, :], ALU.add, ["rg" + sI, "rb"], ["qkf" + sI])
            p6v = Pb[6][:, :].rearrange("p (h n) -> p h n", h=8)
            p7v = Pb[7][:, :].rearrange("p (h n) -> p h n", h=8)
            for h in range(8):
                tr(p6v[0:96, h, :], qkf[i][:, h, :], ident[:, :], ["qkf" + sI, "ident"], ["P6"])
            for h in range(8):
                tr(p7v[0:96, h, :], qkf[i][:, 8 + h, :], ident[:, :], ["qkf" + sI, "ident"], ["P7"])
            ts("dve", qhT[0:96, :, tsl], p6v[0:96, :, :], gcol[0:96, 0:1], None, ALU.mult, None, ["P6", "gcol"],
               ["qhT_%d" % (t // 4)])
            act(khT[0:96, :, tsl], p7v[0:96, :, :], AF.Identity, ["P7", "gcol"], ["khT"], scale=gcol[0:96, 1:2])
        S.barrier()
        A.reset(B1m)
        checkpoint("B1")
        pbuf = [A("pbuf%d" % i, [128, 512], BF16) for i in range(4)]
        lnb = A("lnb", [128, 512], F32)
        rcb = A("rcb", [128, 512], F32)
        scale = 96 ** -0.5
        it = 0
        for h in range(8):
            even = (h % 2 == 0)
            vrows = slice(0, 64) if even else slice(64, 128)
            srows = slice(64, 128) if even else slice(0, 64)
            for qg in range(4):
                qsl = slice(qg * 512, (qg + 1) * 512)
                ob = 4 + (it % 2)
                seq = []
                for kt in range(16):
                    seq.append(("s", kt))
                    if kt >= 2:
                        seq.append(("pv", kt - 2))
                seq += [("pv", 14), ("pv", 15)]
                for kind, kt in seq:
                    sb_ = kt % 3
                    pi = kt % 4
                    if kind == "s":
                        mm(P[sb_][:, :], khT[0:96, h, kt * 128:(kt + 1) * 128], qhT[0:96, h, qsl], True, True,
                           ["khT", "qhT_%d" % qg], [PN[sb_]])
                        act(pbuf[pi][:, :], P[sb_][:, :], AF.Exp, [PN[sb_]], ["pbuf%d" % pi], scale=scale)
                    else:
                        lhsT = vA[:, kt, h, :]
                        mm(P[ob][:, :], lhsT, pbuf[pi][:, :], kt == 0, kt == 15, ["vA", "pbuf%d" % pi], [PN[ob]])
                act(lnb[vrows, :], P[ob][srows, :], AF.Ln, [PN[ob]], ["lnb"])
                act(rcb[vrows, :], lnb[vrows, :], AF.Exp, ["lnb"], ["rcb"], scale=-1.0)
                tt("dve", mixT[vrows, 4 + h // 2, qsl], P[ob][vrows, :], rcb[vrows, :], ALU.mult, [PN[ob], "rcb"], ["mixT_m"])
                it += 1
        S.barrier()
        A.reset(E1)

        checkpoint("C")
        wg = A("w_in_gla", [128, 8, 1568], BF16)
        R2 = A.mark()
        qkT = A("qkT", [128, 4, S_LEN], F32)
        vtok = A("vtok", [128, NT, 512], BF16)
        sgT = A("sgT", [128, 4, S_LEN], BF16)
        lrT = A("lrT", [64, S_LEN], F32)
        wlr = A("wlr", [128, 8, 64], BF16)
        lrb = A("lrb", [64, 1], F32)
        waug = A("waug", [64, 512], F32)
        masks = A("masks", [128, 256], BF16)
        gout = A("gout", [128, 1], F32)
        onesf = A("onesf", [128, 128], F32)
        R4 = A.mark()
        dma("pool", wg[:, :, :], win_d.ap()[:, 0:1568].rearrange("(c p) n -> p c n", p=128), writes=["wg"])
        memset("pool", wlr[:, :, :], 0.0, ["wlr"])
        dma("pool", wlr[:, :, 0:16], win_d.ap()[:, 1536:1552].rearrange("(c p) n -> p c n", p=128), reads=["wlr"], writes=["wlr"])
        dma("pool", wlr[:, :, 32:48], win_d.ap()[:, 1552:1568].rearrange("(c p) n -> p c n", p=128), reads=["wlr"], writes=["wlr"])
        dma("sp", lrb[:, :], lrb_d[:, :], writes=["lrb"])
        memset("pool", waug[:, :], 0.0, ["waug"])
        dma("sp", waug[0:16, 0:256], gkf_w[:, :], reads=["waug"], writes=["waug"])
        dma("sp", waug[16:17, 0:256], gkf_b[:, :], reads=["waug"], writes=["waug"])
        dma("sp", waug[16:17, 256:512], gkb_b[:, :], reads=["waug"], writes=["waug"])
        dma("sp", waug[32:48, 256:512], gkb_w[:, :], reads=["waug"], writes=["waug"])
        dma("sp", masks[:, :], masks_d[:, :], writes=["masks"])
        dma("sp", gout[:, :], go_d[:, :], writes=["gout"])
        memset("pool", onesf[:, :], 1.0, ["onesf"])
        blk = 0
        for kind, idx in [("q", 0), ("q", 1), ("k", 0), ("k", 1), ("g", 0), ("g", 1), ("g", 2), ("g", 3), ("lr", 0)]:
            for tg in range(4):
                pbk = blk % 4
                blk += 1
                tgs = slice(tg * 512, (tg + 1) * 512)
                for kc in range(8):
                    if kind == "q":
                        lhsT = wg[:, kc, idx * 128:(idx + 1) * 128]
                    elif kind == "k":
                        lhsT = wg[:, kc, 256 + idx * 128:256 + (idx + 1) * 128]
                    elif kind == "g":
                        lhsT = wg[:, kc, 1024 + idx * 128:1024 + (idx + 1) * 128]
                    else:
                        lhsT = wlr[:, kc, :]
                    mrows = 64 if kind == "lr" else 128
                    mm(P[pbk][0:mrows, :], lhsT, hT[:, kc, tgs], kc == 0, kc == 7, ["hT_%d" % tg, "wg", "wlr"], [PN[pbk]])
                if kind == "q":
                    act(qkT[:, idx, tgs], P[pbk][:, :], AF.Copy, [PN[pbk]], ["qT"], scale=0.125)
                elif kind == "k":
                    cp("dve", qkT[:, 2 + idx, tgs], P[pbk][:, :], [PN[pbk]], ["kT"])
                elif kind == "g":
                    act(sgT[:, idx, tgs], P[pbk][:, :], AF.Silu, [PN[pbk]], ["sgT"])
                else:
                    act(lrT[:, tgs], P[pbk][0:64, :], AF.Identity, [PN[pbk], "lrb"], ["lrT"], bias=lrb[:, :])
        for t in range(NT):
            pbk = 4 + t % 2
            tsl = slice(t * 128, (t + 1) * 128)
            for kc in range(8):
                mm(P[pbk][:, :], hT[:, kc, tsl], wg[:, kc, 512:1024], kc == 0, kc == 7, ["hT_%d" % (t // 4), "wg"], [PN[pbk]])
            cp("dve" if t % 2 else "act", vtok[:, t, :], P[pbk][:, :], [PN[pbk]], ["vtok"])
        S.barrier()

        checkpoint("B2")
        HTB = L0
        tmp = [A("gt%d" % i, [128, 1024], F32, at=HTB + i * 4096) for i in range(4)]
        prod = {}
        names = [(d_, hp, k_) for d_ in (0, 1) for hp in (0, 1) for k_ in ("qr", "kr", "qb")]
        slots = [HTB + 16384 + i * 4096 for i in range(4)] + [E1 + 16384 + i * 4096 for i in range(2)]
        for i, nm in enumerate(names):
            if i < 6:
                prod[nm] = A("pr", [128, S_LEN], BF16, at=slots[i])
            else:
                prod[nm] = A("pr", [128, S_LEN], BF16)
        kdT = A("kdT", [128, 1024], BF16)
        dec = A("dec", [128, 4, 32], F32)
        smask = A("smask", [128, 1024], F32)
        kd = A("kd", [128, NT, 512], BF16, at=E1)
        memset("pool", smask[:, :], 1.0, ["smask"])
        memset("pool", smask[:, :].rearrange("p (c j) -> p c j", j=64)[:, :, 0:1], 0.0, ["smask"])
        G, Fc, Dt, Eb = tmp
        Dt2 = A("Dt2", [128, 1024], F32)
        Eb2 = A("Eb2", [128, 1024], F32)
        DtL = [(Dt, "Dt"), (Dt2, "Dt2")]
        EbL = [(Eb, "Eb"), (Eb2, "Eb2")]
        cnt = {"d": 0, "e": 0}

        def nextD():
            cnt["d"] += 1
            return DtL[cnt["d"] % 2]

        def nextE():
            cnt["e"] += 1
            return EbL[cnt["e"] % 2]

        def exp_prod(src, sname, scl, dst, base, bname, dname="prod"):
            E_, en = nextE()
            act(E_[:, :], src, AF.Exp, [sname], [en], scale=scl)
            tt("pool", dst, base, E_[:, :], ALU.mult, [bname, en], [dname])
        for d_ in (0, 1):
            for hp in (0, 1):
                dh = d_ * 2 + hp
                qT = qkT[:, hp, :]
                kT = qkT[:, 2 + hp, :]
                for half in range(2):
                    hs = slice(half * 1024, (half + 1) * 1024)
                    for j in range(2):
                        pbk = j
                        cols = slice(half * 1024 + j * 512, half * 1024 + (j + 1) * 512)
                        mm(P[pbk][:, :], waug[0:64, dh * 128:(dh + 1) * 128], lrT[0:64, cols], True, True,
                           ["waug", "lrT"], [PN[pbk]])
                        act(Eb[:, j * 512:(j + 1) * 512], P[pbk][:, :], AF.Exp, [PN[pbk]], ["Eb"], scale=-1.0)
                    act(G[:, :], Eb[:, :], AF.Ln, ["Eb"], ["G"], bias=1.0)
                    op("dve", lambda e: e.tensor_tensor_scan(out=Fc[:, :], data0=smask[:, :], data1=G[:, :], initial=0.0,
                                                             op0=ALU.mult, op1=ALU.add), reads=["smask", "G"], writes=["Fc"])
                    Fv = Fc[:, :].rearrange("p (c j) -> p c j", j=64)
                    Dv = Dt[:, :].rearrange("p (c j) -> p c j", j=64)
                    T63 = bcast_ap(Fc[:, 63:64], [[64, 16], [0, 64]])
                    act(dec[:, dh, half * 16:(half + 1) * 16], Fv[:, :, 63], AF.Exp, ["Fc"], ["dec"], scale=-1.0 / 16)
                    if d_ == 0:
                        ref = bcast_ap(Fc[:, 31:32], [[64, 16], [0, 64]])
                        D_, dn = nextD()
                        tt("dve", D_[:, :].rearrange("p (c j) -> p c j", j=64), Fv, ref, ALU.subtract, ["Fc"], [dn])
                        exp_prod(D_[:, :], dn, -1.0 / 16, prod[(0, hp, "qr")][:, hs], qT[:, hs], "qT")
                        exp_prod(D_[:, :], dn, 1.0 / 16, prod[(0, hp, "kr")][:, hs], kT[:, hs], "kT")
                        exp_prod(Fc[:, :], "Fc", -1.0 / 16, prod[(0, hp, "qb")][:, hs], qT[:, hs], "qT")
                        D_, dn = nextD()
                        tt("dve", D_[:, :].rearrange("p (c j) -> p c j", j=64), Fv, T63, ALU.subtract, ["Fc"], [dn])
                        exp_prod(D_[:, :], dn, 1.0 / 16, kdT[:, :], kT[:, hs], "kT", "kdT")
                    else:
                        tt("dve", G[:, :], Fc[:, :], G[:, :], ALU.subtract, ["Fc", "G"], ["G"])
                        Gv = G[:, :].rearrange("p (c j) -> p c j", j=64)
                        ref = bcast_ap(G[:, 32:33], [[64, 16], [0, 64]])
                        D_, dn = nextD()
                        tt("dve", D_[:, :].rearrange("p (c j) -> p c j", j=64), Gv, ref, ALU.subtract, ["G"], [dn])
                        exp_prod(D_[:, :], dn, 1.0 / 16, prod[(1, hp, "qr")][:, hs], qT[:, hs], "qT")
                        exp_prod(D_[:, :], dn, -1.0 / 16, prod[(1, hp, "kr")][:, hs], kT[:, hs], "kT")
                        D_, dn = nextD()
                        tt("dve", D_[:, :].rearrange("p (c j) -> p c j", j=64), Gv, T63, ALU.subtract, ["G", "Fc"], [dn])
                        exp_prod(D_[:, :], dn, 1.0 / 16, prod[(1, hp, "qb")][:, hs], qT[:, hs], "qT")
                        exp_prod(G[:, :], "G", -1.0 / 16, kdT[:, :], kT[:, hs], "kT", "kdT")
                    for g4 in range(2):
                        pbk = 2 + g4
                        pv = Pb[pbk][:, 0:512].rearrange("p (t n) -> p t n", t=4)
                        for tq in range(4):
                            c0 = (g4 * 4 + tq) * 128
                            tr(pv[:, tq, :], kdT[:, c0:c0 + 128], ident[:, :], ["kdT", "ident"], [PN[pbk]])
                        t0 = half * 8 + g4 * 4
                        cp("dve", kd[:, t0:t0 + 4, dh * 128:(dh + 1) * 128], pv, [PN[pbk]], ["kd"])
        S.barrier()

        checkpoint("D1")
        qk_off = R2
        Sst = [A("Sst%d" % i, [128, 32, 128], BF16, at=qk_off + i * 8192) for i in range(4)]
        Sf = [A("Sf%d" % i, [128, 256], F32, at=HTB + i * 1024) for i in range(4)]
        for dh in range(4):
            memset("pool", Sf[dh][:, :], 0.0, ["Sf%d" % dh])
        for step in range(32):
            for dh in range(4):
                d_, hp = divmod(dh, 2)
                n = step if d_ == 0 else 31 - step
                t, c = divmod(n, 2)
                rows = slice(c * 64, (c + 1) * 64)
                cp("pool", Sst[dh][0:64, n, :], Sf[dh][0:64, 0:128], ["Sf%d" % dh], ["SstA%d" % dh])
                cp("act", Sst[dh][64:128, n, :], Sf[dh][64:128, 128:256], ["Sf%d" % dh], ["SstB%d" % dh])
                if step == 31:
                    continue
                pbk = 4 * c + dh
                mm(P[pbk][:, 0:256], kd[rows, t, dh * 128:(dh + 1) * 128], vtok[rows, t, hp * 256:(hp + 1) * 256],
                   True, True, ["kd", "vtok"], [PN[pbk]])
                stt(Sf[dh][:, :], Sf[dh][:, :], dec[:, dh, n:n + 1], P[pbk][:, 0:256], ALU.mult, ALU.add,
                    ["Sf%d" % dh, "dec", PN[pbk]], ["Sf%d" % dh])
        S.barrier()

        checkpoint("D2")
        smb = [A("smb%d" % i, [128, 2, 2, 128], BF16, at=HTB + 4096 + i * 1024) for i in range(2)]
        sqoL = [A("sqo%d" % i, [128, 256], F32, at=HTB + 6144 + i * 1024) for i in range(2)]
        rsoL = [A("rso%d" % i, [128, 256], F32, at=HTB + 8192 + i * 1024) for i in range(2)]
        t1oL = [A("t1o%d" % i, [128, 256], F32, at=HTB + 10240 + i * 1024) for i in range(2)]
        for t in range(NT):
            tsl = slice(t * 128, (t + 1) * 128)
            for par in range(2):
                rows = slice(par * 64, (par + 1) * 64)
                sbk = par
                obk = 2 + par
                scv = P[sbk][:, :].rearrange("p (a b n) -> p a b n", a=2, b=2)
                for hp in range(2):
                    for d_ in range(2):
                        mm(scv[:, hp, d_, :], prod[(d_, hp, "kr")][rows, tsl], prod[(d_, hp, "qr")][rows, tsl], True, True,
                           ["prod"], [PN[sbk]])
                mk = bcast_ap(masks[:, 0:256], [[0, 2], [1, 256]])
                tt("dve", smb[par][:, :, :, :].rearrange("p a b n -> p a (b n)"),
                   P[sbk][:, :].rearrange("p (a m) -> p a m", a=2), mk, ALU.mult, [PN[sbk], "masks"], ["smb%d" % par])
                ov = P[obk][:, 0:256].rearrange("p (a n) -> p a n", a=2)
                for hp in range(2):
                    h = hp * 2 + par
                    mm(ov[:, hp, :], vtok[:, t, h * 128:(h + 1) * 128], smb[par][:, hp, 0, :], True, False,
                       ["vtok", "smb%d" % par], [PN[obk]])
                    mm(ov[:, hp, :], vtok[:, t, h * 128:(h + 1) * 128], smb[par][:, hp, 1, :], False, False,
                       ["vtok", "smb%d" % par], [PN[obk]])
                    for d_ in range(2):
                        dh = d_ * 2 + hp
                        for c in range(2):
                            n = t * 2 + c
                            csl = slice(t * 128 + c * 64, t * 128 + (c + 1) * 64)
                            last = (d_ == 1 and c == 1)
                            mm(ov[:, hp, c * 64:(c + 1) * 64], Sst[dh][rows, n, :], prod[(d_, hp, "qb")][rows, csl],
                               False, last, ["SstA%d" % dh, "SstB%d" % dh, "prod"], [PN[obk]])
                sqo, rso, t1o = sqoL[par], rsoL[par], t1oL[par]
                sP = "%d" % par
                act(sqo[:, :], P[obk][:, 0:256], AF.Square, [PN[obk]], ["sqo" + sP])
                ebk = 4 + par
                mm(P[ebk][:, 0:256], onesf[:, :], sqo[:, :], True, True, ["onesf", "sqo" + sP], [PN[ebk]])
                rsqrt_act(rso[:, :], P[ebk][:, 0:256], 128, [PN[ebk]], ["rso" + sP])
                stt(t1o[:, :], P[obk][:, 0:256], gout[:, 0:1], rso[:, :], ALU.mult, ALU.mult, [PN[obk], "gout", "rso" + sP], ["t1o" + sP])
                for hp in range(2):
                    h = hp * 2 + par
                    tt("pool", mixT[:, h, tsl], t1o[:, hp * 128:(hp + 1) * 128], sgT[:, h, tsl], ALU.mult,
                       ["t1o" + sP, "sgT"], ["mixT_g"])
        S.barrier()
        A.reset(L0)
        if debug:
            final_ops.append(dma("sp", dbg["mix"][:, :], mixT[:, :, :].rearrange("p c n -> p (c n)"), reads=["mixT_g", "mixT_m"]))

        checkpoint("D3")
        X = A("X", [128, NT, D], F32)
        h2T = A("h2T", [128, 8, S_LEN], BF16)
        wo = A("wo", [128, 8, D], BF16)
        WO_OFF = A.off - 16384
        g2 = A("g2", [128, D], F32)
        wr = A("wr", [128, 8, 36], BF16)
        rbias = A("rbias", [128, 36], F32)
        gT = A("gT", [32, 2, S_LEN], BF16)
        sel = A("sel", [32, 32, 128], BF16)
        lgA = A("lgA", [128, NT, 36], F32)
        hb = [A("hb2_%d" % i, [128, D], BF16) for i in range(2)]
        sqj = A("sqj2", [128, D], BF16)
        st1 = A("st1_2", [128, 4], F32)
        dma("pool", wo[:, :, :], wout_d.ap().rearrange("(c p) n -> p c n", p=128), writes=["wo"])
        dma("sp", g2[:, :], bcast_row(g2_d, D), writes=["g2"])
        dma("pool", wr[:, :, 0:4], wrg_d.ap().rearrange("(c p) n -> p c n", p=128), writes=["wr"])
        dma("pool", wr[:, :, 4:36], wre_d.ap().rearrange("(c p) n -> p c n", p=128), writes=["wr"])
        dma("sp", rbias[:, 0:4], bcast_row(brg_d, 4), writes=["rbias"])
        dma("sp", rbias[:, 4:36], bcast_row(bre_d, 32), writes=["rbias"])
        dma("sp", sel[:, :, :], sel_d.ap().rearrange("p (e n) -> p e n", e=32), writes=["sel"])
        for t in range(NT):
            tsl = slice(t * 128, (t + 1) * 128)
            dma("sp", X[:, t, :], x_d[tsl, :], writes=["X%d" % t])
            for ch in range(2):
                pbk = (t % 2) * 2 + ch
                for kc in range(8):
                    mm(P[pbk][:, :], mixT[:, kc, tsl], wo[:, kc, ch * 512:(ch + 1) * 512], kc == 0, kc == 7,
                       ["mixT_g", "mixT_m", "wo"], [PN[pbk]])
                tt("dve", X[:, t, ch * 512:(ch + 1) * 512], X[:, t, ch * 512:(ch + 1) * 512], P[pbk][:, :], ALU.add,
                   ["X%d" % t, PN[pbk]], ["X%d" % t])
        if debug:
            for t in range(NT):
                final_ops.append(dma("sp", dbg["x1"][t * 128:(t + 1) * 128, :], X[:, t, :], reads=["X%d" % t]))
        checkpoint("E")
        for t in range(NT):
            tsl = slice(t * 128, (t + 1) * 128)
            norm_to_T(lambda t: X[:, t, :], ["X%d" % t], g2, "g2", h2T, "h2T", "F", t, 4 + t % 2)
            rbk = 6 + t % 2
            for kc in range(8):
                mm(P[rbk][:, 0:36], h2T[:, kc, tsl], wr[:, kc, :], kc == 0, kc == 7, ["h2T_%d" % (t // 4), "wr"], [PN[rbk]])
            tt("dve", lgA[:, t, :], P[rbk][:, 0:36], rbias[:, :], ALU.add, [PN[rbk], "rbias"], ["lgA"])

        def bl(ap2, k):
            return bcast_ap(ap2, [list(ap2.ap[1]), [0, k]])

        def red(out, in_, o, reads, writes):
            return op("dve", lambda e: e.tensor_reduce(out=out, in_=in_, axis=AX.X, op=o), reads=reads, writes=writes,
                      n=fsz(in_))

        r16 = lambda nm: A(nm, [128, NT], F32)
        r4 = lambda nm: A(nm, [128, NT, 4], F32)
        r8 = lambda nm: A(nm, [128, NT, 8], F32)
        mg, s4, ptop, m1, m2, dm, e2, den, w1, w2 = [r16("r16_%d" % i) for i in range(10)]
        d4, e4, oh, ohp = [r4("r4_%d" % i) for i in range(4)]
        ls, tmp8, eq1, ls2, eq2, wg8 = [r8("r8_%d" % i) for i in range(6)]
        gate = A("gate", [128, NT, 32], F32)
        gtmp = A("gtmp", [128, NT, 32], F32)
        ghl = A("ghl", [128, 2, NT, 32], BF16)
        red(mg[:, :], lgA[:, :, 0:4], ALU.max, ["lgA"], ["mg"])
        tt("dve", d4[:, :, :], lgA[:, :, 0:4], bl(mg[:, :], 4), ALU.subtract, ["lgA", "mg"], ["d4"])
        act(e4[:, :, :], d4[:, :, :], AF.Exp, ["d4"], ["e4"])
        red(s4[:, :], e4[:, :, :], ALU.add, ["e4"], ["s4"])
        op("dve", lambda e: e.reciprocal(out=ptop[:, :], in_=s4[:, :]), reads=["s4"], writes=["ptop"], n=128)
        ts("dve", oh[:, :, :], d4[:, :, :], 0.0, None, ALU.is_equal, None, ["d4"], ["oh"])
        tt("dve", ohp[:, :, :], oh[:, :, :], bl(ptop[:, :], 4), ALU.mult, ["oh", "ptop"], ["ohp"])
        tt("dve", ls[:, :, :], lgA[:, :, 4:12], bl(oh[:, :, 0], 8), ALU.mult, ["lgA", "oh"], ["ls"])
        for g_ in range(1, 4):
            tt("dve", tmp8[:, :, :], lgA[:, :, 4 + 8 * g_:12 + 8 * g_], bl(oh[:, :, g_], 8), ALU.mult, ["lgA", "oh"], ["tmp8"])
            tt("dve", ls[:, :, :], ls[:, :, :], tmp8[:, :, :], ALU.add, ["ls", "tmp8"], ["ls"])
        red(m1[:, :], ls[:, :, :], ALU.max, ["ls"], ["m1"])
        tt("dve", eq1[:, :, :], ls[:, :, :], bl(m1[:, :], 8), ALU.is_equal, ["ls", "m1"], ["eq1"])
        stt(ls2[:, :, :], eq1[:, :, :], -1e30, ls[:, :, :], ALU.mult, ALU.add, ["eq1", "ls"], ["ls2"])
        red(m2[:, :], ls2[:, :, :], ALU.max, ["ls2"], ["m2"])
        tt("dve", eq2[:, :, :], ls2[:, :, :], bl(m2[:, :], 8), ALU.is_equal, ["ls2", "m2"], ["eq2"])
        tt("dve", dm[:, :], m2[:, :], m1[:, :], ALU.subtract, ["m1", "m2"], ["dm"])
        act(e2[:, :], dm[:, :], AF.Exp, ["dm"], ["e2"])
        ts("dve", den[:, :], e2[:, :], 1.0, None, ALU.add, None, ["e2"], ["den"])
        op("dve", lambda e: e.reciprocal(out=w1[:, :], in_=den[:, :]), reads=["den"], writes=["w1"], n=128)
        tt("dve", w2[:, :], e2[:, :], w1[:, :], ALU.mult, ["e2", "w1"], ["w2"])
        tt("dve", wg8[:, :, :], eq1[:, :, :], bl(w1[:, :], 8), ALU.mult, ["eq1", "w1"], ["wg8"])
        tt("dve", tmp8[:, :, :], eq2[:, :, :], bl(w2[:, :], 8), ALU.mult, ["eq2", "w2"], ["tmp8"])
        tt("dve", wg8[:, :, :], wg8[:, :, :], tmp8[:, :, :], ALU.add, ["wg8", "tmp8"], ["wg8"])
        for g_ in range(4):
            tt("dve", gate[:, :, g_ * 8:(g_ + 1) * 8], wg8[:, :, :], bl(ohp[:, :, g_], 8), ALU.mult, ["wg8", "ohp"], ["gate"])
        if debug:
            for t in range(NT):
                final_ops.append(dma("sp", dbg["gate"][t * 128:(t + 1) * 128, :], gate[:, t, :], reads=["gate"]))
        cp("dve", ghl[:, 0, :, :], gate[:, :, :], ["gate"], ["ghl0"])
        tt("dve", gtmp[:, :, :], gate[:, :, :], ghl[:, 0, :, :], ALU.subtract, ["gate", "ghl0"], ["gtmp"])
        cp("dve", ghl[:, 1, :, :], gtmp[:, :, :], ["gtmp"], ["ghl1"])
        for a_ in range(2):
            for half in range(2):
                bk = 4 + a_ * 2 + half
                pv = Pb[bk][:, :].rearrange("p (t n) -> p t n", t=8)
                for tq in range(8):
                    tr(pv[0:32, tq, :], ghl[:, a_, half * 8 + tq, :], ident[:, :], ["ghl%d" % a_, "ident"], [PN[bk]])
                cp("act" if half else "dve", gT[0:32, a_, half * 1024:(half + 1) * 1024], Pb[bk][0:32, :], [PN[bk]], ["gT"])
        checkpoint("F")

        EG = 2
        NEG = 32 // EG
        MX = SB_BASE + 256
        S.alias(["hid0", "hid1", "sil0", "sil1", "t1m0", "t1m1", "wdn0", "wdn1"], ["mixT_g", "mixT_m"])
        hid = [A("hid%d" % b, [128, EG, 2, 512], BF16, at=MX + b * 4096) for b in range(2)]
        sil = [A("sil%d" % b, [128, 512], F32, at=MX + 8192 + b * 2048) for b in range(2)]
        t1m = [A("t1m%d" % b, [128, 512], F32, at=MX + 12288 + b * 2048) for b in range(2)]
        wdn = [[A("wd%d_%d" % (b, j), [128, 2, D], BF16, at=MX + 16384 + (b * EG + j) * 4096) for j in range(EG)] for b in range(2)]
        wgt = [None, None]
        wup = [None, None]
        wgt[0] = [A("wg0_%d" % j, [128, 8, 256], BF16) for j in range(EG)]
        wup[0] = [A("wu0_%d" % j, [128, 8, 256], BF16) for j in range(EG)]
        wgt[1] = [A("wg1_%d" % j, [128, 8, 256], BF16, at=WO_OFF + j * 4096) for j in range(EG)]
        wup[1] = [A("wu1_%d" % j, [128, 8, 256], BF16, at=WO_OFF + 8192 + j * 4096) for j in range(EG)]
        itc = 0
        for eg in range(NEG):
            b = eg % 2
            extra = ["wo"] if b == 1 else []
            for j in range(EG):
                e_ = eg * EG + j
                dma("pool", wgt[b][j][:, :, :], weg_d.ap()[e_].rearrange("(c p) f -> p c f", p=128), writes=["wgt%d" % b] + extra)
                dma("pool", wup[b][j][:, :, :], weu_d.ap()[e_].rearrange("(c p) f -> p c f", p=128), writes=["wup%d" % b] + extra)
                dma("pool", wdn[b][j][:, :, :], wed_d.ap()[e_].rearrange("(c p) d -> p c d", p=128), writes=["wdn%d" % b])
            for tg in range(4):
                hbi = itc % 2
                itc += 1
                tgs = slice(tg * 512, (tg + 1) * 512)
                for j in range(EG):
                    e_ = eg * EG + j
                    gbk = 6 + j
                    for fh in range(2):
                        k2 = fh
                        gb_, ub_ = 0 + k2, 2 + k2
                        for kc in range(8):
                            mm(P[gb_][:, :], wgt[b][j][:, kc, fh * 128:(fh + 1) * 128], h2T[:, kc, tgs], kc == 0, kc == 7,
                               ["wgt%d" % b, "h2T_%d" % tg], [PN[gb_]])
                        for kc in range(8):
                            mm(P[ub_][:, :], wup[b][j][:, kc, fh * 128:(fh + 1) * 128], h2T[:, kc, tgs], kc == 0, kc == 7,
                               ["wup%d" % b, "h2T_%d" % tg], [PN[ub_]])
                        if fh == 0:
                            mm(P[gbk][:, :], sel[0:32, e_, :], gT[0:32, 0, tgs], True, False, ["sel", "gT"], [PN[gbk]])
                            mm(P[gbk][:, :], sel[0:32, e_, :], gT[0:32, 1, tgs], False, True, ["sel", "gT"], [PN[gbk]])
                        act(sil[k2][:, :], P[gb_][:, :], AF.Silu, [PN[gb_]], ["sil%d" % k2])
                        tt("dve", t1m[k2][:, :], sil[k2][:, :], P[ub_][:, :], ALU.mult, ["sil%d" % k2, PN[ub_]], ["t1m%d" % k2])
                        tt("dve", hid[hbi][:, j, fh, :], t1m[k2][:, :], P[gbk][:, :], ALU.mult, ["t1m%d" % k2, PN[gbk]],
                           ["hid%d" % hbi])
                for tt_ in range(4):
                    t = tg * 4 + tt_
                    for ch in range(2):
                        abk = 4 + (tt_ * 2 + ch) % 2
                        n_acc = EG * 2
                        a_i = 0
                        for j in range(EG):
                            for fh in range(2):
                                mm(P[abk][:, :], hid[hbi][:, j, fh, tt_ * 128:(tt_ + 1) * 128],
                                   wdn[b][j][:, fh, ch * 512:(ch + 1) * 512], a_i == 0, a_i == n_acc - 1,
                                   ["hid%d" % hbi, "wdn%d" % b], [PN[abk]])
                                a_i += 1
                        tt("dve", X[:, t, ch * 512:(ch + 1) * 512], X[:, t, ch * 512:(ch + 1) * 512], P[abk][:, :], ALU.add,
                           ["X%d" % t, PN[abk]], ["X%d" % t])
        for t in range(NT):
            final_ops.append(dma("sp", out_d[t * 128:(t + 1) * 128, :], X[:, t, :], reads=["X%d" % t]))

        S.emit(es, final_wait_ops=final_ops)
    return nc


def make_consts():
    ident = np.eye(128, dtype=np.float32).astype(ml_dtypes.bfloat16)
    j = np.arange(128)[:, None]
    i = np.arange(128)[None, :]
    same = (j // 64) == (i // 64)
    mf = (same & (j <= i)).astype(np.float32)
    mb = (same & (j > i)).astype(np.float32)
    masks = np.concatenate([mf, mb], axis=1).astype(ml_dtypes.bfloat16)
    invf = (10000.0 ** (-np.arange(0, 32, 2, dtype=np.float32) / 32)).astype(np.float32)
    invf = np.broadcast_to(invf[None, :], (128, 16)).copy()
    sel = np.zeros((32, 32, 128), np.float32)
    for e in range(32):
        sel[e, e, :] = 1.0
    sel = sel.reshape(32, 32 * 128).astype(ml_dtypes.bfloat16)
    lrb = np.zeros((64, 1), np.float32)
    lrb[16, 0] = 1.0
    return {"c_ident": ident, "c_masks": masks, "c_invf": invf, "c_sel": sel, "c_lrbias": lrb}


_NC_CACHE = {}


def make_in_maps(inputs, n_cores=8):
    c = make_consts()
    f = lambda k: np.ascontiguousarray(np.asarray(inputs[k], dtype=np.float32)[0])
    shared = {
        "norm1_gain": f("norm1_gain").reshape(1, D),
        "w_in": f("w_in"),
        "gla_gk_fwd_w": f("gla_gk_fwd_w"), "gla_gk_fwd_b": f("gla_gk_fwd_b").reshape(1, 256),
        "gla_gk_bwd_w": f("gla_gk_bwd_w"), "gla_gk_bwd_b": f("gla_gk_bwd_b").reshape(1, 256),
        "gla_out_gain": f("gla_out_gain").reshape(128, 1),
        "mla_q_gain": f("mla_q_gain").reshape(1, 256), "mla_w_qb": f("mla_w_qb"),
        "mla_kv_gain": f("mla_kv_gain").reshape(1, 128), "mla_w_kvb": f("mla_w_kvb"),
        "q_norm_gain": f("q_norm_gain").reshape(1, 96), "k_norm_gain": f("k_norm_gain").reshape(1, 96),
        "w_out": f("w_out"), "norm2_gain": f("norm2_gain").reshape(1, D),
        "w_router_group": f("w_router_group"), "b_router_group": f("b_router_group").reshape(1, 4),
        "w_router_expert": f("w_router_expert"), "b_router_expert": f("b_router_expert").reshape(1, 32),
        "w_expert_gate": f("w_expert_gate").reshape(32, D, 256),
        "w_expert_up": f("w_expert_up").reshape(32, D, 256),
        "w_expert_down": f("w_expert_down").reshape(32, 256, D),
    }
    shared.update(c)
    x = np.asarray(inputs["x"], dtype=np.float32)
    pos = np.asarray(inputs["positions"]).astype(np.int32)
    maps = []
    for b in range(n_cores):
        m = dict(shared)
        m["x"] = np.ascontiguousarray(x[b])
        m["pos"] = np.ascontiguousarray(pos[b].reshape(NT, 128).T)
        maps.append(m)
    return maps


def kernel(**inputs):
    if "nc" not in _NC_CACHE:
        _NC_CACHE["nc"] = build()
    nc = _NC_CACHE["nc"]
    maps = make_in_maps(inputs, 8)
    res = run_bass_kernel_spmd(nc, maps, core_ids=list(range(8)))
    out = np.stack([np.asarray(r["out"], dtype=np.float32) for r in res.results], axis=0)
    return out
```

```python
import contextlib
import math
import numpy as np
import ml_dtypes
import concourse.bass as bass
import concourse.mybir as mybir
from concourse.bass_utils import run_bass_kernel_spmd

F32 = mybir.dt.float32
BF16 = mybir.dt.bfloat16
I32 = mybir.dt.int32
ALU = mybir.AluOpType
AF = mybir.ActivationFunctionType
AX = mybir.AxisListType

S_LEN = 2048
D = 1024
NT = 16
EPS = 1e-6
PI = math.pi


class T:
    __slots__ = ("name", "w", "r")

    def __init__(self, name):
        self.name = name
        self.w = None
        self.r = []


class Op:
    __slots__ = ("eng", "fn", "deps", "signal", "sig", "dma", "dsem", "dval", "alld", "n", "seg", "idx", "nbytes", "tag")


class Sched:
    ENGS = ("pe", "act", "dve", "pool", "sp")

    def __init__(self, nc, n_dma_sems=12):
        self.nc = nc
        self.ops = {e: [] for e in self.ENGS}
        self.n_dma_sems = n_dma_sems
        self.dma_count = {e: 0 for e in self.ENGS}
        self.tiles = {}
        self.pending = {e: [] for e in self.ENGS}
        self.dma_since_barrier = []
        self.stopped = False
        self.seg = 0
        self.nops = 0
        self.noresched = set()

    def t(self, name):
        if name not in self.tiles:
            self.tiles[name] = T(name)
        return self.tiles[name]

    def _tl(self, lst):
        out = []
        for x in lst:
            if isinstance(x, str):
                out.append(self.t(x))
            elif isinstance(x, (list, tuple)):
                out.extend(self._tl(x))
            elif x is not None:
                out.append(x)
        return out

    def alias(self, new_names, old_names):
        if self.stopped:
            return
        for nn in new_names:
            tn = self.t(nn)
            for on in old_names:
                to = self.t(on)
                if to.w is not None:
                    tn.r.append(to.w)
                tn.r.extend(to.r)

    def barrier(self):
        if self.stopped:
            return
        lasts = []
        for e in self.ENGS:
            for o in reversed(self.ops[e]):
                if not o.dma:
                    lasts.append(o)
                    break
        lasts.extend(self.dma_since_barrier)
        self.dma_since_barrier = []
        for e in self.ENGS:
            self.pending[e] = list(lasts)
        self.seg += 1

    def op(self, eng, fn, reads=(), writes=(), dma=False, n=64, nbytes=0):
        if self.stopped:
            return None
        o = Op()
        o.n = n
        o.nbytes = nbytes
        o.seg = self.seg
        o.idx = self.nops
        self.nops += 1
        o.eng = eng
        o.fn = fn
        o.dma = dma
        o.signal = False
        o.sig = 0
        deps = {}
        reads = self._tl(reads)
        writes = self._tl(writes)
        for t in reads:
            if t.w is not None:
                deps[id(t.w)] = (t.w, "raw")
            if t.name[0] == "P" and t.name[1:].isdigit():
                for r in t.r:
                    if id(r) not in deps and r.eng != eng:
                        deps[id(r)] = (r, "war")
        for t in writes:
            if t.w is not None and id(t.w) not in deps:
                deps[id(t.w)] = (t.w, "waw")
            for r in t.r:
                if id(r) not in deps:
                    deps[id(r)] = (r, "war")
        if self.pending[eng]:
            for p in self.pending[eng]:
                deps[id(p)] = (p, "raw")
            self.pending[eng] = []
        o.alld = [p for p, _k in deps.values()]
        o.tag = ("R:" + ",".join(t.name for t in reads) + " W:" + ",".join(t.name for t in writes))
        dl = []
        for p, kind in deps.values():
            if p.eng == eng and not p.dma:
                if eng == "pe":
                    continue
                if kind != "raw" and not dma and eng != "pool":
                    continue
            dl.append(p)
        o.deps = dl
        for p in dl:
            p.signal = True
        for t in reads:
            if not dma:
                for r in t.r:
                    if not r.dma and r.eng == eng:
                        o.alld.append(r)
                t.r = [r for r in t.r if r.dma or r.eng != eng]
            t.r.append(o)
        for t in writes:
            t.w = o
            t.r = []
        if dma:
            self.dma_count[eng] += 1
            self.dma_since_barrier.append(o)
        self.ops[eng].append(o)
        return o

    @staticmethod
    def _dur(o):
        n = o.n
        if o.dma:
            return 60.0 if o.eng == "sp" else 900.0
        if o.eng == "pe":
            return 30.0 + max(n, 64) / 2.0
        if o.eng == "act":
            return 220.0 + n / 1.4
        if o.eng == "dve":
            return 120.0 + n * 1.3
        return 550.0 + n * 0.75

    def reschedule(self):
        allops = []
        for e in self.ENGS:
            allops.extend(self.ops[e])
        allops.sort(key=lambda o: o.idx)
        import heapq
        new = {e: [] for e in self.ENGS}
        segs = {}
        for o in allops:
            segs.setdefault(o.seg, []).append(o)
        LAT = 250.0
        for sg in sorted(segs):
            ops = segs[sg]
            if sg in self.noresched:
                for o in ops:
                    new[o.eng].append(o)
                continue
            inseg = set(id(o) for o in ops)
            done = {}
            users = {}
            indeg = {}
            first = {}
            for o in ops:
                if o.eng not in first:
                    first[o.eng] = o
                elif first[o.eng] not in o.alld:
                    o.alld.append(first[o.eng])
            for o in ops:
                k = 0
                for p in o.alld:
                    if id(p) in inseg:
                        k += 1
                        users.setdefault(id(p), []).append(o)
                indeg[id(o)] = k
            ready = {e: [] for e in self.ENGS}
            efree = {e: 0.0 for e in self.ENGS}
            rtime = {}
            for o in ops:
                if indeg[id(o)] == 0:
                    rtime[id(o)] = 0.0
                    ready[o.eng].append(o)
            left = len(ops)
            SLACK = 0.0
            while left:
                best = None
                for e in self.ENGS:
                    rl = ready[e]
                    if not rl:
                        continue
                    ef = efree[e]
                    oldest = None
                    fill = None
                    for o in rl:
                        st = max(rtime[id(o)], ef)
                        if oldest is None or o.idx < oldest[1].idx:
                            oldest = (st, o)
                        if fill is None or (st, o.idx) < (fill[0], fill[1].idx):
                            fill = (st, o)
                    pick = oldest if oldest[0] <= fill[0] + SLACK else fill
                    if best is None or (pick[0], pick[1].idx) < (best[0], best[1].idx):
                        best = pick
                st, o = best
                e = o.eng
                ready[e].remove(o)
                d = self._dur(o)
                efree[e] = st + d
                fin = st + d
                if o.dma:
                    fin = st + 2000.0 + o.nbytes / 150.0
                done[id(o)] = fin
                new[e].append(o)
                left -= 1
                for u in users.get(id(o), ()):
                    indeg[id(u)] -= 1
                    lat = 0.0 if (u.eng == o.eng and not o.dma) else LAT
                    rtime[id(u)] = max(rtime.get(id(u), 0.0), fin + lat)
                    if indeg[id(u)] == 0:
                        ready[u.eng].append(u)
        self.ops = new

    def emit(self, es, final_wait_ops=()):
        nc = self.nc
        if RESCHEDULE:
            self.reschedule()
        sems = {e: es.enter_context(nc.semaphore("s_" + e)) for e in self.ENGS}
        dsems = {e: [es.enter_context(nc.semaphore("d_%s_%d" % (e, i)))
                     for i in range(self.n_dma_sems)]
                 for e in self.ENGS if self.dma_count[e] > 0}
        for e in self.ENGS:
            c = 0
            i = 0
            for o in self.ops[e]:
                if o.dma:
                    o.dsem = i % self.n_dma_sems
                    o.dval = 16 * (i // self.n_dma_sems + 1)
                    i += 1
                elif o.signal:
                    c += 1
                    o.sig = c
        block = es.enter_context(nc.Block())
        eng_obj = {"pe": block.tensor, "act": block.scalar, "dve": block.vector,
                   "pool": block.gpsimd, "sp": block.sync}
        for e in self.ENGS:
            ops = self.ops[e]
            if not ops:
                continue

            def body(engine, e=e, ops=ops):
                waited = {}

                def wait(sem, key, val):
                    if waited.get(key, 0) >= val:
                        return
                    waited[key] = val
                    engine.wait_ge(sem, val)

                for o in ops:
                    for p in o.deps:
                        if p.dma:
                            wait(dsems[p.eng][p.dsem], ("d", p.eng, p.dsem), p.dval)
                        else:
                            wait(sems[p.eng], ("c", p.eng), p.sig)
                    if o.dma and o.dval > 16:
                        wait(dsems[e][o.dsem], ("d", e, o.dsem), o.dval - 16)
                    ins = o.fn(engine)
                    if o.dma:
                        ins.then_inc(dsems[e][o.dsem], 16)
                    elif o.signal:
                        ins.then_inc(sems[e], 1)
                if e == "sp":
                    for o in final_wait_ops:
                        if o is None:
                            continue
                        wait(dsems[o.eng][o.dsem], ("d", o.eng, o.dsem), o.dval)

            eng_obj[e](body)


RESCHEDULE = True
SB_BASE = 16640
SB_END = 229376


class Alloc:
    def __init__(self, nc):
        self.nc = nc
        self.off = SB_BASE
        self.n = 0

    def mark(self):
        return self.off

    def reset(self, m):
        self.off = m

    def __call__(self, name, shape, dt, at=None):
        esz = 2 if dt == BF16 else 4
        nb = int(np.prod(shape[1:])) * esz
        nb = (nb + 63) // 64 * 64
        self.n += 1
        if at is None:
            at = self.off
            self.off += nb
        assert at + nb <= SB_END, ("SBUF overflow", name, at, nb)
        return self.nc.alloc_sbuf_tensor_at("%s_%d" % (name, self.n), list(shape), dt, offset=at)


def bcast_ap(ap, pattern):
    return bass.AP(ap.tensor, ap.offset, [list(ap.ap[0])] + [list(p) for p in pattern])


def build(debug=False, stop_after=None):
    nc = bass.Bass("TRN2", target_bir_lowering=False)
    dr = lambda n, s, dt=F32: nc.dram_tensor(n, list(s), dt, kind="ExternalInput")
    x_d = dr("x", [S_LEN, D])
    pos_d = dr("pos", [128, NT], I32)
    g1_d = dr("norm1_gain", [1, D])
    win_d = dr("w_in", [D, 1984])
    gkf_w = dr("gla_gk_fwd_w", [16, 256])
    gkf_b = dr("gla_gk_fwd_b", [1, 256])
    gkb_w = dr("gla_gk_bwd_w", [16, 256])
    gkb_b = dr("gla_gk_bwd_b", [1, 256])
    go_d = dr("gla_out_gain", [128, 1])
    gqa_d = dr("mla_q_gain", [1, 256])
    wqb_d = dr("mla_w_qb", [256, 768])
    gkva_d = dr("mla_kv_gain", [1, 128])
    wkvb_d = dr("mla_w_kvb", [128, 1024])
    gqn_d = dr("q_norm_gain", [1, 96])
    gkn_d = dr("k_norm_gain", [1, 96])
    wout_d = dr("w_out", [D, D])
    g2_d = dr("norm2_gain", [1, D])
    wrg_d = dr("w_router_group", [D, 4])
    brg_d = dr("b_router_group", [1, 4])
    wre_d = dr("w_router_expert", [D, 32])
    bre_d = dr("b_router_expert", [1, 32])
    weg_d = dr("w_expert_gate", [32, D, 256])
    weu_d = dr("w_expert_up", [32, D, 256])
    wed_d = dr("w_expert_down", [32, 256, D])
    ident_d = dr("c_ident", [128, 128], BF16)
    masks_d = dr("c_masks", [128, 256], BF16)
    invf_d = dr("c_invf", [128, 16])
    sel_d = dr("c_sel", [32, 32 * 128], BF16)
    lrb_d = dr("c_lrbias", [64, 1])
    out_d = nc.dram_tensor("out", [S_LEN, D], F32, kind="ExternalOutput")
    dbg = {}
    if debug:
        dbg["mix"] = nc.dram_tensor("d_mix", [128, 8 * S_LEN], BF16, kind="ExternalOutput")
        dbg["x1"] = nc.dram_tensor("d_x1", [S_LEN, D], F32, kind="ExternalOutput")
        dbg["gate"] = nc.dram_tensor("d_gate", [S_LEN, 32], F32, kind="ExternalOutput")

    if debug:
        dbg["gen"] = nc.dram_tensor("d_gen", [128, 8 * S_LEN], BF16, kind="ExternalOutput")
    S = Sched(nc)
    A = Alloc(nc)
    op = S.op
    final_ops = []

    with contextlib.ExitStack() as es:
        P = [es.enter_context(nc.psum_tensor("pb%d" % i, [128, 512], F32)) for i in range(8)]
        Pb = [p.bitcast(BF16) for p in P]
        PN = ["P%d" % i for i in range(8)]

        def fsz(ap):
            r = 1
            for d_ in list(ap.shape)[1:]:
                r *= int(d_)
            return r

        def dma(q, out, in_, reads=(), writes=(), **kw):
            return op(q, lambda e: e.dma_start(out=out, in_=in_, **kw), reads=reads, writes=writes, dma=True,
                      nbytes=fsz(out) * 4 * 128)

        def act(out, in_, func, reads, writes, **kw):
            return op("act", lambda e: e.activation(out=out, in_=in_, func=func, **kw), reads=reads, writes=writes,
                      n=fsz(out))

        def rsqrt_act(out, in_, n, reads, writes):
            act(out, in_, AF.Ln, reads, writes, scale=1.0 / n, bias=EPS)
            act(out, out, AF.Exp, writes, writes, scale=-0.5)

        def tt(eng, out, in0, in1, o, reads, writes):
            return op(eng, lambda e: e.tensor_tensor(out=out, in0=in0, in1=in1, op=o), reads=reads, writes=writes,
                      n=fsz(out))

        def ts(eng, out, in0, s1, s2, o0, o1, reads, writes):
            if o1 is None:
                return op(eng, lambda e: e.tensor_scalar(out=out, in0=in0, scalar1=s1, scalar2=None, op0=o0),
                          reads=reads, writes=writes, n=fsz(out))
            return op(eng, lambda e: e.tensor_scalar(out=out, in0=in0, scalar1=s1, scalar2=s2, op0=o0, op1=o1),
                      reads=reads, writes=writes, n=fsz(out))

        def stt(out, in0, sc, in1, o0, o1, reads, writes):
            return op("dve", lambda e: e.scalar_tensor_tensor(out=out, in0=in0, scalar=sc, in1=in1, op0=o0, op1=o1),
                      reads=reads, writes=writes, n=fsz(out))

        def mm(out, lhsT, rhs, start, stop, reads, writes):
            return op("pe", lambda e: e.matmul(out, lhsT=lhsT, rhs=rhs, start=start, stop=stop),
                      reads=reads, writes=writes, n=fsz(rhs) * (4 if rhs.dtype == F32 else 1))

        def tr(out, in_, ident, reads, writes):
            return op("pe", lambda e: e.transpose(out=out, in_=in_, identity=ident), reads=reads, writes=writes, n=128)

        def cp(eng, out, in_, reads, writes):
            if eng == "act":
                return act(out, in_, AF.Copy, reads, writes)
            return op(eng, lambda e: e.tensor_copy(out=out, in_=in_), reads=reads, writes=writes, n=fsz(out))

        def memset(eng, ap, val, writes):
            return op(eng, lambda e: e.memset(ap, val), writes=writes, n=fsz(ap))

        def bcast_row(dram, n):
            return bass.AP(dram, 0, [[0, 128], [1, n]])

        ident = A("ident", [128, 128], BF16)
        mixT = A("mixT", [128, 8, S_LEN], BF16)
        dma("sp", ident[:, :], ident_d[:, :], writes=["ident"])
        L0 = A.mark()

        hT = A("hT", [128, 8, S_LEN], BF16)
        E1 = A.mark()
        g1 = A("g1", [128, D], F32)
        xt = [A("xt%d" % i, [128, D], F32) for i in range(2)]
        hb = [A("hb%d" % i, [128, D], BF16) for i in range(2)]
        sqj = A("sqj", [128, D], F32)
        st1 = A("st1", [128, 4], F32)
        dma("sp", g1[:, :], bcast_row(g1_d, D), writes=["g1"])

        def norm_to_T(src_ap_fn, src_tiles, gain, gname, dstT, dname, pfx, t, pbank):
            i = t % 2
            ssq = st1[:, 0:1]
            rs = st1[:, 1:2]
            act(sqj[:, :], src_ap_fn(t), AF.Square, src_tiles, [pfx + "sqj", pfx + "ssq"], accum_out=ssq)
            rsqrt_act(rs, ssq, D, [pfx + "ssq"], [pfx + "rs"])
            stt(hb[i][:, :], src_ap_fn(t), rs, gain[:, :], ALU.mult, ALU.mult,
                src_tiles + [pfx + "rs", gname], [pfx + "hb%d" % i])
            pbv = Pb[pbank][:, :].rearrange("p (c n) -> p c n", c=8)
            for kc in range(8):
                tr(pbv[:, kc, :], hb[i][:, kc * 128:(kc + 1) * 128], ident[:, :],
                   [pfx + "hb%d" % i, "ident"], [PN[pbank]])
            cp("dve" if t % 2 else "act", dstT[:, :, t * 128:(t + 1) * 128], pbv, [PN[pbank]], [dname + "_%d" % (t // 4)])

        for t in range(NT):
            i = t % 2
            dma("sp", xt[i][:, :], x_d[t * 128:(t + 1) * 128, :], writes=["xt%d" % i])
            norm_to_T(lambda t, i=i: xt[i][:, :], ["xt%d" % i], g1, "g1", hT, "hT", "A", t, t % 2)
        hT_tiles = ["hT_%d" % k for k in range(4)]
        S.barrier()
        A.reset(E1)
        def checkpoint(name, dump=None, reads=()):
            if stop_after == name:
                if dump is not None and debug:
                    S.barrier()
                    final_ops.append(dma("sp", dbg["gen"][:, :], dump, reads=list(reads)))
                S.stopped = True

        checkpoint("A")

        wm = A("w_in_mla", [128, 8, 416], BF16)
        wqb = A("wqb", [128, 2, 768], BF16)
        wkvb = A("wkvb", [128, 1024], BF16)
        cs = A("cs", [128, NT, 64], F32)
        qhT = A("qhT", [128, 8, S_LEN], BF16)
        khT = A("khT", [128, 8, S_LEN], BF16)
        vA = A("vA", [128, NT, 8, 128], BF16)
        gqa = A("gqa", [128, 384], F32)
        gqk = A("gqkr", [128, 16, 32], F32)
        gcol = A("gcol", [128, 2], F32)
        B1m = A.mark()
        dma("pool", wm[:, :, :], win_d.ap()[:, 1568:1984].rearrange("(c p) n -> p c n", p=128), writes=["wm"])
        dma("pool", wqb[:, :, :], wqb_d.ap().rearrange("(c p) n -> p c n", p=128), writes=["wqb"])
        dma("pool", wkvb[:, :], wkvb_d[:, :], writes=["wkvb"])
        dma("sp", gqa[:, 0:256], bcast_row(gqa_d, 256), writes=["gqa"])
        dma("sp", gqa[:, 256:384], bcast_row(gkva_d, 128), writes=["gqa"])
        dma("sp", gqk[:, 0:8, :], bass.AP(gqn_d, 64, [[0, 128], [0, 8], [1, 32]]), writes=["gqk"])
        dma("sp", gqk[:, 8:16, :], bass.AP(gkn_d, 64, [[0, 128], [0, 8], [1, 32]]), writes=["gqk"])
        memset("pool", gcol[:, :], 1.0, ["gcol"])
        dma("sp", gcol[0:64, 0:1], bass.AP(gqn_d, 0, [[1, 64], [1, 1]]), reads=["gcol"], writes=["gcol"])
        dma("sp", gcol[0:64, 1:2], bass.AP(gkn_d, 0, [[1, 64], [1, 1]]), reads=["gcol"], writes=["gcol"])
        posi = A("posi", [128, NT], I32)
        posf = A("posf", [128, NT], F32)
        invf = A("invf", [128, 16], F32)
        ang = A("ang", [128, NT, 16], F32)
        kk = A("kk", [128, NT, 16], F32)
        ki = A("ki", [128, NT, 16], I32)
        rr = A("rr", [128, NT, 16], F32)
        yy = A("yy", [128, NT, 16], F32)
        m_ = A("m_", [128, NT, 16], F32)
        dma("sp", posi[:, :], pos_d[:, :], writes=["posi"])
        dma("sp", invf[:, :], invf_d[:, :], writes=["invf"])
        cp("dve", posf[:, :], posi[:, :], ["posi"], ["posf"])
        for t in range(NT):
            ts("dve", ang[:, t, :], invf[:, :], posf[:, t:t + 1], None, ALU.mult, None, ["invf", "posf"], ["ang"])
        ts("dve", kk[:, :, :], ang[:, :, :], 1.0 / (2 * PI), None, ALU.mult, None, ["ang"], ["kk"])
        cp("dve", ki[:, :, :], kk[:, :, :], ["kk"], ["ki"])
        cp("dve", kk[:, :, :], ki[:, :, :], ["ki"], ["kk"])
        stt(rr[:, :, :], kk[:, :, :], -2 * PI, ang[:, :, :], ALU.mult, ALU.add, ["kk", "ang"], ["rr"])
        for which, shift in ((1, 0.0), (0, PI / 2)):
            ts("dve", yy[:, :, :], rr[:, :, :], shift, None, ALU.add, None, ["rr"], ["yy"])
            ts("dve", m_[:, :, :], yy[:, :, :], PI, None, ALU.is_gt, None, ["yy"], ["m_"])
            stt(yy[:, :, :], m_[:, :, :], -2 * PI, yy[:, :, :], ALU.mult, ALU.add, ["m_", "yy"], ["yy"])
            ts("dve", m_[:, :, :], yy[:, :, :], -PI, None, ALU.is_lt, None, ["yy"], ["m_"])
            stt(yy[:, :, :], m_[:, :, :], 2 * PI, yy[:, :, :], ALU.mult, ALU.add, ["m_", "yy"], ["yy"])
            ts("dve", yy[:, :, :], yy[:, :, :], PI, -PI, ALU.min, ALU.max, ["yy"], ["yy"])
            if which == 0:
                act(cs[:, :, 0:16], yy[:, :, :], AF.Sin, ["yy"], ["cs"])
                act(cs[:, :, 16:32], yy[:, :, :], AF.Sin, ["yy"], ["cs"])
            else:
                act(cs[:, :, 48:64], yy[:, :, :], AF.Sin, ["yy"], ["cs"])
                act(cs[:, :, 32:48], cs[:, :, 48:64], AF.Copy, ["cs"], ["cs"], scale=-1.0)
        memset("pool", vA[:, :, :, :], 1.0, ["vA"])
        S.barrier()
        A.reset(B1m)

        sqjb1 = A("sqjb", [128, 416], BF16)
        sqjb = [sqjb1, sqjb1]
        stq = [A("stq%d" % i, [128, 32], F32) for i in range(2)]
        ab = [A("ab%d" % i, [128, 384], BF16) for i in range(2)]
        abT = [A("abT%d" % i, [128, 3, 128], BF16) for i in range(2)]
        kraw = [A("kraw%d" % i, [128, 8, 96], F32) for i in range(2)]
        sqn = A("sqn", [128, 16, 96], BF16)
        rg = [A("rg%d" % i, [128, 16, 32], F32) for i in range(2)]
        rg2 = A("rg2", [128, 16, 48], F32)
        rb = A("rb", [128, 16, 32], F32)
        qkf = [A("qkf%d" % i, [128, 16, 96], BF16) for i in range(2)]
        SQ2 = math.sqrt(2.0)

        def st_E1a(t):
            i = t % 2
            sI = "_%d" % i
            tsl = slice(t * 128, (t + 1) * 128)
            hTt = "hT_%d" % (t // 4)
            for kc in range(8):
                mm(P[0][:, 0:416], hT[:, kc, tsl], wm[:, kc, :], kc == 0, kc == 7, [hTt, "wm"], ["P0"])
            act(sqjb[i][:, 0:256], P[0][:, 0:256], AF.Square, ["P0"], ["stqA" + sI], accum_out=stq[i][:, 0:1])
            act(sqjb[i][:, 256:384], P[0][:, 256:384], AF.Square, ["P0"], ["stqA" + sI],
                accum_out=stq[i][:, 1:2], scale=SQ2)
            act(kraw[i][:, :, 64:96], bcast_ap(P[0][:, 384:416], [[0, 8], [1, 32]]), AF.Copy, ["P0"], ["krawR" + sI])
            rsqrt_act(stq[i][:, 2:4], stq[i][:, 0:2], 256, ["stqA" + sI], ["stqB" + sI])
            stt(ab[i][:, 0:256], P[0][:, 0:256], stq[i][:, 2:3], gqa[:, 0:256], ALU.mult, ALU.mult,
                ["P0", "stqB" + sI, "gqa"], ["ab" + sI])
            stt(ab[i][:, 256:384], P[0][:, 256:384], stq[i][:, 3:4], gqa[:, 256:384], ALU.mult, ALU.mult,
                ["P0", "stqB" + sI, "gqa"], ["ab" + sI])
        def st_E1b(t):
            i = t % 2
            sI = "_%d" % i
            tsl = slice(t * 128, (t + 1) * 128)
            hTt = "hT_%d" % (t // 4)
            p1v = Pb[1][:, 0:384].rearrange("p (c n) -> p c n", c=3)
            for c in range(3):
                tr(p1v[:, c, :], ab[i][:, c * 128:(c + 1) * 128], ident[:, :], ["ab" + sI, "ident"], ["P1"])
            cp("act", abT[i][:, :, :], p1v, ["P1"], ["abT" + sI])
        def st_E2(t):
            i = t % 2
            sI = "_%d" % i
            tsl = slice(t * 128, (t + 1) * 128)
            hTt = "hT_%d" % (t // 4)
            for nb in range(2):
                for kc in range(2):
                    mm(P[2 + nb][:, 0:384], abT[i][:, kc, :], wqb[:, kc, nb * 384:(nb + 1) * 384], kc == 0, kc == 1,
                       ["abT" + sI, "wqb"], [PN[2 + nb]])
                mm(P[4 + nb][:, :], abT[i][:, 2, :], wkvb[:, nb * 512:(nb + 1) * 512], True, True,
                   ["abT" + sI, "wkvb"], [PN[4 + nb]])
            for nb in range(2):
                srck = P[4 + nb][:, :].rearrange("p (h d) -> p h d", h=4)[:, :, 0:64]
                cp("act", kraw[i][:, nb * 4:nb * 4 + 4, 0:64], srck, [PN[4 + nb]], ["krawN" + sI])
                srcv = P[4 + nb][:, :].rearrange("p (a b d) -> p a b d", a=2, b=2)
                dstv = vA[:, t, nb * 4:nb * 4 + 4, :].rearrange("p (a b) d -> p a b d", b=2)
                cp("act", dstv[:, :, 0, 0:64], srcv[:, :, 0, 64:128], [PN[4 + nb]], ["vA"])
                cp("act", dstv[:, :, 1, 64:128], srcv[:, :, 1, 64:128], [PN[4 + nb]], ["vA"])
            for nb in range(2):
                act(sqn[:, nb * 4:nb * 4 + 4, :], P[2 + nb][:, 0:384].rearrange("p (h d) -> p h d", h=4), AF.Square,
                    [PN[2 + nb]], ["sqn"])
            act(sqn[:, 8:16, :], kraw[i][:, :, :], AF.Square, ["krawN" + sI, "krawR" + sI], ["sqn"])
            op("dve", lambda e, i=i: e.tensor_reduce(out=stq[i][:, 8:24], in_=sqn[:, :, :], axis=AX.X, op=ALU.add),
               reads=["sqn"], writes=["stqC" + sI])
            rsqrt_act(stq[i][:, 8:24], stq[i][:, 8:24], 96, ["stqC" + sI], ["stqC" + sI])
            for nb in range(2):
                pv = P[2 + nb][:, 0:384].rearrange("p (h d) -> p h d", h=4)
                rq = stq[i][:, 8 + nb * 4:9 + nb * 4]
                tt("dve", qkf[i][:, nb * 4:nb * 4 + 4, 0:64], pv[:, :, 0:64], bcast_ap(rq, [[1, 4], [0, 64]]), ALU.mult,
                   [PN[2 + nb], "stqC" + sI], ["qkf" + sI])
                tt("dve", rg[i][:, nb * 4:nb * 4 + 4, :], pv[:, :, 64:96], bcast_ap(rq, [[1, 4], [0, 32]]), ALU.mult,
                   [PN[2 + nb], "stqC" + sI], ["rg" + sI])
            rk = stq[i][:, 16:17]
            tt("dve", qkf[i][:, 8:16, 0:64], kraw[i][:, :, 0:64], bcast_ap(rk, [[1, 8], [0, 64]]), ALU.mult,
               ["krawN" + sI, "stqC" + sI], ["qkf" + sI])
            tt("dve", rg[i][:, 8:16, :], kraw[i][:, :, 64:96], bcast_ap(rk, [[1, 8], [0, 32]]), ALU.mult,
               ["krawR" + sI, "stqC" + sI], ["rg" + sI])
        def st_L(t):
            i = t % 2
            sI = "_%d" % i
            tsl = slice(t * 128, (t + 1) * 128)
            hTt = "hT_%d" % (t // 4)
            tt("pool", rg2[:, :, 0:32], rg[i][:, :, :], gqk[:, :, :], ALU.mult, ["rg" + sI, "gqk"], ["rg2"])
            tt("pool", rg2[:, :, 32:48], rg[i][:, :, 0:16], gqk[:, :, 0:16], ALU.mult, ["rg" + sI, "gqk"], ["rg2"])
            c1 = bcast_ap(cs[:, t, 0:32], [[0, 16], [1, 32]])
            c2 = bcast_ap(cs[:, t, 32:64], [[0, 16], [1, 32]])
            tt("pool", rg[i][:, :, :], rg2[:, :, 0:32], c1, ALU.mult, ["rg2", "cs"], ["rg" + sI])
            tt("pool", rb[:, :, :], rg2[:, :, 16:48], c2, ALU.mult, ["rg2", "cs"], ["rb"])
            tt("pool", qkf[i][:, :, 64:96], rg[i][:, :, :], rb[:, :, :], ALU.add, ["rg" + sI, "rb"], ["qkf" + sI])
            p6v = Pb[6][:, :].rearrange("p (h n) -> p h n", h=8)
            p7v = Pb[7][:, :].rearrange("p (h n) -> p h n", h=8)
            for h in range(8):
                tr(p6v[0:96, h, :], qkf[i][:, h, :], ident[:, :], ["qkf" + sI, "ident"], ["P6"])
            for h in range(8):
                tr(p7v[0:96, h, :], qkf[i][:, 8 + h, :], ident[:, :], ["qkf" + sI, "ident"], ["P7"])
            ts("dve", qhT[0:96, :, tsl], p6v[0:96, :, :], gcol[0:96, 0:1], None, ALU.mult, None, ["P6", "gcol"],
               ["qhT_%d" % (t // 4)])
            act(khT[0:96, :, tsl], p7v[0:96, :, :], AF.Identity, ["P7", "gcol"], ["khT"], scale=gcol[0:96, 1:2])

        S.noresched.add(S.seg)
        for step in range(NT + 2):
            if step < NT:
                st_E1a(step)
            if 0 <= step - 1 < NT:
                st_E2(step - 1)
            if 0 <= step - 2 < NT:
                st_L(step - 2)
            if step < NT:
                st_E1b(step)
        S.barrier()
        A.reset(B1m)
        checkpoint("B1")
        pbuf = [A("pbuf%d" % i, [128, 512], BF16) for i in range(4)]
        lnb = A("lnb", [128, 512], F32)
        rcb = A("rcb", [128, 512], F32)
        scale = 96 ** -0.5
        it = 0
        for h in range(8):
            even = (h % 2 == 0)
            vrows = slice(0, 64) if even else slice(64, 128)
            srows = slice(64, 128) if even else slice(0, 64)
            for qg in range(4):
                qsl = slice(qg * 512, (qg + 1) * 512)
                ob = 4 + (it % 2)
                seq = []
                for kt in range(16):
                    seq.append(("s", kt))
                    if kt >= 2:
                        seq.append(("pv", kt - 2))
                seq += [("pv", 14), ("pv", 15)]
                for kind, kt in seq:
                    sb_ = kt % 3
                    pi = kt % 4
                    if kind == "s":
                        mm(P[sb_][:, :], khT[0:96, h, kt * 128:(kt + 1) * 128], qhT[0:96, h, qsl], True, True,
                           ["khT", "qhT_%d" % qg], [PN[sb_]])
                        act(pbuf[pi][:, :], P[sb_][:, :], AF.Exp, [PN[sb_]], ["pbuf%d" % pi], scale=scale)
                    else:
                        lhsT = vA[:, kt, h, :]
                        mm(P[ob][:, :], lhsT, pbuf[pi][:, :], kt == 0, kt == 15, ["vA", "pbuf%d" % pi], [PN[ob]])
                act(lnb[vrows, :], P[ob][srows, :], AF.Ln, [PN[ob]], ["lnb"])
                act(rcb[vrows, :], lnb[vrows, :], AF.Exp, ["lnb"], ["rcb"], scale=-1.0)
                tt("dve", mixT[vrows, 4 + h // 2, qsl], P[ob][vrows, :], rcb[vrows, :], ALU.mult, [PN[ob], "rcb"], ["mixT_m"])
                it += 1
        S.barrier()
        A.reset(E1)

        checkpoint("C")
        wg = A("w_in_gla", [128, 8, 1568], BF16)
        R2 = A.mark()
        qkT = A("qkT", [128, 4, S_LEN], F32)
        vtok = A("vtok", [128, NT, 512], BF16)
        sgT = A("sgT", [128, 4, S_LEN], BF16)
        lrT = A("lrT", [64, S_LEN], F32)
        wlr = A("wlr", [128, 8, 64], BF16)
        lrb = A("lrb", [64, 1], F32)
        waug = A("waug", [64, 512], F32)
        masks = A("masks", [128, 256], BF16)
        gout = A("gout", [128, 1], F32)
        onesf = A("onesf", [128, 128], F32)
        R4 = A.mark()
        dma("pool", wg[:, :, :], win_d.ap()[:, 0:1568].rearrange("(c p) n -> p c n", p=128), writes=["wg"])
        memset("pool", wlr[:, :, :], 0.0, ["wlr"])
        dma("pool", wlr[:, :, 0:16], win_d.ap()[:, 1536:1552].rearrange("(c p) n -> p c n", p=128), reads=["wlr"], writes=["wlr"])
        dma("pool", wlr[:, :, 32:48], win_d.ap()[:, 1552:1568].rearrange("(c p) n -> p c n", p=128), reads=["wlr"], writes=["wlr"])
        dma("sp", lrb[:, :], lrb_d[:, :], writes=["lrb"])
        memset("pool", waug[:, :], 0.0, ["waug"])
        dma("sp", waug[0:16, 0:256], gkf_w[:, :], reads=["waug"], writes=["waug"])
        dma("sp", waug[16:17, 0:256], gkf_b[:, :], reads=["waug"], writes=["waug"])
        dma("sp", waug[16:17, 256:512], gkb_b[:, :], reads=["waug"], writes=["waug"])
        dma("sp", waug[32:48, 256:512], gkb_w[:, :], reads=["waug"], writes=["waug"])
        dma("sp", masks[:, :], masks_d[:, :], writes=["masks"])
        dma("sp", gout[:, :], go_d[:, :], writes=["gout"])
        memset("pool", onesf[:, :], 1.0, ["onesf"])
        blk = 0
        for kind, idx in [("q", 0), ("q", 1), ("k", 0), ("k", 1), ("g", 0), ("g", 1), ("g", 2), ("g", 3), ("lr", 0)]:
            for tg in range(4):
                pbk = blk % 4
                blk += 1
                tgs = slice(tg * 512, (tg + 1) * 512)
                for kc in range(8):
                    if kind == "q":
                        lhsT = wg[:, kc, idx * 128:(idx + 1) * 128]
                    elif kind == "k":
                        lhsT = wg[:, kc, 256 + idx * 128:256 + (idx + 1) * 128]
                    elif kind == "g":
                        lhsT = wg[:, kc, 1024 + idx * 128:1024 + (idx + 1) * 128]
                    else:
                        lhsT = wlr[:, kc, :]
                    mrows = 64 if kind == "lr" else 128
                    mm(P[pbk][0:mrows, :], lhsT, hT[:, kc, tgs], kc == 0, kc == 7, ["hT_%d" % tg, "wg", "wlr"], [PN[pbk]])
                if kind == "q":
                    act(qkT[:, idx, tgs], P[pbk][:, :], AF.Copy, [PN[pbk]], ["qT"], scale=0.125)
                elif kind == "k":
                    cp("dve", qkT[:, 2 + idx, tgs], P[pbk][:, :], [PN[pbk]], ["kT"])
                elif kind == "g":
                    act(sgT[:, idx, tgs], P[pbk][:, :], AF.Silu, [PN[pbk]], ["sgT"])
                else:
                    act(lrT[:, tgs], P[pbk][0:64, :], AF.Identity, [PN[pbk], "lrb"], ["lrT"], bias=lrb[:, :])
        for t in range(NT):
            pbk = 4 + t % 2
            tsl = slice(t * 128, (t + 1) * 128)
            for kc in range(8):
                mm(P[pbk][:, :], hT[:, kc, tsl], wg[:, kc, 512:1024], kc == 0, kc == 7, ["hT_%d" % (t // 4), "wg"], [PN[pbk]])
            cp("dve" if t % 2 else "act", vtok[:, t, :], P[pbk][:, :], [PN[pbk]], ["vtok"])
        S.barrier()

        checkpoint("B2")
        HTB = L0
        tmp = [A("gt%d" % i, [128, 1024], F32, at=HTB + i * 4096) for i in range(4)]
        prod = {}
        names = [(d_, hp, k_) for d_ in (0, 1) for hp in (0, 1) for k_ in ("qr", "kr", "qb")]
        slots = [HTB + 16384 + i * 4096 for i in range(4)] + [E1 + 16384 + i * 4096 for i in range(2)]
        for i, nm in enumerate(names):
            if i < 6:
                prod[nm] = A("pr", [128, S_LEN], BF16, at=slots[i])
            else:
                prod[nm] = A("pr", [128, S_LEN], BF16)
        kdT = A("kdT", [128, 1024], BF16)
        dec = A("dec", [128, 4, 32], F32)
        smask = A("smask", [128, 1024], F32)
        kd = A("kd", [128, NT, 512], BF16, at=E1)
        memset("pool", smask[:, :], 1.0, ["smask"])
        memset("pool", smask[:, :].rearrange("p (c j) -> p c j", j=64)[:, :, 0:1], 0.0, ["smask"])
        G, Fc, Dt, Eb = tmp
        Dt2 = A("Dt2", [128, 1024], F32)
        Eb2 = A("Eb2", [128, 1024], F32)
        DtL = [(Dt, "Dt"), (Dt2, "Dt2")]
        EbL = [(Eb, "Eb"), (Eb2, "Eb2")]
        cnt = {"d": 0, "e": 0}

        def nextD():
            cnt["d"] += 1
            return DtL[cnt["d"] % 2]

        def nextE():
            cnt["e"] += 1
            return EbL[cnt["e"] % 2]

        def exp_prod(src, sname, scl, dst, base, bname, dname="prod"):
            E_, en = nextE()
            act(E_[:, :], src, AF.Exp, [sname], [en], scale=scl)
            tt("pool", dst, base, E_[:, :], ALU.mult, [bname, en], [dname])
        for d_ in (0, 1):
            for hp in (0, 1):
                dh = d_ * 2 + hp
                qT = qkT[:, hp, :]
                kT = qkT[:, 2 + hp, :]
                for half in range(2):
                    hs = slice(half * 1024, (half + 1) * 1024)
                    for j in range(2):
                        pbk = j
                        cols = slice(half * 1024 + j * 512, half * 1024 + (j + 1) * 512)
                        mm(P[pbk][:, :], waug[0:64, dh * 128:(dh + 1) * 128], lrT[0:64, cols], True, True,
                           ["waug", "lrT"], [PN[pbk]])
                        act(Eb[:, j * 512:(j + 1) * 512], P[pbk][:, :], AF.Exp, [PN[pbk]], ["Eb"], scale=-1.0)
                    act(G[:, :], Eb[:, :], AF.Ln, ["Eb"], ["G"], bias=1.0)
                    op("dve", lambda e: e.tensor_tensor_scan(out=Fc[:, :], data0=smask[:, :], data1=G[:, :], initial=0.0,
                                                             op0=ALU.mult, op1=ALU.add), reads=["smask", "G"], writes=["Fc"])
                    Fv = Fc[:, :].rearrange("p (c j) -> p c j", j=64)
                    Dv = Dt[:, :].rearrange("p (c j) -> p c j", j=64)
                    T63 = bcast_ap(Fc[:, 63:64], [[64, 16], [0, 64]])
                    act(dec[:, dh, half * 16:(half + 1) * 16], Fv[:, :, 63], AF.Exp, ["Fc"], ["dec"], scale=-1.0 / 16)
                    if d_ == 0:
                        ref = bcast_ap(Fc[:, 31:32], [[64, 16], [0, 64]])
                        D_, dn = nextD()
                        tt("dve", D_[:, :].rearrange("p (c j) -> p c j", j=64), Fv, ref, ALU.subtract, ["Fc"], [dn])
                        exp_prod(D_[:, :], dn, -1.0 / 16, prod[(0, hp, "qr")][:, hs], qT[:, hs], "qT")
                        exp_prod(D_[:, :], dn, 1.0 / 16, prod[(0, hp, "kr")][:, hs], kT[:, hs], "kT")
                        exp_prod(Fc[:, :], "Fc", -1.0 / 16, prod[(0, hp, "qb")][:, hs], qT[:, hs], "qT")
                        D_, dn = nextD()
                        tt("dve", D_[:, :].rearrange("p (c j) -> p c j", j=64), Fv, T63, ALU.subtract, ["Fc"], [dn])
                        exp_prod(D_[:, :], dn, 1.0 / 16, kdT[:, :], kT[:, hs], "kT", "kdT")
                    else:
                        tt("dve", G[:, :], Fc[:, :], G[:, :], ALU.subtract, ["Fc", "G"], ["G"])
                        Gv = G[:, :].rearrange("p (c j) -> p c j", j=64)
                        ref = bcast_ap(G[:, 32:33], [[64, 16], [0, 64]])
                        D_, dn = nextD()
                        tt("dve", D_[:, :].rearrange("p (c j) -> p c j", j=64), Gv, ref, ALU.subtract, ["G"], [dn])
                        exp_prod(D_[:, :], dn, 1.0 / 16, prod[(1, hp, "qr")][:, hs], qT[:, hs], "qT")
                        exp_prod(D_[:, :], dn, -1.0 / 16, prod[(1, hp, "kr")][:, hs], kT[:, hs], "kT")
                        D_, dn = nextD()
                        tt("dve", D_[:, :].rearrange("p (c j) -> p c j", j=64), Gv, T63, ALU.subtract, ["G", "Fc"], [dn])
                        exp_prod(D_[:, :], dn, 1.0 / 16, prod[(1, hp, "qb")][:, hs], qT[:, hs], "qT")
                        exp_prod(G[:, :], "G", -1.0 / 16, kdT[:, :], kT[:, hs], "kT", "kdT")
                    for g4 in range(2):
                        pbk = 2 + g4
                        pv = Pb[pbk][:, 0:512].rearrange("p (t n) -> p t n", t=4)
                        for tq in range(4):
                            c0 = (g4 * 4 + tq) * 128
                            tr(pv[:, tq, :], kdT[:, c0:c0 + 128], ident[:, :], ["kdT", "ident"], [PN[pbk]])
                        t0 = half * 8 + g4 * 4
                        cp("dve", kd[:, t0:t0 + 4, dh * 128:(dh + 1) * 128], pv, [PN[pbk]], ["kd"])
        S.barrier()

        checkpoint("D1")
        qk_off = R2
        Sst = [A("Sst%d" % i, [128, 32, 128], BF16, at=qk_off + i * 8192) for i in range(4)]
        Sf = [A("Sf%d" % i, [128, 256], F32, at=HTB + i * 1024) for i in range(4)]
        for dh in range(4):
            memset("pool", Sf[dh][:, :], 0.0, ["Sf%d" % dh])
        for step in range(32):
            for dh in range(4):
                d_, hp = divmod(dh, 2)
                n = step if d_ == 0 else 31 - step
                t, c = divmod(n, 2)
                rows = slice(c * 64, (c + 1) * 64)
                cp("pool", Sst[dh][0:64, n, :], Sf[dh][0:64, 0:128], ["Sf%d" % dh], ["SstA%d" % dh])
                cp("act", Sst[dh][64:128, n, :], Sf[dh][64:128, 128:256], ["Sf%d" % dh], ["SstB%d" % dh])
                if step == 31:
                    continue
                pbk = 4 * c + dh
                mm(P[pbk][:, 0:256], kd[rows, t, dh * 128:(dh + 1) * 128], vtok[rows, t, hp * 256:(hp + 1) * 256],
                   True, True, ["kd", "vtok"], [PN[pbk]])
                stt(Sf[dh][:, :], Sf[dh][:, :], dec[:, dh, n:n + 1], P[pbk][:, 0:256], ALU.mult, ALU.add,
                    ["Sf%d" % dh, "dec", PN[pbk]], ["Sf%d" % dh])
        S.barrier()

        checkpoint("D2")
        smb = [A("smb%d" % i, [128, 2, 2, 128], BF16, at=HTB + 4096 + i * 1024) for i in range(2)]
        sqoL = [A("sqo%d" % i, [128, 256], F32, at=HTB + 6144 + i * 1024) for i in range(2)]
        rsoL = [A("rso%d" % i, [128, 256], F32, at=HTB + 8192 + i * 1024) for i in range(2)]
        t1oL = [A("t1o%d" % i, [128, 256], F32, at=HTB + 10240 + i * 1024) for i in range(2)]
        for t in range(NT):
            tsl = slice(t * 128, (t + 1) * 128)
            for par in range(2):
                rows = slice(par * 64, (par + 1) * 64)
                sbk = par
                obk = 2 + par
                scv = P[sbk][:, :].rearrange("p (a b n) -> p a b n", a=2, b=2)
                for hp in range(2):
                    for d_ in range(2):
                        mm(scv[:, hp, d_, :], prod[(d_, hp, "kr")][rows, tsl], prod[(d_, hp, "qr")][rows, tsl], True, True,
                           ["prod"], [PN[sbk]])
                mk = bcast_ap(masks[:, 0:256], [[0, 2], [1, 256]])
                tt("dve", smb[par][:, :, :, :].rearrange("p a b n -> p a (b n)"),
                   P[sbk][:, :].rearrange("p (a m) -> p a m", a=2), mk, ALU.mult, [PN[sbk], "masks"], ["smb%d" % par])
                ov = P[obk][:, 0:256].rearrange("p (a n) -> p a n", a=2)
                for hp in range(2):
                    h = hp * 2 + par
                    mm(ov[:, hp, :], vtok[:, t, h * 128:(h + 1) * 128], smb[par][:, hp, 0, :], True, False,
                       ["vtok", "smb%d" % par], [PN[obk]])
                    mm(ov[:, hp, :], vtok[:, t, h * 128:(h + 1) * 128], smb[par][:, hp, 1, :], False, False,
                       ["vtok", "smb%d" % par], [PN[obk]])
                    for d_ in range(2):
                        dh = d_ * 2 + hp
                        for c in range(2):
                            n = t * 2 + c
                            csl = slice(t * 128 + c * 64, t * 128 + (c + 1) * 64)
                            last = (d_ == 1 and c == 1)
                            mm(ov[:, hp, c * 64:(c + 1) * 64], Sst[dh][rows, n, :], prod[(d_, hp, "qb")][rows, csl],
                               False, last, ["SstA%d" % dh, "SstB%d" % dh, "prod"], [PN[obk]])
                sqo, rso, t1o = sqoL[par], rsoL[par], t1oL[par]
                sP = "%d" % par
                act(sqo[:, :], P[obk][:, 0:256], AF.Square, [PN[obk]], ["sqo" + sP])
                ebk = 4 + par
                mm(P[ebk][:, 0:256], onesf[:, :], sqo[:, :], True, True, ["onesf", "sqo" + sP], [PN[ebk]])
                rsqrt_act(rso[:, :], P[ebk][:, 0:256], 128, [PN[ebk]], ["rso" + sP])
                stt(t1o[:, :], P[obk][:, 0:256], gout[:, 0:1], rso[:, :], ALU.mult, ALU.mult, [PN[obk], "gout", "rso" + sP], ["t1o" + sP])
                for hp in range(2):
                    h = hp * 2 + par
                    tt("pool", mixT[:, h, tsl], t1o[:, hp * 128:(hp + 1) * 128], sgT[:, h, tsl], ALU.mult,
                       ["t1o" + sP, "sgT"], ["mixT_g"])
        S.barrier()
        A.reset(L0)
        if debug:
            final_ops.append(dma("sp", dbg["mix"][:, :], mixT[:, :, :].rearrange("p c n -> p (c n)"), reads=["mixT_g", "mixT_m"]))

        checkpoint("D3")
        X = A("X", [128, NT, D], F32)
        h2T = A("h2T", [128, 8, S_LEN], BF16)
        wo = A("wo", [128, 8, D], BF16)
        WO_OFF = A.off - 16384
        g2 = A("g2", [128, D], F32)
        wr = A("wr", [128, 8, 36], BF16)
        rbias = A("rbias", [128, 36], F32)
        gT = A("gT", [32, 2, S_LEN], BF16)
        sel = A("sel", [32, 32, 128], BF16)
        lgA = A("lgA", [128, NT, 36], F32)
        hb = [A("hb2_%d" % i, [128, D], BF16) for i in range(2)]
        sqj = A("sqj2", [128, D], BF16)
        st1 = A("st1_2", [128, 4], F32)
        dma("pool", wo[:, :, :], wout_d.ap().rearrange("(c p) n -> p c n", p=128), writes=["wo"])
        dma("sp", g2[:, :], bcast_row(g2_d, D), writes=["g2"])
        dma("pool", wr[:, :, 0:4], wrg_d.ap().rearrange("(c p) n -> p c n", p=128), writes=["wr"])
        dma("pool", wr[:, :, 4:36], wre_d.ap().rearrange("(c p) n -> p c n", p=128), writes=["wr"])
        dma("sp", rbias[:, 0:4], bcast_row(brg_d, 4), writes=["rbias"])
        dma("sp", rbias[:, 4:36], bcast_row(bre_d, 32), writes=["rbias"])
        dma("sp", sel[:, :, :], sel_d.ap().rearrange("p (e n) -> p e n", e=32), writes=["sel"])
        for t in range(NT):
            tsl = slice(t * 128, (t + 1) * 128)
            dma("sp", X[:, t, :], x_d[tsl, :], writes=["X%d" % t])
            for ch in range(2):
                pbk = (t % 2) * 2 + ch
                for kc in range(8):
                    mm(P[pbk][:, :], mixT[:, kc, tsl], wo[:, kc, ch * 512:(ch + 1) * 512], kc == 0, kc == 7,
                       ["mixT_g", "mixT_m", "wo"], [PN[pbk]])
                tt("dve", X[:, t, ch * 512:(ch + 1) * 512], X[:, t, ch * 512:(ch + 1) * 512], P[pbk][:, :], ALU.add,
                   ["X%d" % t, PN[pbk]], ["X%d" % t])
        if debug:
            for t in range(NT):
                final_ops.append(dma("sp", dbg["x1"][t * 128:(t + 1) * 128, :], X[:, t, :], reads=["X%d" % t]))
        checkpoint("E")
        for t in range(NT):
            tsl = slice(t * 128, (t + 1) * 128)
            norm_to_T(lambda t: X[:, t, :], ["X%d" % t], g2, "g2", h2T, "h2T", "F", t, 4 + t % 2)
            rbk = 6 + t % 2
            for kc in range(8):
                mm(P[rbk][:, 0:36], h2T[:, kc, tsl], wr[:, kc, :], kc == 0, kc == 7, ["h2T_%d" % (t // 4), "wr"], [PN[rbk]])
            tt("dve", lgA[:, t, :], P[rbk][:, 0:36], rbias[:, :], ALU.add, [PN[rbk], "rbias"], ["lgA"])

        def bl(ap2, k):
            return bcast_ap(ap2, [list(ap2.ap[1]), [0, k]])

        def red(out, in_, o, reads, writes):
            return op("dve", lambda e: e.tensor_reduce(out=out, in_=in_, axis=AX.X, op=o), reads=reads, writes=writes,
                      n=fsz(in_))

        r16 = lambda nm: A(nm, [128, NT], F32)
        r4 = lambda nm: A(nm, [128, NT, 4], F32)
        r8 = lambda nm: A(nm, [128, NT, 8], F32)
        mg, s4, ptop, m1, m2, dm, e2, den, w1, w2 = [r16("r16_%d" % i) for i in range(10)]
        d4, e4, oh, ohp = [r4("r4_%d" % i) for i in range(4)]
        ls, tmp8, eq1, ls2, eq2, wg8 = [r8("r8_%d" % i) for i in range(6)]
        gate = A("gate", [128, NT, 32], F32)
        gtmp = A("gtmp", [128, NT, 32], F32)
        ghl = A("ghl", [128, 2, NT, 32], BF16)
        red(mg[:, :], lgA[:, :, 0:4], ALU.max, ["lgA"], ["mg"])
        tt("dve", d4[:, :, :], lgA[:, :, 0:4], bl(mg[:, :], 4), ALU.subtract, ["lgA", "mg"], ["d4"])
        act(e4[:, :, :], d4[:, :, :], AF.Exp, ["d4"], ["e4"])
        red(s4[:, :], e4[:, :, :], ALU.add, ["e4"], ["s4"])
        op("dve", lambda e: e.reciprocal(out=ptop[:, :], in_=s4[:, :]), reads=["s4"], writes=["ptop"], n=128)
        ts("dve", oh[:, :, :], d4[:, :, :], 0.0, None, ALU.is_equal, None, ["d4"], ["oh"])
        tt("dve", ohp[:, :, :], oh[:, :, :], bl(ptop[:, :], 4), ALU.mult, ["oh", "ptop"], ["ohp"])
        tt("dve", ls[:, :, :], lgA[:, :, 4:12], bl(oh[:, :, 0], 8), ALU.mult, ["lgA", "oh"], ["ls"])
        for g_ in range(1, 4):
            tt("dve", tmp8[:, :, :], lgA[:, :, 4 + 8 * g_:12 + 8 * g_], bl(oh[:, :, g_], 8), ALU.mult, ["lgA", "oh"], ["tmp8"])
            tt("dve", ls[:, :, :], ls[:, :, :], tmp8[:, :, :], ALU.add, ["ls", "tmp8"], ["ls"])
        red(m1[:, :], ls[:, :, :], ALU.max, ["ls"], ["m1"])
        tt("dve", eq1[:, :, :], ls[:, :, :], bl(m1[:, :], 8), ALU.is_equal, ["ls", "m1"], ["eq1"])
        stt(ls2[:, :, :], eq1[:, :, :], -1e30, ls[:, :, :], ALU.mult, ALU.add, ["eq1", "ls"], ["ls2"])
        red(m2[:, :], ls2[:, :, :], ALU.max, ["ls2"], ["m2"])
        tt("dve", eq2[:, :, :], ls2[:, :, :], bl(m2[:, :], 8), ALU.is_equal, ["ls2", "m2"], ["eq2"])
        tt("dve", dm[:, :], m2[:, :], m1[:, :], ALU.subtract, ["m1", "m2"], ["dm"])
        act(e2[:, :], dm[:, :], AF.Exp, ["dm"], ["e2"])
        ts("dve", den[:, :], e2[:, :], 1.0, None, ALU.add, None, ["e2"], ["den"])
        op("dve", lambda e: e.reciprocal(out=w1[:, :], in_=den[:, :]), reads=["den"], writes=["w1"], n=128)
        tt("dve", w2[:, :], e2[:, :], w1[:, :], ALU.mult, ["e2", "w1"], ["w2"])
        tt("dve", wg8[:, :, :], eq1[:, :, :], bl(w1[:, :], 8), ALU.mult, ["eq1", "w1"], ["wg8"])
        tt("dve", tmp8[:, :, :], eq2[:, :, :], bl(w2[:, :], 8), ALU.mult, ["eq2", "w2"], ["tmp8"])
        tt("dve", wg8[:, :, :], wg8[:, :, :], tmp8[:, :, :], ALU.add, ["wg8", "tmp8"], ["wg8"])
        for g_ in range(4):
            tt("dve", gate[:, :, g_ * 8:(g_ + 1) * 8], wg8[:, :, :], bl(ohp[:, :, g_], 8), ALU.mult, ["wg8", "ohp"], ["gate"])
        if debug:
            for t in range(NT):
                final_ops.append(dma("sp", dbg["gate"][t * 128:(t + 1) * 128, :], gate[:, t, :], reads=["gate"]))
        cp("dve", ghl[:, 0, :, :], gate[:, :, :], ["gate"], ["ghl0"])
        tt("dve", gtmp[:, :, :], gate[:, :, :], ghl[:, 0, :, :], ALU.subtract, ["gate", "ghl0"], ["gtmp"])
        cp("dve", ghl[:, 1, :, :], gtmp[:, :, :], ["gtmp"], ["ghl1"])
        for a_ in range(2):
            for half in range(2):
                bk = 4 + a_ * 2 + half
                pv = Pb[bk][:, :].rearrange("p (t n) -> p t n", t=8)
                for tq in range(8):
                    tr(pv[0:32, tq, :], ghl[:, a_, half * 8 + tq, :], ident[:, :], ["ghl%d" % a_, "ident"], [PN[bk]])
                cp("act" if half else "dve", gT[0:32, a_, half * 1024:(half + 1) * 1024], Pb[bk][0:32, :], [PN[bk]], ["gT"])
        checkpoint("F")

        EG = 2
        NEG = 32 // EG
        MX = SB_BASE + 256
        S.alias(["hid0", "hid1", "sil0", "sil1", "t1m0", "t1m1", "wdn0", "wdn1"], ["mixT_g", "mixT_m"])
        hid = [A("hid%d" % b, [128, EG, 2, 512], BF16, at=MX + b * 4096) for b in range(2)]
        sil = [A("sil%d" % b, [128, 512], F32, at=MX + 8192 + b * 2048) for b in range(2)]
        t1m = [A("t1m%d" % b, [128, 512], F32, at=MX + 12288 + b * 2048) for b in range(2)]
        wdn = [[A("wd%d_%d" % (b, j), [128, 2, D], BF16, at=MX + 16384 + (b * EG + j) * 4096) for j in range(EG)] for b in range(2)]
        wgt = [None, None]
        wup = [None, None]
        wgt[0] = [A("wg0_%d" % j, [128, 8, 256], BF16) for j in range(EG)]
        wup[0] = [A("wu0_%d" % j, [128, 8, 256], BF16) for j in range(EG)]
        wgt[1] = [A("wg1_%d" % j, [128, 8, 256], BF16, at=WO_OFF + j * 4096) for j in range(EG)]
        wup[1] = [A("wu1_%d" % j, [128, 8, 256], BF16, at=WO_OFF + 8192 + j * 4096) for j in range(EG)]
        itc = 0
        for eg in range(NEG):
            b = eg % 2
            extra = ["wo"] if b == 1 else []
            for j in range(EG):
                e_ = eg * EG + j
                dma("pool", wgt[b][j][:, :, :], weg_d.ap()[e_].rearrange("(c p) f -> p c f", p=128), writes=["wgt%d" % b] + extra)
                dma("pool", wup[b][j][:, :, :], weu_d.ap()[e_].rearrange("(c p) f -> p c f", p=128), writes=["wup%d" % b] + extra)
                dma("pool", wdn[b][j][:, :, :], wed_d.ap()[e_].rearrange("(c p) d -> p c d", p=128), writes=["wdn%d" % b])
            for tg in range(4):
                hbi = itc % 2
                itc += 1
                tgs = slice(tg * 512, (tg + 1) * 512)
                for j in range(EG):
                    e_ = eg * EG + j
                    gbk = 6 + j
                    for fh in range(2):
                        k2 = fh
                        gb_, ub_ = 0 + k2, 2 + k2
                        for kc in range(8):
                            mm(P[gb_][:, :], wgt[b][j][:, kc, fh * 128:(fh + 1) * 128], h2T[:, kc, tgs], kc == 0, kc == 7,
                               ["wgt%d" % b, "h2T_%d" % tg], [PN[gb_]])
                        for kc in range(8):
                            mm(P[ub_][:, :], wup[b][j][:, kc, fh * 128:(fh + 1) * 128], h2T[:, kc, tgs], kc == 0, kc == 7,
                               ["wup%d" % b, "h2T_%d" % tg], [PN[ub_]])
                        if fh == 0:
                            mm(P[gbk][:, :], sel[0:32, e_, :], gT[0:32, 0, tgs], True, False, ["sel", "gT"], [PN[gbk]])
                            mm(P[gbk][:, :], sel[0:32, e_, :], gT[0:32, 1, tgs], False, True, ["sel", "gT"], [PN[gbk]])
                        act(sil[k2][:, :], P[gb_][:, :], AF.Silu, [PN[gb_]], ["sil%d" % k2])
                        tt("dve", t1m[k2][:, :], sil[k2][:, :], P[ub_][:, :], ALU.mult, ["sil%d" % k2, PN[ub_]], ["t1m%d" % k2])
                        tt("dve", hid[hbi][:, j, fh, :], t1m[k2][:, :], P[gbk][:, :], ALU.mult, ["t1m%d" % k2, PN[gbk]],
                           ["hid%d" % hbi])
                for tt_ in range(4):
                    t = tg * 4 + tt_
                    for ch in range(2):
                        abk = 4 + (tt_ * 2 + ch) % 2
                        n_acc = EG * 2
                        a_i = 0
                        for j in range(EG):
                            for fh in range(2):
                                mm(P[abk][:, :], hid[hbi][:, j, fh, tt_ * 128:(tt_ + 1) * 128],
                                   wdn[b][j][:, fh, ch * 512:(ch + 1) * 512], a_i == 0, a_i == n_acc - 1,
                                   ["hid%d" % hbi, "wdn%d" % b], [PN[abk]])
                                a_i += 1
                        tt("dve", X[:, t, ch * 512:(ch + 1) * 512], X[:, t, ch * 512:(ch + 1) * 512], P[abk][:, :], ALU.add,
                           ["X%d" % t, PN[abk]], ["X%d" % t])
        for t in range(NT):
            final_ops.append(dma("sp", out_d[t * 128:(t + 1) * 128, :], X[:, t, :], reads=["X%d" % t]))

        S.emit(es, final_wait_ops=final_ops)
    return nc


def make_consts():
    ident = np.eye(128, dtype=np.float32).astype(ml_dtypes.bfloat16)
    j = np.arange(128)[:, None]
    i = np.arange(128)[None, :]
    same = (j // 64) == (i // 64)
    mf = (same & (j <= i)).astype(np.float32)
    mb = (same & (j > i)).astype(np.float32)
    masks = np.concatenate([mf, mb], axis=1).astype(ml_dtypes.bfloat16)
    invf = (10000.0 ** (-np.arange(0, 32, 2, dtype=np.float32) / 32)).astype(np.float32)
    invf = np.broadcast_to(invf[None, :], (128, 16)).copy()
    sel = np.zeros((32, 32, 128), np.float32)
    for e in range(32):
        sel[e, e, :] = 1.0
    sel = sel.reshape(32, 32 * 128).astype(ml_dtypes.bfloat16)
    lrb = np.zeros((64, 1), np.float32)
    lrb[16, 0] = 1.0
    return {"c_ident": ident, "c_masks": masks, "c_invf": invf, "c_sel": sel, "c_lrbias": lrb}


_NC_CACHE = {}


def make_in_maps(inputs, n_cores=8):
    c = make_consts()
    f = lambda k: np.ascontiguousarray(np.asarray(inputs[k], dtype=np.float32)[0])
    shared = {
        "norm1_gain": f("norm1_gain").reshape(1, D),
        "w_in": f("w_in"),
        "gla_gk_fwd_w": f("gla_gk_fwd_w"), "gla_gk_fwd_b": f("gla_gk_fwd_b").reshape(1, 256),
        "gla_gk_bwd_w": f("gla_gk_bwd_w"), "gla_gk_bwd_b": f("gla_gk_bwd_b").reshape(1, 256),
        "gla_out_gain": f("gla_out_gain").reshape(128, 1),
        "mla_q_gain": f("mla_q_gain").reshape(1, 256), "mla_w_qb": f("mla_w_qb"),
        "mla_kv_gain": f("mla_kv_gain").reshape(1, 128), "mla_w_kvb": f("mla_w_kvb"),
        "q_norm_gain": f("q_norm_gain").reshape(1, 96), "k_norm_gain": f("k_norm_gain").reshape(1, 96),
        "w_out": f("w_out"), "norm2_gain": f("norm2_gain").reshape(1, D),
        "w_router_group": f("w_router_group"), "b_router_group": f("b_router_group").reshape(1, 4),
        "w_router_expert": f("w_router_expert"), "b_router_expert": f("b_router_expert").reshape(1, 32),
        "w_expert_gate": f("w_expert_gate").reshape(32, D, 256),
        "w_expert_up": f("w_expert_up").reshape(32, D, 256),
        "w_expert_down": f("w_expert_down").reshape(32, 256, D),
    }
    shared.update(c)
    x = np.asarray(inputs["x"], dtype=np.float32)
    pos = np.asarray(inputs["positions"]).astype(np.int32)
    maps = []
    for b in range(n_cores):
        m = dict(shared)
        m["x"] = np.ascontiguousarray(x[b])
        m["pos"] = np.ascontiguousarray(pos[b].reshape(NT, 128).T)
        maps.append(m)
    return maps


def kernel(**inputs):
    if "nc" not in _NC_CACHE:
        _NC_CACHE["nc"] = build()
    nc = _NC_CACHE["nc"]
    maps = make_in_maps(inputs, 8)
    res = run_bass_kernel_spmd(nc, maps, core_ids=list(range(8)))
    out = np.stack([np.asarray(r["out"], dtype=np.float32) for r in res.results], axis=0)
    return out
```

```python
import contextlib
import math
import numpy as np
import ml_dtypes
import concourse.bass as bass
import concourse.mybir as mybir
from concourse.bass_utils import run_bass_kernel_spmd

F32 = mybir.dt.float32
BF16 = mybir.dt.bfloat16
I32 = mybir.dt.int32
ALU = mybir.AluOpType
AF = mybir.ActivationFunctionType
AX = mybir.AxisListType

S_LEN = 2048
D = 1024
NT = 16
EPS = 1e-6
PI = math.pi


class T:
    __slots__ = ("name", "w", "r")

    def __init__(self, name):
        self.name = name
        self.w = None
        self.r = []


class Op:
    __slots__ = ("eng", "fn", "deps", "signal", "sig", "dma", "dsem", "dval", "alld", "n", "seg", "idx", "nbytes", "tag")


class Sched:
    ENGS = ("pe", "act", "dve", "pool", "sp")

    def __init__(self, nc, n_dma_sems=12):
        self.nc = nc
        self.ops = {e: [] for e in self.ENGS}
        self.n_dma_sems = n_dma_sems
        self.dma_count = {e: 0 for e in self.ENGS}
        self.tiles = {}
        self.pending = {e: [] for e in self.ENGS}
        self.dma_since_barrier = []
        self.stopped = False
        self.seg = 0
        self.nops = 0
        self.noresched = set()

    def t(self, name):
        if name not in self.tiles:
            self.tiles[name] = T(name)
        return self.tiles[name]

    def _tl(self, lst):
        out = []
        for x in lst:
            if isinstance(x, str):
                out.append(self.t(x))
            elif isinstance(x, (list, tuple)):
                out.extend(self._tl(x))
            elif x is not None:
                out.append(x)
        return out

    def alias(self, new_names, old_names):
        if self.stopped:
            return
        for nn in new_names:
            tn = self.t(nn)
            for on in old_names:
                to = self.t(on)
                if to.w is not None:
                    tn.r.append(to.w)
                tn.r.extend(to.r)

    def barrier(self):
        if self.stopped:
            return
        lasts = []
        for e in self.ENGS:
            for o in reversed(self.ops[e]):
                if not o.dma:
                    lasts.append(o)
                    break
        lasts.extend(self.dma_since_barrier)
        self.dma_since_barrier = []
        for e in self.ENGS:
            self.pending[e] = list(lasts)
        self.seg += 1

    def op(self, eng, fn, reads=(), writes=(), dma=False, n=64, nbytes=0):
        if self.stopped:
            return None
        o = Op()
        o.n = n
        o.nbytes = nbytes
        o.seg = self.seg
        o.idx = self.nops
        self.nops += 1
        o.eng = eng
        o.fn = fn
        o.dma = dma
        o.signal = False
        o.sig = 0
        deps = {}
        reads = self._tl(reads)
        writes = self._tl(writes)
        for t in reads:
            if t.w is not None:
                deps[id(t.w)] = (t.w, "raw")
            if t.name[0] == "P" and t.name[1:].isdigit():
                for r in t.r:
                    if id(r) not in deps and r.eng != eng:
                        deps[id(r)] = (r, "war")
        for t in writes:
            if t.w is not None and id(t.w) not in deps:
                deps[id(t.w)] = (t.w, "waw")
            for r in t.r:
                if id(r) not in deps:
                    deps[id(r)] = (r, "war")
        if self.pending[eng]:
            for p in self.pending[eng]:
                deps[id(p)] = (p, "raw")
            self.pending[eng] = []
        o.alld = [p for p, _k in deps.values()]
        o.tag = ("R:" + ",".join(t.name for t in reads) + " W:" + ",".join(t.name for t in writes))
        dl = []
        for p, kind in deps.values():
            if p.eng == eng and not p.dma:
                if eng == "pe":
                    continue
                if kind != "raw" and not dma and eng != "pool":
                    continue
            dl.append(p)
        o.deps = dl
        for p in dl:
            p.signal = True
        for t in reads:
            if not dma:
                for r in t.r:
                    if not r.dma and r.eng == eng:
                        o.alld.append(r)
                t.r = [r for r in t.r if r.dma or r.eng != eng]
            t.r.append(o)
        for t in writes:
            t.w = o
            t.r = []
        if dma:
            self.dma_count[eng] += 1
            self.dma_since_barrier.append(o)
        self.ops[eng].append(o)
        return o

    @staticmethod
    def _dur(o):
        n = o.n
        if o.dma:
            return 60.0 if o.eng == "sp" else 900.0
        if o.eng == "pe":
            return 30.0 + max(n, 64) / 2.0
        if o.eng == "act":
            return 220.0 + n / 1.4
        if o.eng == "dve":
            return 120.0 + n * 1.3
        return 550.0 + n * 0.75

    def reschedule(self):
        allops = []
        for e in self.ENGS:
            allops.extend(self.ops[e])
        allops.sort(key=lambda o: o.idx)
        import heapq
        new = {e: [] for e in self.ENGS}
        segs = {}
        for o in allops:
            segs.setdefault(o.seg, []).append(o)
        LAT = 250.0
        for sg in sorted(segs):
            ops = segs[sg]
            if sg in self.noresched:
                for o in ops:
                    new[o.eng].append(o)
                continue
            inseg = set(id(o) for o in ops)
            done = {}
            users = {}
            indeg = {}
            first = {}
            for o in ops:
                if o.eng not in first:
                    first[o.eng] = o
                elif first[o.eng] not in o.alld:
                    o.alld.append(first[o.eng])
            for o in ops:
                k = 0
                for p in o.alld:
                    if id(p) in inseg:
                        k += 1
                        users.setdefault(id(p), []).append(o)
                indeg[id(o)] = k
            ready = {e: [] for e in self.ENGS}
            efree = {e: 0.0 for e in self.ENGS}
            rtime = {}
            for o in ops:
                if indeg[id(o)] == 0:
                    rtime[id(o)] = 0.0
                    ready[o.eng].append(o)
            left = len(ops)
            SLACK = 0.0
            while left:
                best = None
                for e in self.ENGS:
                    rl = ready[e]
                    if not rl:
                        continue
                    ef = efree[e]
                    oldest = None
                    fill = None
                    for o in rl:
                        st = max(rtime[id(o)], ef)
                        if oldest is None or o.idx < oldest[1].idx:
                            oldest = (st, o)
                        if fill is None or (st, o.idx) < (fill[0], fill[1].idx):
                            fill = (st, o)
                    pick = oldest if oldest[0] <= fill[0] + SLACK else fill
                    if best is None or (pick[0], pick[1].idx) < (best[0], best[1].idx):
                        best = pick
                st, o = best
                e = o.eng
                ready[e].remove(o)
                d = self._dur(o)
                efree[e] = st + d
                fin = st + d
                if o.dma:
                    fin = st + 2000.0 + o.nbytes / 150.0
                done[id(o)] = fin
                new[e].append(o)
                left -= 1
                for u in users.get(id(o), ()):
                    indeg[id(u)] -= 1
                    lat = 0.0 if (u.eng == o.eng and not o.dma) else LAT
                    rtime[id(u)] = max(rtime.get(id(u), 0.0), fin + lat)
                    if indeg[id(u)] == 0:
                        ready[u.eng].append(u)
        self.ops = new

    def emit(self, es, final_wait_ops=()):
        nc = self.nc
        if RESCHEDULE:
            self.reschedule()
        sems = {e: es.enter_context(nc.semaphore("s_" + e)) for e in self.ENGS}
        dsems = {e: [es.enter_context(nc.semaphore("d_%s_%d" % (e, i)))
                     for i in range(self.n_dma_sems)]
                 for e in self.ENGS if self.dma_count[e] > 0}
        for e in self.ENGS:
            c = 0
            i = 0
            for o in self.ops[e]:
                if o.dma:
                    o.dsem = i % self.n_dma_sems
                    o.dval = 16 * (i // self.n_dma_sems + 1)
                    i += 1
                elif o.signal:
                    c += 1
                    o.sig = c
        block = es.enter_context(nc.Block())
        eng_obj = {"pe": block.tensor, "act": block.scalar, "dve": block.vector,
                   "pool": block.gpsimd, "sp": block.sync}
        for e in self.ENGS:
            ops = self.ops[e]
            if not ops:
                continue

            def body(engine, e=e, ops=ops):
                waited = {}

                def wait(sem, key, val):
                    if waited.get(key, 0) >= val:
                        return
                    waited[key] = val
                    engine.wait_ge(sem, val)

                for o in ops:
                    for p in o.deps:
                        if p.dma:
                            wait(dsems[p.eng][p.dsem], ("d", p.eng, p.dsem), p.dval)
                        else:
                            wait(sems[p.eng], ("c", p.eng), p.sig)
                    if o.dma and o.dval > 16:
                        wait(dsems[e][o.dsem], ("d", e, o.dsem), o.dval - 16)
                    ins = o.fn(engine)
                    if o.dma:
                        ins.then_inc(dsems[e][o.dsem], 16)
                    elif o.signal:
                        ins.then_inc(sems[e], 1)
                if e == "sp":
                    for o in final_wait_ops:
                        if o is None:
                            continue
                        wait(dsems[o.eng][o.dsem], ("d", o.eng, o.dsem), o.dval)

            eng_obj[e](body)


RESCHEDULE = True
SB_BASE = 16640
SB_END = 229376


class Alloc:
    def __init__(self, nc):
        self.nc = nc
        self.off = SB_BASE
        self.n = 0

    def mark(self):
        return self.off

    def reset(self, m):
        self.off = m

    def __call__(self, name, shape, dt, at=None):
        esz = 2 if dt == BF16 else 4
        nb = int(np.prod(shape[1:])) * esz
        nb = (nb + 63) // 64 * 64
        self.n += 1
        if at is None:
            at = self.off
            self.off += nb
        assert at + nb <= SB_END, ("SBUF overflow", name, at, nb)
        return self.nc.alloc_sbuf_tensor_at("%s_%d" % (name, self.n), list(shape), dt, offset=at)


def bcast_ap(ap, pattern):
    return bass.AP(ap.tensor, ap.offset, [list(ap.ap[0])] + [list(p) for p in pattern])


def build(debug=False, stop_after=None):
    nc = bass.Bass("TRN2", target_bir_lowering=False)
    dr = lambda n, s, dt=F32: nc.dram_tensor(n, list(s), dt, kind="ExternalInput")
    x_d = dr("x", [S_LEN, D])
    pos_d = dr("pos", [128, NT], I32)
    g1_d = dr("norm1_gain", [1, D])
    win_d = dr("w_in", [D, 1984])
    gkf_w = dr("gla_gk_fwd_w", [16, 256])
    gkf_b = dr("gla_gk_fwd_b", [1, 256])
    gkb_w = dr("gla_gk_bwd_w", [16, 256])
    gkb_b = dr("gla_gk_bwd_b", [1, 256])
    go_d = dr("gla_out_gain", [128, 1])
    gqa_d = dr("mla_q_gain", [1, 256])
    wqb_d = dr("mla_w_qb", [256, 768])
    gkva_d = dr("mla_kv_gain", [1, 128])
    wkvb_d = dr("mla_w_kvb", [128, 1024])
    gqn_d = dr("q_norm_gain", [1, 96])
    gkn_d = dr("k_norm_gain", [1, 96])
    wout_d = dr("w_out", [D, D])
    g2_d = dr("norm2_gain", [1, D])
    wrg_d = dr("w_router_group", [D, 4])
    brg_d = dr("b_router_group", [1, 4])
    wre_d = dr("w_router_expert", [D, 32])
    bre_d = dr("b_router_expert", [1, 32])
    weg_d = dr("w_expert_gate", [32, D, 256])
    weu_d = dr("w_expert_up", [32, D, 256])
    wed_d = dr("w_expert_down", [32, 256, D])
    ident_d = dr("c_ident", [128, 128], BF16)
    masks_d = dr("c_masks", [128, 256], BF16)
    invf_d = dr("c_invf", [128, 16])
    sel_d = dr("c_sel", [32, 32 * 128], BF16)
    lrb_d = dr("c_lrbias", [64, 1])
    out_d = nc.dram_tensor("out", [S_LEN, D], F32, kind="ExternalOutput")
    dbg = {}
    if debug:
        dbg["mix"] = nc.dram_tensor("d_mix", [128, 8 * S_LEN], BF16, kind="ExternalOutput")
        dbg["x1"] = nc.dram_tensor("d_x1", [S_LEN, D], F32, kind="ExternalOutput")
        dbg["gate"] = nc.dram_tensor("d_gate", [S_LEN, 32], F32, kind="ExternalOutput")

    if debug:
        dbg["gen"] = nc.dram_tensor("d_gen", [128, 8 * S_LEN], BF16, kind="ExternalOutput")
    S = Sched(nc)
    A = Alloc(nc)
    op = S.op
    final_ops = []

    with contextlib.ExitStack() as es:
        P = [es.enter_context(nc.psum_tensor("pb%d" % i, [128, 512], F32)) for i in range(8)]
        Pb = [p.bitcast(BF16) for p in P]
        PN = ["P%d" % i for i in range(8)]

        def fsz(ap):
            r = 1
            for d_ in list(ap.shape)[1:]:
                r *= int(d_)
            return r

        def dma(q, out, in_, reads=(), writes=(), **kw):
            return op(q, lambda e: e.dma_start(out=out, in_=in_, **kw), reads=reads, writes=writes, dma=True,
                      nbytes=fsz(out) * 4 * 128)

        def act(out, in_, func, reads, writes, **kw):
            return op("act", lambda e: e.activation(out=out, in_=in_, func=func, **kw), reads=reads, writes=writes,
                      n=fsz(out))

        def rsqrt_act(out, in_, n, reads, writes):
            act(out, in_, AF.Ln, reads, writes, scale=1.0 / n, bias=EPS)
            act(out, out, AF.Exp, writes, writes, scale=-0.5)

        def tt(eng, out, in0, in1, o, reads, writes):
            return op(eng, lambda e: e.tensor_tensor(out=out, in0=in0, in1=in1, op=o), reads=reads, writes=writes,
                      n=fsz(out))

        def ts(eng, out, in0, s1, s2, o0, o1, reads, writes):
            if o1 is None:
                return op(eng, lambda e: e.tensor_scalar(out=out, in0=in0, scalar1=s1, scalar2=None, op0=o0),
                          reads=reads, writes=writes, n=fsz(out))
            return op(eng, lambda e: e.tensor_scalar(out=out, in0=in0, scalar1=s1, scalar2=s2, op0=o0, op1=o1),
                      reads=reads, writes=writes, n=fsz(out))

        def stt(out, in0, sc, in1, o0, o1, reads, writes):
            return op("dve", lambda e: e.scalar_tensor_tensor(out=out, in0=in0, scalar=sc, in1=in1, op0=o0, op1=o1),
                      reads=reads, writes=writes, n=fsz(out))

        def mm(out, lhsT, rhs, start, stop, reads, writes):
            return op("pe", lambda e: e.matmul(out, lhsT=lhsT, rhs=rhs, start=start, stop=stop),
                      reads=reads, writes=writes, n=fsz(rhs) * (4 if rhs.dtype == F32 else 1))

        def tr(out, in_, ident, reads, writes):
            return op("pe", lambda e: e.transpose(out=out, in_=in_, identity=ident), reads=reads, writes=writes, n=128)

        def cp(eng, out, in_, reads, writes):
            if eng == "act":
                return act(out, in_, AF.Copy, reads, writes)
            return op(eng, lambda e: e.tensor_copy(out=out, in_=in_), reads=reads, writes=writes, n=fsz(out))

        def memset(eng, ap, val, writes):
            return op(eng, lambda e: e.memset(ap, val), writes=writes, n=fsz(ap))

        def bcast_row(dram, n):
            return bass.AP(dram, 0, [[0, 128], [1, n]])

        ident = A("ident", [128, 128], BF16)
        mixT = A("mixT", [128, 8, S_LEN], BF16)
        dma("sp", ident[:, :], ident_d[:, :], writes=["ident"])
        L0 = A.mark()

        hT = A("hT", [128, 8, S_LEN], BF16)
        E1 = A.mark()
        g1 = A("g1", [128, D], F32)
        xt = [A("xt%d" % i, [128, D], F32) for i in range(2)]
        hb = [A("hb%d" % i, [128, D], BF16) for i in range(2)]
        sqj = A("sqj", [128, D], F32)
        st1 = A("st1", [128, 4], F32)
        dma("sp", g1[:, :], bcast_row(g1_d, D), writes=["g1"])

        def norm_to_T(src_ap_fn, src_tiles, gain, gname, dstT, dname, pfx, t, pbank):
            i = t % 2
            ssq = st1[:, 0:1]
            rs = st1[:, 1:2]
            act(sqj[:, :], src_ap_fn(t), AF.Square, src_tiles, [pfx + "sqj", pfx + "ssq"], accum_out=ssq)
            rsqrt_act(rs, ssq, D, [pfx + "ssq"], [pfx + "rs"])
            stt(hb[i][:, :], src_ap_fn(t), rs, gain[:, :], ALU.mult, ALU.mult,
                src_tiles + [pfx + "rs", gname], [pfx + "hb%d" % i])
            pbv = Pb[pbank][:, :].rearrange("p (c n) -> p c n", c=8)
            for kc in range(8):
                tr(pbv[:, kc, :], hb[i][:, kc * 128:(kc + 1) * 128], ident[:, :],
                   [pfx + "hb%d" % i, "ident"], [PN[pbank]])
            cp("dve" if t % 2 else "act", dstT[:, :, t * 128:(t + 1) * 128], pbv, [PN[pbank]], [dname + "_%d" % (t // 4)])

        for t in range(NT):
            i = t % 2
            dma("sp", xt[i][:, :], x_d[t * 128:(t + 1) * 128, :], writes=["xt%d" % i])
            norm_to_T(lambda t, i=i: xt[i][:, :], ["xt%d" % i], g1, "g1", hT, "hT", "A", t, t % 2)
        hT_tiles = ["hT_%d" % k for k in range(4)]
        S.barrier()
        A.reset(E1)
        def checkpoint(name, dump=None, reads=()):
            if stop_after == name:
                if dump is not None and debug:
                    S.barrier()
                    final_ops.append(dma("sp", dbg["gen"][:, :], dump, reads=list(reads)))
                S.stopped = True

        checkpoint("A")

        wm = A("w_in_mla", [128, 8, 416], BF16)
        wqb = A("wqb", [128, 2, 768], BF16)
        wkvb = A("wkvb", [128, 1024], BF16)
        cs = A("cs", [128, NT, 64], F32)
        qhT = A("qhT", [128, 8, S_LEN], BF16)
        khT = A("khT", [128, 8, S_LEN], BF16)
        vA = A("vA", [128, NT, 8, 128], BF16)
        gqa = A("gqa", [128, 384], F32)
        gqk = A("gqkr", [128, 16, 32], F32)
        gcol = A("gcol", [128, 2], F32)
        B1m = A.mark()
        dma("pool", wm[:, :, :], win_d.ap()[:, 1568:1984].rearrange("(c p) n -> p c n", p=128), writes=["wm"])
        dma("pool", wqb[:, :, :], wqb_d.ap().rearrange("(c p) n -> p c n", p=128), writes=["wqb"])
        dma("pool", wkvb[:, :], wkvb_d[:, :], writes=["wkvb"])
        dma("sp", gqa[:, 0:256], bcast_row(gqa_d, 256), writes=["gqa"])
        dma("sp", gqa[:, 256:384], bcast_row(gkva_d, 128), writes=["gqa"])
        dma("sp", gqk[:, 0:8, :], bass.AP(gqn_d, 64, [[0, 128], [0, 8], [1, 32]]), writes=["gqk"])
        dma("sp", gqk[:, 8:16, :], bass.AP(gkn_d, 64, [[0, 128], [0, 8], [1, 32]]), writes=["gqk"])
        memset("pool", gcol[:, :], 1.0, ["gcol"])
        dma("sp", gcol[0:64, 0:1], bass.AP(gqn_d, 0, [[1, 64], [1, 1]]), reads=["gcol"], writes=["gcol"])
        dma("sp", gcol[0:64, 1:2], bass.AP(gkn_d, 0, [[1, 64], [1, 1]]), reads=["gcol"], writes=["gcol"])
        posi = A("posi", [128, NT], I32)
        posf = A("posf", [128, NT], F32)
        invf = A("invf", [128, 16], F32)
        ang = A("ang", [128, NT, 16], F32)
        kk = A("kk", [128, NT, 16], F32)
        ki = A("ki", [128, NT, 16], I32)
        rr = A("rr", [128, NT, 16], F32)
        yy = A("yy", [128, NT, 16], F32)
        m_ = A("m_", [128, NT, 16], F32)
        dma("sp", posi[:, :], pos_d[:, :], writes=["posi"])
        dma("sp", invf[:, :], invf_d[:, :], writes=["invf"])
        cp("dve", posf[:, :], posi[:, :], ["posi"], ["posf"])
        for t in range(NT):
            ts("dve", ang[:, t, :], invf[:, :], posf[:, t:t + 1], None, ALU.mult, None, ["invf", "posf"], ["ang"])
        ts("dve", kk[:, :, :], ang[:, :, :], 1.0 / (2 * PI), None, ALU.mult, None, ["ang"], ["kk"])
        cp("dve", ki[:, :, :], kk[:, :, :], ["kk"], ["ki"])
        cp("dve", kk[:, :, :], ki[:, :, :], ["ki"], ["kk"])
        stt(rr[:, :, :], kk[:, :, :], -2 * PI, ang[:, :, :], ALU.mult, ALU.add, ["kk", "ang"], ["rr"])
        for which, shift in ((1, 0.0), (0, PI / 2)):
            ts("dve", yy[:, :, :], rr[:, :, :], shift, None, ALU.add, None, ["rr"], ["yy"])
            ts("dve", m_[:, :, :], yy[:, :, :], PI, None, ALU.is_gt, None, ["yy"], ["m_"])
            stt(yy[:, :, :], m_[:, :, :], -2 * PI, yy[:, :, :], ALU.mult, ALU.add, ["m_", "yy"], ["yy"])
            ts("dve", m_[:, :, :], yy[:, :, :], -PI, None, ALU.is_lt, None, ["yy"], ["m_"])
            stt(yy[:, :, :], m_[:, :, :], 2 * PI, yy[:, :, :], ALU.mult, ALU.add, ["m_", "yy"], ["yy"])
            ts("dve", yy[:, :, :], yy[:, :, :], PI, -PI, ALU.min, ALU.max, ["yy"], ["yy"])
            if which == 0:
                act(cs[:, :, 0:16], yy[:, :, :], AF.Sin, ["yy"], ["cs"])
                act(cs[:, :, 16:32], yy[:, :, :], AF.Sin, ["yy"], ["cs"])
            else:
                act(cs[:, :, 48:64], yy[:, :, :], AF.Sin, ["yy"], ["cs"])
                act(cs[:, :, 32:48], cs[:, :, 48:64], AF.Copy, ["cs"], ["cs"], scale=-1.0)
        memset("pool", vA[:, :, :, :], 1.0, ["vA"])
        S.barrier()
        A.reset(B1m)

        sqjb1 = A("sqjb", [128, 416], BF16)
        sqjb = [sqjb1, sqjb1]
        stq = [A("stq%d" % i, [128, 32], F32) for i in range(2)]
        ab = [A("ab%d" % i, [128, 384], BF16) for i in range(2)]
        abT = [A("abT%d" % i, [128, 3, 128], BF16) for i in range(2)]
        kraw = [A("kraw%d" % i, [128, 8, 96], F32) for i in range(2)]
        sqn = A("sqn", [128, 16, 96], BF16)
        rg = [A("rg%d" % i, [128, 16, 32], F32) for i in range(2)]
        rg2 = A("rg2", [128, 16, 48], F32)
        rb = A("rb", [128, 16, 32], F32)
        qkf = [A("qkf%d" % i, [128, 16, 96], BF16) for i in range(2)]
        SQ2 = math.sqrt(2.0)

        def st_E1a(t):
            i = t % 2
            sI = "_%d" % i
            tsl = slice(t * 128, (t + 1) * 128)
            hTt = "hT_%d" % (t // 4)
            for kc in range(8):
                mm(P[0][:, 0:416], hT[:, kc, tsl], wm[:, kc, :], kc == 0, kc == 7, [hTt, "wm"], ["P0"])
            act(sqjb[i][:, 0:256], P[0][:, 0:256], AF.Square, ["P0"], ["stqA" + sI], accum_out=stq[i][:, 0:1])
            act(sqjb[i][:, 256:384], P[0][:, 256:384], AF.Square, ["P0"], ["stqA" + sI],
                accum_out=stq[i][:, 1:2], scale=SQ2)
            act(kraw[i][:, :, 64:96], bcast_ap(P[0][:, 384:416], [[0, 8], [1, 32]]), AF.Copy, ["P0"], ["krawR" + sI])
            rsqrt_act(stq[i][:, 2:4], stq[i][:, 0:2], 256, ["stqA" + sI], ["stqB" + sI])
            stt(ab[i][:, 0:256], P[0][:, 0:256], stq[i][:, 2:3], gqa[:, 0:256], ALU.mult, ALU.mult,
                ["P0", "stqB" + sI, "gqa"], ["ab" + sI])
            stt(ab[i][:, 256:384], P[0][:, 256:384], stq[i][:, 3:4], gqa[:, 256:384], ALU.mult, ALU.mult,
                ["P0", "stqB" + sI, "gqa"], ["ab" + sI])
        def st_E1b(t):
            i = t % 2
            sI = "_%d" % i
            tsl = slice(t * 128, (t + 1) * 128)
            hTt = "hT_%d" % (t // 4)
            p1v = Pb[1][:, 0:384].rearrange("p (c n) -> p c n", c=3)
            for c in range(3):
                tr(p1v[:, c, :], ab[i][:, c * 128:(c + 1) * 128], ident[:, :], ["ab" + sI, "ident"], ["P1"])
            cp("act", abT[i][:, :, :], p1v, ["P1"], ["abT" + sI])
        def st_E2(t):
            i = t % 2
            sI = "_%d" % i
            tsl = slice(t * 128, (t + 1) * 128)
            hTt = "hT_%d" % (t // 4)
            for nb in range(2):
                for kc in range(2):
                    mm(P[2 + nb][:, 0:384], abT[i][:, kc, :], wqb[:, kc, nb * 384:(nb + 1) * 384], kc == 0, kc == 1,
                       ["abT" + sI, "wqb"], [PN[2 + nb]])
                mm(P[4 + nb][:, :], abT[i][:, 2, :], wkvb[:, nb * 512:(nb + 1) * 512], True, True,
                   ["abT" + sI, "wkvb"], [PN[4 + nb]])
            for nb in range(2):
                srck = P[4 + nb][:, :].rearrange("p (h d) -> p h d", h=4)[:, :, 0:64]
                cp("act", kraw[i][:, nb * 4:nb * 4 + 4, 0:64], srck, [PN[4 + nb]], ["krawN" + sI])
                srcv = P[4 + nb][:, :].rearrange("p (a b d) -> p a b d", a=2, b=2)
                dstv = vA[:, t, nb * 4:nb * 4 + 4, :].rearrange("p (a b) d -> p a b d", b=2)
                cp("act", dstv[:, :, 0, 0:64], srcv[:, :, 0, 64:128], [PN[4 + nb]], ["vA"])
                cp("act", dstv[:, :, 1, 64:128], srcv[:, :, 1, 64:128], [PN[4 + nb]], ["vA"])
            for nb in range(2):
                act(sqn[:, nb * 4:nb * 4 + 4, :], P[2 + nb][:, 0:384].rearrange("p (h d) -> p h d", h=4), AF.Square,
                    [PN[2 + nb]], ["sqn"])
            act(sqn[:, 8:16, :], kraw[i][:, :, :], AF.Square, ["krawN" + sI, "krawR" + sI], ["sqn"])
            op("dve", lambda e, i=i: e.tensor_reduce(out=stq[i][:, 8:24], in_=sqn[:, :, :], axis=AX.X, op=ALU.add),
               reads=["sqn"], writes=["stqC" + sI])
            rsqrt_act(stq[i][:, 8:24], stq[i][:, 8:24], 96, ["stqC" + sI], ["stqC" + sI])
            for nb in range(2):
                pv = P[2 + nb][:, 0:384].rearrange("p (h d) -> p h d", h=4)
                rq = stq[i][:, 8 + nb * 4:9 + nb * 4]
                tt("dve", qkf[i][:, nb * 4:nb * 4 + 4, 0:64], pv[:, :, 0:64], bcast_ap(rq, [[1, 4], [0, 64]]), ALU.mult,
                   [PN[2 + nb], "stqC" + sI], ["qkf" + sI])
                tt("dve", rg[i][:, nb * 4:nb * 4 + 4, :], pv[:, :, 64:96], bcast_ap(rq, [[1, 4], [0, 32]]), ALU.mult,
                   [PN[2 + nb], "stqC" + sI], ["rg" + sI])
            rk = stq[i][:, 16:17]
            tt("dve", qkf[i][:, 8:16, 0:64], kraw[i][:, :, 0:64], bcast_ap(rk, [[1, 8], [0, 64]]), ALU.mult,
               ["krawN" + sI, "stqC" + sI], ["qkf" + sI])
            tt("dve", rg[i][:, 8:16, :], kraw[i][:, :, 64:96], bcast_ap(rk, [[1, 8], [0, 32]]), ALU.mult,
               ["krawR" + sI, "stqC" + sI], ["rg" + sI])
        def st_L(t):
            i = t % 2
            sI = "_%d" % i
            tsl = slice(t * 128, (t + 1) * 128)
            hTt = "hT_%d" % (t // 4)
            tt("pool", rg2[:, :, 0:32], rg[i][:, :, :], gqk[:, :, :], ALU.mult, ["rg" + sI, "gqk"], ["rg2"])
            tt("pool", rg2[:, :, 32:48], rg[i][:, :, 0:16], gqk[:, :, 0:16], ALU.mult, ["rg" + sI, "gqk"], ["rg2"])
            c1 = bcast_ap(cs[:, t, 0:32], [[0, 16], [1, 32]])
            c2 = bcast_ap(cs[:, t, 32:64], [[0, 16], [1, 32]])
            tt("pool", rg[i][:, :, :], rg2[:, :, 0:32], c1, ALU.mult, ["rg2", "cs"], ["rg" + sI])
            tt("pool", rb[:, :, :], rg2[:, :, 16:48], c2, ALU.mult, ["rg2", "cs"], ["rb"])
            tt("pool", qkf[i][:, :, 64:96], rg[i][:, :, :], rb[:, :, :], ALU.add, ["rg" + sI, "rb"], ["qkf" + sI])
            p6v = Pb[6][:, :].rearrange("p (h n) -> p h n", h=8)
            p7v = Pb[7][:, :].rearrange("p (h n) -> p h n", h=8)
            for h in range(8):
                tr(p6v[0:96, h, :], qkf[i][:, h, :], ident[:, :], ["qkf" + sI, "ident"], ["P6"])
            for h in range(8):
                tr(p7v[0:96, h, :], qkf[i][:, 8 + h, :], ident[:, :], ["qkf" + sI, "ident"], ["P7"])
            ts("dve", qhT[0:96, :, tsl], p6v[0:96, :, :], gcol[0:96, 0:1], None, ALU.mult, None, ["P6", "gcol"],
               ["qhT_%d" % (t // 4)])
            act(khT[0:96, :, tsl], p7v[0:96, :, :], AF.Identity, ["P7", "gcol"], ["khT"], scale=gcol[0:96, 1:2])

        S.noresched.add(S.seg)
        for step in range(NT + 2):
            if step < NT:
                st_E1a(step)
            if 0 <= step - 1 < NT:
                st_E2(step - 1)
            if 0 <= step - 2 < NT:
                st_L(step - 2)
            if step < NT:
                st_E1b(step)
        S.barrier()
        A.reset(B1m)
        checkpoint("B1")
        pbuf = [A("pbuf%d" % i, [128, 512], BF16) for i in range(4)]
        lnb = A("lnb", [128, 512], F32)
        rcb = A("rcb", [128, 512], F32)
        scale = 96 ** -0.5
        it = 0
        for h in range(8):
            even = (h % 2 == 0)
            vrows = slice(0, 64) if even else slice(64, 128)
            srows = slice(64, 128) if even else slice(0, 64)
            for qg in range(4):
                qsl = slice(qg * 512, (qg + 1) * 512)
                ob = 4 + (it % 2)
                seq = []
                for kt in range(16):
                    seq.append(("s", kt))
                    if kt >= 2:
                        seq.append(("pv", kt - 2))
                seq += [("pv", 14), ("pv", 15)]
                for kind, kt in seq:
                    sb_ = kt % 3
                    pi = kt % 4
                    if kind == "s":
                        mm(P[sb_][:, :], khT[0:96, h, kt * 128:(kt + 1) * 128], qhT[0:96, h, qsl], True, True,
                           ["khT", "qhT_%d" % qg], [PN[sb_]])
                        act(pbuf[pi][:, :], P[sb_][:, :], AF.Exp, [PN[sb_]], ["pbuf%d" % pi], scale=scale)
                    else:
                        lhsT = vA[:, kt, h, :]
                        mm(P[ob][:, :], lhsT, pbuf[pi][:, :], kt == 0, kt == 15, ["vA", "pbuf%d" % pi], [PN[ob]])
                op("dve", lambda e, vrows=vrows, srows=srows, ob=ob: e.reciprocal(out=rcb[vrows, :], in_=P[ob][srows, :]),
                   reads=[PN[ob]], writes=["rcb"], n=4096)
                tt("dve", mixT[vrows, 4 + h // 2, qsl], P[ob][vrows, :], rcb[vrows, :], ALU.mult, [PN[ob], "rcb"], ["mixT_m"])
                it += 1
        S.barrier()
        A.reset(E1)

        checkpoint("C")
        wg = A("w_in_gla", [128, 8, 1568], BF16)
        R2 = A.mark()
        qkT = A("qkT", [128, 4, S_LEN], F32)
        vtok = A("vtok", [128, NT, 512], BF16)
        sgT = A("sgT", [128, 4, S_LEN], BF16)
        lrT = A("lrT", [64, S_LEN], F32)
        wlr = A("wlr", [128, 8, 64], BF16)
        lrb = A("lrb", [64, 1], F32)
        waug = A("waug", [64, 512], F32)
        masks = A("masks", [128, 256], BF16)
        gout = A("gout", [128, 1], F32)
        onesf = A("onesf", [128, 128], F32)
        R4 = A.mark()
        dma("pool", wg[:, :, :], win_d.ap()[:, 0:1568].rearrange("(c p) n -> p c n", p=128), writes=["wg"])
        memset("pool", wlr[:, :, :], 0.0, ["wlr"])
        dma("pool", wlr[:, :, 0:16], win_d.ap()[:, 1536:1552].rearrange("(c p) n -> p c n", p=128), reads=["wlr"], writes=["wlr"])
        dma("pool", wlr[:, :, 32:48], win_d.ap()[:, 1552:1568].rearrange("(c p) n -> p c n", p=128), reads=["wlr"], writes=["wlr"])
        dma("sp", lrb[:, :], lrb_d[:, :], writes=["lrb"])
        memset("pool", waug[:, :], 0.0, ["waug"])
        dma("sp", waug[0:16, 0:256], gkf_w[:, :], reads=["waug"], writes=["waug"])
        dma("sp", waug[16:17, 0:256], gkf_b[:, :], reads=["waug"], writes=["waug"])
        dma("sp", waug[16:17, 256:512], gkb_b[:, :], reads=["waug"], writes=["waug"])
        dma("sp", waug[32:48, 256:512], gkb_w[:, :], reads=["waug"], writes=["waug"])
        dma("sp", masks[:, :], masks_d[:, :], writes=["masks"])
        dma("sp", gout[:, :], go_d[:, :], writes=["gout"])
        memset("pool", onesf[:, :], 1.0, ["onesf"])
        blk = 0
        for kind, idx in [("q", 0), ("q", 1), ("k", 0), ("k", 1), ("g", 0), ("g", 1), ("g", 2), ("g", 3), ("lr", 0)]:
            for tg in range(4):
                pbk = blk % 4
                blk += 1
                tgs = slice(tg * 512, (tg + 1) * 512)
                for kc in range(8):
                    if kind == "q":
                        lhsT = wg[:, kc, idx * 128:(idx + 1) * 128]
                    elif kind == "k":
                        lhsT = wg[:, kc, 256 + idx * 128:256 + (idx + 1) * 128]
                    elif kind == "g":
                        lhsT = wg[:, kc, 1024 + idx * 128:1024 + (idx + 1) * 128]
                    else:
                        lhsT = wlr[:, kc, :]
                    mrows = 64 if kind == "lr" else 128
                    mm(P[pbk][0:mrows, :], lhsT, hT[:, kc, tgs], kc == 0, kc == 7, ["hT_%d" % tg, "wg", "wlr"], [PN[pbk]])
                if kind == "q":
                    act(qkT[:, idx, tgs], P[pbk][:, :], AF.Copy, [PN[pbk]], ["qT"], scale=0.125)
                elif kind == "k":
                    cp("dve", qkT[:, 2 + idx, tgs], P[pbk][:, :], [PN[pbk]], ["kT"])
                elif kind == "g":
                    act(sgT[:, idx, tgs], P[pbk][:, :], AF.Silu, [PN[pbk]], ["sgT"])
                else:
                    act(lrT[:, tgs], P[pbk][0:64, :], AF.Identity, [PN[pbk], "lrb"], ["lrT"], bias=lrb[:, :])
        for t in range(NT):
            pbk = 4 + t % 2
            tsl = slice(t * 128, (t + 1) * 128)
            for kc in range(8):
                mm(P[pbk][:, :], hT[:, kc, tsl], wg[:, kc, 512:1024], kc == 0, kc == 7, ["hT_%d" % (t // 4), "wg"], [PN[pbk]])
            cp("dve" if t % 2 else "act", vtok[:, t, :], P[pbk][:, :], [PN[pbk]], ["vtok"])
        S.barrier()

        checkpoint("B2")
        HTB = L0
        tmp = [A("gt%d" % i, [128, 1024], F32, at=HTB + i * 4096) for i in range(4)]
        prod = {}
        names = [(d_, hp, k_) for d_ in (0, 1) for hp in (0, 1) for k_ in ("qr", "kr", "qb")]
        slots = [HTB + 16384 + i * 4096 for i in range(4)] + [E1 + 16384 + i * 4096 for i in range(2)]
        for i, nm in enumerate(names):
            if i < 6:
                prod[nm] = A("pr", [128, S_LEN], BF16, at=slots[i])
            else:
                prod[nm] = A("pr", [128, S_LEN], BF16)
        kdT = A("kdT", [128, 1024], BF16)
        dec = A("dec", [128, 4, 32], F32)
        smask = A("smask", [128, 1024], F32)
        kd = A("kd", [128, NT, 512], BF16, at=E1)
        memset("pool", smask[:, :], 1.0, ["smask"])
        memset("pool", smask[:, :].rearrange("p (c j) -> p c j", j=64)[:, :, 0:1], 0.0, ["smask"])
        G, Fc, Dt, Eb = tmp
        Dt2 = A("Dt2", [128, 1024], F32)
        Eb2 = A("Eb2", [128, 1024], F32)
        DtL = [(Dt, "Dt"), (Dt2, "Dt2")]
        EbL = [(Eb, "Eb"), (Eb2, "Eb2")]
        cnt = {"d": 0, "e": 0}

        def nextD():
            cnt["d"] += 1
            return DtL[cnt["d"] % 2]

        def nextE():
            cnt["e"] += 1
            return EbL[cnt["e"] % 2]

        def exp_prod(src, sname, scl, dst, base, bname, dname="prod"):
            E_, en = nextE()
            act(E_[:, :], src, AF.Exp, [sname], [en], scale=scl)
            tt("pool", dst, base, E_[:, :], ALU.mult, [bname, en], [dname])
        for d_ in (0, 1):
            for hp in (0, 1):
                dh = d_ * 2 + hp
                qT = qkT[:, hp, :]
                kT = qkT[:, 2 + hp, :]
                for half in range(2):
                    hs = slice(half * 1024, (half + 1) * 1024)
                    for j in range(2):
                        pbk = j
                        cols = slice(half * 1024 + j * 512, half * 1024 + (j + 1) * 512)
                        mm(P[pbk][:, :], waug[0:64, dh * 128:(dh + 1) * 128], lrT[0:64, cols], True, True,
                           ["waug", "lrT"], [PN[pbk]])
                        act(Eb[:, j * 512:(j + 1) * 512], P[pbk][:, :], AF.Exp, [PN[pbk]], ["Eb"], scale=-1.0)
                    act(G[:, :], Eb[:, :], AF.Ln, ["Eb"], ["G"], bias=1.0)
                    op("dve", lambda e: e.tensor_tensor_scan(out=Fc[:, :], data0=smask[:, :], data1=G[:, :], initial=0.0,
                                                             op0=ALU.mult, op1=ALU.add), reads=["smask", "G"], writes=["Fc"])
                    Fv = Fc[:, :].rearrange("p (c j) -> p c j", j=64)
                    Dv = Dt[:, :].rearrange("p (c j) -> p c j", j=64)
                    T63 = bcast_ap(Fc[:, 63:64], [[64, 16], [0, 64]])
                    act(dec[:, dh, half * 16:(half + 1) * 16], Fv[:, :, 63], AF.Exp, ["Fc"], ["dec"], scale=-1.0 / 16)
                    if d_ == 0:
                        ref = bcast_ap(Fc[:, 31:32], [[64, 16], [0, 64]])
                        D_, dn = nextD()
                        tt("dve", D_[:, :].rearrange("p (c j) -> p c j", j=64), Fv, ref, ALU.subtract, ["Fc"], [dn])
                        exp_prod(D_[:, :], dn, -1.0 / 16, prod[(0, hp, "qr")][:, hs], qT[:, hs], "qT")
                        exp_prod(D_[:, :], dn, 1.0 / 16, prod[(0, hp, "kr")][:, hs], kT[:, hs], "kT")
                        exp_prod(Fc[:, :], "Fc", -1.0 / 16, prod[(0, hp, "qb")][:, hs], qT[:, hs], "qT")
                        D_, dn = nextD()
                        tt("dve", D_[:, :].rearrange("p (c j) -> p c j", j=64), Fv, T63, ALU.subtract, ["Fc"], [dn])
                        exp_prod(D_[:, :], dn, 1.0 / 16, kdT[:, :], kT[:, hs], "kT", "kdT")
                    else:
                        tt("dve", G[:, :], Fc[:, :], G[:, :], ALU.subtract, ["Fc", "G"], ["G"])
                        Gv = G[:, :].rearrange("p (c j) -> p c j", j=64)
                        ref = bcast_ap(G[:, 32:33], [[64, 16], [0, 64]])
                        D_, dn = nextD()
                        tt("dve", D_[:, :].rearrange("p (c j) -> p c j", j=64), Gv, ref, ALU.subtract, ["G"], [dn])
                        exp_prod(D_[:, :], dn, 1.0 / 16, prod[(1, hp, "qr")][:, hs], qT[:, hs], "qT")
                        exp_prod(D_[:, :], dn, -1.0 / 16, prod[(1, hp, "kr")][:, hs], kT[:, hs], "kT")
                        D_, dn = nextD()
                        tt("dve", D_[:, :].rearrange("p (c j) -> p c j", j=64), Gv, T63, ALU.subtract, ["G", "Fc"], [dn])
                        exp_prod(D_[:, :], dn, 1.0 / 16, prod[(1, hp, "qb")][:, hs], qT[:, hs], "qT")
                        exp_prod(G[:, :], "G", -1.0 / 16, kdT[:, :], kT[:, hs], "kT", "kdT")
                    for g4 in range(2):
                        pbk = 2 + g4
                        pv = Pb[pbk][:, 0:512].rearrange("p (t n) -> p t n", t=4)
                        for tq in range(4):
                            c0 = (g4 * 4 + tq) * 128
                            tr(pv[:, tq, :], kdT[:, c0:c0 + 128], ident[:, :], ["kdT", "ident"], [PN[pbk]])
                        t0 = half * 8 + g4 * 4
                        cp("dve", kd[:, t0:t0 + 4, dh * 128:(dh + 1) * 128], pv, [PN[pbk]], ["kd"])
        S.barrier()

        checkpoint("D1")
        qk_off = R2
        Sst = [A("Sst%d" % i, [128, 32, 128], BF16, at=qk_off + i * 8192) for i in range(4)]
        Sf = [A("Sf%d" % i, [128, 256], F32, at=HTB + i * 1024) for i in range(4)]
        for dh in range(4):
            memset("pool", Sf[dh][:, :], 0.0, ["Sf%d" % dh])
        for step in range(32):
            for dh in range(4):
                d_, hp = divmod(dh, 2)
                n = step if d_ == 0 else 31 - step
                t, c = divmod(n, 2)
                rows = slice(c * 64, (c + 1) * 64)
                cp("pool", Sst[dh][0:64, n, :], Sf[dh][0:64, 0:128], ["Sf%d" % dh], ["SstA%d" % dh])
                cp("act", Sst[dh][64:128, n, :], Sf[dh][64:128, 128:256], ["Sf%d" % dh], ["SstB%d" % dh])
                if step == 31:
                    continue
                pbk = 4 * c + dh
                mm(P[pbk][:, 0:256], kd[rows, t, dh * 128:(dh + 1) * 128], vtok[rows, t, hp * 256:(hp + 1) * 256],
                   True, True, ["kd", "vtok"], [PN[pbk]])
                stt(Sf[dh][:, :], Sf[dh][:, :], dec[:, dh, n:n + 1], P[pbk][:, 0:256], ALU.mult, ALU.add,
                    ["Sf%d" % dh, "dec", PN[pbk]], ["Sf%d" % dh])
        S.barrier()

        checkpoint("D2")
        smb = [A("smb%d" % i, [128, 2, 2, 128], BF16, at=HTB + 4096 + i * 1024) for i in range(2)]
        sqoL = [A("sqo%d" % i, [128, 256], F32, at=HTB + 6144 + i * 1024) for i in range(2)]
        rsoL = [A("rso%d" % i, [128, 256], F32, at=HTB + 8192 + i * 1024) for i in range(2)]
        t1oL = [A("t1o%d" % i, [128, 256], F32, at=HTB + 10240 + i * 1024) for i in range(2)]
        for t in range(NT):
            tsl = slice(t * 128, (t + 1) * 128)
            for par in range(2):
                rows = slice(par * 64, (par + 1) * 64)
                sbk = par
                obk = 2 + par
                scv = P[sbk][:, :].rearrange("p (a b n) -> p a b n", a=2, b=2)
                for hp in range(2):
                    for d_ in range(2):
                        mm(scv[:, hp, d_, :], prod[(d_, hp, "kr")][rows, tsl], prod[(d_, hp, "qr")][rows, tsl], True, True,
                           ["prod"], [PN[sbk]])
                mk = bcast_ap(masks[:, 0:256], [[0, 2], [1, 256]])
                tt("dve", smb[par][:, :, :, :].rearrange("p a b n -> p a (b n)"),
                   P[sbk][:, :].rearrange("p (a m) -> p a m", a=2), mk, ALU.mult, [PN[sbk], "masks"], ["smb%d" % par])
                ov = P[obk][:, 0:256].rearrange("p (a n) -> p a n", a=2)
                for hp in range(2):
                    h = hp * 2 + par
                    mm(ov[:, hp, :], vtok[:, t, h * 128:(h + 1) * 128], smb[par][:, hp, 0, :], True, False,
                       ["vtok", "smb%d" % par], [PN[obk]])
                    mm(ov[:, hp, :], vtok[:, t, h * 128:(h + 1) * 128], smb[par][:, hp, 1, :], False, False,
                       ["vtok", "smb%d" % par], [PN[obk]])
                    for d_ in range(2):
                        dh = d_ * 2 + hp
                        for c in range(2):
                            n = t * 2 + c
                            csl = slice(t * 128 + c * 64, t * 128 + (c + 1) * 64)
                            last = (d_ == 1 and c == 1)
                            mm(ov[:, hp, c * 64:(c + 1) * 64], Sst[dh][rows, n, :], prod[(d_, hp, "qb")][rows, csl],
                               False, last, ["SstA%d" % dh, "SstB%d" % dh, "prod"], [PN[obk]])
                sqo, rso, t1o = sqoL[par], rsoL[par], t1oL[par]
                sP = "%d" % par
                act(sqo[:, :], P[obk][:, 0:256], AF.Square, [PN[obk]], ["sqo" + sP])
                ebk = 4 + par
                mm(P[ebk][:, 0:256], onesf[:, :], sqo[:, :], True, True, ["onesf", "sqo" + sP], [PN[ebk]])
                rsqrt_act(rso[:, :], P[ebk][:, 0:256], 128, [PN[ebk]], ["rso" + sP])
                stt(t1o[:, :], P[obk][:, 0:256], gout[:, 0:1], rso[:, :], ALU.mult, ALU.mult, [PN[obk], "gout", "rso" + sP], ["t1o" + sP])
                for hp in range(2):
                    h = hp * 2 + par
                    tt("pool", mixT[:, h, tsl], t1o[:, hp * 128:(hp + 1) * 128], sgT[:, h, tsl], ALU.mult,
                       ["t1o" + sP, "sgT"], ["mixT_g"])
        S.barrier()
        A.reset(L0)
        if debug:
            final_ops.append(dma("sp", dbg["mix"][:, :], mixT[:, :, :].rearrange("p c n -> p (c n)"), reads=["mixT_g", "mixT_m"]))

        checkpoint("D3")
        X = A("X", [128, NT, D], F32)
        h2T = A("h2T", [128, 8, S_LEN], BF16)
        wo = A("wo", [128, 8, D], BF16)
        WO_OFF = A.off - 16384
        g2 = A("g2", [128, D], F32)
        wr = A("wr", [128, 8, 36], BF16)
        rbias = A("rbias", [128, 36], F32)
        gT = A("gT", [32, 2, S_LEN], BF16)
        sel = A("sel", [32, 32, 128], BF16)
        lgA = A("lgA", [128, NT, 36], F32)
        hb = [A("hb2_%d" % i, [128, D], BF16) for i in range(2)]
        sqj = A("sqj2", [128, D], BF16)
        st1 = A("st1_2", [128, 4], F32)
        dma("pool", wo[:, :, :], wout_d.ap().rearrange("(c p) n -> p c n", p=128), writes=["wo"])
        dma("sp", g2[:, :], bcast_row(g2_d, D), writes=["g2"])
        dma("pool", wr[:, :, 0:4], wrg_d.ap().rearrange("(c p) n -> p c n", p=128), writes=["wr"])
        dma("pool", wr[:, :, 4:36], wre_d.ap().rearrange("(c p) n -> p c n", p=128), writes=["wr"])
        dma("sp", rbias[:, 0:4], bcast_row(brg_d, 4), writes=["rbias"])
        dma("sp", rbias[:, 4:36], bcast_row(bre_d, 32), writes=["rbias"])
        dma("sp", sel[:, :, :], sel_d.ap().rearrange("p (e n) -> p e n", e=32), writes=["sel"])
        for t in range(NT):
            tsl = slice(t * 128, (t + 1) * 128)
            dma("sp", X[:, t, :], x_d[tsl, :], writes=["X%d" % t])
            for ch in range(2):
                pbk = (t % 2) * 2 + ch
                for kc in range(8):
                    mm(P[pbk][:, :], mixT[:, kc, tsl], wo[:, kc, ch * 512:(ch + 1) * 512], kc == 0, kc == 7,
                       ["mixT_g", "mixT_m", "wo"], [PN[pbk]])
                tt("dve", X[:, t, ch * 512:(ch + 1) * 512], X[:, t, ch * 512:(ch + 1) * 512], P[pbk][:, :], ALU.add,
                   ["X%d" % t, PN[pbk]], ["X%d" % t])
        if debug:
            for t in range(NT):
                final_ops.append(dma("sp", dbg["x1"][t * 128:(t + 1) * 128, :], X[:, t, :], reads=["X%d" % t]))
        checkpoint("E")
        for t in range(NT):
            tsl = slice(t * 128, (t + 1) * 128)
            norm_to_T(lambda t: X[:, t, :], ["X%d" % t], g2, "g2", h2T, "h2T", "F", t, 4 + t % 2)
            rbk = 6 + t % 2
            for kc in range(8):
                mm(P[rbk][:, 0:36], h2T[:, kc, tsl], wr[:, kc, :], kc == 0, kc == 7, ["h2T_%d" % (t // 4), "wr"], [PN[rbk]])
            tt("dve", lgA[:, t, :], P[rbk][:, 0:36], rbias[:, :], ALU.add, [PN[rbk], "rbias"], ["lgA"])

        def bl(ap2, k):
            return bcast_ap(ap2, [list(ap2.ap[1]), [0, k]])

        def red(out, in_, o, reads, writes):
            return op("dve", lambda e: e.tensor_reduce(out=out, in_=in_, axis=AX.X, op=o), reads=reads, writes=writes,
                      n=fsz(in_))

        r16 = lambda nm: A(nm, [128, NT], F32)
        r4 = lambda nm: A(nm, [128, NT, 4], F32)
        r8 = lambda nm: A(nm, [128, NT, 8], F32)
        mg, s4, ptop, m1, m2, dm, e2, den, w1, w2 = [r16("r16_%d" % i) for i in range(10)]
        d4, e4, oh, ohp = [r4("r4_%d" % i) for i in range(4)]
        ls, tmp8, eq1, ls2, eq2, wg8 = [r8("r8_%d" % i) for i in range(6)]
        gate = A("gate", [128, NT, 32], F32)
        gtmp = A("gtmp", [128, NT, 32], F32)
        ghl = A("ghl", [128, 2, NT, 32], BF16)
        red(mg[:, :], lgA[:, :, 0:4], ALU.max, ["lgA"], ["mg"])
        tt("dve", d4[:, :, :], lgA[:, :, 0:4], bl(mg[:, :], 4), ALU.subtract, ["lgA", "mg"], ["d4"])
        act(e4[:, :, :], d4[:, :, :], AF.Exp, ["d4"], ["e4"])
        red(s4[:, :], e4[:, :, :], ALU.add, ["e4"], ["s4"])
        op("dve", lambda e: e.reciprocal(out=ptop[:, :], in_=s4[:, :]), reads=["s4"], writes=["ptop"], n=128)
        ts("dve", oh[:, :, :], d4[:, :, :], 0.0, None, ALU.is_equal, None, ["d4"], ["oh"])
        tt("dve", ohp[:, :, :], oh[:, :, :], bl(ptop[:, :], 4), ALU.mult, ["oh", "ptop"], ["ohp"])
        tt("dve", ls[:, :, :], lgA[:, :, 4:12], bl(oh[:, :, 0], 8), ALU.mult, ["lgA", "oh"], ["ls"])
        for g_ in range(1, 4):
            tt("dve", tmp8[:, :, :], lgA[:, :, 4 + 8 * g_:12 + 8 * g_], bl(oh[:, :, g_], 8), ALU.mult, ["lgA", "oh"], ["tmp8"])
            tt("dve", ls[:, :, :], ls[:, :, :], tmp8[:, :, :], ALU.add, ["ls", "tmp8"], ["ls"])
        red(m1[:, :], ls[:, :, :], ALU.max, ["ls"], ["m1"])
        tt("dve", eq1[:, :, :], ls[:, :, :], bl(m1[:, :], 8), ALU.is_equal, ["ls", "m1"], ["eq1"])
        stt(ls2[:, :, :], eq1[:, :, :], -1e30, ls[:, :, :], ALU.mult, ALU.add, ["eq1", "ls"], ["ls2"])
        red(m2[:, :], ls2[:, :, :], ALU.max, ["ls2"], ["m2"])
        tt("dve", eq2[:, :, :], ls2[:, :, :], bl(m2[:, :], 8), ALU.is_equal, ["ls2", "m2"], ["eq2"])
        tt("dve", dm[:, :], m2[:, :], m1[:, :], ALU.subtract, ["m1", "m2"], ["dm"])
        act(e2[:, :], dm[:, :], AF.Exp, ["dm"], ["e2"])
        ts("dve", den[:, :], e2[:, :], 1.0, None, ALU.add, None, ["e2"], ["den"])
        op("dve", lambda e: e.reciprocal(out=w1[:, :], in_=den[:, :]), reads=["den"], writes=["w1"], n=128)
        tt("dve", w2[:, :], e2[:, :], w1[:, :], ALU.mult, ["e2", "w1"], ["w2"])
        tt("dve", wg8[:, :, :], eq1[:, :, :], bl(w1[:, :], 8), ALU.mult, ["eq1", "w1"], ["wg8"])
        tt("dve", tmp8[:, :, :], eq2[:, :, :], bl(w2[:, :], 8), ALU.mult, ["eq2", "w2"], ["tmp8"])
        tt("dve", wg8[:, :, :], wg8[:, :, :], tmp8[:, :, :], ALU.add, ["wg8", "tmp8"], ["wg8"])
        for g_ in range(4):
            tt("dve", gate[:, :, g_ * 8:(g_ + 1) * 8], wg8[:, :, :], bl(ohp[:, :, g_], 8), ALU.mult, ["wg8", "ohp"], ["gate"])
        if debug:
            for t in range(NT):
                final_ops.append(dma("sp", dbg["gate"][t * 128:(t + 1) * 128, :], gate[:, t, :], reads=["gate"]))
        cp("dve", ghl[:, 0, :, :], gate[:, :, :], ["gate"], ["ghl0"])
        tt("dve", gtmp[:, :, :], gate[:, :, :], ghl[:, 0, :, :], ALU.subtract, ["gate", "ghl0"], ["gtmp"])
        cp("dve", ghl[:, 1, :, :], gtmp[:, :, :], ["gtmp"], ["ghl1"])
        for a_ in range(2):
            for half in range(2):
                bk = 4 + a_ * 2 + half
                pv = Pb[bk][:, :].rearrange("p (t n) -> p t n", t=8)
                for tq in range(8):
                    tr(pv[0:32, tq, :], ghl[:, a_, half * 8 + tq, :], ident[:, :], ["ghl%d" % a_, "ident"], [PN[bk]])
                cp("act" if half else "dve", gT[0:32, a_, half * 1024:(half + 1) * 1024], Pb[bk][0:32, :], [PN[bk]], ["gT"])
        checkpoint("F")

        EG = 2
        NEG = 32 // EG
        MX = SB_BASE + 256
        S.alias(["hid0", "hid1", "sil0", "sil1", "t1m0", "t1m1", "wdn0", "wdn1"], ["mixT_g", "mixT_m"])
        hid = [A("hid%d" % b, [128, EG, 2, 512], BF16, at=MX + b * 4096) for b in range(2)]
        sil = [A("sil%d" % b, [128, 512], F32, at=MX + 8192 + b * 2048) for b in range(2)]
        t1m = [A("t1m%d" % b, [128, 512], F32, at=MX + 12288 + b * 2048) for b in range(2)]
        wdn = [[A("wd%d_%d" % (b, j), [128, 2, D], BF16, at=MX + 16384 + (b * EG + j) * 4096) for j in range(EG)] for b in range(2)]
        wgt = [None, None]
        wup = [None, None]
        wgt[0] = [A("wg0_%d" % j, [128, 8, 256], BF16) for j in range(EG)]
        wup[0] = [A("wu0_%d" % j, [128, 8, 256], BF16) for j in range(EG)]
        wgt[1] = [A("wg1_%d" % j, [128, 8, 256], BF16, at=WO_OFF + j * 4096) for j in range(EG)]
        wup[1] = [A("wu1_%d" % j, [128, 8, 256], BF16, at=WO_OFF + 8192 + j * 4096) for j in range(EG)]
        itc = 0
        for eg in range(NEG):
            b = eg % 2
            extra = ["wo"] if b == 1 else []
            for j in range(EG):
                e_ = eg * EG + j
                dma("pool", wgt[b][j][:, :, :], weg_d.ap()[e_].rearrange("(c p) f -> p c f", p=128), writes=["wgt%d" % b] + extra)
                dma("pool", wup[b][j][:, :, :], weu_d.ap()[e_].rearrange("(c p) f -> p c f", p=128), writes=["wup%d" % b] + extra)
                dma("pool", wdn[b][j][:, :, :], wed_d.ap()[e_].rearrange("(c p) d -> p c d", p=128), writes=["wdn%d" % b])
            for tg in range(4):
                hbi = itc % 2
                itc += 1
                tgs = slice(tg * 512, (tg + 1) * 512)
                for j in range(EG):
                    e_ = eg * EG + j
                    gbk = 6 + j
                    for fh in range(2):
                        k2 = fh
                        gb_, ub_ = 0 + k2, 2 + k2
                        for kc in range(8):
                            mm(P[gb_][:, :], wgt[b][j][:, kc, fh * 128:(fh + 1) * 128], h2T[:, kc, tgs], kc == 0, kc == 7,
                               ["wgt%d" % b, "h2T_%d" % tg], [PN[gb_]])
                        for kc in range(8):
                            mm(P[ub_][:, :], wup[b][j][:, kc, fh * 128:(fh + 1) * 128], h2T[:, kc, tgs], kc == 0, kc == 7,
                               ["wup%d" % b, "h2T_%d" % tg], [PN[ub_]])
                        if fh == 0:
                            mm(P[gbk][:, :], sel[0:32, e_, :], gT[0:32, 0, tgs], True, False, ["sel", "gT"], [PN[gbk]])
                            mm(P[gbk][:, :], sel[0:32, e_, :], gT[0:32, 1, tgs], False, True, ["sel", "gT"], [PN[gbk]])
                        act(sil[k2][:, :], P[gb_][:, :], AF.Silu, [PN[gb_]], ["sil%d" % k2])
                        tt("dve", t1m[k2][:, :], sil[k2][:, :], P[ub_][:, :], ALU.mult, ["sil%d" % k2, PN[ub_]], ["t1m%d" % k2])
                        tt("dve", hid[hbi][:, j, fh, :], t1m[k2][:, :], P[gbk][:, :], ALU.mult, ["t1m%d" % k2, PN[gbk]],
                           ["hid%d" % hbi])
                for tt_ in range(4):
                    t = tg * 4 + tt_
                    for ch in range(2):
                        abk = 4 + (tt_ * 2 + ch) % 2
                        n_acc = EG * 2
                        a_i = 0
                        for j in range(EG):
                            for fh in range(2):
                                mm(P[abk][:, :], hid[hbi][:, j, fh, tt_ * 128:(tt_ + 1) * 128],
                                   wdn[b][j][:, fh, ch * 512:(ch + 1) * 512], a_i == 0, a_i == n_acc - 1,
                                   ["hid%d" % hbi, "wdn%d" % b], [PN[abk]])
                                a_i += 1
                        tt("dve", X[:, t, ch * 512:(ch + 1) * 512], X[:, t, ch * 512:(ch + 1) * 512], P[abk][:, :], ALU.add,
                           ["X%d" % t, PN[abk]], ["X%d" % t])
        for t in range(NT):
            final_ops.append(dma("sp", out_d[t * 128:(t + 1) * 128, :], X[:, t, :], reads=["X%d" % t]))

        S.emit(es, final_wait_ops=final_ops)
    return nc


def make_consts():
    ident = np.eye(128, dtype=np.float32).astype(ml_dtypes.bfloat16)
    j = np.arange(128)[:, None]
    i = np.arange(128)[None, :]
    same = (j // 64) == (i // 64)
    mf = (same & (j <= i)).astype(np.float32)
    mb = (same & (j > i)).astype(np.float32)
    masks = np.concatenate([mf, mb], axis=1).astype(ml_dtypes.bfloat16)
    invf = (10000.0 ** (-np.arange(0, 32, 2, dtype=np.float32) / 32)).astype(np.float32)
    invf = np.broadcast_to(invf[None, :], (128, 16)).copy()
    sel = np.zeros((32, 32, 128), np.float32)
    for e in range(32):
        sel[e, e, :] = 1.0
    sel = sel.reshape(32, 32 * 128).astype(ml_dtypes.bfloat16)
    lrb = np.zeros((64, 1), np.float32)
    lrb[16, 0] = 1.0
    return {"c_ident": ident, "c_masks": masks, "c_invf": invf, "c_sel": sel, "c_lrbias": lrb}


_NC_CACHE = {}


def make_in_maps(inputs, n_cores=8):
    c = make_consts()
    f = lambda k: np.ascontiguousarray(np.asarray(inputs[k], dtype=np.float32)[0])
    shared = {
        "norm1_gain": f("norm1_gain").reshape(1, D),
        "w_in": f("w_in"),
        "gla_gk_fwd_w": f("gla_gk_fwd_w"), "gla_gk_fwd_b": f("gla_gk_fwd_b").reshape(1, 256),
        "gla_gk_bwd_w": f("gla_gk_bwd_w"), "gla_gk_bwd_b": f("gla_gk_bwd_b").reshape(1, 256),
        "gla_out_gain": f("gla_out_gain").reshape(128, 1),
        "mla_q_gain": f("mla_q_gain").reshape(1, 256), "mla_w_qb": f("mla_w_qb"),
        "mla_kv_gain": f("mla_kv_gain").reshape(1, 128), "mla_w_kvb": f("mla_w_kvb"),
        "q_norm_gain": f("q_norm_gain").reshape(1, 96), "k_norm_gain": f("k_norm_gain").reshape(1, 96),
        "w_out": f("w_out"), "norm2_gain": f("norm2_gain").reshape(1, D),
        "w_router_group": f("w_router_group"), "b_router_group": f("b_router_group").reshape(1, 4),
        "w_router_expert": f("w_router_expert"), "b_router_expert": f("b_router_expert").reshape(1, 32),
        "w_expert_gate": f("w_expert_gate").reshape(32, D, 256),
        "w_expert_up": f("w_expert_up").reshape(32, D, 256),
        "w_expert_down": f("w_expert_down").reshape(32, 256, D),
    }
    shared.update(c)
    x = np.asarray(inputs["x"], dtype=np.float32)
    pos = np.asarray(inputs["positions"]).astype(np.int32)
    maps = []
    for b in range(n_cores):
        m = dict(shared)
        m["x"] = np.ascontiguousarray(x[b])
        m["pos"] = np.ascontiguousarray(pos[b].reshape(NT, 128).T)
        maps.append(m)
    return maps


def kernel(**inputs):
    if "nc" not in _NC_CACHE:
        _NC_CACHE["nc"] = build()
    nc = _NC_CACHE["nc"]
    maps = make_in_maps(inputs, 8)
    res = run_bass_kernel_spmd(nc, maps, core_ids=list(range(8)))
    out = np.stack([np.asarray(r["out"], dtype=np.float32) for r in res.results], axis=0)
    return out
```

```python
import contextlib
import math
import numpy as np
import ml_dtypes
import concourse.bass as bass
import concourse.mybir as mybir
from concourse.bass_utils import run_bass_kernel_spmd

F32 = mybir.dt.float32
BF16 = mybir.dt.bfloat16
I32 = mybir.dt.int32
ALU = mybir.AluOpType
AF = mybir.ActivationFunctionType
AX = mybir.AxisListType

S_LEN = 2048
D = 1024
NT = 16
EPS = 1e-6
PI = math.pi


class T:
    __slots__ = ("name", "w", "r")

    def __init__(self, name):
        self.name = name
        self.w = None
        self.r = []


class Op:
    __slots__ = ("eng", "fn", "deps", "signal", "sig", "dma", "dsem", "dval", "alld", "n", "seg", "idx", "nbytes", "tag")


class Sched:
    ENGS = ("pe", "act", "dve", "pool", "sp")

    def __init__(self, nc, n_dma_sems=12):
        self.nc = nc
        self.ops = {e: [] for e in self.ENGS}
        self.n_dma_sems = n_dma_sems
        self.dma_count = {e: 0 for e in self.ENGS}
        self.tiles = {}
        self.pending = {e: [] for e in self.ENGS}
        self.dma_since_barrier = []
        self.stopped = False
        self.seg = 0
        self.nops = 0
        self.noresched = set()

    def t(self, name):
        if name not in self.tiles:
            self.tiles[name] = T(name)
        return self.tiles[name]

    def _tl(self, lst):
        out = []
        for x in lst:
            if isinstance(x, str):
                out.append(self.t(x))
            elif isinstance(x, (list, tuple)):
                out.extend(self._tl(x))
            elif x is not None:
                out.append(x)
        return out

    def alias(self, new_names, old_names):
        if self.stopped:
            return
        for nn in new_names:
            tn = self.t(nn)
            for on in old_names:
                to = self.t(on)
                if to.w is not None:
                    tn.r.append(to.w)
                tn.r.extend(to.r)

    def barrier(self):
        if self.stopped:
            return
        lasts = []
        for e in self.ENGS:
            for o in reversed(self.ops[e]):
                if not o.dma:
                    lasts.append(o)
                    break
        lasts.extend(self.dma_since_barrier)
        self.dma_since_barrier = []
        for e in self.ENGS:
            self.pending[e] = list(lasts)
        self.seg += 1

    def op(self, eng, fn, reads=(), writes=(), dma=False, n=64, nbytes=0):
        if self.stopped:
            return None
        o = Op()
        o.n = n
        o.nbytes = nbytes
        o.seg = self.seg
        o.idx = self.nops
        self.nops += 1
        o.eng = eng
        o.fn = fn
        o.dma = dma
        o.signal = False
        o.sig = 0
        deps = {}
        reads = self._tl(reads)
        writes = self._tl(writes)
        for t in reads:
            if t.w is not None:
                deps[id(t.w)] = (t.w, "raw")
            if t.name[0] == "P" and t.name[1:].isdigit():
                for r in t.r:
                    if id(r) not in deps and r.eng != eng:
                        deps[id(r)] = (r, "war")
        for t in writes:
            if t.w is not None and id(t.w) not in deps:
                deps[id(t.w)] = (t.w, "waw")
            for r in t.r:
                if id(r) not in deps:
                    deps[id(r)] = (r, "war")
        if self.pending[eng]:
            for p in self.pending[eng]:
                deps[id(p)] = (p, "raw")
            self.pending[eng] = []
        o.alld = [p for p, _k in deps.values()]
        o.tag = ("R:" + ",".join(t.name for t in reads) + " W:" + ",".join(t.name for t in writes))
        dl = []
        for p, kind in deps.values():
            if p.eng == eng and not p.dma:
                if eng == "pe":
                    continue
                if kind != "raw" and not dma and not STRICT_SAME_ENGINE:
                    continue
            dl.append(p)
        o.deps = dl
        for p in dl:
            p.signal = True
        for t in reads:
            if not dma:
                for r in t.r:
                    if not r.dma and r.eng == eng:
                        o.alld.append(r)
                t.r = [r for r in t.r if r.dma or r.eng != eng]
            t.r.append(o)
        for t in writes:
            t.w = o
            t.r = []
        if dma:
            self.dma_count[eng] += 1
            self.dma_since_barrier.append(o)
        self.ops[eng].append(o)
        return o

    @staticmethod
    def _dur(o):
        n = o.n
        if o.dma:
            return 60.0 if o.eng == "sp" else 900.0
        if o.eng == "pe":
            return 30.0 + max(n, 64) / 2.0
        if o.eng == "act":
            return 220.0 + n / 1.4
        if o.eng == "dve":
            return 120.0 + n * 1.3
        return 550.0 + n * 0.75

    def reschedule(self):
        allops = []
        for e in self.ENGS:
            allops.extend(self.ops[e])
        allops.sort(key=lambda o: o.idx)
        import heapq
        new = {e: [] for e in self.ENGS}
        segs = {}
        for o in allops:
            segs.setdefault(o.seg, []).append(o)
        LAT = 250.0
        for sg in sorted(segs):
            ops = segs[sg]
            if sg in self.noresched:
                for o in ops:
                    new[o.eng].append(o)
                continue
            inseg = set(id(o) for o in ops)
            done = {}
            users = {}
            indeg = {}
            first = {}
            for o in ops:
                if o.eng not in first:
                    first[o.eng] = o
                elif first[o.eng] not in o.alld:
                    o.alld.append(first[o.eng])
            for o in ops:
                k = 0
                for p in o.alld:
                    if id(p) in inseg:
                        k += 1
                        users.setdefault(id(p), []).append(o)
                indeg[id(o)] = k
            ready = {e: [] for e in self.ENGS}
            efree = {e: 0.0 for e in self.ENGS}
            rtime = {}
            for o in ops:
                if indeg[id(o)] == 0:
                    rtime[id(o)] = 0.0
                    ready[o.eng].append(o)
            left = len(ops)
            SLACK = 0.0
            while left:
                best = None
                for e in self.ENGS:
                    rl = ready[e]
                    if not rl:
                        continue
                    ef = efree[e]
                    oldest = None
                    fill = None
                    for o in rl:
                        st = max(rtime[id(o)], ef)
                        if oldest is None or o.idx < oldest[1].idx:
                            oldest = (st, o)
                        if fill is None or (st, o.idx) < (fill[0], fill[1].idx):
                            fill = (st, o)
                    pick = oldest if oldest[0] <= fill[0] + SLACK else fill
                    if best is None or (pick[0], pick[1].idx) < (best[0], best[1].idx):
                        best = pick
                st, o = best
                e = o.eng
                ready[e].remove(o)
                d = self._dur(o)
                efree[e] = st + d
                fin = st + d
                if o.dma:
                    fin = st + 2000.0 + o.nbytes / 150.0
                done[id(o)] = fin
                new[e].append(o)
                left -= 1
                for u in users.get(id(o), ()):
                    indeg[id(u)] -= 1
                    lat = 0.0 if (u.eng == o.eng and not o.dma) else LAT
                    rtime[id(u)] = max(rtime.get(id(u), 0.0), fin + lat)
                    if indeg[id(u)] == 0:
                        ready[u.eng].append(u)
        self.ops = new

    def emit(self, es, final_wait_ops=()):
        nc = self.nc
        if RESCHEDULE:
            self.reschedule()
        sems = {e: es.enter_context(nc.semaphore("s_" + e)) for e in self.ENGS}
        dsems = {e: [es.enter_context(nc.semaphore("d_%s_%d" % (e, i)))
                     for i in range(self.n_dma_sems)]
                 for e in self.ENGS if self.dma_count[e] > 0}
        for e in self.ENGS:
            c = 0
            i = 0
            for o in self.ops[e]:
                if o.dma:
                    o.dsem = i % self.n_dma_sems
                    o.dval = 16 * (i // self.n_dma_sems + 1)
                    i += 1
                elif o.signal:
                    c += 1
                    o.sig = c
        block = es.enter_context(nc.Block())
        eng_obj = {"pe": block.tensor, "act": block.scalar, "dve": block.vector,
                   "pool": block.gpsimd, "sp": block.sync}
        for e in self.ENGS:
            ops = self.ops[e]
            if not ops:
                continue

            def body(engine, e=e, ops=ops):
                waited = {}

                def wait(sem, key, val):
                    if waited.get(key, 0) >= val:
                        return
                    waited[key] = val
                    engine.wait_ge(sem, val)

                for o in ops:
                    for p in o.deps:
                        if p.dma:
                            wait(dsems[p.eng][p.dsem], ("d", p.eng, p.dsem), p.dval)
                        else:
                            wait(sems[p.eng], ("c", p.eng), p.sig)
                    if o.dma and o.dval > 16:
                        wait(dsems[e][o.dsem], ("d", e, o.dsem), o.dval - 16)
                    ins = o.fn(engine)
                    if o.dma:
                        ins.then_inc(dsems[e][o.dsem], 16)
                    elif o.signal:
                        ins.then_inc(sems[e], 1)
                if e == "sp":
                    for o in final_wait_ops:
                        if o is None:
                            continue
                        wait(dsems[o.eng][o.dsem], ("d", o.eng, o.dsem), o.dval)

            eng_obj[e](body)


RESCHEDULE = True
STRICT_SAME_ENGINE = True
SB_BASE = 16640
SB_END = 229376


class Alloc:
    def __init__(self, nc):
        self.nc = nc
        self.off = SB_BASE
        self.n = 0

    def mark(self):
        return self.off

    def reset(self, m):
        self.off = m

    def __call__(self, name, shape, dt, at=None):
        esz = 2 if dt == BF16 else 4
        nb = int(np.prod(shape[1:])) * esz
        nb = (nb + 63) // 64 * 64
        self.n += 1
        if at is None:
            at = self.off
            self.off += nb
        assert at + nb <= SB_END, ("SBUF overflow", name, at, nb)
        return self.nc.alloc_sbuf_tensor_at("%s_%d" % (name, self.n), list(shape), dt, offset=at)


def bcast_ap(ap, pattern):
    return bass.AP(ap.tensor, ap.offset, [list(ap.ap[0])] + [list(p) for p in pattern])


def build(debug=False, stop_after=None):
    nc = bass.Bass("TRN2", target_bir_lowering=False)
    dr = lambda n, s, dt=F32: nc.dram_tensor(n, list(s), dt, kind="ExternalInput")
    x_d = dr("x", [S_LEN, D])
    pos_d = dr("pos", [128, NT], I32)
    g1_d = dr("norm1_gain", [1, D])
    win_d = dr("w_in", [D, 1984])
    gkf_w = dr("gla_gk_fwd_w", [16, 256])
    gkf_b = dr("gla_gk_fwd_b", [1, 256])
    gkb_w = dr("gla_gk_bwd_w", [16, 256])
    gkb_b = dr("gla_gk_bwd_b", [1, 256])
    go_d = dr("gla_out_gain", [128, 1])
    gqa_d = dr("mla_q_gain", [1, 256])
    wqb_d = dr("mla_w_qb", [256, 768])
    gkva_d = dr("mla_kv_gain", [1, 128])
    wkvb_d = dr("mla_w_kvb", [128, 1024])
    gqn_d = dr("q_norm_gain", [1, 96])
    gkn_d = dr("k_norm_gain", [1, 96])
    wout_d = dr("w_out", [D, D])
    g2_d = dr("norm2_gain", [1, D])
    wrg_d = dr("w_router_group", [D, 4])
    brg_d = dr("b_router_group", [1, 4])
    wre_d = dr("w_router_expert", [D, 32])
    bre_d = dr("b_router_expert", [1, 32])
    weg_d = dr("w_expert_gate", [32, D, 256])
    weu_d = dr("w_expert_up", [32, D, 256])
    wed_d = dr("w_expert_down", [32, 256, D])
    ident_d = dr("c_ident", [128, 128], BF16)
    masks_d = dr("c_masks", [128, 256], BF16)
    invf_d = dr("c_invf", [128, 16])
    sel_d = dr("c_sel", [32, 32 * 128], BF16)
    lrb_d = dr("c_lrbias", [64, 1])
    out_d = nc.dram_tensor("out", [S_LEN, D], F32, kind="ExternalOutput")
    gdr = nc.dram_tensor("gate_scratch", [32, S_LEN], F32, kind="Internal")
    dbg = {}
    if debug:
        dbg["mix"] = nc.dram_tensor("d_mix", [128, 8 * S_LEN], BF16, kind="ExternalOutput")
        dbg["x1"] = nc.dram_tensor("d_x1", [S_LEN, D], F32, kind="ExternalOutput")
        dbg["gate"] = nc.dram_tensor("d_gate", [S_LEN, 32], F32, kind="ExternalOutput")

    if debug:
        dbg["gen"] = nc.dram_tensor("d_gen", [128, 8 * S_LEN], BF16, kind="ExternalOutput")
    S = Sched(nc)
    A = Alloc(nc)
    op = S.op
    final_ops = []

    with contextlib.ExitStack() as es:
        P = [es.enter_context(nc.psum_tensor("pb%d" % i, [128, 512], F32)) for i in range(8)]
        Pb = [p.bitcast(BF16) for p in P]
        PN = ["P%d" % i for i in range(8)]

        def fsz(ap):
            r = 1
            for d_ in list(ap.shape)[1:]:
                r *= int(d_)
            return r

        def dma(q, out, in_, reads=(), writes=(), **kw):
            return op(q, lambda e: e.dma_start(out=out, in_=in_, **kw), reads=reads, writes=writes, dma=True,
                      nbytes=fsz(out) * 4 * 128)

        def act(out, in_, func, reads, writes, **kw):
            return op("act", lambda e: e.activation(out=out, in_=in_, func=func, **kw), reads=reads, writes=writes,
                      n=fsz(out))

        def rsqrt_act(out, in_, n, reads, writes):
            act(out, in_, AF.Ln, reads, writes, scale=1.0 / n, bias=EPS)
            act(out, out, AF.Exp, writes, writes, scale=-0.5)

        def tt(eng, out, in0, in1, o, reads, writes):
            return op(eng, lambda e: e.tensor_tensor(out=out, in0=in0, in1=in1, op=o), reads=reads, writes=writes,
                      n=fsz(out))

        def ts(eng, out, in0, s1, s2, o0, o1, reads, writes):
            if o1 is None:
                return op(eng, lambda e: e.tensor_scalar(out=out, in0=in0, scalar1=s1, scalar2=None, op0=o0),
                          reads=reads, writes=writes, n=fsz(out))
            return op(eng, lambda e: e.tensor_scalar(out=out, in0=in0, scalar1=s1, scalar2=s2, op0=o0, op1=o1),
                      reads=reads, writes=writes, n=fsz(out))

        def stt(out, in0, sc, in1, o0, o1, reads, writes):
            return op("dve", lambda e: e.scalar_tensor_tensor(out=out, in0=in0, scalar=sc, in1=in1, op0=o0, op1=o1),
                      reads=reads, writes=writes, n=fsz(out))

        def mm(out, lhsT, rhs, start, stop, reads, writes):
            return op("pe", lambda e: e.matmul(out, lhsT=lhsT, rhs=rhs, start=start, stop=stop),
                      reads=reads, writes=writes, n=fsz(rhs) * (4 if rhs.dtype == F32 else 1))

        def tr(out, in_, ident, reads, writes):
            return op("pe", lambda e: e.transpose(out=out, in_=in_, identity=ident), reads=reads, writes=writes, n=128)

        def cp(eng, out, in_, reads, writes):
            if eng == "act":
                return act(out, in_, AF.Copy, reads, writes)
            return op(eng, lambda e: e.tensor_copy(out=out, in_=in_), reads=reads, writes=writes, n=fsz(out))

        def memset(eng, ap, val, writes):
            return op(eng, lambda e: e.memset(ap, val), writes=writes, n=fsz(ap))

        def bcast_row(dram, n):
            return bass.AP(dram, 0, [[0, 128], [1, n]])

        ident = A("ident", [128, 128], BF16)
        mixT = A("mixT", [128, 8, S_LEN], BF16)
        dma("sp", ident[:, :], ident_d[:, :], writes=["ident"])
        L0 = A.mark()

        hT = A("hT", [128, 8, S_LEN], BF16)
        E1 = A.mark()
        g1 = A("g1", [128, D], F32)
        xt = [A("xt%d" % i, [128, D], F32) for i in range(2)]
        hb = [A("hb%d" % i, [128, D], BF16) for i in range(2)]
        sqj = A("sqj", [128, D], F32)
        st1 = A("st1", [128, 4], F32)
        dma("sp", g1[:, :], bcast_row(g1_d, D), writes=["g1"])

        def norm_to_T(src_ap_fn, src_tiles, gain, gname, dstT, dname, pfx, t, pbank):
            i = t % 2
            ssq = st1[:, 0:1]
            rs = st1[:, 1:2]
            act(sqj[:, :], src_ap_fn(t), AF.Square, src_tiles, [pfx + "sqj", pfx + "ssq"], accum_out=ssq)
            rsqrt_act(rs, ssq, D, [pfx + "ssq"], [pfx + "rs"])
            stt(hb[i][:, :], src_ap_fn(t), rs, gain[:, :], ALU.mult, ALU.mult,
                src_tiles + [pfx + "rs", gname], [pfx + "hb%d" % i])
            pbv = Pb[pbank][:, :].rearrange("p (c n) -> p c n", c=8)
            for kc in range(8):
                tr(pbv[:, kc, :], hb[i][:, kc * 128:(kc + 1) * 128], ident[:, :],
                   [pfx + "hb%d" % i, "ident"], [PN[pbank]])
            cp("dve" if t % 2 else "act", dstT[:, :, t * 128:(t + 1) * 128], pbv, [PN[pbank]], [dname + "_%d" % (t // 4)])

        for t in range(NT):
            i = t % 2
            dma("sp", xt[i][:, :], x_d[t * 128:(t + 1) * 128, :], writes=["xt%d" % i])
            norm_to_T(lambda t, i=i: xt[i][:, :], ["xt%d" % i], g1, "g1", hT, "hT", "A", t, t % 2)
        hT_tiles = ["hT_%d" % k for k in range(4)]
        S.barrier()
        A.reset(E1)
        def checkpoint(name, dump=None, reads=()):
            if stop_after == name:
                if dump is not None and debug:
                    S.barrier()
                    final_ops.append(dma("sp", dbg["gen"][:, :], dump, reads=list(reads)))
                S.stopped = True

        checkpoint("A")

        wm = A("w_in_mla", [128, 8, 416], BF16)
        wqb = A("wqb", [128, 2, 768], BF16)
        wkvb = A("wkvb", [128, 1024], BF16)
        cs = A("cs", [128, NT, 64], F32)
        qhT = A("qhT", [128, 8, S_LEN], BF16)
        khT = A("khT", [128, 8, S_LEN], BF16)
        vA = A("vA", [128, NT, 8, 128], BF16)
        gqa = A("gqa", [128, 384], F32)
        gqk = A("gqkr", [128, 16, 32], F32)
        gcol = A("gcol", [128, 2], F32)
        B1m = A.mark()
        dma("pool", wm[:, :, :], win_d.ap()[:, 1568:1984].rearrange("(c p) n -> p c n", p=128), writes=["wm"])
        dma("pool", wqb[:, :, :], wqb_d.ap().rearrange("(c p) n -> p c n", p=128), writes=["wqb"])
        dma("pool", wkvb[:, :], wkvb_d[:, :], writes=["wkvb"])
        dma("sp", gqa[:, 0:256], bcast_row(gqa_d, 256), writes=["gqa"])
        dma("sp", gqa[:, 256:384], bcast_row(gkva_d, 128), writes=["gqa"])
        dma("sp", gqk[:, 0:8, :], bass.AP(gqn_d, 64, [[0, 128], [0, 8], [1, 32]]), writes=["gqk"])
        dma("sp", gqk[:, 8:16, :], bass.AP(gkn_d, 64, [[0, 128], [0, 8], [1, 32]]), writes=["gqk"])
        memset("pool", gcol[:, :], 1.0, ["gcol"])
        dma("sp", gcol[0:64, 0:1], bass.AP(gqn_d, 0, [[1, 64], [1, 1]]), reads=["gcol"], writes=["gcol"])
        dma("sp", gcol[0:64, 1:2], bass.AP(gkn_d, 0, [[1, 64], [1, 1]]), reads=["gcol"], writes=["gcol"])
        posi = A("posi", [128, NT], I32)
        posf = A("posf", [128, NT], F32)
        invf = A("invf", [128, 16], F32)
        ang = A("ang", [128, NT, 16], F32)
        kk = A("kk", [128, NT, 16], F32)
        ki = A("ki", [128, NT, 16], I32)
        rr = A("rr", [128, NT, 16], F32)
        yy = A("yy", [128, NT, 16], F32)
        m_ = A("m_", [128, NT, 16], F32)
        dma("sp", posi[:, :], pos_d[:, :], writes=["posi"])
        dma("sp", invf[:, :], invf_d[:, :], writes=["invf"])
        cp("dve", posf[:, :], posi[:, :], ["posi"], ["posf"])
        for t in range(NT):
            ts("dve", ang[:, t, :], invf[:, :], posf[:, t:t + 1], None, ALU.mult, None, ["invf", "posf"], ["ang"])
        ts("dve", kk[:, :, :], ang[:, :, :], 1.0 / (2 * PI), None, ALU.mult, None, ["ang"], ["kk"])
        cp("dve", ki[:, :, :], kk[:, :, :], ["kk"], ["ki"])
        cp("dve", kk[:, :, :], ki[:, :, :], ["ki"], ["kk"])
        C1 = 6.28125
        C2 = 2 * PI - C1
        stt(rr[:, :, :], kk[:, :, :], -C1, ang[:, :, :], ALU.mult, ALU.add, ["kk", "ang"], ["rr"])
        stt(rr[:, :, :], kk[:, :, :], -C2, rr[:, :, :], ALU.mult, ALU.add, ["kk", "rr"], ["rr"])
        for which, shift in ((1, 0.0), (0, PI / 2)):
            ts("dve", yy[:, :, :], rr[:, :, :], shift, None, ALU.add, None, ["rr"], ["yy"])
            ts("dve", m_[:, :, :], yy[:, :, :], PI, None, ALU.is_gt, None, ["yy"], ["m_"])
            stt(yy[:, :, :], m_[:, :, :], -2 * PI, yy[:, :, :], ALU.mult, ALU.add, ["m_", "yy"], ["yy"])
            ts("dve", m_[:, :, :], yy[:, :, :], -PI, None, ALU.is_lt, None, ["yy"], ["m_"])
            stt(yy[:, :, :], m_[:, :, :], 2 * PI, yy[:, :, :], ALU.mult, ALU.add, ["m_", "yy"], ["yy"])
            ts("dve", yy[:, :, :], yy[:, :, :], PI, -PI, ALU.min, ALU.max, ["yy"], ["yy"])
            if which == 0:
                act(cs[:, :, 0:16], yy[:, :, :], AF.Sin, ["yy"], ["cs"])
                act(cs[:, :, 16:32], yy[:, :, :], AF.Sin, ["yy"], ["cs"])
            else:
                act(cs[:, :, 48:64], yy[:, :, :], AF.Sin, ["yy"], ["cs"])
                act(cs[:, :, 32:48], cs[:, :, 48:64], AF.Copy, ["cs"], ["cs"], scale=-1.0)
        memset("pool", vA[:, :, :, :], 1.0, ["vA"])
        S.barrier()
        A.reset(B1m)

        sqjb1 = A("sqjb", [128, 416], BF16)
        sqjb = [sqjb1, sqjb1]
        stq = [A("stq%d" % i, [128, 32], F32) for i in range(2)]
        ab = [A("ab%d" % i, [128, 384], BF16) for i in range(2)]
        abT = [A("abT%d" % i, [128, 3, 128], BF16) for i in range(2)]
        kraw = [A("kraw%d" % i, [128, 8, 96], F32) for i in range(2)]
        sqn = A("sqn", [128, 16, 96], BF16)
        rg = [A("rg%d" % i, [128, 16, 32], F32) for i in range(2)]
        rg2 = A("rg2", [128, 16, 48], F32)
        rb = A("rb", [128, 16, 32], F32)
        qkf = [A("qkf%d" % i, [128, 16, 96], BF16) for i in range(2)]
        SQ2 = math.sqrt(2.0)

        def st_E1a(t):
            i = t % 2
            sI = "_%d" % i
            tsl = slice(t * 128, (t + 1) * 128)
            hTt = "hT_%d" % (t // 4)
            for kc in range(8):
                mm(P[0][:, 0:416], hT[:, kc, tsl], wm[:, kc, :], kc == 0, kc == 7, [hTt, "wm"], ["P0"])
            act(sqjb[i][:, 0:256], P[0][:, 0:256], AF.Square, ["P0"], ["stqA" + sI], accum_out=stq[i][:, 0:1])
            act(sqjb[i][:, 256:384], P[0][:, 256:384], AF.Square, ["P0"], ["stqA" + sI],
                accum_out=stq[i][:, 1:2], scale=SQ2)
            act(kraw[i][:, :, 64:96], bcast_ap(P[0][:, 384:416], [[0, 8], [1, 32]]), AF.Copy, ["P0"], ["krawR" + sI])
            rsqrt_act(stq[i][:, 2:4], stq[i][:, 0:2], 256, ["stqA" + sI], ["stqB" + sI])
            stt(ab[i][:, 0:256], P[0][:, 0:256], stq[i][:, 2:3], gqa[:, 0:256], ALU.mult, ALU.mult,
                ["P0", "stqB" + sI, "gqa"], ["ab" + sI])
            stt(ab[i][:, 256:384], P[0][:, 256:384], stq[i][:, 3:4], gqa[:, 256:384], ALU.mult, ALU.mult,
                ["P0", "stqB" + sI, "gqa"], ["ab" + sI])
        def st_E1b(t):
            i = t % 2
            sI = "_%d" % i
            tsl = slice(t * 128, (t + 1) * 128)
            hTt = "hT_%d" % (t // 4)
            p1v = Pb[1][:, 0:384].rearrange("p (c n) -> p c n", c=3)
            for c in range(3):
                tr(p1v[:, c, :], ab[i][:, c * 128:(c + 1) * 128], ident[:, :], ["ab" + sI, "ident"], ["P1"])
            cp("act", abT[i][:, :, :], p1v, ["P1"], ["abT" + sI])
        def st_E2(t):
            i = t % 2
            sI = "_%d" % i
            tsl = slice(t * 128, (t + 1) * 128)
            hTt = "hT_%d" % (t // 4)
            for nb in range(2):
                for kc in range(2):
                    mm(P[2 + nb][:, 0:384], abT[i][:, kc, :], wqb[:, kc, nb * 384:(nb + 1) * 384], kc == 0, kc == 1,
                       ["abT" + sI, "wqb"], [PN[2 + nb]])
                mm(P[4 + nb][:, :], abT[i][:, 2, :], wkvb[:, nb * 512:(nb + 1) * 512], True, True,
                   ["abT" + sI, "wkvb"], [PN[4 + nb]])
            for nb in range(2):
                srck = P[4 + nb][:, :].rearrange("p (h d) -> p h d", h=4)[:, :, 0:64]
                cp("act", kraw[i][:, nb * 4:nb * 4 + 4, 0:64], srck, [PN[4 + nb]], ["krawN" + sI])
                srcv = P[4 + nb][:, :].rearrange("p (a b d) -> p a b d", a=2, b=2)
                dstv = vA[:, t, nb * 4:nb * 4 + 4, :].rearrange("p (a b) d -> p a b d", b=2)
                cp("act", dstv[:, :, 0, 0:64], srcv[:, :, 0, 64:128], [PN[4 + nb]], ["vA"])
                cp("act", dstv[:, :, 1, 64:128], srcv[:, :, 1, 64:128], [PN[4 + nb]], ["vA"])
            for nb in range(2):
                act(sqn[:, nb * 4:nb * 4 + 4, :], P[2 + nb][:, 0:384].rearrange("p (h d) -> p h d", h=4), AF.Square,
                    [PN[2 + nb]], ["sqn"])
            act(sqn[:, 8:16, :], kraw[i][:, :, :], AF.Square, ["krawN" + sI, "krawR" + sI], ["sqn"])
            op("dve", lambda e, i=i: e.tensor_reduce(out=stq[i][:, 8:24], in_=sqn[:, :, :], axis=AX.X, op=ALU.add),
               reads=["sqn"], writes=["stqC" + sI])
            rsqrt_act(stq[i][:, 8:24], stq[i][:, 8:24], 96, ["stqC" + sI], ["stqC" + sI])
            for nb in range(2):
                pv = P[2 + nb][:, 0:384].rearrange("p (h d) -> p h d", h=4)
                rq = stq[i][:, 8 + nb * 4:9 + nb * 4]
                tt("dve", qkf[i][:, nb * 4:nb * 4 + 4, 0:64], pv[:, :, 0:64], bcast_ap(rq, [[1, 4], [0, 64]]), ALU.mult,
                   [PN[2 + nb], "stqC" + sI], ["qkf" + sI])
                tt("dve", rg[i][:, nb * 4:nb * 4 + 4, :], pv[:, :, 64:96], bcast_ap(rq, [[1, 4], [0, 32]]), ALU.mult,
                   [PN[2 + nb], "stqC" + sI], ["rg" + sI])
            rk = stq[i][:, 16:17]
            tt("dve", qkf[i][:, 8:16, 0:64], kraw[i][:, :, 0:64], bcast_ap(rk, [[1, 8], [0, 64]]), ALU.mult,
               ["krawN" + sI, "stqC" + sI], ["qkf" + sI])
            tt("dve", rg[i][:, 8:16, :], kraw[i][:, :, 64:96], bcast_ap(rk, [[1, 8], [0, 32]]), ALU.mult,
               ["krawR" + sI, "stqC" + sI], ["rg" + sI])
        def st_L(t):
            i = t % 2
            sI = "_%d" % i
            tsl = slice(t * 128, (t + 1) * 128)
            hTt = "hT_%d" % (t // 4)
            tt("pool", rg2[:, :, 0:32], rg[i][:, :, :], gqk[:, :, :], ALU.mult, ["rg" + sI, "gqk"], ["rg2"])
            tt("pool", rg2[:, :, 32:48], rg[i][:, :, 0:16], gqk[:, :, 0:16], ALU.mult, ["rg" + sI, "gqk"], ["rg2"])
            c1 = bcast_ap(cs[:, t, 0:32], [[0, 16], [1, 32]])
            c2 = bcast_ap(cs[:, t, 32:64], [[0, 16], [1, 32]])
            tt("pool", rg[i][:, :, :], rg2[:, :, 0:32], c1, ALU.mult, ["rg2", "cs"], ["rg" + sI])
            tt("pool", rb[:, :, :], rg2[:, :, 16:48], c2, ALU.mult, ["rg2", "cs"], ["rb"])
            tt("pool", qkf[i][:, :, 64:96], rg[i][:, :, :], rb[:, :, :], ALU.add, ["rg" + sI, "rb"], ["qkf" + sI])
            p6v = Pb[6][:, :].rearrange("p (h n) -> p h n", h=8)
            p7v = Pb[7][:, :].rearrange("p (h n) -> p h n", h=8)
            for h in range(8):
                tr(p6v[0:96, h, :], qkf[i][:, h, :], ident[:, :], ["qkf" + sI, "ident"], ["P6"])
            for h in range(8):
                tr(p7v[0:96, h, :], qkf[i][:, 8 + h, :], ident[:, :], ["qkf" + sI, "ident"], ["P7"])
            ts("dve", qhT[0:96, :, tsl], p6v[0:96, :, :], gcol[0:96, 0:1], None, ALU.mult, None, ["P6", "gcol"],
               ["qhT_%d" % (t // 4)])
            act(khT[0:96, :, tsl], p7v[0:96, :, :], AF.Identity, ["P7", "gcol"], ["khT"], scale=gcol[0:96, 1:2])

        S.noresched.add(S.seg)
        for step in range(NT + 2):
            if step < NT:
                st_E1a(step)
            if 0 <= step - 1 < NT:
                st_E2(step - 1)
            if 0 <= step - 2 < NT:
                st_L(step - 2)
            if step < NT:
                st_E1b(step)
        S.barrier()
        A.reset(B1m)
        checkpoint("B1")
        pbuf = [A("pbuf%d" % i, [128, 512], BF16) for i in range(4)]
        lnb = A("lnb", [128, 512], F32)
        rcb = A("rcb", [128, 512], F32)
        scale = 96 ** -0.5
        it = 0
        for h in range(8):
            even = (h % 2 == 0)
            vrows = slice(0, 64) if even else slice(64, 128)
            srows = slice(64, 128) if even else slice(0, 64)
            for qg in range(4):
                qsl = slice(qg * 512, (qg + 1) * 512)
                ob = 4 + (it % 2)
                seq = []
                for kt in range(16):
                    seq.append(("s", kt))
                    if kt >= 2:
                        seq.append(("pv", kt - 2))
                seq += [("pv", 14), ("pv", 15)]
                for kind, kt in seq:
                    sb_ = kt % 3
                    pi = kt % 4
                    if kind == "s":
                        mm(P[sb_][:, :], khT[0:96, h, kt * 128:(kt + 1) * 128], qhT[0:96, h, qsl], True, True,
                           ["khT", "qhT_%d" % qg], [PN[sb_]])
                        act(pbuf[pi][:, :], P[sb_][:, :], AF.Exp, [PN[sb_]], ["pbuf%d" % pi], scale=scale)
                    else:
                        lhsT = vA[:, kt, h, :]
                        mm(P[ob][:, :], lhsT, pbuf[pi][:, :], kt == 0, kt == 15, ["vA", "pbuf%d" % pi], [PN[ob]])
                op("dve", lambda e, vrows=vrows, srows=srows, ob=ob: e.reciprocal(out=rcb[vrows, :], in_=P[ob][srows, :]),
                   reads=[PN[ob]], writes=["rcb"], n=4096)
                tt("dve", mixT[vrows, 4 + h // 2, qsl], P[ob][vrows, :], rcb[vrows, :], ALU.mult, [PN[ob], "rcb"], ["mixT_m"])
                it += 1
        S.barrier()
        A.reset(E1)

        checkpoint("C")
        wg = A("w_in_gla", [128, 8, 1568], BF16)
        R2 = A.mark()
        qkT = A("qkT", [128, 4, S_LEN], F32)
        vtok = A("vtok", [128, NT, 512], BF16)
        sgT = A("sgT", [128, 4, S_LEN], BF16)
        lrT = A("lrT", [64, S_LEN], F32)
        wlr = A("wlr", [128, 8, 64], BF16)
        lrb = A("lrb", [64, 1], F32)
        waug = A("waug", [64, 512], F32)
        masks = A("masks", [128, 256], BF16)
        gout = A("gout", [128, 1], F32)
        onesf = A("onesf", [128, 128], F32)
        R4 = A.mark()
        dma("pool", wg[:, :, :], win_d.ap()[:, 0:1568].rearrange("(c p) n -> p c n", p=128), writes=["wg"])
        memset("pool", wlr[:, :, :], 0.0, ["wlr"])
        dma("pool", wlr[:, :, 0:16], win_d.ap()[:, 1536:1552].rearrange("(c p) n -> p c n", p=128), reads=["wlr"], writes=["wlr"])
        dma("pool", wlr[:, :, 32:48], win_d.ap()[:, 1552:1568].rearrange("(c p) n -> p c n", p=128), reads=["wlr"], writes=["wlr"])
        dma("sp", lrb[:, :], lrb_d[:, :], writes=["lrb"])
        memset("pool", waug[:, :], 0.0, ["waug"])
        dma("sp", waug[0:16, 0:256], gkf_w[:, :], reads=["waug"], writes=["waug"])
        dma("sp", waug[16:17, 0:256], gkf_b[:, :], reads=["waug"], writes=["waug"])
        dma("sp", waug[16:17, 256:512], gkb_b[:, :], reads=["waug"], writes=["waug"])
        dma("sp", waug[32:48, 256:512], gkb_w[:, :], reads=["waug"], writes=["waug"])
        dma("sp", masks[:, :], masks_d[:, :], writes=["masks"])
        dma("sp", gout[:, :], go_d[:, :], writes=["gout"])
        memset("pool", onesf[:, :], 1.0, ["onesf"])
        blk = 0
        for kind, idx in [("q", 0), ("q", 1), ("k", 0), ("k", 1), ("g", 0), ("g", 1), ("g", 2), ("g", 3), ("lr", 0)]:
            for tg in range(4):
                pbk = blk % 4
                blk += 1
                tgs = slice(tg * 512, (tg + 1) * 512)
                for kc in range(8):
                    if kind == "q":
                        lhsT = wg[:, kc, idx * 128:(idx + 1) * 128]
                    elif kind == "k":
                        lhsT = wg[:, kc, 256 + idx * 128:256 + (idx + 1) * 128]
                    elif kind == "g":
                        lhsT = wg[:, kc, 1024 + idx * 128:1024 + (idx + 1) * 128]
                    else:
                        lhsT = wlr[:, kc, :]
                    mrows = 64 if kind == "lr" else 128
                    mm(P[pbk][0:mrows, :], lhsT, hT[:, kc, tgs], kc == 0, kc == 7, ["hT_%d" % tg, "wg", "wlr"], [PN[pbk]])
                if kind == "q":
                    act(qkT[:, idx, tgs], P[pbk][:, :], AF.Copy, [PN[pbk]], ["qT"], scale=0.125)
                elif kind == "k":
                    cp("dve", qkT[:, 2 + idx, tgs], P[pbk][:, :], [PN[pbk]], ["kT"])
                elif kind == "g":
                    act(sgT[:, idx, tgs], P[pbk][:, :], AF.Silu, [PN[pbk]], ["sgT"])
                else:
                    act(lrT[:, tgs], P[pbk][0:64, :], AF.Identity, [PN[pbk], "lrb"], ["lrT"], bias=lrb[:, :])
        for t in range(NT):
            pbk = 4 + t % 2
            tsl = slice(t * 128, (t + 1) * 128)
            for kc in range(8):
                mm(P[pbk][:, :], hT[:, kc, tsl], wg[:, kc, 512:1024], kc == 0, kc == 7, ["hT_%d" % (t // 4), "wg"], [PN[pbk]])
            cp("dve" if t % 2 else "act", vtok[:, t, :], P[pbk][:, :], [PN[pbk]], ["vtok"])
        S.barrier()

        checkpoint("B2")
        HTB = L0
        tmp = [A("gt%d" % i, [128, 1024], F32, at=HTB + i * 4096) for i in range(4)]
        prod = {}
        names = [(d_, hp, k_) for d_ in (0, 1) for hp in (0, 1) for k_ in ("qr", "kr", "qb")]
        slots = [HTB + 16384 + i * 4096 for i in range(4)] + [E1 + 16384 + i * 4096 for i in range(2)]
        for i, nm in enumerate(names):
            if i < 6:
                prod[nm] = A("pr", [128, S_LEN], BF16, at=slots[i])
            else:
                prod[nm] = A("pr", [128, S_LEN], BF16)
        kdT = A("kdT", [128, 1024], BF16)
        dec = A("dec", [128, 4, 32], F32)
        smask = A("smask", [128, 1024], F32)
        kd = A("kd", [128, NT, 512], BF16, at=E1)
        memset("pool", smask[:, :], 1.0, ["smask"])
        memset("pool", smask[:, :].rearrange("p (c j) -> p c j", j=64)[:, :, 0:1], 0.0, ["smask"])
        G, Fc, Dt, Eb = tmp
        Dt2 = A("Dt2", [128, 1024], F32)
        Eb2 = A("Eb2", [128, 1024], F32)
        DtL = [(Dt, "Dt"), (Dt2, "Dt2")]
        EbL = [(Eb, "Eb"), (Eb2, "Eb2")]
        cnt = {"d": 0, "e": 0}

        def nextD():
            cnt["d"] += 1
            return DtL[cnt["d"] % 2]

        def nextE():
            cnt["e"] += 1
            return EbL[cnt["e"] % 2]

        def exp_prod(src, sname, scl, dst, base, bname, dname="prod"):
            E_, en = nextE()
            act(E_[:, :], src, AF.Exp, [sname], [en], scale=scl)
            tt("pool", dst, base, E_[:, :], ALU.mult, [bname, en], [dname])
        for d_ in (0, 1):
            for hp in (0, 1):
                dh = d_ * 2 + hp
                qT = qkT[:, hp, :]
                kT = qkT[:, 2 + hp, :]
                for half in range(2):
                    hs = slice(half * 1024, (half + 1) * 1024)
                    for j in range(2):
                        pbk = j
                        cols = slice(half * 1024 + j * 512, half * 1024 + (j + 1) * 512)
                        mm(P[pbk][:, :], waug[0:64, dh * 128:(dh + 1) * 128], lrT[0:64, cols], True, True,
                           ["waug", "lrT"], [PN[pbk]])
                        act(Eb[:, j * 512:(j + 1) * 512], P[pbk][:, :], AF.Exp, [PN[pbk]], ["Eb"], scale=-1.0)
                    act(G[:, :], Eb[:, :], AF.Ln, ["Eb"], ["G"], bias=1.0)
                    op("dve", lambda e: e.tensor_tensor_scan(out=Fc[:, :], data0=smask[:, :], data1=G[:, :], initial=0.0,
                                                             op0=ALU.mult, op1=ALU.add), reads=["smask", "G"], writes=["Fc"])
                    Fv = Fc[:, :].rearrange("p (c j) -> p c j", j=64)
                    Dv = Dt[:, :].rearrange("p (c j) -> p c j", j=64)
                    T63 = bcast_ap(Fc[:, 63:64], [[64, 16], [0, 64]])
                    act(dec[:, dh, half * 16:(half + 1) * 16], Fv[:, :, 63], AF.Exp, ["Fc"], ["dec"], scale=-1.0 / 16)
                    if d_ == 0:
                        ref = bcast_ap(Fc[:, 31:32], [[64, 16], [0, 64]])
                        D_, dn = nextD()
                        tt("dve", D_[:, :].rearrange("p (c j) -> p c j", j=64), Fv, ref, ALU.subtract, ["Fc"], [dn])
                        exp_prod(D_[:, :], dn, -1.0 / 16, prod[(0, hp, "qr")][:, hs], qT[:, hs], "qT")
                        exp_prod(D_[:, :], dn, 1.0 / 16, prod[(0, hp, "kr")][:, hs], kT[:, hs], "kT")
                        exp_prod(Fc[:, :], "Fc", -1.0 / 16, prod[(0, hp, "qb")][:, hs], qT[:, hs], "qT")
                        D_, dn = nextD()
                        tt("dve", D_[:, :].rearrange("p (c j) -> p c j", j=64), Fv, T63, ALU.subtract, ["Fc"], [dn])
                        exp_prod(D_[:, :], dn, 1.0 / 16, kdT[:, :], kT[:, hs], "kT", "kdT")
                    else:
                        tt("dve", G[:, :], Fc[:, :], G[:, :], ALU.subtract, ["Fc", "G"], ["G"])
                        Gv = G[:, :].rearrange("p (c j) -> p c j", j=64)
                        ref = bcast_ap(G[:, 32:33], [[64, 16], [0, 64]])
                        D_, dn = nextD()
                        tt("dve", D_[:, :].rearrange("p (c j) -> p c j", j=64), Gv, ref, ALU.subtract, ["G"], [dn])
                        exp_prod(D_[:, :], dn, 1.0 / 16, prod[(1, hp, "qr")][:, hs], qT[:, hs], "qT")
                        exp_prod(D_[:, :], dn, -1.0 / 16, prod[(1, hp, "kr")][:, hs], kT[:, hs], "kT")
                        D_, dn = nextD()
                        tt("dve", D_[:, :].rearrange("p (c j) -> p c j", j=64), Gv, T63, ALU.subtract, ["G", "Fc"], [dn])
                        exp_prod(D_[:, :], dn, 1.0 / 16, prod[(1, hp, "qb")][:, hs], qT[:, hs], "qT")
                        exp_prod(G[:, :], "G", -1.0 / 16, kdT[:, :], kT[:, hs], "kT", "kdT")
                    for g4 in range(2):
                        pbk = 2 + g4
                        pv = Pb[pbk][:, 0:512].rearrange("p (t n) -> p t n", t=4)
                        for tq in range(4):
                            c0 = (g4 * 4 + tq) * 128
                            tr(pv[:, tq, :], kdT[:, c0:c0 + 128], ident[:, :], ["kdT", "ident"], [PN[pbk]])
                        t0 = half * 8 + g4 * 4
                        cp("dve", kd[:, t0:t0 + 4, dh * 128:(dh + 1) * 128], pv, [PN[pbk]], ["kd"])
        S.barrier()

        checkpoint("D1")
        qk_off = R2
        Sst = [A("Sst%d" % i, [128, 32, 128], BF16, at=qk_off + i * 8192) for i in range(4)]
        Sf = [A("Sf%d" % i, [128, 256], F32, at=HTB + i * 1024) for i in range(4)]
        for dh in range(4):
            memset("pool", Sf[dh][:, :], 0.0, ["Sf%d" % dh])
        for step in range(32):
            for dh in range(4):
                d_, hp = divmod(dh, 2)
                n = step if d_ == 0 else 31 - step
                t, c = divmod(n, 2)
                rows = slice(c * 64, (c + 1) * 64)
                cp("pool", Sst[dh][0:64, n, :], Sf[dh][0:64, 0:128], ["Sf%d" % dh], ["SstA%d" % dh])
                cp("act", Sst[dh][64:128, n, :], Sf[dh][64:128, 128:256], ["Sf%d" % dh], ["SstB%d" % dh])
                if step == 31:
                    continue
                pbk = 4 * c + dh
                mm(P[pbk][:, 0:256], kd[rows, t, dh * 128:(dh + 1) * 128], vtok[rows, t, hp * 256:(hp + 1) * 256],
                   True, True, ["kd", "vtok"], [PN[pbk]])
                stt(Sf[dh][:, :], Sf[dh][:, :], dec[:, dh, n:n + 1], P[pbk][:, 0:256], ALU.mult, ALU.add,
                    ["Sf%d" % dh, "dec", PN[pbk]], ["Sf%d" % dh])
        S.barrier()

        checkpoint("D2")
        smb = [A("smb%d" % i, [128, 2, 2, 128], BF16, at=HTB + 4096 + i * 1024) for i in range(2)]
        sqoL = [A("sqo%d" % i, [128, 256], F32, at=HTB + 6144 + i * 1024) for i in range(2)]
        rsoL = [A("rso%d" % i, [128, 256], F32, at=HTB + 8192 + i * 1024) for i in range(2)]
        t1oL = [A("t1o%d" % i, [128, 256], F32, at=HTB + 10240 + i * 1024) for i in range(2)]
        for t in range(NT):
            tsl = slice(t * 128, (t + 1) * 128)
            for par in range(2):
                rows = slice(par * 64, (par + 1) * 64)
                sbk = par
                obk = 2 + par
                scv = P[sbk][:, :].rearrange("p (a b n) -> p a b n", a=2, b=2)
                for hp in range(2):
                    for d_ in range(2):
                        mm(scv[:, hp, d_, :], prod[(d_, hp, "kr")][rows, tsl], prod[(d_, hp, "qr")][rows, tsl], True, True,
                           ["prod"], [PN[sbk]])
                mk = bcast_ap(masks[:, 0:256], [[0, 2], [1, 256]])
                tt("dve", smb[par][:, :, :, :].rearrange("p a b n -> p a (b n)"),
                   P[sbk][:, :].rearrange("p (a m) -> p a m", a=2), mk, ALU.mult, [PN[sbk], "masks"], ["smb%d" % par])
                ov = P[obk][:, 0:256].rearrange("p (a n) -> p a n", a=2)
                for hp in range(2):
                    h = hp * 2 + par
                    mm(ov[:, hp, :], vtok[:, t, h * 128:(h + 1) * 128], smb[par][:, hp, 0, :], True, False,
                       ["vtok", "smb%d" % par], [PN[obk]])
                    mm(ov[:, hp, :], vtok[:, t, h * 128:(h + 1) * 128], smb[par][:, hp, 1, :], False, False,
                       ["vtok", "smb%d" % par], [PN[obk]])
                    for d_ in range(2):
                        dh = d_ * 2 + hp
                        for c in range(2):
                            n = t * 2 + c
                            csl = slice(t * 128 + c * 64, t * 128 + (c + 1) * 64)
                            last = (d_ == 1 and c == 1)
                            mm(ov[:, hp, c * 64:(c + 1) * 64], Sst[dh][rows, n, :], prod[(d_, hp, "qb")][rows, csl],
                               False, last, ["SstA%d" % dh, "SstB%d" % dh, "prod"], [PN[obk]])
                sqo, rso, t1o = sqoL[par], rsoL[par], t1oL[par]
                sP = "%d" % par
                act(sqo[:, :], P[obk][:, 0:256], AF.Square, [PN[obk]], ["sqo" + sP])
                ebk = 4 + par
                mm(P[ebk][:, 0:256], onesf[:, :], sqo[:, :], True, True, ["onesf", "sqo" + sP], [PN[ebk]])
                rsqrt_act(rso[:, :], P[ebk][:, 0:256], 128, [PN[ebk]], ["rso" + sP])
                stt(t1o[:, :], P[obk][:, 0:256], gout[:, 0:1], rso[:, :], ALU.mult, ALU.mult, [PN[obk], "gout", "rso" + sP], ["t1o" + sP])
                for hp in range(2):
                    h = hp * 2 + par
                    tt("pool", mixT[:, h, tsl], t1o[:, hp * 128:(hp + 1) * 128], sgT[:, h, tsl], ALU.mult,
                       ["t1o" + sP, "sgT"], ["mixT_g"])
        S.barrier()
        A.reset(L0)
        if debug:
            final_ops.append(dma("sp", dbg["mix"][:, :], mixT[:, :, :].rearrange("p c n -> p (c n)"), reads=["mixT_g", "mixT_m"]))

        checkpoint("D3")
        X = A("X", [128, NT, D], F32)
        h2T = A("h2T", [128, 8, S_LEN], BF16)
        wo = A("wo", [128, 8, D], BF16)
        WO_OFF = A.off - 16384
        g2 = A("g2", [128, D], F32)
        wr = A("wr", [128, 8, 36], BF16)
        rbias = A("rbias", [128, 36], F32)
        gTf = A("gTf", [32, S_LEN], F32)
        identf = A("identf", [128, 128], F32)
        cp("dve", identf[:, :], ident[:, :], ["ident"], ["identf"])
        gwb = [A("gwb%d" % i, [128, 512], F32) for i in range(4)]
        lgA = A("lgA", [128, NT, 36], F32)
        hb = [A("hb2_%d" % i, [128, D], BF16) for i in range(2)]
        sqj = A("sqj2", [128, D], BF16)
        st1 = A("st1_2", [128, 4], F32)
        dma("pool", wo[:, :, :], wout_d.ap().rearrange("(c p) n -> p c n", p=128), writes=["wo"])
        dma("sp", g2[:, :], bcast_row(g2_d, D), writes=["g2"])
        dma("pool", wr[:, :, 0:4], wrg_d.ap().rearrange("(c p) n -> p c n", p=128), writes=["wr"])
        dma("pool", wr[:, :, 4:36], wre_d.ap().rearrange("(c p) n -> p c n", p=128), writes=["wr"])
        dma("sp", rbias[:, 0:4], bcast_row(brg_d, 4), writes=["rbias"])
        dma("sp", rbias[:, 4:36], bcast_row(bre_d, 32), writes=["rbias"])
        for t in range(NT):
            tsl = slice(t * 128, (t + 1) * 128)
            dma("sp", X[:, t, :], x_d[tsl, :], writes=["X%d" % t])
            for ch in range(2):
                pbk = (t % 2) * 2 + ch
                for kc in range(8):
                    mm(P[pbk][:, :], mixT[:, kc, tsl], wo[:, kc, ch * 512:(ch + 1) * 512], kc == 0, kc == 7,
                       ["mixT_g", "mixT_m", "wo"], [PN[pbk]])
                tt("dve", X[:, t, ch * 512:(ch + 1) * 512], X[:, t, ch * 512:(ch + 1) * 512], P[pbk][:, :], ALU.add,
                   ["X%d" % t, PN[pbk]], ["X%d" % t])
        if debug:
            for t in range(NT):
                final_ops.append(dma("sp", dbg["x1"][t * 128:(t + 1) * 128, :], X[:, t, :], reads=["X%d" % t]))
        checkpoint("E")
        for t in range(NT):
            tsl = slice(t * 128, (t + 1) * 128)
            norm_to_T(lambda t: X[:, t, :], ["X%d" % t], g2, "g2", h2T, "h2T", "F", t, 4 + t % 2)
            rbk = 6 + t % 2
            for kc in range(8):
                mm(P[rbk][:, 0:36], h2T[:, kc, tsl], wr[:, kc, :], kc == 0, kc == 7, ["h2T_%d" % (t // 4), "wr"], [PN[rbk]])
            tt("dve", lgA[:, t, :], P[rbk][:, 0:36], rbias[:, :], ALU.add, [PN[rbk], "rbias"], ["lgA"])

        def bl(ap2, k):
            return bcast_ap(ap2, [list(ap2.ap[1]), [0, k]])

        def red(out, in_, o, reads, writes):
            return op("dve", lambda e: e.tensor_reduce(out=out, in_=in_, axis=AX.X, op=o), reads=reads, writes=writes,
                      n=fsz(in_))

        r16 = lambda nm: A(nm, [128, NT], F32)
        r4 = lambda nm: A(nm, [128, NT, 4], F32)
        r8 = lambda nm: A(nm, [128, NT, 8], F32)
        mg, s4, ptop, m1, m2, dm, e2, den, w1, w2 = [r16("r16_%d" % i) for i in range(10)]
        d4, e4, oh, ohp = [r4("r4_%d" % i) for i in range(4)]
        ls, tmp8, eq1, ls2, eq2, wg8 = [r8("r8_%d" % i) for i in range(6)]
        gate = A("gate", [128, NT, 32], F32)
        red(mg[:, :], lgA[:, :, 0:4], ALU.max, ["lgA"], ["mg"])
        tt("dve", d4[:, :, :], lgA[:, :, 0:4], bl(mg[:, :], 4), ALU.subtract, ["lgA", "mg"], ["d4"])
        act(e4[:, :, :], d4[:, :, :], AF.Exp, ["d4"], ["e4"])
        red(s4[:, :], e4[:, :, :], ALU.add, ["e4"], ["s4"])
        op("dve", lambda e: e.reciprocal(out=ptop[:, :], in_=s4[:, :]), reads=["s4"], writes=["ptop"], n=128)
        ts("dve", oh[:, :, :], d4[:, :, :], 0.0, None, ALU.is_equal, None, ["d4"], ["oh"])
        tt("dve", ohp[:, :, :], oh[:, :, :], bl(ptop[:, :], 4), ALU.mult, ["oh", "ptop"], ["ohp"])
        tt("dve", ls[:, :, :], lgA[:, :, 4:12], bl(oh[:, :, 0], 8), ALU.mult, ["lgA", "oh"], ["ls"])
        for g_ in range(1, 4):
            tt("dve", tmp8[:, :, :], lgA[:, :, 4 + 8 * g_:12 + 8 * g_], bl(oh[:, :, g_], 8), ALU.mult, ["lgA", "oh"], ["tmp8"])
            tt("dve", ls[:, :, :], ls[:, :, :], tmp8[:, :, :], ALU.add, ["ls", "tmp8"], ["ls"])
        red(m1[:, :], ls[:, :, :], ALU.max, ["ls"], ["m1"])
        tt("dve", eq1[:, :, :], ls[:, :, :], bl(m1[:, :], 8), ALU.is_equal, ["ls", "m1"], ["eq1"])
        stt(ls2[:, :, :], eq1[:, :, :], -1e30, ls[:, :, :], ALU.mult, ALU.add, ["eq1", "ls"], ["ls2"])
        red(m2[:, :], ls2[:, :, :], ALU.max, ["ls2"], ["m2"])
        tt("dve", eq2[:, :, :], ls2[:, :, :], bl(m2[:, :], 8), ALU.is_equal, ["ls2", "m2"], ["eq2"])
        tt("dve", dm[:, :], m2[:, :], m1[:, :], ALU.subtract, ["m1", "m2"], ["dm"])
        act(e2[:, :], dm[:, :], AF.Exp, ["dm"], ["e2"])
        ts("dve", den[:, :], e2[:, :], 1.0, None, ALU.add, None, ["e2"], ["den"])
        op("dve", lambda e: e.reciprocal(out=w1[:, :], in_=den[:, :]), reads=["den"], writes=["w1"], n=128)
        tt("dve", w2[:, :], e2[:, :], w1[:, :], ALU.mult, ["e2", "w1"], ["w2"])
        tt("dve", wg8[:, :, :], eq1[:, :, :], bl(w1[:, :], 8), ALU.mult, ["eq1", "w1"], ["wg8"])
        tt("dve", tmp8[:, :, :], eq2[:, :, :], bl(w2[:, :], 8), ALU.mult, ["eq2", "w2"], ["tmp8"])
        tt("dve", wg8[:, :, :], wg8[:, :, :], tmp8[:, :, :], ALU.add, ["wg8", "tmp8"], ["wg8"])
        for g_ in range(4):
            tt("dve", gate[:, :, g_ * 8:(g_ + 1) * 8], wg8[:, :, :], bl(ohp[:, :, g_], 8), ALU.mult, ["wg8", "ohp"], ["gate"])
        if debug:
            for t in range(NT):
                final_ops.append(dma("sp", dbg["gate"][t * 128:(t + 1) * 128, :], gate[:, t, :], reads=["gate"]))
        for q4 in range(4):
            bk = 4 + q4
            for tq in range(4):
                t = q4 * 4 + tq
                tr(P[bk][0:32, tq * 128:(tq + 1) * 128], gate[:, t, :], identf[:, :], ["gate", "identf"], [PN[bk]])
            cp("act" if q4 % 2 else "dve", gTf[0:32, q4 * 512:(q4 + 1) * 512], P[bk][0:32, :], [PN[bk]], ["gTf"])
        dma("sp", gdr[:, :], gTf[0:32, :], reads=["gTf"], writes=["gdr"])
        checkpoint("F")

        EG = 2
        NEG = 32 // EG
        MX = SB_BASE + 256
        S.alias(["hid0", "hid1", "sil0", "sil1", "t1m0", "t1m1", "wdn0", "wdn1"], ["mixT_g", "mixT_m"])
        hid = [A("hid%d" % b, [128, EG, 2, 512], BF16, at=MX + b * 4096) for b in range(2)]
        sil = [A("sil%d" % b, [128, 512], F32, at=MX + 8192 + b * 2048) for b in range(2)]
        t1m = [A("t1m%d" % b, [128, 512], F32, at=MX + 12288 + b * 2048) for b in range(2)]
        wdn = [[A("wd%d_%d" % (b, j), [128, 2, D], BF16, at=MX + 16384 + (b * EG + j) * 4096) for j in range(EG)] for b in range(2)]
        wgt = [None, None]
        wup = [None, None]
        wgt[0] = [A("wg0_%d" % j, [128, 8, 256], BF16) for j in range(EG)]
        wup[0] = [A("wu0_%d" % j, [128, 8, 256], BF16) for j in range(EG)]
        wgt[1] = [A("wg1_%d" % j, [128, 8, 256], BF16, at=WO_OFF + j * 4096) for j in range(EG)]
        wup[1] = [A("wu1_%d" % j, [128, 8, 256], BF16, at=WO_OFF + 8192 + j * 4096) for j in range(EG)]
        itc = 0
        gcnt = [0]
        for eg in range(NEG):
            b = eg % 2
            extra = ["wo"] if b == 1 else []
            for j in range(EG):
                e_ = eg * EG + j
                dma("pool", wgt[b][j][:, :, :], weg_d.ap()[e_].rearrange("(c p) f -> p c f", p=128), writes=["wgt%d" % b] + extra)
                dma("pool", wup[b][j][:, :, :], weu_d.ap()[e_].rearrange("(c p) f -> p c f", p=128), writes=["wup%d" % b] + extra)
                dma("pool", wdn[b][j][:, :, :], wed_d.ap()[e_].rearrange("(c p) d -> p c d", p=128), writes=["wdn%d" % b])
            for tg in range(4):
                hbi = itc % 2
                itc += 1
                tgs = slice(tg * 512, (tg + 1) * 512)
                for j in range(EG):
                    e_ = eg * EG + j
                    gbk = 6 + j
                    for fh in range(2):
                        k2 = fh
                        gb_, ub_ = 0 + k2, 2 + k2
                        for kc in range(8):
                            mm(P[gb_][:, :], wgt[b][j][:, kc, fh * 128:(fh + 1) * 128], h2T[:, kc, tgs], kc == 0, kc == 7,
                               ["wgt%d" % b, "h2T_%d" % tg], [PN[gb_]])
                        for kc in range(8):
                            mm(P[ub_][:, :], wup[b][j][:, kc, fh * 128:(fh + 1) * 128], h2T[:, kc, tgs], kc == 0, kc == 7,
                               ["wup%d" % b, "h2T_%d" % tg], [PN[ub_]])
                        if fh == 0:
                            gk = gcnt[0] % 4
                            gcnt[0] += 1
                            dma("sp", gwb[gk][:, :], bass.AP(gdr, e_ * S_LEN + tg * 512, [[0, 128], [1, 512]]),
                                reads=["gdr"], writes=["gwb%d" % gk])
                        act(sil[k2][:, :], P[gb_][:, :], AF.Silu, [PN[gb_]], ["sil%d" % k2])
                        tt("dve", t1m[k2][:, :], sil[k2][:, :], P[ub_][:, :], ALU.mult, ["sil%d" % k2, PN[ub_]], ["t1m%d" % k2])
                        tt("dve", hid[hbi][:, j, fh, :], t1m[k2][:, :], gwb[gk][:, :], ALU.mult, ["t1m%d" % k2, "gwb%d" % gk],
                           ["hid%d" % hbi])
                for tt_ in range(4):
                    t = tg * 4 + tt_
                    for ch in range(2):
                        abk = 4 + (tt_ * 2 + ch) % 2
                        n_acc = EG * 2
                        a_i = 0
                        for j in range(EG):
                            for fh in range(2):
                                mm(P[abk][:, :], hid[hbi][:, j, fh, tt_ * 128:(tt_ + 1) * 128],
                                   wdn[b][j][:, fh, ch * 512:(ch + 1) * 512], a_i == 0, a_i == n_acc - 1,
                                   ["hid%d" % hbi, "wdn%d" % b], [PN[abk]])
                                a_i += 1
                        tt("dve", X[:, t, ch * 512:(ch + 1) * 512], X[:, t, ch * 512:(ch + 1) * 512], P[abk][:, :], ALU.add,
                           ["X%d" % t, PN[abk]], ["X%d" % t])
        for t in range(NT):
            final_ops.append(dma("sp", out_d[t * 128:(t + 1) * 128, :], X[:, t, :], reads=["X%d" % t]))

        S.emit(es, final_wait_ops=final_ops)
    return nc


def make_consts():
    ident = np.eye(128, dtype=np.float32).astype(ml_dtypes.bfloat16)
    j = np.arange(128)[:, None]
    i = np.arange(128)[None, :]
    same = (j // 64) == (i // 64)
    mf = (same & (j <= i)).astype(np.float32)
    mb = (same & (j > i)).astype(np.float32)
    masks = np.concatenate([mf, mb], axis=1).astype(ml_dtypes.bfloat16)
    invf = (10000.0 ** (-np.arange(0, 32, 2, dtype=np.float32) / 32)).astype(np.float32)
    invf = np.broadcast_to(invf[None, :], (128, 16)).copy()
    sel = np.zeros((32, 32, 128), np.float32)
    for e in range(32):
        sel[e, e, :] = 1.0
    sel = sel.reshape(32, 32 * 128).astype(ml_dtypes.bfloat16)
    lrb = np.zeros((64, 1), np.float32)
    lrb[16, 0] = 1.0
    return {"c_ident": ident, "c_masks": masks, "c_invf": invf, "c_sel": sel, "c_lrbias": lrb}


_NC_CACHE = {}


def make_in_maps(inputs, n_cores=8):
    c = make_consts()
    f = lambda k: np.ascontiguousarray(np.asarray(inputs[k], dtype=np.float32)[0])
    shared = {
        "norm1_gain": f("norm1_gain").reshape(1, D),
        "w_in": f("w_in"),
        "gla_gk_fwd_w": f("gla_gk_fwd_w"), "gla_gk_fwd_b": f("gla_gk_fwd_b").reshape(1, 256),
        "gla_gk_bwd_w": f("gla_gk_bwd_w"), "gla_gk_bwd_b": f("gla_gk_bwd_b").reshape(1, 256),
        "gla_out_gain": f("gla_out_gain").reshape(128, 1),
        "mla_q_gain": f("mla_q_gain").reshape(1, 256), "mla_w_qb": f("mla_w_qb"),
        "mla_kv_gain": f("mla_kv_gain").reshape(1, 128), "mla_w_kvb": f("mla_w_kvb"),
        "q_norm_gain": f("q_norm_gain").reshape(1, 96), "k_norm_gain": f("k_norm_gain").reshape(1, 96),
        "w_out": f("w_out"), "norm2_gain": f("norm2_gain").reshape(1, D),
        "w_router_group": f("w_router_group"), "b_router_group": f("b_router_group").reshape(1, 4),
        "w_router_expert": f("w_router_expert"), "b_router_expert": f("b_router_expert").reshape(1, 32),
        "w_expert_gate": f("w_expert_gate").reshape(32, D, 256),
        "w_expert_up": f("w_expert_up").reshape(32, D, 256),
        "w_expert_down": f("w_expert_down").reshape(32, 256, D),
    }
    shared.update(c)
    x = np.asarray(inputs["x"], dtype=np.float32)
    pos = np.asarray(inputs["positions"]).astype(np.int32)
    maps = []
    for b in range(n_cores):
        m = dict(shared)
        m["x"] = np.ascontiguousarray(x[b])
        m["pos"] = np.ascontiguousarray(pos[b].reshape(NT, 128).T)
        maps.append(m)
    return maps


def kernel(**inputs):
    if "nc" not in _NC_CACHE:
        _NC_CACHE["nc"] = build()
    nc = _NC_CACHE["nc"]
    maps = make_in_maps(inputs, 8)
    res = run_bass_kernel_spmd(nc, maps, core_ids=list(range(8)))
    out = np.stack([np.asarray(r["out"], dtype=np.float32) for r in res.results], axis=0)
    return out
```

```python
import contextlib
import math
import numpy as np
import ml_dtypes
import concourse.bass as bass
import concourse.mybir as mybir
from concourse.bass_utils import run_bass_kernel_spmd

F32 = mybir.dt.float32
BF16 = mybir.dt.bfloat16
I32 = mybir.dt.int32
ALU = mybir.AluOpType
AF = mybir.ActivationFunctionType
AX = mybir.AxisListType

S_LEN = 2048
D = 1024
NT = 16
EPS = 1e-6
PI = math.pi


class T:
    __slots__ = ("name", "w", "r")

    def __init__(self, name):
        self.name = name
        self.w = None
        self.r = []


class Op:
    __slots__ = ("eng", "fn", "deps", "signal", "sig", "dma", "dsem", "dval", "alld", "n", "seg", "idx", "nbytes", "tag")


class Sched:
    ENGS = ("pe", "act", "dve", "pool", "sp")

    def __init__(self, nc, n_dma_sems=12):
        self.nc = nc
        self.ops = {e: [] for e in self.ENGS}
        self.n_dma_sems = n_dma_sems
        self.dma_count = {e: 0 for e in self.ENGS}
        self.tiles = {}
        self.pending = {e: [] for e in self.ENGS}
        self.dma_since_barrier = []
        self.stopped = False
        self.seg = 0
        self.nops = 0
        self.noresched = set()

    def t(self, name):
        if name not in self.tiles:
            self.tiles[name] = T(name)
        return self.tiles[name]

    def _tl(self, lst):
        out = []
        for x in lst:
            if isinstance(x, str):
                out.append(self.t(x))
            elif isinstance(x, (list, tuple)):
                out.extend(self._tl(x))
            elif x is not None:
                out.append(x)
        return out

    def alias(self, new_names, old_names):
        if self.stopped:
            return
        for nn in new_names:
            tn = self.t(nn)
            for on in old_names:
                to = self.t(on)
                if to.w is not None:
                    tn.r.append(to.w)
                tn.r.extend(to.r)

    def barrier(self):
        if self.stopped:
            return
        lasts = []
        for e in self.ENGS:
            for o in reversed(self.ops[e]):
                if not o.dma:
                    lasts.append(o)
                    break
        lasts.extend(self.dma_since_barrier)
        self.dma_since_barrier = []
        for e in self.ENGS:
            self.pending[e] = list(lasts)
        self.seg += 1

    def op(self, eng, fn, reads=(), writes=(), dma=False, n=64, nbytes=0):
        if self.stopped:
            return None
        o = Op()
        o.n = n
        o.nbytes = nbytes
        o.seg = self.seg
        o.idx = self.nops
        self.nops += 1
        o.eng = eng
        o.fn = fn
        o.dma = dma
        o.signal = False
        o.sig = 0
        deps = {}
        reads = self._tl(reads)
        writes = self._tl(writes)
        for t in reads:
            if t.w is not None:
                deps[id(t.w)] = (t.w, "raw")
            if t.name[0] == "P" and t.name[1:].isdigit():
                for r in t.r:
                    if id(r) not in deps and r.eng != eng:
                        deps[id(r)] = (r, "war")
        for t in writes:
            if t.w is not None and id(t.w) not in deps:
                deps[id(t.w)] = (t.w, "waw")
            for r in t.r:
                if id(r) not in deps:
                    deps[id(r)] = (r, "war")
        if self.pending[eng]:
            for p in self.pending[eng]:
                deps[id(p)] = (p, "raw")
            self.pending[eng] = []
        o.alld = [p for p, _k in deps.values()]
        o.tag = ("R:" + ",".join(t.name for t in reads) + " W:" + ",".join(t.name for t in writes))
        dl = []
        for p, kind in deps.values():
            if p.eng == eng and not p.dma:
                if eng == "pe":
                    continue
                if kind != "raw" and not dma and not STRICT_SAME_ENGINE:
                    continue
            dl.append(p)
        o.deps = dl
        for p in dl:
            p.signal = True
        for t in reads:
            if not dma:
                for r in t.r:
                    if not r.dma and r.eng == eng:
                        o.alld.append(r)
                t.r = [r for r in t.r if r.dma or r.eng != eng]
            t.r.append(o)
        for t in writes:
            t.w = o
            t.r = []
        if dma:
            self.dma_count[eng] += 1
            self.dma_since_barrier.append(o)
        self.ops[eng].append(o)
        return o

    @staticmethod
    def _dur(o):
        n = o.n
        if o.dma:
            return 60.0 if o.eng == "sp" else 900.0
        if o.eng == "pe":
            return 30.0 + max(n, 64) / 2.0
        if o.eng == "act":
            return 220.0 + n / 1.4
        if o.eng == "dve":
            return 120.0 + n * 1.3
        return 550.0 + n * 0.75

    def reschedule(self):
        allops = []
        for e in self.ENGS:
            allops.extend(self.ops[e])
        allops.sort(key=lambda o: o.idx)
        import heapq
        new = {e: [] for e in self.ENGS}
        segs = {}
        for o in allops:
            segs.setdefault(o.seg, []).append(o)
        LAT = 250.0
        for sg in sorted(segs):
            ops = segs[sg]
            if sg in self.noresched:
                for o in ops:
                    new[o.eng].append(o)
                continue
            inseg = set(id(o) for o in ops)
            done = {}
            users = {}
            indeg = {}
            first = {}
            for o in ops:
                if o.eng not in first:
                    first[o.eng] = o
                elif first[o.eng] not in o.alld:
                    o.alld.append(first[o.eng])
            for o in ops:
                k = 0
                for p in o.alld:
                    if id(p) in inseg:
                        k += 1
                        users.setdefault(id(p), []).append(o)
                indeg[id(o)] = k
            ready = {e: [] for e in self.ENGS}
            efree = {e: 0.0 for e in self.ENGS}
            rtime = {}
            for o in ops:
                if indeg[id(o)] == 0:
                    rtime[id(o)] = 0.0
                    ready[o.eng].append(o)
            left = len(ops)
            SLACK = 0.0
            while left:
                best = None
                for e in self.ENGS:
                    rl = ready[e]
                    if not rl:
                        continue
                    ef = efree[e]
                    oldest = None
                    fill = None
                    for o in rl:
                        st = max(rtime[id(o)], ef)
                        if oldest is None or o.idx < oldest[1].idx:
                            oldest = (st, o)
                        if fill is None or (st, o.idx) < (fill[0], fill[1].idx):
                            fill = (st, o)
                    pick = oldest if oldest[0] <= fill[0] + SLACK else fill
                    if best is None or (pick[0], pick[1].idx) < (best[0], best[1].idx):
                        best = pick
                st, o = best
                e = o.eng
                ready[e].remove(o)
                d = self._dur(o)
                efree[e] = st + d
                fin = st + d
                if o.dma:
                    fin = st + 2000.0 + o.nbytes / 150.0
                done[id(o)] = fin
                new[e].append(o)
                left -= 1
                for u in users.get(id(o), ()):
                    indeg[id(u)] -= 1
                    lat = 0.0 if (u.eng == o.eng and not o.dma) else LAT
                    rtime[id(u)] = max(rtime.get(id(u), 0.0), fin + lat)
                    if indeg[id(u)] == 0:
                        ready[u.eng].append(u)
        self.ops = new

    def emit(self, es, final_wait_ops=()):
        nc = self.nc
        if RESCHEDULE:
            self.reschedule()
        sems = {e: es.enter_context(nc.semaphore("s_" + e)) for e in self.ENGS}
        dsems = {e: [es.enter_context(nc.semaphore("d_%s_%d" % (e, i)))
                     for i in range(self.n_dma_sems)]
                 for e in self.ENGS if self.dma_count[e] > 0}
        for e in self.ENGS:
            c = 0
            i = 0
            for o in self.ops[e]:
                if o.dma:
                    o.dsem = i % self.n_dma_sems
                    o.dval = 16 * (i // self.n_dma_sems + 1)
                    i += 1
                elif o.signal:
                    c += 1
                    o.sig = c
        block = es.enter_context(nc.Block())
        eng_obj = {"pe": block.tensor, "act": block.scalar, "dve": block.vector,
                   "pool": block.gpsimd, "sp": block.sync}
        for e in self.ENGS:
            ops = self.ops[e]
            if not ops:
                continue

            def body(engine, e=e, ops=ops):
                waited = {}

                def wait(sem, key, val):
                    if waited.get(key, 0) >= val:
                        return
                    waited[key] = val
                    engine.wait_ge(sem, val)

                for o in ops:
                    for p in o.deps:
                        if p.dma:
                            wait(dsems[p.eng][p.dsem], ("d", p.eng, p.dsem), p.dval)
                        else:
                            wait(sems[p.eng], ("c", p.eng), p.sig)
                    if o.dma and o.dval > 16:
                        wait(dsems[e][o.dsem], ("d", e, o.dsem), o.dval - 16)
                    ins = o.fn(engine)
                    if o.dma:
                        ins.then_inc(dsems[e][o.dsem], 16)
                    elif o.signal:
                        ins.then_inc(sems[e], 1)
                if e == "sp":
                    for o in final_wait_ops:
                        if o is None:
                            continue
                        wait(dsems[o.eng][o.dsem], ("d", o.eng, o.dsem), o.dval)

            eng_obj[e](body)


RESCHEDULE = True
STRICT_SAME_ENGINE = True
SB_BASE = 16640
SB_END = 229376


class Alloc:
    def __init__(self, nc):
        self.nc = nc
        self.off = SB_BASE
        self.n = 0

    def mark(self):
        return self.off

    def reset(self, m):
        self.off = m

    def __call__(self, name, shape, dt, at=None):
        esz = 2 if dt == BF16 else 4
        nb = int(np.prod(shape[1:])) * esz
        nb = (nb + 63) // 64 * 64
        self.n += 1
        if at is None:
            at = self.off
            self.off += nb
        assert at + nb <= SB_END, ("SBUF overflow", name, at, nb)
        return self.nc.alloc_sbuf_tensor_at("%s_%d" % (name, self.n), list(shape), dt, offset=at)


def bcast_ap(ap, pattern):
    return bass.AP(ap.tensor, ap.offset, [list(ap.ap[0])] + [list(p) for p in pattern])


def build(debug=False, stop_after=None):
    nc = bass.Bass("TRN2", target_bir_lowering=False)
    dr = lambda n, s, dt=F32: nc.dram_tensor(n, list(s), dt, kind="ExternalInput")
    x_d = dr("x", [S_LEN, D])
    pos_d = dr("pos", [128, NT], I32)
    g1_d = dr("norm1_gain", [1, D])
    win_d = dr("w_in", [D, 1984])
    gkf_w = dr("gla_gk_fwd_w", [16, 256])
    gkf_b = dr("gla_gk_fwd_b", [1, 256])
    gkb_w = dr("gla_gk_bwd_w", [16, 256])
    gkb_b = dr("gla_gk_bwd_b", [1, 256])
    go_d = dr("gla_out_gain", [128, 1])
    gqa_d = dr("mla_q_gain", [1, 256])
    wqb_d = dr("mla_w_qb", [256, 768])
    gkva_d = dr("mla_kv_gain", [1, 128])
    wkvb_d = dr("mla_w_kvb", [128, 1024])
    gqn_d = dr("q_norm_gain", [1, 96])
    gkn_d = dr("k_norm_gain", [1, 96])
    wout_d = dr("w_out", [D, D])
    g2_d = dr("norm2_gain", [1, D])
    wrg_d = dr("w_router_group", [D, 4])
    brg_d = dr("b_router_group", [1, 4])
    wre_d = dr("w_router_expert", [D, 32])
    bre_d = dr("b_router_expert", [1, 32])
    weg_d = dr("w_expert_gate", [32, D, 256])
    weu_d = dr("w_expert_up", [32, D, 256])
    wed_d = dr("w_expert_down", [32, 256, D])
    ident_d = dr("c_ident", [128, 128], BF16)
    masks_d = dr("c_masks", [128, 256], BF16)
    invf_d = dr("c_invf", [128, 16])
    sel_d = dr("c_sel", [32, 32 * 128], BF16)
    lrb_d = dr("c_lrbias", [64, 1])
    out_d = nc.dram_tensor("out", [S_LEN, D], F32, kind="ExternalOutput")
    gdr = nc.dram_tensor("gate_scratch", [32, S_LEN], F32, kind="Internal")
    dbg = {}
    if debug:
        dbg["mix"] = nc.dram_tensor("d_mix", [128, 8 * S_LEN], BF16, kind="ExternalOutput")
        dbg["x1"] = nc.dram_tensor("d_x1", [S_LEN, D], F32, kind="ExternalOutput")
        dbg["gate"] = nc.dram_tensor("d_gate", [S_LEN, 32], F32, kind="ExternalOutput")

    if debug:
        dbg["gen"] = nc.dram_tensor("d_gen", [128, 8 * S_LEN], BF16, kind="ExternalOutput")
    S = Sched(nc)
    A = Alloc(nc)
    op = S.op
    final_ops = []

    with contextlib.ExitStack() as es:
        P = [es.enter_context(nc.psum_tensor("pb%d" % i, [128, 512], F32)) for i in range(8)]
        Pb = [p.bitcast(BF16) for p in P]
        PN = ["P%d" % i for i in range(8)]

        def fsz(ap):
            r = 1
            for d_ in list(ap.shape)[1:]:
                r *= int(d_)
            return r

        def dma(q, out, in_, reads=(), writes=(), **kw):
            return op(q, lambda e: e.dma_start(out=out, in_=in_, **kw), reads=reads, writes=writes, dma=True,
                      nbytes=fsz(out) * 4 * 128)

        def act(out, in_, func, reads, writes, **kw):
            return op("act", lambda e: e.activation(out=out, in_=in_, func=func, **kw), reads=reads, writes=writes,
                      n=fsz(out))

        def rsqrt_act(out, in_, n, reads, writes):
            act(out, in_, AF.Ln, reads, writes, scale=1.0 / n, bias=EPS)
            act(out, out, AF.Exp, writes, writes, scale=-0.5)

        def tt(eng, out, in0, in1, o, reads, writes):
            return op(eng, lambda e: e.tensor_tensor(out=out, in0=in0, in1=in1, op=o), reads=reads, writes=writes,
                      n=fsz(out))

        def ts(eng, out, in0, s1, s2, o0, o1, reads, writes):
            if o1 is None:
                return op(eng, lambda e: e.tensor_scalar(out=out, in0=in0, scalar1=s1, scalar2=None, op0=o0),
                          reads=reads, writes=writes, n=fsz(out))
            return op(eng, lambda e: e.tensor_scalar(out=out, in0=in0, scalar1=s1, scalar2=s2, op0=o0, op1=o1),
                      reads=reads, writes=writes, n=fsz(out))

        def stt(out, in0, sc, in1, o0, o1, reads, writes):
            return op("dve", lambda e: e.scalar_tensor_tensor(out=out, in0=in0, scalar=sc, in1=in1, op0=o0, op1=o1),
                      reads=reads, writes=writes, n=fsz(out))

        def mm(out, lhsT, rhs, start, stop, reads, writes):
            return op("pe", lambda e: e.matmul(out, lhsT=lhsT, rhs=rhs, start=start, stop=stop),
                      reads=reads, writes=writes, n=fsz(rhs) * (4 if rhs.dtype == F32 else 1))

        def tr(out, in_, ident, reads, writes):
            return op("pe", lambda e: e.transpose(out=out, in_=in_, identity=ident), reads=reads, writes=writes, n=128)

        def cp(eng, out, in_, reads, writes):
            if eng == "act":
                return act(out, in_, AF.Copy, reads, writes)
            return op(eng, lambda e: e.tensor_copy(out=out, in_=in_), reads=reads, writes=writes, n=fsz(out))

        def memset(eng, ap, val, writes):
            return op(eng, lambda e: e.memset(ap, val), writes=writes, n=fsz(ap))

        def bcast_row(dram, n):
            return bass.AP(dram, 0, [[0, 128], [1, n]])

        ident = A("ident", [128, 128], BF16)
        mixT = A("mixT", [128, 8, S_LEN], BF16)
        dma("sp", ident[:, :], ident_d[:, :], writes=["ident"])
        L0 = A.mark()

        hT = A("hT", [128, 8, S_LEN], BF16)
        E1 = A.mark()
        g1 = A("g1", [128, D], F32)
        xt = [A("xt%d" % i, [128, D], F32) for i in range(2)]
        hb = [A("hb%d" % i, [128, D], BF16) for i in range(2)]
        sqj = A("sqj", [128, D], F32)
        st1 = A("st1", [128, 4], F32)
        dma("sp", g1[:, :], bcast_row(g1_d, D), writes=["g1"])

        def norm_to_T(src_ap_fn, src_tiles, gain, gname, dstT, dname, pfx, t, pbank):
            i = t % 2
            ssq = st1[:, 0:1]
            rs = st1[:, 1:2]
            act(sqj[:, :], src_ap_fn(t), AF.Square, src_tiles, [pfx + "sqj", pfx + "ssq"], accum_out=ssq)
            rsqrt_act(rs, ssq, D, [pfx + "ssq"], [pfx + "rs"])
            stt(hb[i][:, :], src_ap_fn(t), rs, gain[:, :], ALU.mult, ALU.mult,
                src_tiles + [pfx + "rs", gname], [pfx + "hb%d" % i])
            pbv = Pb[pbank][:, :].rearrange("p (c n) -> p c n", c=8)
            for kc in range(8):
                tr(pbv[:, kc, :], hb[i][:, kc * 128:(kc + 1) * 128], ident[:, :],
                   [pfx + "hb%d" % i, "ident"], [PN[pbank]])
            cp("dve" if t % 2 else "act", dstT[:, :, t * 128:(t + 1) * 128], pbv, [PN[pbank]], [dname + "_%d" % (t // 4)])

        for t in range(NT):
            i = t % 2
            dma("sp", xt[i][:, :], x_d[t * 128:(t + 1) * 128, :], writes=["xt%d" % i])
            norm_to_T(lambda t, i=i: xt[i][:, :], ["xt%d" % i], g1, "g1", hT, "hT", "A", t, t % 2)
        hT_tiles = ["hT_%d" % k for k in range(4)]
        S.barrier()
        A.reset(E1)
        def checkpoint(name, dump=None, reads=()):
            if stop_after == name:
                if dump is not None and debug:
                    S.barrier()
                    final_ops.append(dma("sp", dbg["gen"][:, :], dump, reads=list(reads)))
                S.stopped = True

        checkpoint("A")

        wm = A("w_in_mla", [128, 8, 416], BF16)
        wqb = A("wqb", [128, 2, 768], BF16)
        wkvb = A("wkvb", [128, 1024], BF16)
        cs = A("cs", [128, NT, 64], F32)
        qhT = A("qhT", [128, 8, S_LEN], BF16)
        khT = A("khT", [128, 8, S_LEN], BF16)
        vA = A("vA", [128, NT, 8, 128], BF16)
        gqa = A("gqa", [128, 384], F32)
        gqk = A("gqkr", [128, 16, 32], F32)
        gcol = A("gcol", [128, 2], F32)
        B1m = A.mark()
        dma("pool", wm[:, :, :], win_d.ap()[:, 1568:1984].rearrange("(c p) n -> p c n", p=128), writes=["wm"])
        dma("pool", wqb[:, :, :], wqb_d.ap().rearrange("(c p) n -> p c n", p=128), writes=["wqb"])
        dma("pool", wkvb[:, :], wkvb_d[:, :], writes=["wkvb"])
        dma("sp", gqa[:, 0:256], bcast_row(gqa_d, 256), writes=["gqa"])
        dma("sp", gqa[:, 256:384], bcast_row(gkva_d, 128), writes=["gqa"])
        dma("sp", gqk[:, 0:8, :], bass.AP(gqn_d, 64, [[0, 128], [0, 8], [1, 32]]), writes=["gqk"])
        dma("sp", gqk[:, 8:16, :], bass.AP(gkn_d, 64, [[0, 128], [0, 8], [1, 32]]), writes=["gqk"])
        memset("pool", gcol[:, :], 1.0, ["gcol"])
        dma("sp", gcol[0:64, 0:1], bass.AP(gqn_d, 0, [[1, 64], [1, 1]]), reads=["gcol"], writes=["gcol"])
        dma("sp", gcol[0:64, 1:2], bass.AP(gkn_d, 0, [[1, 64], [1, 1]]), reads=["gcol"], writes=["gcol"])
        posi = A("posi", [128, NT], I32)
        posf = A("posf", [128, NT], F32)
        invf = A("invf", [128, 16], F32)
        ang = A("ang", [128, NT, 16], F32)
        kk = A("kk", [128, NT, 16], F32)
        ki = A("ki", [128, NT, 16], I32)
        rr = A("rr", [128, NT, 16], F32)
        yy = A("yy", [128, NT, 16], F32)
        m_ = A("m_", [128, NT, 16], F32)
        dma("sp", posi[:, :], pos_d[:, :], writes=["posi"])
        dma("sp", invf[:, :], invf_d[:, :], writes=["invf"])
        cp("dve", posf[:, :], posi[:, :], ["posi"], ["posf"])
        for t in range(NT):
            ts("dve", ang[:, t, :], invf[:, :], posf[:, t:t + 1], None, ALU.mult, None, ["invf", "posf"], ["ang"])
        ts("dve", kk[:, :, :], ang[:, :, :], 1.0 / (2 * PI), None, ALU.mult, None, ["ang"], ["kk"])
        cp("dve", ki[:, :, :], kk[:, :, :], ["kk"], ["ki"])
        cp("dve", kk[:, :, :], ki[:, :, :], ["ki"], ["kk"])
        C1 = 6.28125
        C2 = 2 * PI - C1
        stt(rr[:, :, :], kk[:, :, :], -C1, ang[:, :, :], ALU.mult, ALU.add, ["kk", "ang"], ["rr"])
        stt(rr[:, :, :], kk[:, :, :], -C2, rr[:, :, :], ALU.mult, ALU.add, ["kk", "rr"], ["rr"])
        for which, shift in ((1, 0.0), (0, PI / 2)):
            ts("dve", yy[:, :, :], rr[:, :, :], shift, None, ALU.add, None, ["rr"], ["yy"])
            ts("dve", m_[:, :, :], yy[:, :, :], PI, None, ALU.is_gt, None, ["yy"], ["m_"])
            stt(yy[:, :, :], m_[:, :, :], -2 * PI, yy[:, :, :], ALU.mult, ALU.add, ["m_", "yy"], ["yy"])
            ts("dve", m_[:, :, :], yy[:, :, :], -PI, None, ALU.is_lt, None, ["yy"], ["m_"])
            stt(yy[:, :, :], m_[:, :, :], 2 * PI, yy[:, :, :], ALU.mult, ALU.add, ["m_", "yy"], ["yy"])
            ts("dve", yy[:, :, :], yy[:, :, :], PI, -PI, ALU.min, ALU.max, ["yy"], ["yy"])
            if which == 0:
                act(cs[:, :, 0:16], yy[:, :, :], AF.Sin, ["yy"], ["cs"])
                act(cs[:, :, 16:32], yy[:, :, :], AF.Sin, ["yy"], ["cs"])
            else:
                act(cs[:, :, 48:64], yy[:, :, :], AF.Sin, ["yy"], ["cs"])
                act(cs[:, :, 32:48], cs[:, :, 48:64], AF.Copy, ["cs"], ["cs"], scale=-1.0)
        memset("pool", vA[:, :, :, :], 1.0, ["vA"])
        S.barrier()
        A.reset(B1m)

        sqjb1 = A("sqjb", [128, 416], BF16)
        sqjb = [sqjb1, sqjb1]
        stq = [A("stq%d" % i, [128, 32], F32) for i in range(2)]
        ab = [A("ab%d" % i, [128, 384], BF16) for i in range(2)]
        abT = [A("abT%d" % i, [128, 3, 128], BF16) for i in range(2)]
        kraw = [A("kraw%d" % i, [128, 8, 96], F32) for i in range(2)]
        sqn = A("sqn", [128, 16, 96], BF16)
        rg = [A("rg%d" % i, [128, 16, 32], F32) for i in range(2)]
        rg2 = A("rg2", [128, 16, 48], F32)
        rb = A("rb", [128, 16, 32], F32)
        qkf = [A("qkf%d" % i, [128, 16, 96], BF16) for i in range(2)]
        SQ2 = math.sqrt(2.0)

        def st_E1a(t):
            i = t % 2
            sI = "_%d" % i
            tsl = slice(t * 128, (t + 1) * 128)
            hTt = "hT_%d" % (t // 4)
            for kc in range(8):
                mm(P[0][:, 0:416], hT[:, kc, tsl], wm[:, kc, :], kc == 0, kc == 7, [hTt, "wm"], ["P0"])
            act(sqjb[i][:, 0:256], P[0][:, 0:256], AF.Square, ["P0"], ["stqA" + sI], accum_out=stq[i][:, 0:1])
            act(sqjb[i][:, 256:384], P[0][:, 256:384], AF.Square, ["P0"], ["stqA" + sI],
                accum_out=stq[i][:, 1:2], scale=SQ2)
            act(kraw[i][:, :, 64:96], bcast_ap(P[0][:, 384:416], [[0, 8], [1, 32]]), AF.Copy, ["P0"], ["krawR" + sI])
            rsqrt_act(stq[i][:, 2:4], stq[i][:, 0:2], 256, ["stqA" + sI], ["stqB" + sI])
            stt(ab[i][:, 0:256], P[0][:, 0:256], stq[i][:, 2:3], gqa[:, 0:256], ALU.mult, ALU.mult,
                ["P0", "stqB" + sI, "gqa"], ["ab" + sI])
            stt(ab[i][:, 256:384], P[0][:, 256:384], stq[i][:, 3:4], gqa[:, 256:384], ALU.mult, ALU.mult,
                ["P0", "stqB" + sI, "gqa"], ["ab" + sI])
        def st_E1b(t):
            i = t % 2
            sI = "_%d" % i
            tsl = slice(t * 128, (t + 1) * 128)
            hTt = "hT_%d" % (t // 4)
            p1v = Pb[1][:, 0:384].rearrange("p (c n) -> p c n", c=3)
            for c in range(3):
                tr(p1v[:, c, :], ab[i][:, c * 128:(c + 1) * 128], ident[:, :], ["ab" + sI, "ident"], ["P1"])
            cp("act", abT[i][:, :, :], p1v, ["P1"], ["abT" + sI])
        def st_E2(t):
            i = t % 2
            sI = "_%d" % i
            tsl = slice(t * 128, (t + 1) * 128)
            hTt = "hT_%d" % (t // 4)
            for nb in range(2):
                for kc in range(2):
                    mm(P[2 + nb][:, 0:384], abT[i][:, kc, :], wqb[:, kc, nb * 384:(nb + 1) * 384], kc == 0, kc == 1,
                       ["abT" + sI, "wqb"], [PN[2 + nb]])
                mm(P[4 + nb][:, :], abT[i][:, 2, :], wkvb[:, nb * 512:(nb + 1) * 512], True, True,
                   ["abT" + sI, "wkvb"], [PN[4 + nb]])
            for nb in range(2):
                srck = P[4 + nb][:, :].rearrange("p (h d) -> p h d", h=4)[:, :, 0:64]
                cp("act", kraw[i][:, nb * 4:nb * 4 + 4, 0:64], srck, [PN[4 + nb]], ["krawN" + sI])
                srcv = P[4 + nb][:, :].rearrange("p (a b d) -> p a b d", a=2, b=2)
                dstv = vA[:, t, nb * 4:nb * 4 + 4, :].rearrange("p (a b) d -> p a b d", b=2)
                cp("act", dstv[:, :, 0, 0:64], srcv[:, :, 0, 64:128], [PN[4 + nb]], ["vA"])
                cp("act", dstv[:, :, 1, 64:128], srcv[:, :, 1, 64:128], [PN[4 + nb]], ["vA"])
            for nb in range(2):
                act(sqn[:, nb * 4:nb * 4 + 4, :], P[2 + nb][:, 0:384].rearrange("p (h d) -> p h d", h=4), AF.Square,
                    [PN[2 + nb]], ["sqn"])
            act(sqn[:, 8:16, :], kraw[i][:, :, :], AF.Square, ["krawN" + sI, "krawR" + sI], ["sqn"])
            op("dve", lambda e, i=i: e.tensor_reduce(out=stq[i][:, 8:24], in_=sqn[:, :, :], axis=AX.X, op=ALU.add),
               reads=["sqn"], writes=["stqC" + sI])
            rsqrt_act(stq[i][:, 8:24], stq[i][:, 8:24], 96, ["stqC" + sI], ["stqC" + sI])
            for nb in range(2):
                pv = P[2 + nb][:, 0:384].rearrange("p (h d) -> p h d", h=4)
                rq = stq[i][:, 8 + nb * 4:9 + nb * 4]
                tt("dve", qkf[i][:, nb * 4:nb * 4 + 4, 0:64], pv[:, :, 0:64], bcast_ap(rq, [[1, 4], [0, 64]]), ALU.mult,
                   [PN[2 + nb], "stqC" + sI], ["qkf" + sI])
                tt("dve", rg[i][:, nb * 4:nb * 4 + 4, :], pv[:, :, 64:96], bcast_ap(rq, [[1, 4], [0, 32]]), ALU.mult,
                   [PN[2 + nb], "stqC" + sI], ["rg" + sI])
            rk = stq[i][:, 16:17]
            tt("dve", qkf[i][:, 8:16, 0:64], kraw[i][:, :, 0:64], bcast_ap(rk, [[1, 8], [0, 64]]), ALU.mult,
               ["krawN" + sI, "stqC" + sI], ["qkf" + sI])
            tt("dve", rg[i][:, 8:16, :], kraw[i][:, :, 64:96], bcast_ap(rk, [[1, 8], [0, 32]]), ALU.mult,
               ["krawR" + sI, "stqC" + sI], ["rg" + sI])
        def st_L(t):
            i = t % 2
            sI = "_%d" % i
            tsl = slice(t * 128, (t + 1) * 128)
            hTt = "hT_%d" % (t // 4)
            tt("pool", rg2[:, :, 0:32], rg[i][:, :, :], gqk[:, :, :], ALU.mult, ["rg" + sI, "gqk"], ["rg2"])
            tt("pool", rg2[:, :, 32:48], rg[i][:, :, 0:16], gqk[:, :, 0:16], ALU.mult, ["rg" + sI, "gqk"], ["rg2"])
            c1 = bcast_ap(cs[:, t, 0:32], [[0, 16], [1, 32]])
            c2 = bcast_ap(cs[:, t, 32:64], [[0, 16], [1, 32]])
            tt("pool", rg[i][:, :, :], rg2[:, :, 0:32], c1, ALU.mult, ["rg2", "cs"], ["rg" + sI])
            tt("pool", rb[:, :, :], rg2[:, :, 16:48], c2, ALU.mult, ["rg2", "cs"], ["rb"])
            tt("pool", qkf[i][:, :, 64:96], rg[i][:, :, :], rb[:, :, :], ALU.add, ["rg" + sI, "rb"], ["qkf" + sI])
            p6v = Pb[6][:, :].rearrange("p (h n) -> p h n", h=8)
            p7v = Pb[7][:, :].rearrange("p (h n) -> p h n", h=8)
            for h in range(8):
                tr(p6v[0:96, h, :], qkf[i][:, h, :], ident[:, :], ["qkf" + sI, "ident"], ["P6"])
            for h in range(8):
                tr(p7v[0:96, h, :], qkf[i][:, 8 + h, :], ident[:, :], ["qkf" + sI, "ident"], ["P7"])
            ts("dve", qhT[0:96, :, tsl], p6v[0:96, :, :], gcol[0:96, 0:1], None, ALU.mult, None, ["P6", "gcol"],
               ["qhT_%d" % (t // 4)])
            act(khT[0:96, :, tsl], p7v[0:96, :, :], AF.Identity, ["P7", "gcol"], ["khT"], scale=gcol[0:96, 1:2])

        S.noresched.add(S.seg)
        for step in range(NT + 2):
            if step < NT:
                st_E1a(step)
            if 0 <= step - 1 < NT:
                st_E2(step - 1)
            if 0 <= step - 2 < NT:
                st_L(step - 2)
            if step < NT:
                st_E1b(step)
        S.barrier()
        A.reset(B1m)
        checkpoint("B1")
        pbuf = [A("pbuf%d" % i, [128, 512], BF16, at=E1 + i * 1024) for i in range(4)]
        rcb = A("rcb", [128, 512], F32, at=E1 + 4096)
        wg = A("w_in_gla", [128, 8, 1568], BF16)
        wlr = A("wlr", [128, 8, 64], BF16)
        dma("pool", wg[:, :, :], win_d.ap()[:, 0:1568].rearrange("(c p) n -> p c n", p=128), writes=["wg"])
        memset("pool", wlr[:, :, :], 0.0, ["wlr"])
        dma("pool", wlr[:, :, 0:16], win_d.ap()[:, 1536:1552].rearrange("(c p) n -> p c n", p=128), reads=["wlr"], writes=["wlr"])
        dma("pool", wlr[:, :, 32:48], win_d.ap()[:, 1552:1568].rearrange("(c p) n -> p c n", p=128), reads=["wlr"], writes=["wlr"])
        scale = 96 ** -0.5
        it = 0
        for h in range(8):
            even = (h % 2 == 0)
            vrows = slice(0, 64) if even else slice(64, 128)
            srows = slice(64, 128) if even else slice(0, 64)
            for qg in range(4):
                qsl = slice(qg * 512, (qg + 1) * 512)
                ob = 4 + (it % 2)
                seq = []
                for kt in range(16):
                    seq.append(("s", kt))
                    if kt >= 2:
                        seq.append(("pv", kt - 2))
                seq += [("pv", 14), ("pv", 15)]
                for kind, kt in seq:
                    sb_ = kt % 3
                    pi = kt % 4
                    if kind == "s":
                        mm(P[sb_][:, :], khT[0:96, h, kt * 128:(kt + 1) * 128], qhT[0:96, h, qsl], True, True,
                           ["khT", "qhT_%d" % qg], [PN[sb_]])
                        act(pbuf[pi][:, :], P[sb_][:, :], AF.Exp, [PN[sb_]], ["pbuf%d" % pi], scale=scale)
                    else:
                        lhsT = vA[:, kt, h, :]
                        mm(P[ob][:, :], lhsT, pbuf[pi][:, :], kt == 0, kt == 15, ["vA", "pbuf%d" % pi], [PN[ob]])
                op("dve", lambda e, vrows=vrows, srows=srows, ob=ob: e.reciprocal(out=rcb[vrows, :], in_=P[ob][srows, :]),
                   reads=[PN[ob]], writes=["rcb"], n=4096)
                tt("dve", mixT[vrows, 4 + h // 2, qsl], P[ob][vrows, :], rcb[vrows, :], ALU.mult, [PN[ob], "rcb"], ["mixT_m"])
                it += 1
        S.barrier()
        A.reset(E1)

        checkpoint("C")
        A.off += 25088
        R2 = A.mark()
        qkT = A("qkT", [128, 4, S_LEN], F32)
        vtok = A("vtok", [128, NT, 512], BF16)
        sgT = A("sgT", [128, 4, S_LEN], BF16)
        lrT = A("lrT", [64, S_LEN], F32)
        lrb = A("lrb", [64, 1], F32)
        waug = A("waug", [64, 512], F32)
        masks = A("masks", [128, 256], BF16)
        gout = A("gout", [128, 1], F32)
        onesf = A("onesf", [128, 128], F32)
        R4 = A.mark()
        dma("sp", lrb[:, :], lrb_d[:, :], writes=["lrb"])
        memset("pool", waug[:, :], 0.0, ["waug"])
        dma("sp", waug[0:16, 0:256], gkf_w[:, :], reads=["waug"], writes=["waug"])
        dma("sp", waug[16:17, 0:256], gkf_b[:, :], reads=["waug"], writes=["waug"])
        dma("sp", waug[16:17, 256:512], gkb_b[:, :], reads=["waug"], writes=["waug"])
        dma("sp", waug[32:48, 256:512], gkb_w[:, :], reads=["waug"], writes=["waug"])
        dma("sp", masks[:, :], masks_d[:, :], writes=["masks"])
        dma("sp", gout[:, :], go_d[:, :], writes=["gout"])
        memset("pool", onesf[:, :], 1.0, ["onesf"])
        blk = 0
        for kind, idx in [("q", 0), ("q", 1), ("k", 0), ("k", 1), ("g", 0), ("g", 1), ("g", 2), ("g", 3), ("lr", 0)]:
            for tg in range(4):
                pbk = blk % 4
                blk += 1
                tgs = slice(tg * 512, (tg + 1) * 512)
                for kc in range(8):
                    if kind == "q":
                        lhsT = wg[:, kc, idx * 128:(idx + 1) * 128]
                    elif kind == "k":
                        lhsT = wg[:, kc, 256 + idx * 128:256 + (idx + 1) * 128]
                    elif kind == "g":
                        lhsT = wg[:, kc, 1024 + idx * 128:1024 + (idx + 1) * 128]
                    else:
                        lhsT = wlr[:, kc, :]
                    mrows = 64 if kind == "lr" else 128
                    mm(P[pbk][0:mrows, :], lhsT, hT[:, kc, tgs], kc == 0, kc == 7, ["hT_%d" % tg, "wg", "wlr"], [PN[pbk]])
                if kind == "q":
                    act(qkT[:, idx, tgs], P[pbk][:, :], AF.Copy, [PN[pbk]], ["qT"], scale=0.125)
                elif kind == "k":
                    cp("dve", qkT[:, 2 + idx, tgs], P[pbk][:, :], [PN[pbk]], ["kT"])
                elif kind == "g":
                    act(sgT[:, idx, tgs], P[pbk][:, :], AF.Silu, [PN[pbk]], ["sgT"])
                else:
                    act(lrT[:, tgs], P[pbk][0:64, :], AF.Identity, [PN[pbk], "lrb"], ["lrT"], bias=lrb[:, :])
        for t in range(NT):
            pbk = 4 + t % 2
            tsl = slice(t * 128, (t + 1) * 128)
            for kc in range(8):
                mm(P[pbk][:, :], hT[:, kc, tsl], wg[:, kc, 512:1024], kc == 0, kc == 7, ["hT_%d" % (t // 4), "wg"], [PN[pbk]])
            cp("dve" if t % 2 else "act", vtok[:, t, :], P[pbk][:, :], [PN[pbk]], ["vtok"])
        S.barrier()

        checkpoint("B2")
        HTB = L0
        tmp = [A("gt%d" % i, [128, 1024], F32, at=HTB + i * 4096) for i in range(4)]
        prod = {}
        names = [(d_, hp, k_) for d_ in (0, 1) for hp in (0, 1) for k_ in ("qr", "kr", "qb")]
        slots = [HTB + 16384 + i * 4096 for i in range(4)] + [E1 + 16384 + i * 4096 for i in range(2)]
        for i, nm in enumerate(names):
            if i < 6:
                prod[nm] = A("pr", [128, S_LEN], BF16, at=slots[i])
            else:
                prod[nm] = A("pr", [128, S_LEN], BF16)
        kdT = A("kdT", [128, 1024], BF16)
        dec = A("dec", [128, 4, 32], F32)
        smask = A("smask", [128, 1024], F32)
        kd = A("kd", [128, NT, 512], BF16, at=E1)
        memset("pool", smask[:, :], 1.0, ["smask"])
        memset("pool", smask[:, :].rearrange("p (c j) -> p c j", j=64)[:, :, 0:1], 0.0, ["smask"])
        G, Fc, Dt, Eb = tmp
        Dt2 = A("Dt2", [128, 1024], F32)
        Eb2 = A("Eb2", [128, 1024], F32)
        DtL = [(Dt, "Dt"), (Dt2, "Dt2")]
        EbL = [(Eb, "Eb"), (Eb2, "Eb2")]
        cnt = {"d": 0, "e": 0}

        def nextD():
            cnt["d"] += 1
            return DtL[cnt["d"] % 2]

        def nextE():
            cnt["e"] += 1
            return EbL[cnt["e"] % 2]

        def exp_prod(src, sname, scl, dst, base, bname, dname="prod"):
            E_, en = nextE()
            act(E_[:, :], src, AF.Exp, [sname], [en], scale=scl)
            tt("pool", dst, base, E_[:, :], ALU.mult, [bname, en], [dname])
        for d_ in (0, 1):
            for hp in (0, 1):
                dh = d_ * 2 + hp
                qT = qkT[:, hp, :]
                kT = qkT[:, 2 + hp, :]
                for half in range(2):
                    hs = slice(half * 1024, (half + 1) * 1024)
                    for j in range(2):
                        pbk = j
                        cols = slice(half * 1024 + j * 512, half * 1024 + (j + 1) * 512)
                        mm(P[pbk][:, :], waug[0:64, dh * 128:(dh + 1) * 128], lrT[0:64, cols], True, True,
                           ["waug", "lrT"], [PN[pbk]])
                        act(Eb[:, j * 512:(j + 1) * 512], P[pbk][:, :], AF.Exp, [PN[pbk]], ["Eb"], scale=-1.0)
                    act(G[:, :], Eb[:, :], AF.Ln, ["Eb"], ["G"], bias=1.0)
                    op("dve", lambda e: e.tensor_tensor_scan(out=Fc[:, :], data0=smask[:, :], data1=G[:, :], initial=0.0,
                                                             op0=ALU.mult, op1=ALU.add), reads=["smask", "G"], writes=["Fc"])
                    Fv = Fc[:, :].rearrange("p (c j) -> p c j", j=64)
                    Dv = Dt[:, :].rearrange("p (c j) -> p c j", j=64)
                    T63 = bcast_ap(Fc[:, 63:64], [[64, 16], [0, 64]])
                    act(dec[:, dh, half * 16:(half + 1) * 16], Fv[:, :, 63], AF.Exp, ["Fc"], ["dec"], scale=-1.0 / 16)
                    if d_ == 0:
                        ref = bcast_ap(Fc[:, 31:32], [[64, 16], [0, 64]])
                        D_, dn = nextD()
                        tt("dve", D_[:, :].rearrange("p (c j) -> p c j", j=64), Fv, ref, ALU.subtract, ["Fc"], [dn])
                        exp_prod(D_[:, :], dn, -1.0 / 16, prod[(0, hp, "qr")][:, hs], qT[:, hs], "qT")
                        exp_prod(D_[:, :], dn, 1.0 / 16, prod[(0, hp, "kr")][:, hs], kT[:, hs], "kT")
                        exp_prod(Fc[:, :], "Fc", -1.0 / 16, prod[(0, hp, "qb")][:, hs], qT[:, hs], "qT")
                        D_, dn = nextD()
                        tt("dve", D_[:, :].rearrange("p (c j) -> p c j", j=64), Fv, T63, ALU.subtract, ["Fc"], [dn])
                        exp_prod(D_[:, :], dn, 1.0 / 16, kdT[:, :], kT[:, hs], "kT", "kdT")
                    else:
                        tt("dve", G[:, :], Fc[:, :], G[:, :], ALU.subtract, ["Fc", "G"], ["G"])
                        Gv = G[:, :].rearrange("p (c j) -> p c j", j=64)
                        ref = bcast_ap(G[:, 32:33], [[64, 16], [0, 64]])
                        D_, dn = nextD()
                        tt("dve", D_[:, :].rearrange("p (c j) -> p c j", j=64), Gv, ref, ALU.subtract, ["G"], [dn])
                        exp_prod(D_[:, :], dn, 1.0 / 16, prod[(1, hp, "qr")][:, hs], qT[:, hs], "qT")
                        exp_prod(D_[:, :], dn, -1.0 / 16, prod[(1, hp, "kr")][:, hs], kT[:, hs], "kT")
                        D_, dn = nextD()
                        tt("dve", D_[:, :].rearrange("p (c j) -> p c j", j=64), Gv, T63, ALU.subtract, ["G", "Fc"], [dn])
                        exp_prod(D_[:, :], dn, 1.0 / 16, prod[(1, hp, "qb")][:, hs], qT[:, hs], "qT")
                        exp_prod(G[:, :], "G", -1.0 / 16, kdT[:, :], kT[:, hs], "kT", "kdT")
                    for g4 in range(2):
                        pbk = 2 + g4
                        pv = Pb[pbk][:, 0:512].rearrange("p (t n) -> p t n", t=4)
                        for tq in range(4):
                            c0 = (g4 * 4 + tq) * 128
                            tr(pv[:, tq, :], kdT[:, c0:c0 + 128], ident[:, :], ["kdT", "ident"], [PN[pbk]])
                        t0 = half * 8 + g4 * 4
                        cp("dve", kd[:, t0:t0 + 4, dh * 128:(dh + 1) * 128], pv, [PN[pbk]], ["kd"])
        S.barrier()

        checkpoint("D1")
        qk_off = R2
        Sst = [A("Sst%d" % i, [128, 32, 128], BF16, at=qk_off + i * 8192) for i in range(4)]
        Sf = [A("Sf%d" % i, [128, 256], F32, at=HTB + i * 1024) for i in range(4)]
        for dh in range(4):
            memset("pool", Sf[dh][:, :], 0.0, ["Sf%d" % dh])
        for step in range(32):
            for dh in range(4):
                d_, hp = divmod(dh, 2)
                n = step if d_ == 0 else 31 - step
                t, c = divmod(n, 2)
                rows = slice(c * 64, (c + 1) * 64)
                cp("pool", Sst[dh][0:64, n, :], Sf[dh][0:64, 0:128], ["Sf%d" % dh], ["SstA%d" % dh])
                cp("act", Sst[dh][64:128, n, :], Sf[dh][64:128, 128:256], ["Sf%d" % dh], ["SstB%d" % dh])
                if step == 31:
                    continue
                pbk = 4 * c + dh
                mm(P[pbk][:, 0:256], kd[rows, t, dh * 128:(dh + 1) * 128], vtok[rows, t, hp * 256:(hp + 1) * 256],
                   True, True, ["kd", "vtok"], [PN[pbk]])
                stt(Sf[dh][:, :], Sf[dh][:, :], dec[:, dh, n:n + 1], P[pbk][:, 0:256], ALU.mult, ALU.add,
                    ["Sf%d" % dh, "dec", PN[pbk]], ["Sf%d" % dh])
        S.barrier()

        checkpoint("D2")
        smb = [A("smb%d" % i, [128, 2, 2, 128], BF16, at=HTB + 4096 + i * 1024) for i in range(2)]
        sqoL = [A("sqo%d" % i, [128, 256], F32, at=HTB + 6144 + i * 1024) for i in range(2)]
        rsoL = [A("rso%d" % i, [128, 256], F32, at=HTB + 8192 + i * 1024) for i in range(2)]
        t1oL = [A("t1o%d" % i, [128, 256], F32, at=HTB + 10240 + i * 1024) for i in range(2)]
        for t in range(NT):
            tsl = slice(t * 128, (t + 1) * 128)
            for par in range(2):
                rows = slice(par * 64, (par + 1) * 64)
                sbk = par
                obk = 2 + par
                scv = P[sbk][:, :].rearrange("p (a b n) -> p a b n", a=2, b=2)
                for hp in range(2):
                    for d_ in range(2):
                        mm(scv[:, hp, d_, :], prod[(d_, hp, "kr")][rows, tsl], prod[(d_, hp, "qr")][rows, tsl], True, True,
                           ["prod"], [PN[sbk]])
                mk = bcast_ap(masks[:, 0:256], [[0, 2], [1, 256]])
                tt("dve", smb[par][:, :, :, :].rearrange("p a b n -> p a (b n)"),
                   P[sbk][:, :].rearrange("p (a m) -> p a m", a=2), mk, ALU.mult, [PN[sbk], "masks"], ["smb%d" % par])
                ov = P[obk][:, 0:256].rearrange("p (a n) -> p a n", a=2)
                for hp in range(2):
                    h = hp * 2 + par
                    mm(ov[:, hp, :], vtok[:, t, h * 128:(h + 1) * 128], smb[par][:, hp, 0, :], True, False,
                       ["vtok", "smb%d" % par], [PN[obk]])
                    mm(ov[:, hp, :], vtok[:, t, h * 128:(h + 1) * 128], smb[par][:, hp, 1, :], False, False,
                       ["vtok", "smb%d" % par], [PN[obk]])
                    for d_ in range(2):
                        dh = d_ * 2 + hp
                        for c in range(2):
                            n = t * 2 + c
                            csl = slice(t * 128 + c * 64, t * 128 + (c + 1) * 64)
                            last = (d_ == 1 and c == 1)
                            mm(ov[:, hp, c * 64:(c + 1) * 64], Sst[dh][rows, n, :], prod[(d_, hp, "qb")][rows, csl],
                               False, last, ["SstA%d" % dh, "SstB%d" % dh, "prod"], [PN[obk]])
                sqo, rso, t1o = sqoL[par], rsoL[par], t1oL[par]
                sP = "%d" % par
                act(sqo[:, :], P[obk][:, 0:256], AF.Square, [PN[obk]], ["sqo" + sP])
                ebk = 4 + par
                mm(P[ebk][:, 0:256], onesf[:, :], sqo[:, :], True, True, ["onesf", "sqo" + sP], [PN[ebk]])
                rsqrt_act(rso[:, :], P[ebk][:, 0:256], 128, [PN[ebk]], ["rso" + sP])
                stt(t1o[:, :], P[obk][:, 0:256], gout[:, 0:1], rso[:, :], ALU.mult, ALU.mult, [PN[obk], "gout", "rso" + sP], ["t1o" + sP])
                for hp in range(2):
                    h = hp * 2 + par
                    tt("pool", mixT[:, h, tsl], t1o[:, hp * 128:(hp + 1) * 128], sgT[:, h, tsl], ALU.mult,
                       ["t1o" + sP, "sgT"], ["mixT_g"])
        S.barrier()
        A.reset(L0)
        if debug:
            final_ops.append(dma("sp", dbg["mix"][:, :], mixT[:, :, :].rearrange("p c n -> p (c n)"), reads=["mixT_g", "mixT_m"]))

        checkpoint("D3")
        X = A("X", [128, NT, D], F32)
        h2T = A("h2T", [128, 8, S_LEN], BF16)
        wo = A("wo", [128, 8, D], BF16)
        WO_OFF = A.off - 16384
        g2 = A("g2", [128, D], F32)
        wr = A("wr", [128, 8, 36], BF16)
        rbias = A("rbias", [128, 36], F32)
        gTf = A("gTf", [32, S_LEN], F32)
        identf = A("identf", [128, 128], F32)
        cp("dve", identf[:, :], ident[:, :], ["ident"], ["identf"])
        gwb = [A("gwb%d" % i, [128, 512], F32) for i in range(4)]
        lgA = A("lgA", [128, NT, 36], F32)
        hb = [A("hb2_%d" % i, [128, D], BF16) for i in range(2)]
        sqj = A("sqj2", [128, D], BF16)
        st1 = A("st1_2", [128, 4], F32)
        dma("pool", wo[:, :, :], wout_d.ap().rearrange("(c p) n -> p c n", p=128), writes=["wo"])
        dma("sp", g2[:, :], bcast_row(g2_d, D), writes=["g2"])
        dma("pool", wr[:, :, 0:4], wrg_d.ap().rearrange("(c p) n -> p c n", p=128), writes=["wr"])
        dma("pool", wr[:, :, 4:36], wre_d.ap().rearrange("(c p) n -> p c n", p=128), writes=["wr"])
        dma("sp", rbias[:, 0:4], bcast_row(brg_d, 4), writes=["rbias"])
        dma("sp", rbias[:, 4:36], bcast_row(bre_d, 32), writes=["rbias"])
        for t in range(NT):
            tsl = slice(t * 128, (t + 1) * 128)
            dma("sp", X[:, t, :], x_d[tsl, :], writes=["X%d" % t])
            for ch in range(2):
                pbk = (t % 2) * 2 + ch
                for kc in range(8):
                    mm(P[pbk][:, :], mixT[:, kc, tsl], wo[:, kc, ch * 512:(ch + 1) * 512], kc == 0, kc == 7,
                       ["mixT_g", "mixT_m", "wo"], [PN[pbk]])
                tt("dve", X[:, t, ch * 512:(ch + 1) * 512], X[:, t, ch * 512:(ch + 1) * 512], P[pbk][:, :], ALU.add,
                   ["X%d" % t, PN[pbk]], ["X%d" % t])
        if debug:
            for t in range(NT):
                final_ops.append(dma("sp", dbg["x1"][t * 128:(t + 1) * 128, :], X[:, t, :], reads=["X%d" % t]))
        checkpoint("E")
        for t in range(NT):
            tsl = slice(t * 128, (t + 1) * 128)
            norm_to_T(lambda t: X[:, t, :], ["X%d" % t], g2, "g2", h2T, "h2T", "F", t, 4 + t % 2)
            rbk = 6 + t % 2
            for kc in range(8):
                mm(P[rbk][:, 0:36], h2T[:, kc, tsl], wr[:, kc, :], kc == 0, kc == 7, ["h2T_%d" % (t // 4), "wr"], [PN[rbk]])
            tt("dve", lgA[:, t, :], P[rbk][:, 0:36], rbias[:, :], ALU.add, [PN[rbk], "rbias"], ["lgA"])

        def bl(ap2, k):
            return bcast_ap(ap2, [list(ap2.ap[1]), [0, k]])

        def red(out, in_, o, reads, writes):
            return op("dve", lambda e: e.tensor_reduce(out=out, in_=in_, axis=AX.X, op=o), reads=reads, writes=writes,
                      n=fsz(in_))

        r16 = lambda nm: A(nm, [128, NT], F32)
        r4 = lambda nm: A(nm, [128, NT, 4], F32)
        r8 = lambda nm: A(nm, [128, NT, 8], F32)
        mg, s4, ptop, m1, m2, dm, e2, den, w1, w2 = [r16("r16_%d" % i) for i in range(10)]
        d4, e4, oh, ohp = [r4("r4_%d" % i) for i in range(4)]
        ls, tmp8, eq1, ls2, eq2, wg8 = [r8("r8_%d" % i) for i in range(6)]
        gate = A("gate", [128, NT, 32], F32)
        red(mg[:, :], lgA[:, :, 0:4], ALU.max, ["lgA"], ["mg"])
        tt("dve", d4[:, :, :], lgA[:, :, 0:4], bl(mg[:, :], 4), ALU.subtract, ["lgA", "mg"], ["d4"])
        act(e4[:, :, :], d4[:, :, :], AF.Exp, ["d4"], ["e4"])
        red(s4[:, :], e4[:, :, :], ALU.add, ["e4"], ["s4"])
        op("dve", lambda e: e.reciprocal(out=ptop[:, :], in_=s4[:, :]), reads=["s4"], writes=["ptop"], n=128)
        ts("dve", oh[:, :, :], d4[:, :, :], 0.0, None, ALU.is_equal, None, ["d4"], ["oh"])
        tt("dve", ohp[:, :, :], oh[:, :, :], bl(ptop[:, :], 4), ALU.mult, ["oh", "ptop"], ["ohp"])
        tt("dve", ls[:, :, :], lgA[:, :, 4:12], bl(oh[:, :, 0], 8), ALU.mult, ["lgA", "oh"], ["ls"])
        for g_ in range(1, 4):
            tt("dve", tmp8[:, :, :], lgA[:, :, 4 + 8 * g_:12 + 8 * g_], bl(oh[:, :, g_], 8), ALU.mult, ["lgA", "oh"], ["tmp8"])
            tt("dve", ls[:, :, :], ls[:, :, :], tmp8[:, :, :], ALU.add, ["ls", "tmp8"], ["ls"])
        red(m1[:, :], ls[:, :, :], ALU.max, ["ls"], ["m1"])
        tt("dve", eq1[:, :, :], ls[:, :, :], bl(m1[:, :], 8), ALU.is_equal, ["ls", "m1"], ["eq1"])
        stt(ls2[:, :, :], eq1[:, :, :], -1e30, ls[:, :, :], ALU.mult, ALU.add, ["eq1", "ls"], ["ls2"])
        red(m2[:, :], ls2[:, :, :], ALU.max, ["ls2"], ["m2"])
        tt("dve", eq2[:, :, :], ls2[:, :, :], bl(m2[:, :], 8), ALU.is_equal, ["ls2", "m2"], ["eq2"])
        tt("dve", dm[:, :], m2[:, :], m1[:, :], ALU.subtract, ["m1", "m2"], ["dm"])
        act(e2[:, :], dm[:, :], AF.Exp, ["dm"], ["e2"])
        ts("dve", den[:, :], e2[:, :], 1.0, None, ALU.add, None, ["e2"], ["den"])
        op("dve", lambda e: e.reciprocal(out=w1[:, :], in_=den[:, :]), reads=["den"], writes=["w1"], n=128)
        tt("dve", w2[:, :], e2[:, :], w1[:, :], ALU.mult, ["e2", "w1"], ["w2"])
        tt("dve", wg8[:, :, :], eq1[:, :, :], bl(w1[:, :], 8), ALU.mult, ["eq1", "w1"], ["wg8"])
        tt("dve", tmp8[:, :, :], eq2[:, :, :], bl(w2[:, :], 8), ALU.mult, ["eq2", "w2"], ["tmp8"])
        tt("dve", wg8[:, :, :], wg8[:, :, :], tmp8[:, :, :], ALU.add, ["wg8", "tmp8"], ["wg8"])
        for g_ in range(4):
            tt("dve", gate[:, :, g_ * 8:(g_ + 1) * 8], wg8[:, :, :], bl(ohp[:, :, g_], 8), ALU.mult, ["wg8", "ohp"], ["gate"])
        if debug:
            for t in range(NT):
                final_ops.append(dma("sp", dbg["gate"][t * 128:(t + 1) * 128, :], gate[:, t, :], reads=["gate"]))
        for q4 in range(4):
            bk = 4 + q4
            for tq in range(4):
                t = q4 * 4 + tq
                tr(P[bk][0:32, tq * 128:(tq + 1) * 128], gate[:, t, :], identf[:, :], ["gate", "identf"], [PN[bk]])
            cp("act" if q4 % 2 else "dve", gTf[0:32, q4 * 512:(q4 + 1) * 512], P[bk][0:32, :], [PN[bk]], ["gTf"])
        dma("sp", gdr[:, :], gTf[0:32, :], reads=["gTf"], writes=["gdr"])
        checkpoint("F")

        EG = 2
        NEG = 32 // EG
        MX = SB_BASE + 256
        S.alias(["hid0", "hid1", "sil0", "sil1", "t1m0", "t1m1", "wdn0", "wdn1"], ["mixT_g", "mixT_m"])
        hid = [A("hid%d" % b, [128, EG, 2, 512], BF16, at=MX + b * 4096) for b in range(2)]
        sil = [A("sil%d" % b, [128, 512], F32, at=MX + 8192 + b * 2048) for b in range(2)]
        t1m = [A("t1m%d" % b, [128, 512], F32, at=MX + 12288 + b * 2048) for b in range(2)]
        wdn = [[A("wd%d_%d" % (b, j), [128, 2, D], BF16, at=MX + 16384 + (b * EG + j) * 4096) for j in range(EG)] for b in range(2)]
        wgt = [None, None]
        wup = [None, None]
        wgt[0] = [A("wg0_%d" % j, [128, 8, 256], BF16) for j in range(EG)]
        wup[0] = [A("wu0_%d" % j, [128, 8, 256], BF16) for j in range(EG)]
        wgt[1] = [A("wg1_%d" % j, [128, 8, 256], BF16, at=WO_OFF + j * 4096) for j in range(EG)]
        wup[1] = [A("wu1_%d" % j, [128, 8, 256], BF16, at=WO_OFF + 8192 + j * 4096) for j in range(EG)]
        itc = 0
        gcnt = [0]
        for eg in range(NEG):
            b = eg % 2
            extra = ["wo"] if b == 1 else []
            for j in range(EG):
                e_ = eg * EG + j
                dma("pool", wgt[b][j][:, :, :], weg_d.ap()[e_].rearrange("(c p) f -> p c f", p=128), writes=["wgt%d" % b] + extra)
                dma("pool", wup[b][j][:, :, :], weu_d.ap()[e_].rearrange("(c p) f -> p c f", p=128), writes=["wup%d" % b] + extra)
                dma("pool", wdn[b][j][:, :, :], wed_d.ap()[e_].rearrange("(c p) d -> p c d", p=128), writes=["wdn%d" % b])
            for tg in range(4):
                hbi = itc % 2
                itc += 1
                tgs = slice(tg * 512, (tg + 1) * 512)
                for j in range(EG):
                    e_ = eg * EG + j
                    gbk = 6 + j
                    for fh in range(2):
                        k2 = fh
                        gb_, ub_ = 0 + k2, 2 + k2
                        for kc in range(8):
                            mm(P[gb_][:, :], wgt[b][j][:, kc, fh * 128:(fh + 1) * 128], h2T[:, kc, tgs], kc == 0, kc == 7,
                               ["wgt%d" % b, "h2T_%d" % tg], [PN[gb_]])
                        for kc in range(8):
                            mm(P[ub_][:, :], wup[b][j][:, kc, fh * 128:(fh + 1) * 128], h2T[:, kc, tgs], kc == 0, kc == 7,
                               ["wup%d" % b, "h2T_%d" % tg], [PN[ub_]])
                        if fh == 0:
                            gk = gcnt[0] % 4
                            gcnt[0] += 1
                            dma("sp", gwb[gk][:, :], bass.AP(gdr, e_ * S_LEN + tg * 512, [[0, 128], [1, 512]]),
                                reads=["gdr"], writes=["gwb%d" % gk])
                        act(sil[k2][:, :], P[gb_][:, :], AF.Silu, [PN[gb_]], ["sil%d" % k2])
                        tt("dve", t1m[k2][:, :], sil[k2][:, :], P[ub_][:, :], ALU.mult, ["sil%d" % k2, PN[ub_]], ["t1m%d" % k2])
                        tt("dve", hid[hbi][:, j, fh, :], t1m[k2][:, :], gwb[gk][:, :], ALU.mult, ["t1m%d" % k2, "gwb%d" % gk],
                           ["hid%d" % hbi])
                for tt_ in range(4):
                    t = tg * 4 + tt_
                    for ch in range(2):
                        abk = 4 + (tt_ * 2 + ch) % 2
                        n_acc = EG * 2
                        a_i = 0
                        for j in range(EG):
                            for fh in range(2):
                                mm(P[abk][:, :], hid[hbi][:, j, fh, tt_ * 128:(tt_ + 1) * 128],
                                   wdn[b][j][:, fh, ch * 512:(ch + 1) * 512], a_i == 0, a_i == n_acc - 1,
                                   ["hid%d" % hbi, "wdn%d" % b], [PN[abk]])
                                a_i += 1
                        tt("dve", X[:, t, ch * 512:(ch + 1) * 512], X[:, t, ch * 512:(ch + 1) * 512], P[abk][:, :], ALU.add,
                           ["X%d" % t, PN[abk]], ["X%d" % t])
        for t in range(NT):
            final_ops.append(dma("sp", out_d[t * 128:(t + 1) * 128, :], X[:, t, :], reads=["X%d" % t]))

        S.emit(es, final_wait_ops=final_ops)
    return nc


def make_consts():
    ident = np.eye(128, dtype=np.float32).astype(ml_dtypes.bfloat16)
    j = np.arange(128)[:, None]
    i = np.arange(128)[None, :]
    same = (j // 64) == (i // 64)
    mf = (same & (j <= i)).astype(np.float32)
    mb = (same & (j > i)).astype(np.float32)
    masks = np.concatenate([mf, mb], axis=1).astype(ml_dtypes.bfloat16)
    invf = (10000.0 ** (-np.arange(0, 32, 2, dtype=np.float32) / 32)).astype(np.float32)
    invf = np.broadcast_to(invf[None, :], (128, 16)).copy()
    sel = np.zeros((32, 32, 128), np.float32)
    for e in range(32):
        sel[e, e, :] = 1.0
    sel = sel.reshape(32, 32 * 128).astype(ml_dtypes.bfloat16)
    lrb = np.zeros((64, 1), np.float32)
    lrb[16, 0] = 1.0
    return {"c_ident": ident, "c_masks": masks, "c_invf": invf, "c_sel": sel, "c_lrbias": lrb}


_NC_CACHE = {}


def make_in_maps(inputs, n_cores=8):
    c = make_consts()
    f = lambda k: np.ascontiguousarray(np.asarray(inputs[k], dtype=np.float32)[0])
    shared = {
        "norm1_gain": f("norm1_gain").reshape(1, D),
        "w_in": f("w_in"),
        "gla_gk_fwd_w": f("gla_gk_fwd_w"), "gla_gk_fwd_b": f("gla_gk_fwd_b").reshape(1, 256),
        "gla_gk_bwd_w": f("gla_gk_bwd_w"), "gla_gk_bwd_b": f("gla_gk_bwd_b").reshape(1, 256),
        "gla_out_gain": f("gla_out_gain").reshape(128, 1),
        "mla_q_gain": f("mla_q_gain").reshape(1, 256), "mla_w_qb": f("mla_w_qb"),
        "mla_kv_gain": f("mla_kv_gain").reshape(1, 128), "mla_w_kvb": f("mla_w_kvb"),
        "q_norm_gain": f("q_norm_gain").reshape(1, 96), "k_norm_gain": f("k_norm_gain").reshape(1, 96),
        "w_out": f("w_out"), "norm2_gain": f("norm2_gain").reshape(1, D),
        "w_router_group": f("w_router_group"), "b_router_group": f("b_router_group").reshape(1, 4),
        "w_router_expert": f("w_router_expert"), "b_router_expert": f("b_router_expert").reshape(1, 32),
        "w_expert_gate": f("w_expert_gate").reshape(32, D, 256),
        "w_expert_up": f("w_expert_up").reshape(32, D, 256),
        "w_expert_down": f("w_expert_down").reshape(32, 256, D),
    }
    shared.update(c)
    x = np.asarray(inputs["x"], dtype=np.float32)
    pos = np.asarray(inputs["positions"]).astype(np.int32)
    maps = []
    for b in range(n_cores):
        m = dict(shared)
        m["x"] = np.ascontiguousarray(x[b])
        m["pos"] = np.ascontiguousarray(pos[b].reshape(NT, 128).T)
        maps.append(m)
    return maps


def kernel(**inputs):
    if "nc" not in _NC_CACHE:
        _NC_CACHE["nc"] = build()
    nc = _NC_CACHE["nc"]
    maps = make_in_maps(inputs, 8)
    res = run_bass_kernel_spmd(nc, maps, core_ids=list(range(8)))
    out = np.stack([np.asarray(r["out"], dtype=np.float32) for r in res.results], axis=0)
    return out
```

```python
import contextlib
import math
import numpy as np
import ml_dtypes
import concourse.bass as bass
import concourse.mybir as mybir
from concourse.bass_utils import run_bass_kernel_spmd

F32 = mybir.dt.float32
BF16 = mybir.dt.bfloat16
I32 = mybir.dt.int32
ALU = mybir.AluOpType
AF = mybir.ActivationFunctionType
AX = mybir.AxisListType

S_LEN = 2048
D = 1024
NT = 16
EPS = 1e-6
PI = math.pi


class T:
    __slots__ = ("name", "w", "r")

    def __init__(self, name):
        self.name = name
        self.w = None
        self.r = []


class Op:
    __slots__ = ("eng", "fn", "deps", "signal", "sig", "dma", "dsem", "dval", "alld", "n", "seg", "idx", "nbytes", "tag")


class Sched:
    ENGS = ("pe", "act", "dve", "pool", "sp")

    def __init__(self, nc, n_dma_sems=12):
        self.nc = nc
        self.ops = {e: [] for e in self.ENGS}
        self.n_dma_sems = n_dma_sems
        self.dma_count = {e: 0 for e in self.ENGS}
        self.tiles = {}
        self.pending = {e: [] for e in self.ENGS}
        self.dma_since_barrier = []
        self.stopped = False
        self.seg = 0
        self.nops = 0
        self.noresched = set()

    def t(self, name):
        if name not in self.tiles:
            self.tiles[name] = T(name)
        return self.tiles[name]

    def _tl(self, lst):
        out = []
        for x in lst:
            if isinstance(x, str):
                out.append(self.t(x))
            elif isinstance(x, (list, tuple)):
                out.extend(self._tl(x))
            elif x is not None:
                out.append(x)
        return out

    def alias(self, new_names, old_names):
        if self.stopped:
            return
        for nn in new_names:
            tn = self.t(nn)
            for on in old_names:
                to = self.t(on)
                if to.w is not None:
                    tn.r.append(to.w)
                tn.r.extend(to.r)

    def barrier(self):
        if self.stopped:
            return
        lasts = []
        for e in self.ENGS:
            for o in reversed(self.ops[e]):
                if not o.dma:
                    lasts.append(o)
                    break
        lasts.extend(self.dma_since_barrier)
        self.dma_since_barrier = []
        for e in self.ENGS:
            self.pending[e] = list(lasts)
        self.seg += 1

    def op(self, eng, fn, reads=(), writes=(), dma=False, n=64, nbytes=0):
        if self.stopped:
            return None
        o = Op()
        o.n = n
        o.nbytes = nbytes
        o.seg = self.seg
        o.idx = self.nops
        self.nops += 1
        o.eng = eng
        o.fn = fn
        o.dma = dma
        o.signal = False
        o.sig = 0
        deps = {}
        reads = self._tl(reads)
        writes = self._tl(writes)
        for t in reads:
            if t.w is not None:
                deps[id(t.w)] = (t.w, "raw")
            if t.name[0] == "P" and t.name[1:].isdigit():
                for r in t.r:
                    if id(r) not in deps and r.eng != eng:
                        deps[id(r)] = (r, "war")
        for t in writes:
            if t.w is not None and id(t.w) not in deps:
                deps[id(t.w)] = (t.w, "waw")
            for r in t.r:
                if id(r) not in deps:
                    deps[id(r)] = (r, "war")
        if self.pending[eng]:
            for p in self.pending[eng]:
                deps[id(p)] = (p, "raw")
            self.pending[eng] = []
        o.alld = [p for p, _k in deps.values()]
        o.tag = ("R:" + ",".join(t.name for t in reads) + " W:" + ",".join(t.name for t in writes))
        dl = []
        for p, kind in deps.values():
            if p.eng == eng and not p.dma:
                if eng == "pe":
                    continue
                if kind != "raw" and not dma and not STRICT_SAME_ENGINE:
                    continue
            dl.append(p)
        o.deps = dl
        for p in dl:
            p.signal = True
        for t in reads:
            if not dma:
                for r in t.r:
                    if not r.dma and r.eng == eng:
                        o.alld.append(r)
                t.r = [r for r in t.r if r.dma or r.eng != eng]
            t.r.append(o)
        for t in writes:
            t.w = o
            t.r = []
        if dma:
            self.dma_count[eng] += 1
            self.dma_since_barrier.append(o)
        self.ops[eng].append(o)
        return o

    @staticmethod
    def _dur(o):
        n = o.n
        if o.dma:
            return 60.0 if o.eng == "sp" else 900.0
        if o.eng == "pe":
            return 30.0 + max(n, 64) / 2.0
        if o.eng == "act":
            return 220.0 + n / 1.4
        if o.eng == "dve":
            return 120.0 + n * 1.3
        return 550.0 + n * 0.75

    def reschedule(self):
        allops = []
        for e in self.ENGS:
            allops.extend(self.ops[e])
        allops.sort(key=lambda o: o.idx)
        import heapq
        new = {e: [] for e in self.ENGS}
        segs = {}
        for o in allops:
            segs.setdefault(o.seg, []).append(o)
        LAT = 600.0
        for sg in sorted(segs):
            ops = segs[sg]
            if sg in self.noresched:
                for o in ops:
                    new[o.eng].append(o)
                continue
            inseg = set(id(o) for o in ops)
            done = {}
            users = {}
            indeg = {}
            first = {}
            for o in ops:
                if o.eng not in first:
                    first[o.eng] = o
                elif first[o.eng] not in o.alld:
                    o.alld.append(first[o.eng])
            for o in ops:
                k = 0
                for p in o.alld:
                    if id(p) in inseg:
                        k += 1
                        users.setdefault(id(p), []).append(o)
                indeg[id(o)] = k
            ready = {e: [] for e in self.ENGS}
            efree = {e: 0.0 for e in self.ENGS}
            rtime = {}
            for o in ops:
                if indeg[id(o)] == 0:
                    rtime[id(o)] = 0.0
                    ready[o.eng].append(o)
            left = len(ops)
            SLACK = 0.0
            while left:
                best = None
                for e in self.ENGS:
                    rl = ready[e]
                    if not rl:
                        continue
                    ef = efree[e]
                    oldest = None
                    fill = None
                    for o in rl:
                        st = max(rtime[id(o)], ef)
                        if oldest is None or o.idx < oldest[1].idx:
                            oldest = (st, o)
                        if fill is None or (st, o.idx) < (fill[0], fill[1].idx):
                            fill = (st, o)
                    pick = oldest if oldest[0] <= fill[0] + SLACK else fill
                    if best is None or (pick[0], pick[1].idx) < (best[0], best[1].idx):
                        best = pick
                st, o = best
                e = o.eng
                ready[e].remove(o)
                d = self._dur(o)
                efree[e] = st + d
                fin = st + d
                if o.dma:
                    fin = st + 2000.0 + o.nbytes / 150.0
                done[id(o)] = fin
                new[e].append(o)
                left -= 1
                for u in users.get(id(o), ()):
                    indeg[id(u)] -= 1
                    lat = 0.0 if (u.eng == o.eng and not o.dma) else LAT
                    rtime[id(u)] = max(rtime.get(id(u), 0.0), fin + lat)
                    if indeg[id(u)] == 0:
                        ready[u.eng].append(u)
        self.ops = new

    def emit(self, es, final_wait_ops=()):
        nc = self.nc
        if RESCHEDULE:
            self.reschedule()
        sems = {e: es.enter_context(nc.semaphore("s_" + e)) for e in self.ENGS}
        dsems = {e: [es.enter_context(nc.semaphore("d_%s_%d" % (e, i)))
                     for i in range(self.n_dma_sems)]
                 for e in self.ENGS if self.dma_count[e] > 0}
        for e in self.ENGS:
            c = 0
            i = 0
            for o in self.ops[e]:
                if o.dma:
                    o.dsem = i % self.n_dma_sems
                    o.dval = 16 * (i // self.n_dma_sems + 1)
                    i += 1
                elif o.signal:
                    c += 1
                    o.sig = c
        block = es.enter_context(nc.Block())
        eng_obj = {"pe": block.tensor, "act": block.scalar, "dve": block.vector,
                   "pool": block.gpsimd, "sp": block.sync}
        for e in self.ENGS:
            ops = self.ops[e]
            if not ops:
                continue

            def body(engine, e=e, ops=ops):
                waited = {}

                def wait(sem, key, val):
                    if waited.get(key, 0) >= val:
                        return
                    waited[key] = val
                    engine.wait_ge(sem, val)

                for o in ops:
                    for p in o.deps:
                        if p.dma:
                            wait(dsems[p.eng][p.dsem], ("d", p.eng, p.dsem), p.dval)
                        else:
                            wait(sems[p.eng], ("c", p.eng), p.sig)
                    if o.dma and o.dval > 16:
                        wait(dsems[e][o.dsem], ("d", e, o.dsem), o.dval - 16)
                    ins = o.fn(engine)
                    if o.dma:
                        ins.then_inc(dsems[e][o.dsem], 16)
                    elif o.signal:
                        ins.then_inc(sems[e], 1)
                if e == "sp":
                    for o in final_wait_ops:
                        if o is None:
                            continue
                        wait(dsems[o.eng][o.dsem], ("d", o.eng, o.dsem), o.dval)

            eng_obj[e](body)


RESCHEDULE = True
STRICT_SAME_ENGINE = True
SB_BASE = 16640
SB_END = 229376


class Alloc:
    def __init__(self, nc):
        self.nc = nc
        self.off = SB_BASE
        self.n = 0

    def mark(self):
        return self.off

    def reset(self, m):
        self.off = m

    def __call__(self, name, shape, dt, at=None):
        esz = 2 if dt == BF16 else 4
        nb = int(np.prod(shape[1:])) * esz
        nb = (nb + 63) // 64 * 64
        self.n += 1
        if at is None:
            at = self.off
            self.off += nb
        assert at + nb <= SB_END, ("SBUF overflow", name, at, nb)
        return self.nc.alloc_sbuf_tensor_at("%s_%d" % (name, self.n), list(shape), dt, offset=at)


def bcast_ap(ap, pattern):
    return bass.AP(ap.tensor, ap.offset, [list(ap.ap[0])] + [list(p) for p in pattern])


def build(debug=False, stop_after=None):
    nc = bass.Bass("TRN2", target_bir_lowering=False)
    dr = lambda n, s, dt=F32: nc.dram_tensor(n, list(s), dt, kind="ExternalInput")
    x_d = dr("x", [S_LEN, D])
    pos_d = dr("pos", [128, NT], I32)
    g1_d = dr("norm1_gain", [1, D])
    win_d = dr("w_in", [D, 1984])
    gkf_w = dr("gla_gk_fwd_w", [16, 256])
    gkf_b = dr("gla_gk_fwd_b", [1, 256])
    gkb_w = dr("gla_gk_bwd_w", [16, 256])
    gkb_b = dr("gla_gk_bwd_b", [1, 256])
    go_d = dr("gla_out_gain", [128, 1])
    gqa_d = dr("mla_q_gain", [1, 256])
    wqb_d = dr("mla_w_qb", [256, 768])
    gkva_d = dr("mla_kv_gain", [1, 128])
    wkvb_d = dr("mla_w_kvb", [128, 1024])
    gqn_d = dr("q_norm_gain", [1, 96])
    gkn_d = dr("k_norm_gain", [1, 96])
    wout_d = dr("w_out", [D, D])
    g2_d = dr("norm2_gain", [1, D])
    wrg_d = dr("w_router_group", [D, 4])
    brg_d = dr("b_router_group", [1, 4])
    wre_d = dr("w_router_expert", [D, 32])
    bre_d = dr("b_router_expert", [1, 32])
    weg_d = dr("w_expert_gate", [32, D, 256])
    weu_d = dr("w_expert_up", [32, D, 256])
    wed_d = dr("w_expert_down", [32, 256, D])
    ident_d = dr("c_ident", [128, 128], BF16)
    masks_d = dr("c_masks", [128, 256], BF16)
    invf_d = dr("c_invf", [128, 32])
    sel_d = dr("c_sel", [32, 32 * 128], BF16)
    lrb_d = dr("c_lrbias", [64, 1])
    out_d = nc.dram_tensor("out", [S_LEN, D], F32, kind="ExternalOutput")
    gdr = nc.dram_tensor("gate_scratch", [32, S_LEN], F32, kind="Internal")
    dbg = {}
    if debug:
        dbg["mix"] = nc.dram_tensor("d_mix", [128, 8 * S_LEN], BF16, kind="ExternalOutput")
        dbg["x1"] = nc.dram_tensor("d_x1", [S_LEN, D], F32, kind="ExternalOutput")
        dbg["gate"] = nc.dram_tensor("d_gate", [S_LEN, 32], F32, kind="ExternalOutput")

    if debug:
        dbg["gen"] = nc.dram_tensor("d_gen", [128, 8 * S_LEN], BF16, kind="ExternalOutput")
    S = Sched(nc)
    A = Alloc(nc)
    op = S.op
    final_ops = []

    with contextlib.ExitStack() as es:
        P = [es.enter_context(nc.psum_tensor("pb%d" % i, [128, 512], F32)) for i in range(8)]
        Pb = [p.bitcast(BF16) for p in P]
        PN = ["P%d" % i for i in range(8)]

        def fsz(ap):
            r = 1
            for d_ in list(ap.shape)[1:]:
                r *= int(d_)
            return r

        def dma(q, out, in_, reads=(), writes=(), **kw):
            return op(q, lambda e: e.dma_start(out=out, in_=in_, **kw), reads=reads, writes=writes, dma=True,
                      nbytes=fsz(out) * 4 * 128)

        def act(out, in_, func, reads, writes, **kw):
            return op("act", lambda e: e.activation(out=out, in_=in_, func=func, **kw), reads=reads, writes=writes,
                      n=fsz(out))

        def rsqrt_act(out, in_, n, reads, writes):
            act(out, in_, AF.Ln, reads, writes, scale=1.0 / n, bias=EPS)
            act(out, out, AF.Exp, writes, writes, scale=-0.5)

        def tt(eng, out, in0, in1, o, reads, writes):
            return op(eng, lambda e: e.tensor_tensor(out=out, in0=in0, in1=in1, op=o), reads=reads, writes=writes,
                      n=fsz(out))

        def ts(eng, out, in0, s1, s2, o0, o1, reads, writes):
            if o1 is None:
                return op(eng, lambda e: e.tensor_scalar(out=out, in0=in0, scalar1=s1, scalar2=None, op0=o0),
                          reads=reads, writes=writes, n=fsz(out))
            return op(eng, lambda e: e.tensor_scalar(out=out, in0=in0, scalar1=s1, scalar2=s2, op0=o0, op1=o1),
                      reads=reads, writes=writes, n=fsz(out))

        def stt(out, in0, sc, in1, o0, o1, reads, writes):
            return op("dve", lambda e: e.scalar_tensor_tensor(out=out, in0=in0, scalar=sc, in1=in1, op0=o0, op1=o1),
                      reads=reads, writes=writes, n=fsz(out))

        def mm(out, lhsT, rhs, start, stop, reads, writes):
            return op("pe", lambda e: e.matmul(out, lhsT=lhsT, rhs=rhs, start=start, stop=stop),
                      reads=reads, writes=writes, n=fsz(rhs) * (4 if rhs.dtype == F32 else 1))

        def tr(out, in_, ident, reads, writes):
            return op("pe", lambda e: e.transpose(out=out, in_=in_, identity=ident), reads=reads, writes=writes, n=128)

        def cp(eng, out, in_, reads, writes):
            if eng == "act":
                return act(out, in_, AF.Copy, reads, writes)
            return op(eng, lambda e: e.tensor_copy(out=out, in_=in_), reads=reads, writes=writes, n=fsz(out))

        def memset(eng, ap, val, writes):
            return op(eng, lambda e: e.memset(ap, val), writes=writes, n=fsz(ap))

        def bcast_row(dram, n):
            return bass.AP(dram, 0, [[0, 128], [1, n]])

        ident = A("ident", [128, 128], BF16)
        mixT = A("mixT", [128, 8, S_LEN], BF16)
        dma("sp", ident[:, :], ident_d[:, :], writes=["ident"])
        L0 = A.mark()

        hT = A("hT", [128, 8, S_LEN], BF16)
        E1 = A.mark()
        g1 = A("g1", [128, D], F32)
        xt = [A("xt%d" % i, [128, D], F32) for i in range(2)]
        hb = [A("hb%d" % i, [128, D], BF16) for i in range(2)]
        sqj = A("sqj", [128, D], F32)
        st1 = A("st1", [128, 4], F32)
        dma("sp", g1[:, :], bcast_row(g1_d, D), writes=["g1"])

        def norm_to_T(src_ap_fn, src_tiles, gain, gname, dstT, dname, pfx, t, pbank):
            i = t % 2
            ssq = st1[:, 0:1]
            rs = st1[:, 1:2]
            act(sqj[:, :], src_ap_fn(t), AF.Square, src_tiles, [pfx + "sqj", pfx + "ssq"], accum_out=ssq)
            rsqrt_act(rs, ssq, D, [pfx + "ssq"], [pfx + "rs"])
            stt(hb[i][:, :], src_ap_fn(t), rs, gain[:, :], ALU.mult, ALU.mult,
                src_tiles + [pfx + "rs", gname], [pfx + "hb%d" % i])
            pbv = Pb[pbank][:, :].rearrange("p (c n) -> p c n", c=8)
            for kc in range(8):
                tr(pbv[:, kc, :], hb[i][:, kc * 128:(kc + 1) * 128], ident[:, :],
                   [pfx + "hb%d" % i, "ident"], [PN[pbank]])
            cp("dve" if t % 2 else "act", dstT[:, :, t * 128:(t + 1) * 128], pbv, [PN[pbank]], [dname + "_%d" % (t // 4)])

        for t in range(NT):
            i = t % 2
            dma("sp", xt[i][:, :], x_d[t * 128:(t + 1) * 128, :], writes=["xt%d" % i])
            norm_to_T(lambda t, i=i: xt[i][:, :], ["xt%d" % i], g1, "g1", hT, "hT", "A", t, t % 2)
        hT_tiles = ["hT_%d" % k for k in range(4)]
        S.barrier()
        A.reset(E1)
        def checkpoint(name, dump=None, reads=()):
            if stop_after == name:
                if dump is not None and debug:
                    S.barrier()
                    final_ops.append(dma("sp", dbg["gen"][:, :], dump, reads=list(reads)))
                S.stopped = True

        checkpoint("A")

        wm = A("w_in_mla", [128, 8, 416], BF16)
        wqb = A("wqb", [128, 2, 768], BF16)
        wkvb = A("wkvb", [128, 1024], BF16)
        cs = A("cs", [128, NT, 64], F32)
        qhT = A("qhT", [128, 8, S_LEN], BF16)
        khT = A("khT", [128, 8, S_LEN], BF16)
        vA = A("vA", [128, NT, 8, 128], BF16)
        gqa = A("gqa", [128, 384], F32)
        gqk = A("gqkr", [128, 16, 32], F32)
        gcol = A("gcol", [128, 2], F32)
        B1m = A.mark()
        dma("pool", wm[:, :, :], win_d.ap()[:, 1568:1984].rearrange("(c p) n -> p c n", p=128), writes=["wm"])
        dma("pool", wqb[:, :, :], wqb_d.ap().rearrange("(c p) n -> p c n", p=128), writes=["wqb"])
        dma("pool", wkvb[:, :], wkvb_d[:, :], writes=["wkvb"])
        dma("sp", gqa[:, 0:256], bcast_row(gqa_d, 256), writes=["gqa"])
        dma("sp", gqa[:, 256:384], bcast_row(gkva_d, 128), writes=["gqa"])
        dma("sp", gqk[:, 0:8, :], bass.AP(gqn_d, 64, [[0, 128], [0, 8], [1, 32]]), writes=["gqk"])
        dma("sp", gqk[:, 8:16, :], bass.AP(gkn_d, 64, [[0, 128], [0, 8], [1, 32]]), writes=["gqk"])
        memset("pool", gcol[:, :], 1.0, ["gcol"])
        dma("sp", gcol[0:64, 0:1], bass.AP(gqn_d, 0, [[1, 64], [1, 1]]), reads=["gcol"], writes=["gcol"])
        dma("sp", gcol[0:64, 1:2], bass.AP(gkn_d, 0, [[1, 64], [1, 1]]), reads=["gcol"], writes=["gcol"])
        posi = A("posi", [128, NT], I32)
        posf = A("posf", [128, NT], F32)
        invf = A("invf", [128, 32], F32)
        ang = A("ang", [128, NT, 16], F32)
        kk = A("kk", [128, NT, 16], F32)
        ki = A("ki", [128, NT, 16], I32)
        rr = A("rr", [128, NT, 16], F32)
        yy = A("yy", [128, NT, 16], F32)
        m_ = A("m_", [128, NT, 16], F32)
        dma("sp", posi[:, :], pos_d[:, :], writes=["posi"])
        dma("sp", invf[:, :], invf_d[:, :], writes=["invf"])
        cp("dve", posf[:, :], posi[:, :], ["posi"], ["posf"])
        for t in range(NT):
            ts("dve", ang[:, t, :], invf[:, 0:16], posf[:, t:t + 1], None, ALU.mult, None, ["invf", "posf"], ["ang"])
            stt(ang[:, t, :], invf[:, 16:32], posf[:, t:t + 1], ang[:, t, :], ALU.mult, ALU.add, ["invf", "posf", "ang"], ["ang"])
        ts("dve", kk[:, :, :], ang[:, :, :], 1.0 / (2 * PI), None, ALU.mult, None, ["ang"], ["kk"])
        cp("dve", ki[:, :, :], kk[:, :, :], ["kk"], ["ki"])
        cp("dve", kk[:, :, :], ki[:, :, :], ["ki"], ["kk"])
        C1 = 6.28125
        C2 = 2 * PI - C1
        stt(rr[:, :, :], kk[:, :, :], -C1, ang[:, :, :], ALU.mult, ALU.add, ["kk", "ang"], ["rr"])
        stt(rr[:, :, :], kk[:, :, :], -C2, rr[:, :, :], ALU.mult, ALU.add, ["kk", "rr"], ["rr"])
        for which, shift in ((1, 0.0), (0, PI / 2)):
            ts("dve", yy[:, :, :], rr[:, :, :], shift, None, ALU.add, None, ["rr"], ["yy"])
            ts("dve", m_[:, :, :], yy[:, :, :], PI, None, ALU.is_gt, None, ["yy"], ["m_"])
            stt(yy[:, :, :], m_[:, :, :], -2 * PI, yy[:, :, :], ALU.mult, ALU.add, ["m_", "yy"], ["yy"])
            ts("dve", m_[:, :, :], yy[:, :, :], -PI, None, ALU.is_lt, None, ["yy"], ["m_"])
            stt(yy[:, :, :], m_[:, :, :], 2 * PI, yy[:, :, :], ALU.mult, ALU.add, ["m_", "yy"], ["yy"])
            ts("dve", yy[:, :, :], yy[:, :, :], PI, -PI, ALU.min, ALU.max, ["yy"], ["yy"])
            if which == 0:
                act(cs[:, :, 0:16], yy[:, :, :], AF.Sin, ["yy"], ["cs"])
                act(cs[:, :, 16:32], yy[:, :, :], AF.Sin, ["yy"], ["cs"])
            else:
                act(cs[:, :, 48:64], yy[:, :, :], AF.Sin, ["yy"], ["cs"])
                act(cs[:, :, 32:48], cs[:, :, 48:64], AF.Copy, ["cs"], ["cs"], scale=-1.0)
        memset("pool", vA[:, :, :, :], 1.0, ["vA"])
        S.barrier()
        A.reset(B1m)

        sqjb1 = A("sqjb", [128, 416], BF16)
        sqjb = [sqjb1, sqjb1]
        stq = [A("stq%d" % i, [128, 32], F32) for i in range(2)]
        ab = [A("ab%d" % i, [128, 384], BF16) for i in range(2)]
        abT = [A("abT%d" % i, [128, 3, 128], BF16) for i in range(2)]
        kraw = [A("kraw%d" % i, [128, 8, 96], F32) for i in range(2)]
        sqn = A("sqn", [128, 16, 96], BF16)
        rg = [A("rg%d" % i, [128, 16, 32], F32) for i in range(2)]
        rg2 = A("rg2", [128, 16, 48], F32)
        rb = A("rb", [128, 16, 32], F32)
        qkf = [A("qkf%d" % i, [128, 16, 96], BF16) for i in range(2)]
        SQ2 = math.sqrt(2.0)

        def st_E1a(t):
            i = t % 2
            sI = "_%d" % i
            tsl = slice(t * 128, (t + 1) * 128)
            hTt = "hT_%d" % (t // 4)
            for kc in range(8):
                mm(P[0][:, 0:416], hT[:, kc, tsl], wm[:, kc, :], kc == 0, kc == 7, [hTt, "wm"], ["P0"])
            act(sqjb[i][:, 0:256], P[0][:, 0:256], AF.Square, ["P0"], ["stqA" + sI], accum_out=stq[i][:, 0:1])
            act(sqjb[i][:, 256:384], P[0][:, 256:384], AF.Square, ["P0"], ["stqA" + sI],
                accum_out=stq[i][:, 1:2], scale=SQ2)
            act(kraw[i][:, :, 64:96], bcast_ap(P[0][:, 384:416], [[0, 8], [1, 32]]), AF.Copy, ["P0"], ["krawR" + sI])
            rsqrt_act(stq[i][:, 2:4], stq[i][:, 0:2], 256, ["stqA" + sI], ["stqB" + sI])
            stt(ab[i][:, 0:256], P[0][:, 0:256], stq[i][:, 2:3], gqa[:, 0:256], ALU.mult, ALU.mult,
                ["P0", "stqB" + sI, "gqa"], ["ab" + sI])
            stt(ab[i][:, 256:384], P[0][:, 256:384], stq[i][:, 3:4], gqa[:, 256:384], ALU.mult, ALU.mult,
                ["P0", "stqB" + sI, "gqa"], ["ab" + sI])
        def st_E1b(t):
            i = t % 2
            sI = "_%d" % i
            tsl = slice(t * 128, (t + 1) * 128)
            hTt = "hT_%d" % (t // 4)
            p1v = Pb[1][:, 0:384].rearrange("p (c n) -> p c n", c=3)
            for c in range(3):
                tr(p1v[:, c, :], ab[i][:, c * 128:(c + 1) * 128], ident[:, :], ["ab" + sI, "ident"], ["P1"])
            cp("act", abT[i][:, :, :], p1v, ["P1"], ["abT" + sI])
        def st_E2(t):
            i = t % 2
            sI = "_%d" % i
            tsl = slice(t * 128, (t + 1) * 128)
            hTt = "hT_%d" % (t // 4)
            for nb in range(2):
                for kc in range(2):
                    mm(P[2 + nb][:, 0:384], abT[i][:, kc, :], wqb[:, kc, nb * 384:(nb + 1) * 384], kc == 0, kc == 1,
                       ["abT" + sI, "wqb"], [PN[2 + nb]])
                mm(P[4 + nb][:, :], abT[i][:, 2, :], wkvb[:, nb * 512:(nb + 1) * 512], True, True,
                   ["abT" + sI, "wkvb"], [PN[4 + nb]])
            for nb in range(2):
                srck = P[4 + nb][:, :].rearrange("p (h d) -> p h d", h=4)[:, :, 0:64]
                cp("act", kraw[i][:, nb * 4:nb * 4 + 4, 0:64], srck, [PN[4 + nb]], ["krawN" + sI])
                srcv = P[4 + nb][:, :].rearrange("p (a b d) -> p a b d", a=2, b=2)
                dstv = vA[:, t, nb * 4:nb * 4 + 4, :].rearrange("p (a b) d -> p a b d", b=2)
                cp("act", dstv[:, :, 0, 0:64], srcv[:, :, 0, 64:128], [PN[4 + nb]], ["vA"])
                cp("act", dstv[:, :, 1, 64:128], srcv[:, :, 1, 64:128], [PN[4 + nb]], ["vA"])
            for nb in range(2):
                act(sqn[:, nb * 4:nb * 4 + 4, :], P[2 + nb][:, 0:384].rearrange("p (h d) -> p h d", h=4), AF.Square,
                    [PN[2 + nb]], ["sqn"])
            act(sqn[:, 8:16, :], kraw[i][:, :, :], AF.Square, ["krawN" + sI, "krawR" + sI], ["sqn"])
            op("dve", lambda e, i=i: e.tensor_reduce(out=stq[i][:, 8:24], in_=sqn[:, :, :], axis=AX.X, op=ALU.add),
               reads=["sqn"], writes=["stqC" + sI])
            rsqrt_act(stq[i][:, 8:24], stq[i][:, 8:24], 96, ["stqC" + sI], ["stqC" + sI])
            for nb in range(2):
                pv = P[2 + nb][:, 0:384].rearrange("p (h d) -> p h d", h=4)
                rq = stq[i][:, 8 + nb * 4:9 + nb * 4]
                tt("dve", qkf[i][:, nb * 4:nb * 4 + 4, 0:64], pv[:, :, 0:64], bcast_ap(rq, [[1, 4], [0, 64]]), ALU.mult,
                   [PN[2 + nb], "stqC" + sI], ["qkf" + sI])
                tt("dve", rg[i][:, nb * 4:nb * 4 + 4, :], pv[:, :, 64:96], bcast_ap(rq, [[1, 4], [0, 32]]), ALU.mult,
                   [PN[2 + nb], "stqC" + sI], ["rg" + sI])
            rk = stq[i][:, 16:17]
            tt("dve", qkf[i][:, 8:16, 0:64], kraw[i][:, :, 0:64], bcast_ap(rk, [[1, 8], [0, 64]]), ALU.mult,
               ["krawN" + sI, "stqC" + sI], ["qkf" + sI])
            tt("dve", rg[i][:, 8:16, :], kraw[i][:, :, 64:96], bcast_ap(rk, [[1, 8], [0, 32]]), ALU.mult,
               ["krawR" + sI, "stqC" + sI], ["rg" + sI])
        def st_L(t):
            i = t % 2
            sI = "_%d" % i
            tsl = slice(t * 128, (t + 1) * 128)
            hTt = "hT_%d" % (t // 4)
            tt("pool", rg2[:, :, 0:32], rg[i][:, :, :], gqk[:, :, :], ALU.mult, ["rg" + sI, "gqk"], ["rg2"])
            tt("pool", rg2[:, :, 32:48], rg[i][:, :, 0:16], gqk[:, :, 0:16], ALU.mult, ["rg" + sI, "gqk"], ["rg2"])
            c1 = bcast_ap(cs[:, t, 0:32], [[0, 16], [1, 32]])
            c2 = bcast_ap(cs[:, t, 32:64], [[0, 16], [1, 32]])
            tt("pool", rg[i][:, :, :], rg2[:, :, 0:32], c1, ALU.mult, ["rg2", "cs"], ["rg" + sI])
            tt("pool", rb[:, :, :], rg2[:, :, 16:48], c2, ALU.mult, ["rg2", "cs"], ["rb"])
            tt("pool", qkf[i][:, :, 64:96], rg[i][:, :, :], rb[:, :, :], ALU.add, ["rg" + sI, "rb"], ["qkf" + sI])
            p6v = Pb[6][:, :].rearrange("p (h n) -> p h n", h=8)
            p7v = Pb[7][:, :].rearrange("p (h n) -> p h n", h=8)
            for h in range(8):
                tr(p6v[0:96, h, :], qkf[i][:, h, :], ident[:, :], ["qkf" + sI, "ident"], ["P6"])
            for h in range(8):
                tr(p7v[0:96, h, :], qkf[i][:, 8 + h, :], ident[:, :], ["qkf" + sI, "ident"], ["P7"])
            ts("dve", qhT[0:96, :, tsl], p6v[0:96, :, :], gcol[0:96, 0:1], None, ALU.mult, None, ["P6", "gcol"],
               ["qhT_%d" % (t // 4)])
            act(khT[0:96, :, tsl], p7v[0:96, :, :], AF.Identity, ["P7", "gcol"], ["khT"], scale=gcol[0:96, 1:2])

        S.noresched.add(S.seg)
        for step in range(NT + 2):
            if step < NT:
                st_E1a(step)
            if 0 <= step - 1 < NT:
                st_E2(step - 1)
            if 0 <= step - 2 < NT:
                st_L(step - 2)
            if step < NT:
                st_E1b(step)
        S.barrier()
        A.reset(B1m)
        checkpoint("B1")
        pbuf = [A("pbuf%d" % i, [128, 512], BF16, at=E1 + i * 1024) for i in range(4)]
        rcb = A("rcb", [128, 512], F32, at=E1 + 4096)
        wg = A("w_in_gla", [128, 8, 1568], BF16)
        wlr = A("wlr", [128, 8, 64], BF16)
        dma("pool", wg[:, :, :], win_d.ap()[:, 0:1568].rearrange("(c p) n -> p c n", p=128), writes=["wg"])
        memset("pool", wlr[:, :, :], 0.0, ["wlr"])
        dma("pool", wlr[:, :, 0:16], win_d.ap()[:, 1536:1552].rearrange("(c p) n -> p c n", p=128), reads=["wlr"], writes=["wlr"])
        dma("pool", wlr[:, :, 32:48], win_d.ap()[:, 1552:1568].rearrange("(c p) n -> p c n", p=128), reads=["wlr"], writes=["wlr"])
        scale = 96 ** -0.5
        it = 0
        for h in range(8):
            even = (h % 2 == 0)
            vrows = slice(0, 64) if even else slice(64, 128)
            srows = slice(64, 128) if even else slice(0, 64)
            for qg in range(4):
                qsl = slice(qg * 512, (qg + 1) * 512)
                ob = 4 + (it % 2)
                seq = []
                for kt in range(16):
                    seq.append(("s", kt))
                    if kt >= 2:
                        seq.append(("pv", kt - 2))
                seq += [("pv", 14), ("pv", 15)]
                for kind, kt in seq:
                    sb_ = kt % 3
                    pi = kt % 4
                    if kind == "s":
                        mm(P[sb_][:, :], khT[0:96, h, kt * 128:(kt + 1) * 128], qhT[0:96, h, qsl], True, True,
                           ["khT", "qhT_%d" % qg], [PN[sb_]])
                        act(pbuf[pi][:, :], P[sb_][:, :], AF.Exp, [PN[sb_]], ["pbuf%d" % pi], scale=scale)
                    else:
                        lhsT = vA[:, kt, h, :]
                        mm(P[ob][:, :], lhsT, pbuf[pi][:, :], kt == 0, kt == 15, ["vA", "pbuf%d" % pi], [PN[ob]])
                op("dve", lambda e, vrows=vrows, srows=srows, ob=ob: e.reciprocal(out=rcb[vrows, :], in_=P[ob][srows, :]),
                   reads=[PN[ob]], writes=["rcb"], n=4096)
                tt("dve", mixT[vrows, 4 + h // 2, qsl], P[ob][vrows, :], rcb[vrows, :], ALU.mult, [PN[ob], "rcb"], ["mixT_m"])
                it += 1
        S.barrier()
        A.reset(E1)

        checkpoint("C")
        A.off += 25088
        R2 = A.mark()
        qkT = A("qkT", [128, 4, S_LEN], F32)
        vtok = A("vtok", [128, NT, 512], BF16)
        sgT = A("sgT", [128, 4, S_LEN], BF16)
        lrT = A("lrT", [64, S_LEN], F32)
        lrb = A("lrb", [64, 1], F32)
        waug = A("waug", [64, 512], F32)
        masks = A("masks", [128, 256], BF16)
        gout = A("gout", [128, 1], F32)
        onesf = A("onesf", [128, 128], F32)
        R4 = A.mark()
        dma("sp", lrb[:, :], lrb_d[:, :], writes=["lrb"])
        memset("pool", waug[:, :], 0.0, ["waug"])
        dma("sp", waug[0:16, 0:256], gkf_w[:, :], reads=["waug"], writes=["waug"])
        dma("sp", waug[16:17, 0:256], gkf_b[:, :], reads=["waug"], writes=["waug"])
        dma("sp", waug[16:17, 256:512], gkb_b[:, :], reads=["waug"], writes=["waug"])
        dma("sp", waug[32:48, 256:512], gkb_w[:, :], reads=["waug"], writes=["waug"])
        dma("sp", masks[:, :], masks_d[:, :], writes=["masks"])
        dma("sp", gout[:, :], go_d[:, :], writes=["gout"])
        memset("pool", onesf[:, :], 1.0, ["onesf"])
        blk = 0
        for kind, idx in [("q", 0), ("q", 1), ("k", 0), ("k", 1), ("g", 0), ("g", 1), ("g", 2), ("g", 3), ("lr", 0)]:
            for tg in range(4):
                pbk = blk % 4
                blk += 1
                tgs = slice(tg * 512, (tg + 1) * 512)
                for kc in range(8):
                    if kind == "q":
                        lhsT = wg[:, kc, idx * 128:(idx + 1) * 128]
                    elif kind == "k":
                        lhsT = wg[:, kc, 256 + idx * 128:256 + (idx + 1) * 128]
                    elif kind == "g":
                        lhsT = wg[:, kc, 1024 + idx * 128:1024 + (idx + 1) * 128]
                    else:
                        lhsT = wlr[:, kc, :]
                    mrows = 64 if kind == "lr" else 128
                    mm(P[pbk][0:mrows, :], lhsT, hT[:, kc, tgs], kc == 0, kc == 7, ["hT_%d" % tg, "wg", "wlr"], [PN[pbk]])
                if kind == "q":
                    act(qkT[:, idx, tgs], P[pbk][:, :], AF.Copy, [PN[pbk]], ["qT"], scale=0.125)
                elif kind == "k":
                    cp("dve", qkT[:, 2 + idx, tgs], P[pbk][:, :], [PN[pbk]], ["kT"])
                elif kind == "g":
                    act(sgT[:, idx, tgs], P[pbk][:, :], AF.Silu, [PN[pbk]], ["sgT"])
                else:
                    act(lrT[:, tgs], P[pbk][0:64, :], AF.Identity, [PN[pbk], "lrb"], ["lrT"], bias=lrb[:, :])
        for t in range(NT):
            pbk = 4 + t % 2
            tsl = slice(t * 128, (t + 1) * 128)
            for kc in range(8):
                mm(P[pbk][:, :], hT[:, kc, tsl], wg[:, kc, 512:1024], kc == 0, kc == 7, ["hT_%d" % (t // 4), "wg"], [PN[pbk]])
            cp("dve" if t % 2 else "act", vtok[:, t, :], P[pbk][:, :], [PN[pbk]], ["vtok"])
        S.barrier()

        checkpoint("B2")
        HTB = L0
        tmp = [A("gt%d" % i, [128, 1024], F32, at=HTB + i * 4096) for i in range(4)]
        prod = {}
        names = [(d_, hp, k_) for d_ in (0, 1) for hp in (0, 1) for k_ in ("qr", "kr", "qb")]
        slots = [HTB + 16384 + i * 4096 for i in range(4)] + [E1 + 16384 + i * 4096 for i in range(2)]
        for i, nm in enumerate(names):
            if i < 6:
                prod[nm] = A("pr", [128, S_LEN], BF16, at=slots[i])
            else:
                prod[nm] = A("pr", [128, S_LEN], BF16)
        kdT = A("kdT", [128, 1024], BF16)
        dec = A("dec", [128, 4, 32], F32)
        smask = A("smask", [128, 1024], F32)
        kd = A("kd", [128, NT, 512], BF16, at=E1)
        memset("pool", smask[:, :], 1.0, ["smask"])
        memset("pool", smask[:, :].rearrange("p (c j) -> p c j", j=64)[:, :, 0:1], 0.0, ["smask"])
        G, Fc, Dt, Eb = tmp
        Dt2 = A("Dt2", [128, 1024], F32)
        Eb2 = A("Eb2", [128, 1024], F32)
        DtL = [(Dt, "Dt"), (Dt2, "Dt2")]
        EbL = [(Eb, "Eb"), (Eb2, "Eb2")]
        cnt = {"d": 0, "e": 0}

        def nextD():
            cnt["d"] += 1
            return DtL[cnt["d"] % 2]

        def nextE():
            cnt["e"] += 1
            return EbL[cnt["e"] % 2]

        def exp_prod(src, sname, scl, dst, base, bname, dname="prod"):
            E_, en = nextE()
            act(E_[:, :], src, AF.Exp, [sname], [en], scale=scl)
            tt("pool", dst, base, E_[:, :], ALU.mult, [bname, en], [dname])
        for d_ in (0, 1):
            for hp in (0, 1):
                dh = d_ * 2 + hp
                qT = qkT[:, hp, :]
                kT = qkT[:, 2 + hp, :]
                for half in range(2):
                    hs = slice(half * 1024, (half + 1) * 1024)
                    for j in range(2):
                        pbk = j
                        cols = slice(half * 1024 + j * 512, half * 1024 + (j + 1) * 512)
                        mm(P[pbk][:, :], waug[0:64, dh * 128:(dh + 1) * 128], lrT[0:64, cols], True, True,
                           ["waug", "lrT"], [PN[pbk]])
                        act(Eb[:, j * 512:(j + 1) * 512], P[pbk][:, :], AF.Exp, [PN[pbk]], ["Eb"], scale=-1.0)
                    act(G[:, :], Eb[:, :], AF.Ln, ["Eb"], ["G"], bias=1.0)
                    op("dve", lambda e: e.tensor_tensor_scan(out=Fc[:, :], data0=smask[:, :], data1=G[:, :], initial=0.0,
                                                             op0=ALU.mult, op1=ALU.add), reads=["smask", "G"], writes=["Fc"])
                    Fv = Fc[:, :].rearrange("p (c j) -> p c j", j=64)
                    Dv = Dt[:, :].rearrange("p (c j) -> p c j", j=64)
                    T63 = bcast_ap(Fc[:, 63:64], [[64, 16], [0, 64]])
                    act(dec[:, dh, half * 16:(half + 1) * 16], Fv[:, :, 63], AF.Exp, ["Fc"], ["dec"], scale=-1.0 / 16)
                    if d_ == 0:
                        ref = bcast_ap(Fc[:, 31:32], [[64, 16], [0, 64]])
                        D_, dn = nextD()
                        tt("dve", D_[:, :].rearrange("p (c j) -> p c j", j=64), Fv, ref, ALU.subtract, ["Fc"], [dn])
                        exp_prod(D_[:, :], dn, -1.0 / 16, prod[(0, hp, "qr")][:, hs], qT[:, hs], "qT")
                        exp_prod(D_[:, :], dn, 1.0 / 16, prod[(0, hp, "kr")][:, hs], kT[:, hs], "kT")
                        exp_prod(Fc[:, :], "Fc", -1.0 / 16, prod[(0, hp, "qb")][:, hs], qT[:, hs], "qT")
                        D_, dn = nextD()
                        tt("dve", D_[:, :].rearrange("p (c j) -> p c j", j=64), Fv, T63, ALU.subtract, ["Fc"], [dn])
                        exp_prod(D_[:, :], dn, 1.0 / 16, kdT[:, :], kT[:, hs], "kT", "kdT")
                    else:
                        tt("dve", G[:, :], Fc[:, :], G[:, :], ALU.subtract, ["Fc", "G"], ["G"])
                        Gv = G[:, :].rearrange("p (c j) -> p c j", j=64)
                        ref = bcast_ap(G[:, 32:33], [[64, 16], [0, 64]])
                        D_, dn = nextD()
                        tt("dve", D_[:, :].rearrange("p (c j) -> p c j", j=64), Gv, ref, ALU.subtract, ["G"], [dn])
                        exp_prod(D_[:, :], dn, 1.0 / 16, prod[(1, hp, "qr")][:, hs], qT[:, hs], "qT")
                        exp_prod(D_[:, :], dn, -1.0 / 16, prod[(1, hp, "kr")][:, hs], kT[:, hs], "kT")
                        D_, dn = nextD()
                        tt("dve", D_[:, :].rearrange("p (c j) -> p c j", j=64), Gv, T63, ALU.subtract, ["G", "Fc"], [dn])
                        exp_prod(D_[:, :], dn, 1.0 / 16, prod[(1, hp, "qb")][:, hs], qT[:, hs], "qT")
                        exp_prod(G[:, :], "G", -1.0 / 16, kdT[:, :], kT[:, hs], "kT", "kdT")
                    for g4 in range(2):
                        pbk = 2 + g4
                        pv = Pb[pbk][:, 0:512].rearrange("p (t n) -> p t n", t=4)
                        for tq in range(4):
                            c0 = (g4 * 4 + tq) * 128
                            tr(pv[:, tq, :], kdT[:, c0:c0 + 128], ident[:, :], ["kdT", "ident"], [PN[pbk]])
                        t0 = half * 8 + g4 * 4
                        cp("dve", kd[:, t0:t0 + 4, dh * 128:(dh + 1) * 128], pv, [PN[pbk]], ["kd"])
        S.barrier()

        checkpoint("D1")
        qk_off = R2
        Sst = [A("Sst%d" % i, [128, 32, 128], BF16, at=qk_off + i * 8192) for i in range(4)]
        Sf = [A("Sf%d" % i, [128, 256], F32, at=HTB + i * 1024) for i in range(4)]
        for dh in range(4):
            memset("pool", Sf[dh][:, :], 0.0, ["Sf%d" % dh])
        for step in range(32):
            for dh in range(4):
                d_, hp = divmod(dh, 2)
                n = step if d_ == 0 else 31 - step
                t, c = divmod(n, 2)
                rows = slice(c * 64, (c + 1) * 64)
                cp("pool", Sst[dh][0:64, n, :], Sf[dh][0:64, 0:128], ["Sf%d" % dh], ["SstA%d" % dh])
                cp("act", Sst[dh][64:128, n, :], Sf[dh][64:128, 128:256], ["Sf%d" % dh], ["SstB%d" % dh])
                if step == 31:
                    continue
                pbk = 4 * c + dh
                mm(P[pbk][:, 0:256], kd[rows, t, dh * 128:(dh + 1) * 128], vtok[rows, t, hp * 256:(hp + 1) * 256],
                   True, True, ["kd", "vtok"], [PN[pbk]])
                stt(Sf[dh][:, :], Sf[dh][:, :], dec[:, dh, n:n + 1], P[pbk][:, 0:256], ALU.mult, ALU.add,
                    ["Sf%d" % dh, "dec", PN[pbk]], ["Sf%d" % dh])
        S.barrier()

        checkpoint("D2")
        smb = [A("smb%d" % i, [128, 2, 2, 128], BF16, at=HTB + 4096 + i * 1024) for i in range(2)]
        sqoL = [A("sqo%d" % i, [128, 256], F32, at=HTB + 6144 + i * 1024) for i in range(2)]
        rsoL = [A("rso%d" % i, [128, 256], F32, at=HTB + 8192 + i * 1024) for i in range(2)]
        t1oL = [A("t1o%d" % i, [128, 256], F32, at=HTB + 10240 + i * 1024) for i in range(2)]
        for t in range(NT):
            tsl = slice(t * 128, (t + 1) * 128)
            for par in range(2):
                rows = slice(par * 64, (par + 1) * 64)
                sbk = par
                obk = 2 + par
                scv = P[sbk][:, :].rearrange("p (a b n) -> p a b n", a=2, b=2)
                for hp in range(2):
                    for d_ in range(2):
                        mm(scv[:, hp, d_, :], prod[(d_, hp, "kr")][rows, tsl], prod[(d_, hp, "qr")][rows, tsl], True, True,
                           ["prod"], [PN[sbk]])
                mk = bcast_ap(masks[:, 0:256], [[0, 2], [1, 256]])
                tt("dve", smb[par][:, :, :, :].rearrange("p a b n -> p a (b n)"),
                   P[sbk][:, :].rearrange("p (a m) -> p a m", a=2), mk, ALU.mult, [PN[sbk], "masks"], ["smb%d" % par])
                ov = P[obk][:, 0:256].rearrange("p (a n) -> p a n", a=2)
                for hp in range(2):
                    h = hp * 2 + par
                    mm(ov[:, hp, :], vtok[:, t, h * 128:(h + 1) * 128], smb[par][:, hp, 0, :], True, False,
                       ["vtok", "smb%d" % par], [PN[obk]])
                    mm(ov[:, hp, :], vtok[:, t, h * 128:(h + 1) * 128], smb[par][:, hp, 1, :], False, False,
                       ["vtok", "smb%d" % par], [PN[obk]])
                    for d_ in range(2):
                        dh = d_ * 2 + hp
                        for c in range(2):
                            n = t * 2 + c
                            csl = slice(t * 128 + c * 64, t * 128 + (c + 1) * 64)
                            last = (d_ == 1 and c == 1)
                            mm(ov[:, hp, c * 64:(c + 1) * 64], Sst[dh][rows, n, :], prod[(d_, hp, "qb")][rows, csl],
                               False, last, ["SstA%d" % dh, "SstB%d" % dh, "prod"], [PN[obk]])
                sqo, rso, t1o = sqoL[par], rsoL[par], t1oL[par]
                sP = "%d" % par
                act(sqo[:, :], P[obk][:, 0:256], AF.Square, [PN[obk]], ["sqo" + sP])
                ebk = 4 + par
                mm(P[ebk][:, 0:256], onesf[:, :], sqo[:, :], True, True, ["onesf", "sqo" + sP], [PN[ebk]])
                rsqrt_act(rso[:, :], P[ebk][:, 0:256], 128, [PN[ebk]], ["rso" + sP])
                stt(t1o[:, :], P[obk][:, 0:256], gout[:, 0:1], rso[:, :], ALU.mult, ALU.mult, [PN[obk], "gout", "rso" + sP], ["t1o" + sP])
                for hp in range(2):
                    h = hp * 2 + par
                    tt("pool", mixT[:, h, tsl], t1o[:, hp * 128:(hp + 1) * 128], sgT[:, h, tsl], ALU.mult,
                       ["t1o" + sP, "sgT"], ["mixT_g"])
        S.barrier()
        A.reset(L0)
        if debug:
            final_ops.append(dma("sp", dbg["mix"][:, :], mixT[:, :, :].rearrange("p c n -> p (c n)"), reads=["mixT_g", "mixT_m"]))

        checkpoint("D3")
        X = A("X", [128, NT, D], F32)
        h2T = A("h2T", [128, 8, S_LEN], BF16)
        wo = A("wo", [128, 8, D], BF16)
        WO_OFF = A.off - 16384
        g2 = A("g2", [128, D], F32)
        wr = A("wr", [128, 8, 36], BF16)
        rbias = A("rbias", [128, 36], F32)
        gTf = A("gTf", [32, S_LEN], F32)
        identf = A("identf", [128, 128], F32)
        cp("dve", identf[:, :], ident[:, :], ["ident"], ["identf"])
        gwb = [A("gwb%d" % i, [128, 512], F32) for i in range(4)]
        lgA = A("lgA", [128, NT, 36], F32)
        hb = [A("hb2_%d" % i, [128, D], BF16) for i in range(2)]
        sqj = A("sqj2", [128, D], BF16)
        st1 = A("st1_2", [128, 4], F32)
        dma("pool", wo[:, :, :], wout_d.ap().rearrange("(c p) n -> p c n", p=128), writes=["wo"])
        dma("sp", g2[:, :], bcast_row(g2_d, D), writes=["g2"])
        dma("pool", wr[:, :, 0:4], wrg_d.ap().rearrange("(c p) n -> p c n", p=128), writes=["wr"])
        dma("pool", wr[:, :, 4:36], wre_d.ap().rearrange("(c p) n -> p c n", p=128), writes=["wr"])
        dma("sp", rbias[:, 0:4], bcast_row(brg_d, 4), writes=["rbias"])
        dma("sp", rbias[:, 4:36], bcast_row(bre_d, 32), writes=["rbias"])
        for t in range(NT):
            tsl = slice(t * 128, (t + 1) * 128)
            dma("sp", X[:, t, :], x_d[tsl, :], writes=["X%d" % t])
            for ch in range(2):
                pbk = (t % 2) * 2 + ch
                for kc in range(8):
                    mm(P[pbk][:, :], mixT[:, kc, tsl], wo[:, kc, ch * 512:(ch + 1) * 512], kc == 0, kc == 7,
                       ["mixT_g", "mixT_m", "wo"], [PN[pbk]])
                tt("dve", X[:, t, ch * 512:(ch + 1) * 512], X[:, t, ch * 512:(ch + 1) * 512], P[pbk][:, :], ALU.add,
                   ["X%d" % t, PN[pbk]], ["X%d" % t])
        if debug:
            for t in range(NT):
                final_ops.append(dma("sp", dbg["x1"][t * 128:(t + 1) * 128, :], X[:, t, :], reads=["X%d" % t]))
        checkpoint("E")
        for t in range(NT):
            tsl = slice(t * 128, (t + 1) * 128)
            norm_to_T(lambda t: X[:, t, :], ["X%d" % t], g2, "g2", h2T, "h2T", "F", t, 4 + t % 2)
            rbk = 6 + t % 2
            for kc in range(8):
                mm(P[rbk][:, 0:36], h2T[:, kc, tsl], wr[:, kc, :], kc == 0, kc == 7, ["h2T_%d" % (t // 4), "wr"], [PN[rbk]])
            tt("dve", lgA[:, t, :], P[rbk][:, 0:36], rbias[:, :], ALU.add, [PN[rbk], "rbias"], ["lgA"])

        def bl(ap2, k):
            return bcast_ap(ap2, [list(ap2.ap[1]), [0, k]])

        def red(out, in_, o, reads, writes):
            return op("dve", lambda e: e.tensor_reduce(out=out, in_=in_, axis=AX.X, op=o), reads=reads, writes=writes,
                      n=fsz(in_))

        r16 = lambda nm: A(nm, [128, NT], F32)
        r4 = lambda nm: A(nm, [128, NT, 4], F32)
        r8 = lambda nm: A(nm, [128, NT, 8], F32)
        mg, s4, ptop, m1, m2, dm, e2, den, w1, w2 = [r16("r16_%d" % i) for i in range(10)]
        d4, e4, oh, ohp = [r4("r4_%d" % i) for i in range(4)]
        ls, tmp8, eq1, ls2, eq2, wg8 = [r8("r8_%d" % i) for i in range(6)]
        gate = A("gate", [128, NT, 32], F32)
        red(mg[:, :], lgA[:, :, 0:4], ALU.max, ["lgA"], ["mg"])
        tt("dve", d4[:, :, :], lgA[:, :, 0:4], bl(mg[:, :], 4), ALU.subtract, ["lgA", "mg"], ["d4"])
        act(e4[:, :, :], d4[:, :, :], AF.Exp, ["d4"], ["e4"])
        red(s4[:, :], e4[:, :, :], ALU.add, ["e4"], ["s4"])
        op("dve", lambda e: e.reciprocal(out=ptop[:, :], in_=s4[:, :]), reads=["s4"], writes=["ptop"], n=128)
        ts("dve", oh[:, :, :], d4[:, :, :], 0.0, None, ALU.is_equal, None, ["d4"], ["oh"])
        tt("dve", ohp[:, :, :], oh[:, :, :], bl(ptop[:, :], 4), ALU.mult, ["oh", "ptop"], ["ohp"])
        tt("dve", ls[:, :, :], lgA[:, :, 4:12], bl(oh[:, :, 0], 8), ALU.mult, ["lgA", "oh"], ["ls"])
        for g_ in range(1, 4):
            tt("dve", tmp8[:, :, :], lgA[:, :, 4 + 8 * g_:12 + 8 * g_], bl(oh[:, :, g_], 8), ALU.mult, ["lgA", "oh"], ["tmp8"])
            tt("dve", ls[:, :, :], ls[:, :, :], tmp8[:, :, :], ALU.add, ["ls", "tmp8"], ["ls"])
        red(m1[:, :], ls[:, :, :], ALU.max, ["ls"], ["m1"])
        tt("dve", eq1[:, :, :], ls[:, :, :], bl(m1[:, :], 8), ALU.is_equal, ["ls", "m1"], ["eq1"])
        stt(ls2[:, :, :], eq1[:, :, :], -1e30, ls[:, :, :], ALU.mult, ALU.add, ["eq1", "ls"], ["ls2"])
        red(m2[:, :], ls2[:, :, :], ALU.max, ["ls2"], ["m2"])
        tt("dve", eq2[:, :, :], ls2[:, :, :], bl(m2[:, :], 8), ALU.is_equal, ["ls2", "m2"], ["eq2"])
        tt("dve", dm[:, :], m2[:, :], m1[:, :], ALU.subtract, ["m1", "m2"], ["dm"])
        act(e2[:, :], dm[:, :], AF.Exp, ["dm"], ["e2"])
        ts("dve", den[:, :], e2[:, :], 1.0, None, ALU.add, None, ["e2"], ["den"])
        op("dve", lambda e: e.reciprocal(out=w1[:, :], in_=den[:, :]), reads=["den"], writes=["w1"], n=128)
        tt("dve", w2[:, :], e2[:, :], w1[:, :], ALU.mult, ["e2", "w1"], ["w2"])
        tt("dve", wg8[:, :, :], eq1[:, :, :], bl(w1[:, :], 8), ALU.mult, ["eq1", "w1"], ["wg8"])
        tt("dve", tmp8[:, :, :], eq2[:, :, :], bl(w2[:, :], 8), ALU.mult, ["eq2", "w2"], ["tmp8"])
        tt("dve", wg8[:, :, :], wg8[:, :, :], tmp8[:, :, :], ALU.add, ["wg8", "tmp8"], ["wg8"])
        for g_ in range(4):
            tt("dve", gate[:, :, g_ * 8:(g_ + 1) * 8], wg8[:, :, :], bl(ohp[:, :, g_], 8), ALU.mult, ["wg8", "ohp"], ["gate"])
        if debug:
            for t in range(NT):
                final_ops.append(dma("sp", dbg["gate"][t * 128:(t + 1) * 128, :], gate[:, t, :], reads=["gate"]))
        for q4 in range(4):
            bk = 4 + q4
            for tq in range(4):
                t = q4 * 4 + tq
                tr(P[bk][0:32, tq * 128:(tq + 1) * 128], gate[:, t, :], identf[:, :], ["gate", "identf"], [PN[bk]])
            cp("act" if q4 % 2 else "dve", gTf[0:32, q4 * 512:(q4 + 1) * 512], P[bk][0:32, :], [PN[bk]], ["gTf"])
        dma("sp", gdr[:, :], gTf[0:32, :], reads=["gTf"], writes=["gdr"])
        checkpoint("F")

        EG = 2
        NEG = 32 // EG
        MX = SB_BASE + 256
        S.alias(["hid0", "hid1", "sil0", "sil1", "t1m0", "t1m1", "wdn0", "wdn1"], ["mixT_g", "mixT_m"])
        hid = [A("hid%d" % b, [128, EG, 2, 512], BF16, at=MX + b * 4096) for b in range(2)]
        sil = [A("sil%d" % b, [128, 512], F32, at=MX + 8192 + b * 2048) for b in range(2)]
        t1m = [A("t1m%d" % b, [128, 512], F32, at=MX + 12288 + b * 2048) for b in range(2)]
        wdn = [[A("wd%d_%d" % (b, j), [128, 2, D], BF16, at=MX + 16384 + (b * EG + j) * 4096) for j in range(EG)] for b in range(2)]
        wgt = [None, None]
        wup = [None, None]
        wgt[0] = [A("wg0_%d" % j, [128, 8, 256], BF16) for j in range(EG)]
        wup[0] = [A("wu0_%d" % j, [128, 8, 256], BF16) for j in range(EG)]
        wgt[1] = [A("wg1_%d" % j, [128, 8, 256], BF16, at=WO_OFF + j * 4096) for j in range(EG)]
        wup[1] = [A("wu1_%d" % j, [128, 8, 256], BF16, at=WO_OFF + 8192 + j * 4096) for j in range(EG)]
        itc = 0
        gcnt = [0]
        for eg in range(NEG):
            b = eg % 2
            extra = ["wo"] if b == 1 else []
            for j in range(EG):
                e_ = eg * EG + j
                dma("pool", wgt[b][j][:, :, :], weg_d.ap()[e_].rearrange("(c p) f -> p c f", p=128), writes=["wgt%d" % b] + extra)
                dma("pool", wup[b][j][:, :, :], weu_d.ap()[e_].rearrange("(c p) f -> p c f", p=128), writes=["wup%d" % b] + extra)
                dma("pool", wdn[b][j][:, :, :], wed_d.ap()[e_].rearrange("(c p) d -> p c d", p=128), writes=["wdn%d" % b])
            for tg in range(4):
                hbi = itc % 2
                itc += 1
                tgs = slice(tg * 512, (tg + 1) * 512)
                for j in range(EG):
                    e_ = eg * EG + j
                    gbk = 6 + j
                    for fh in range(2):
                        k2 = fh
                        gb_, ub_ = 0 + k2, 2 + k2
                        for kc in range(8):
                            mm(P[gb_][:, :], wgt[b][j][:, kc, fh * 128:(fh + 1) * 128], h2T[:, kc, tgs], kc == 0, kc == 7,
                               ["wgt%d" % b, "h2T_%d" % tg], [PN[gb_]])
                        for kc in range(8):
                            mm(P[ub_][:, :], wup[b][j][:, kc, fh * 128:(fh + 1) * 128], h2T[:, kc, tgs], kc == 0, kc == 7,
                               ["wup%d" % b, "h2T_%d" % tg], [PN[ub_]])
                        if fh == 0:
                            gk = gcnt[0] % 4
                            gcnt[0] += 1
                            dma("sp", gwb[gk][:, :], bass.AP(gdr, e_ * S_LEN + tg * 512, [[0, 128], [1, 512]]),
                                reads=["gdr"], writes=["gwb%d" % gk])
                        act(sil[k2][:, :], P[gb_][:, :], AF.Silu, [PN[gb_]], ["sil%d" % k2])
                        tt("dve", t1m[k2][:, :], sil[k2][:, :], P[ub_][:, :], ALU.mult, ["sil%d" % k2, PN[ub_]], ["t1m%d" % k2])
                        tt("dve", hid[hbi][:, j, fh, :], t1m[k2][:, :], gwb[gk][:, :], ALU.mult, ["t1m%d" % k2, "gwb%d" % gk],
                           ["hid%d" % hbi])
                for tt_ in range(4):
                    t = tg * 4 + tt_
                    for ch in range(2):
                        abk = 4 + (tt_ * 2 + ch) % 2
                        n_acc = EG * 2
                        a_i = 0
                        for j in range(EG):
                            for fh in range(2):
                                mm(P[abk][:, :], hid[hbi][:, j, fh, tt_ * 128:(tt_ + 1) * 128],
                                   wdn[b][j][:, fh, ch * 512:(ch + 1) * 512], a_i == 0, a_i == n_acc - 1,
                                   ["hid%d" % hbi, "wdn%d" % b], [PN[abk]])
                                a_i += 1
                        tt("dve", X[:, t, ch * 512:(ch + 1) * 512], X[:, t, ch * 512:(ch + 1) * 512], P[abk][:, :], ALU.add,
                           ["X%d" % t, PN[abk]], ["X%d" % t])
        for t in range(NT):
            final_ops.append(dma("sp", out_d[t * 128:(t + 1) * 128, :], X[:, t, :], reads=["X%d" % t]))

        S.emit(es, final_wait_ops=final_ops)
    return nc


def make_consts():
    ident = np.eye(128, dtype=np.float32).astype(ml_dtypes.bfloat16)
    j = np.arange(128)[:, None]
    i = np.arange(128)[None, :]
    same = (j // 64) == (i // 64)
    mf = (same & (j <= i)).astype(np.float32)
    mb = (same & (j > i)).astype(np.float32)
    masks = np.concatenate([mf, mb], axis=1).astype(ml_dtypes.bfloat16)
    invf64 = 10000.0 ** (-np.arange(0, 32, 2, dtype=np.float64) / 32)
    invf_hi = invf64.astype(np.float32)
    invf_lo = (invf64 - invf_hi.astype(np.float64)).astype(np.float32)
    invf = np.broadcast_to(np.concatenate([invf_hi, invf_lo])[None, :], (128, 32)).copy()
    sel = np.zeros((32, 32, 128), np.float32)
    for e in range(32):
        sel[e, e, :] = 1.0
    sel = sel.reshape(32, 32 * 128).astype(ml_dtypes.bfloat16)
    lrb = np.zeros((64, 1), np.float32)
    lrb[16, 0] = 1.0
    return {"c_ident": ident, "c_masks": masks, "c_invf": invf, "c_sel": sel, "c_lrbias": lrb}


_NC_CACHE = {}


def make_in_maps(inputs, n_cores=8):
    c = make_consts()
    f = lambda k: np.ascontiguousarray(np.asarray(inputs[k], dtype=np.float32)[0])
    shared = {
        "norm1_gain": f("norm1_gain").reshape(1, D),
        "w_in": f("w_in"),
        "gla_gk_fwd_w": f("gla_gk_fwd_w"), "gla_gk_fwd_b": f("gla_gk_fwd_b").reshape(1, 256),
        "gla_gk_bwd_w": f("gla_gk_bwd_w"), "gla_gk_bwd_b": f("gla_gk_bwd_b").reshape(1, 256),
        "gla_out_gain": f("gla_out_gain").reshape(128, 1),
        "mla_q_gain": f("mla_q_gain").reshape(1, 256), "mla_w_qb": f("mla_w_qb"),
        "mla_kv_gain": f("mla_kv_gain").reshape(1, 128), "mla_w_kvb": f("mla_w_kvb"),
        "q_norm_gain": f("q_norm_gain").reshape(1, 96), "k_norm_gain": f("k_norm_gain").reshape(1, 96),
        "w_out": f("w_out"), "norm2_gain": f("norm2_gain").reshape(1, D),
        "w_router_group": f("w_router_group"), "b_router_group": f("b_router_group").reshape(1, 4),
        "w_router_expert": f("w_router_expert"), "b_router_expert": f("b_router_expert").reshape(1, 32),
        "w_expert_gate": f("w_expert_gate").reshape(32, D, 256),
        "w_expert_up": f("w_expert_up").reshape(32, D, 256),
        "w_expert_down": f("w_expert_down").reshape(32, 256, D),
    }
    shared.update(c)
    x = np.asarray(inputs["x"], dtype=np.float32)
    pos = np.asarray(inputs["positions"]).astype(np.int32)
    maps = []
    for b in range(n_cores):
        m = dict(shared)
        m["x"] = np.ascontiguousarray(x[b])
        m["pos"] = np.ascontiguousarray(pos[b].reshape(NT, 128).T)
        maps.append(m)
    return maps


def kernel(**inputs):
    if "nc" not in _NC_CACHE:
        _NC_CACHE["nc"] = build()
    nc = _NC_CACHE["nc"]
    maps = make_in_maps(inputs, 8)
    res = run_bass_kernel_spmd(nc, maps, core_ids=list(range(8)))
    out = np.stack([np.asarray(r["out"], dtype=np.float32) for r in res.results], axis=0)
    return out
```

```python
import contextlib
import math
import numpy as np
import ml_dtypes
import concourse.bass as bass
import concourse.mybir as mybir
from concourse.bass_utils import run_bass_kernel_spmd

F32 = mybir.dt.float32
BF16 = mybir.dt.bfloat16
I32 = mybir.dt.int32
ALU = mybir.AluOpType
AF = mybir.ActivationFunctionType
AX = mybir.AxisListType

S_LEN = 2048
D = 1024
NT = 16
EPS = 1e-6
PI = math.pi


class T:
    __slots__ = ("name", "w", "r")

    def __init__(self, name):
        self.name = name
        self.w = None
        self.r = []


class Op:
    __slots__ = ("eng", "fn", "deps", "signal", "sig", "dma", "dsem", "dval", "alld", "n", "seg", "idx", "nbytes", "tag")


class Sched:
    ENGS = ("pe", "act", "dve", "pool", "sp")

    def __init__(self, nc, n_dma_sems=12):
        self.nc = nc
        self.ops = {e: [] for e in self.ENGS}
        self.n_dma_sems = n_dma_sems
        self.dma_count = {e: 0 for e in self.ENGS}
        self.tiles = {}
        self.pending = {e: [] for e in self.ENGS}
        self.dma_since_barrier = []
        self.stopped = False
        self.seg = 0
        self.nops = 0
        self.noresched = set()

    def t(self, name):
        if name not in self.tiles:
            self.tiles[name] = T(name)
        return self.tiles[name]

    def _tl(self, lst):
        out = []
        for x in lst:
            if isinstance(x, str):
                out.append(self.t(x))
            elif isinstance(x, (list, tuple)):
                out.extend(self._tl(x))
            elif x is not None:
                out.append(x)
        return out

    def alias(self, new_names, old_names):
        if self.stopped:
            return
        for nn in new_names:
            tn = self.t(nn)
            for on in old_names:
                to = self.t(on)
                if to.w is not None:
                    tn.r.append(to.w)
                tn.r.extend(to.r)

    def barrier(self):
        if self.stopped:
            return
        lasts = []
        for e in self.ENGS:
            for o in reversed(self.ops[e]):
                if not o.dma:
                    lasts.append(o)
                    break
        lasts.extend(self.dma_since_barrier)
        self.dma_since_barrier = []
        for e in self.ENGS:
            self.pending[e] = list(lasts)
        self.seg += 1

    def op(self, eng, fn, reads=(), writes=(), dma=False, n=64, nbytes=0):
        if self.stopped:
            return None
        o = Op()
        o.n = n
        o.nbytes = nbytes
        o.seg = self.seg
        o.idx = self.nops
        self.nops += 1
        o.eng = eng
        o.fn = fn
        o.dma = dma
        o.signal = False
        o.sig = 0
        deps = {}
        reads = self._tl(reads)
        writes = self._tl(writes)
        for t in reads:
            if t.w is not None:
                deps[id(t.w)] = (t.w, "raw")
            if t.name[0] == "P" and t.name[1:].isdigit():
                for r in t.r:
                    if id(r) not in deps and r.eng != eng:
                        deps[id(r)] = (r, "war")
        for t in writes:
            if t.w is not None and id(t.w) not in deps:
                deps[id(t.w)] = (t.w, "waw")
            for r in t.r:
                if id(r) not in deps:
                    deps[id(r)] = (r, "war")
        if self.pending[eng]:
            for p in self.pending[eng]:
                deps[id(p)] = (p, "raw")
            self.pending[eng] = []
        o.alld = [p for p, _k in deps.values()]
        o.tag = ("R:" + ",".join(t.name for t in reads) + " W:" + ",".join(t.name for t in writes))
        dl = []
        for p, kind in deps.values():
            if p.eng == eng and not p.dma:
                if eng == "pe":
                    continue
                if kind != "raw" and not dma and not STRICT_SAME_ENGINE:
                    continue
            dl.append(p)
        o.deps = dl
        for p in dl:
            p.signal = True
        for t in reads:
            if not dma:
                for r in t.r:
                    if not r.dma and r.eng == eng:
                        o.alld.append(r)
                t.r = [r for r in t.r if r.dma or r.eng != eng]
            t.r.append(o)
        for t in writes:
            t.w = o
            t.r = []
        if dma:
            self.dma_count[eng] += 1
            self.dma_since_barrier.append(o)
        self.ops[eng].append(o)
        return o

    @staticmethod
    def _dur(o):
        n = o.n
        if o.dma:
            return 60.0 if o.eng == "sp" else 900.0
        if o.eng == "pe":
            return 30.0 + max(n, 64) / 2.0
        if o.eng == "act":
            return 220.0 + n / 1.4
        if o.eng == "dve":
            return 120.0 + n * 1.3
        return 550.0 + n * 0.75

    def reschedule(self):
        allops = []
        for e in self.ENGS:
            allops.extend(self.ops[e])
        allops.sort(key=lambda o: o.idx)
        import heapq
        new = {e: [] for e in self.ENGS}
        segs = {}
        for o in allops:
            segs.setdefault(o.seg, []).append(o)
        LAT = 600.0
        for sg in sorted(segs):
            ops = segs[sg]
            if sg in self.noresched:
                for o in ops:
                    new[o.eng].append(o)
                continue
            inseg = set(id(o) for o in ops)
            done = {}
            users = {}
            indeg = {}
            first = {}
            for o in ops:
                if o.eng not in first:
                    first[o.eng] = o
                elif first[o.eng] not in o.alld:
                    o.alld.append(first[o.eng])
            for o in ops:
                k = 0
                for p in o.alld:
                    if id(p) in inseg:
                        k += 1
                        users.setdefault(id(p), []).append(o)
                indeg[id(o)] = k
            ready = {e: [] for e in self.ENGS}
            efree = {e: 0.0 for e in self.ENGS}
            rtime = {}
            for o in ops:
                if indeg[id(o)] == 0:
                    rtime[id(o)] = 0.0
                    ready[o.eng].append(o)
            left = len(ops)
            SLACK = 0.0
            while left:
                best = None
                for e in self.ENGS:
                    rl = ready[e]
                    if not rl:
                        continue
                    ef = efree[e]
                    oldest = None
                    fill = None
                    for o in rl:
                        st = max(rtime[id(o)], ef)
                        if oldest is None or o.idx < oldest[1].idx:
                            oldest = (st, o)
                        if fill is None or (st, o.idx) < (fill[0], fill[1].idx):
                            fill = (st, o)
                    pick = oldest if oldest[0] <= fill[0] + SLACK else fill
                    if best is None or (pick[0], pick[1].idx) < (best[0], best[1].idx):
                        best = pick
                st, o = best
                e = o.eng
                ready[e].remove(o)
                d = self._dur(o)
                efree[e] = st + d
                fin = st + d
                if o.dma:
                    fin = st + 2000.0 + o.nbytes / 150.0
                done[id(o)] = fin
                new[e].append(o)
                left -= 1
                for u in users.get(id(o), ()):
                    indeg[id(u)] -= 1
                    lat = 0.0 if (u.eng == o.eng and not o.dma) else LAT
                    rtime[id(u)] = max(rtime.get(id(u), 0.0), fin + lat)
                    if indeg[id(u)] == 0:
                        ready[u.eng].append(u)
        self.ops = new

    def emit(self, es, final_wait_ops=()):
        nc = self.nc
        if RESCHEDULE:
            self.reschedule()
        sems = {e: es.enter_context(nc.semaphore("s_" + e)) for e in self.ENGS}
        dsems = {e: [es.enter_context(nc.semaphore("d_%s_%d" % (e, i)))
                     for i in range(self.n_dma_sems)]
                 for e in self.ENGS if self.dma_count[e] > 0}
        for e in self.ENGS:
            c = 0
            i = 0
            for o in self.ops[e]:
                if o.dma:
                    o.dsem = i % self.n_dma_sems
                    o.dval = 16 * (i // self.n_dma_sems + 1)
                    i += 1
                elif o.signal:
                    c += 1
                    o.sig = c
        block = es.enter_context(nc.Block())
        eng_obj = {"pe": block.tensor, "act": block.scalar, "dve": block.vector,
                   "pool": block.gpsimd, "sp": block.sync}
        for e in self.ENGS:
            ops = self.ops[e]
            if not ops:
                continue

            def body(engine, e=e, ops=ops):
                waited = {}

                def wait(sem, key, val):
                    if waited.get(key, 0) >= val:
                        return
                    waited[key] = val
                    engine.wait_ge(sem, val)

                for o in ops:
                    for p in o.deps:
                        if p.dma:
                            wait(dsems[p.eng][p.dsem], ("d", p.eng, p.dsem), p.dval)
                        else:
                            wait(sems[p.eng], ("c", p.eng), p.sig)
                    if o.dma and o.dval > 16:
                        wait(dsems[e][o.dsem], ("d", e, o.dsem), o.dval - 16)
                    ins = o.fn(engine)
                    if o.dma:
                        ins.then_inc(dsems[e][o.dsem], 16)
                    elif o.signal:
                        ins.then_inc(sems[e], 1)
                if e == "sp":
                    for o in final_wait_ops:
                        if o is None:
                            continue
                        wait(dsems[o.eng][o.dsem], ("d", o.eng, o.dsem), o.dval)

            eng_obj[e](body)


RESCHEDULE = True
STRICT_SAME_ENGINE = True
SB_BASE = 16640
SB_END = 229376


class Alloc:
    def __init__(self, nc):
        self.nc = nc
        self.off = SB_BASE
        self.n = 0

    def mark(self):
        return self.off

    def reset(self, m):
        self.off = m

    def __call__(self, name, shape, dt, at=None):
        esz = 2 if dt == BF16 else 4
        nb = int(np.prod(shape[1:])) * esz
        nb = (nb + 63) // 64 * 64
        self.n += 1
        if at is None:
            at = self.off
            self.off += nb
        assert at + nb <= SB_END, ("SBUF overflow", name, at, nb)
        return self.nc.alloc_sbuf_tensor_at("%s_%d" % (name, self.n), list(shape), dt, offset=at)


def bcast_ap(ap, pattern):
    return bass.AP(ap.tensor, ap.offset, [list(ap.ap[0])] + [list(p) for p in pattern])


def build(debug=False, stop_after=None):
    nc = bass.Bass("TRN2", target_bir_lowering=False)
    dr = lambda n, s, dt=F32: nc.dram_tensor(n, list(s), dt, kind="ExternalInput")
    x_d = dr("x", [S_LEN, D])
    pos_d = dr("pos", [128, NT], I32)
    g1_d = dr("norm1_gain", [1, D])
    win_d = dr("w_in", [D, 1984])
    gkf_w = dr("gla_gk_fwd_w", [16, 256])
    gkf_b = dr("gla_gk_fwd_b", [1, 256])
    gkb_w = dr("gla_gk_bwd_w", [16, 256])
    gkb_b = dr("gla_gk_bwd_b", [1, 256])
    go_d = dr("gla_out_gain", [128, 1])
    gqa_d = dr("mla_q_gain", [1, 256])
    wqb_d = dr("mla_w_qb", [256, 768])
    gkva_d = dr("mla_kv_gain", [1, 128])
    wkvb_d = dr("mla_w_kvb", [128, 1024])
    gqn_d = dr("q_norm_gain", [1, 96])
    gkn_d = dr("k_norm_gain", [1, 96])
    wout_d = dr("w_out", [D, D])
    g2_d = dr("norm2_gain", [1, D])
    wrg_d = dr("w_router_group", [D, 4])
    brg_d = dr("b_router_group", [1, 4])
    wre_d = dr("w_router_expert", [D, 32])
    bre_d = dr("b_router_expert", [1, 32])
    weg_d = dr("w_expert_gate", [32, D, 256])
    weu_d = dr("w_expert_up", [32, D, 256])
    wed_d = dr("w_expert_down", [32, 256, D])
    ident_d = dr("c_ident", [128, 128], BF16)
    masks_d = dr("c_masks", [128, 256], BF16)
    invf_d = dr("c_invf", [128, 32])
    sel_d = dr("c_sel", [32, 32 * 128], BF16)
    lrb_d = dr("c_lrbias", [64, 1])
    out_d = nc.dram_tensor("out", [S_LEN, D], F32, kind="ExternalOutput")
    gdr = nc.dram_tensor("gate_scratch", [32, S_LEN], F32, kind="Internal")
    dbg = {}
    if debug:
        dbg["mix"] = nc.dram_tensor("d_mix", [128, 8 * S_LEN], BF16, kind="ExternalOutput")
        dbg["x1"] = nc.dram_tensor("d_x1", [S_LEN, D], F32, kind="ExternalOutput")
        dbg["gate"] = nc.dram_tensor("d_gate", [S_LEN, 32], F32, kind="ExternalOutput")

    if debug:
        dbg["gen"] = nc.dram_tensor("d_gen", [128, 8 * S_LEN], BF16, kind="ExternalOutput")
    S = Sched(nc)
    A = Alloc(nc)
    op = S.op
    final_ops = []

    with contextlib.ExitStack() as es:
        P = [es.enter_context(nc.psum_tensor("pb%d" % i, [128, 512], F32)) for i in range(8)]
        Pb = [p.bitcast(BF16) for p in P]
        PN = ["P%d" % i for i in range(8)]

        def fsz(ap):
            r = 1
            for d_ in list(ap.shape)[1:]:
                r *= int(d_)
            return r

        def dma(q, out, in_, reads=(), writes=(), **kw):
            return op(q, lambda e: e.dma_start(out=out, in_=in_, **kw), reads=reads, writes=writes, dma=True,
                      nbytes=fsz(out) * 4 * 128)

        def act(out, in_, func, reads, writes, **kw):
            return op("act", lambda e: e.activation(out=out, in_=in_, func=func, **kw), reads=reads, writes=writes,
                      n=fsz(out))

        def rsqrt_act(out, in_, n, reads, writes):
            act(out, in_, AF.Ln, reads, writes, scale=1.0 / n, bias=EPS)
            act(out, out, AF.Exp, writes, writes, scale=-0.5)

        def tt(eng, out, in0, in1, o, reads, writes):
            return op(eng, lambda e: e.tensor_tensor(out=out, in0=in0, in1=in1, op=o), reads=reads, writes=writes,
                      n=fsz(out))

        def ts(eng, out, in0, s1, s2, o0, o1, reads, writes):
            if o1 is None:
                return op(eng, lambda e: e.tensor_scalar(out=out, in0=in0, scalar1=s1, scalar2=None, op0=o0),
                          reads=reads, writes=writes, n=fsz(out))
            return op(eng, lambda e: e.tensor_scalar(out=out, in0=in0, scalar1=s1, scalar2=s2, op0=o0, op1=o1),
                      reads=reads, writes=writes, n=fsz(out))

        def stt(out, in0, sc, in1, o0, o1, reads, writes):
            return op("dve", lambda e: e.scalar_tensor_tensor(out=out, in0=in0, scalar=sc, in1=in1, op0=o0, op1=o1),
                      reads=reads, writes=writes, n=fsz(out))

        def mm(out, lhsT, rhs, start, stop, reads, writes):
            return op("pe", lambda e: e.matmul(out, lhsT=lhsT, rhs=rhs, start=start, stop=stop),
                      reads=reads, writes=writes, n=fsz(rhs) * (4 if rhs.dtype == F32 else 1))

        def tr(out, in_, ident, reads, writes):
            return op("pe", lambda e: e.transpose(out=out, in_=in_, identity=ident), reads=reads, writes=writes, n=128)

        def cp(eng, out, in_, reads, writes):
            if eng == "act":
                return act(out, in_, AF.Copy, reads, writes)
            return op(eng, lambda e: e.tensor_copy(out=out, in_=in_), reads=reads, writes=writes, n=fsz(out))

        def memset(eng, ap, val, writes):
            return op(eng, lambda e: e.memset(ap, val), writes=writes, n=fsz(ap))

        def bcast_row(dram, n):
            return bass.AP(dram, 0, [[0, 128], [1, n]])

        ident = A("ident", [128, 128], BF16)
        mixT = A("mixT", [128, 8, S_LEN], BF16)
        dma("sp", ident[:, :], ident_d[:, :], writes=["ident"])
        L0 = A.mark()

        hT = A("hT", [128, 8, S_LEN], BF16)
        E1 = A.mark()
        g1 = A("g1", [128, D], F32)
        xt = [A("xt%d" % i, [128, D], F32) for i in range(2)]
        hb = [A("hb%d" % i, [128, D], BF16) for i in range(2)]
        sqj = A("sqj", [128, D], F32)
        st1 = A("st1", [128, 4], F32)
        dma("sp", g1[:, :], bcast_row(g1_d, D), writes=["g1"])

        def norm_to_T(src_ap_fn, src_tiles, gain, gname, dstT, dname, pfx, t, pbank):
            i = t % 2
            ssq = st1[:, 0:1]
            rs = st1[:, 1:2]
            act(sqj[:, :], src_ap_fn(t), AF.Square, src_tiles, [pfx + "sqj", pfx + "ssq"], accum_out=ssq)
            rsqrt_act(rs, ssq, D, [pfx + "ssq"], [pfx + "rs"])
            stt(hb[i][:, :], src_ap_fn(t), rs, gain[:, :], ALU.mult, ALU.mult,
                src_tiles + [pfx + "rs", gname], [pfx + "hb%d" % i])
            pbv = Pb[pbank][:, :].rearrange("p (c n) -> p c n", c=8)
            for kc in range(8):
                tr(pbv[:, kc, :], hb[i][:, kc * 128:(kc + 1) * 128], ident[:, :],
                   [pfx + "hb%d" % i, "ident"], [PN[pbank]])
            cp("dve" if t % 2 else "act", dstT[:, :, t * 128:(t + 1) * 128], pbv, [PN[pbank]], [dname + "_%d" % (t // 4)])

        for t in range(NT):
            i = t % 2
            dma("sp", xt[i][:, :], x_d[t * 128:(t + 1) * 128, :], writes=["xt%d" % i])
            norm_to_T(lambda t, i=i: xt[i][:, :], ["xt%d" % i], g1, "g1", hT, "hT", "A", t, t % 2)
        hT_tiles = ["hT_%d" % k for k in range(4)]
        S.barrier()
        A.reset(E1)
        def checkpoint(name, dump=None, reads=()):
            if stop_after == name:
                if dump is not None and debug:
                    S.barrier()
                    final_ops.append(dma("sp", dbg["gen"][:, :], dump, reads=list(reads)))
                S.stopped = True

        checkpoint("A")

        wm = A("w_in_mla", [128, 8, 416], BF16)
        wqb = A("wqb", [128, 2, 768], BF16)
        wkvb = A("wkvb", [128, 1024], BF16)
        cs = A("cs", [128, NT, 64], F32)
        qhT = A("qhT", [128, 8, S_LEN], BF16)
        khT = A("khT", [128, 8, S_LEN], BF16)
        vA = A("vA", [128, NT, 8, 128], BF16)
        gqa = A("gqa", [128, 384], F32)
        gqk = A("gqkr", [128, 16, 32], F32)
        gcol = A("gcol", [128, 2], F32)
        B1m = A.mark()
        dma("pool", wm[:, :, :], win_d.ap()[:, 1568:1984].rearrange("(c p) n -> p c n", p=128), writes=["wm"])
        dma("pool", wqb[:, :, :], wqb_d.ap().rearrange("(c p) n -> p c n", p=128), writes=["wqb"])
        dma("pool", wkvb[:, :], wkvb_d[:, :], writes=["wkvb"])
        dma("sp", gqa[:, 0:256], bcast_row(gqa_d, 256), writes=["gqa"])
        dma("sp", gqa[:, 256:384], bcast_row(gkva_d, 128), writes=["gqa"])
        dma("sp", gqk[:, 0:8, :], bass.AP(gqn_d, 64, [[0, 128], [0, 8], [1, 32]]), writes=["gqk"])
        dma("sp", gqk[:, 8:16, :], bass.AP(gkn_d, 64, [[0, 128], [0, 8], [1, 32]]), writes=["gqk"])
        memset("pool", gcol[:, :], 1.0, ["gcol"])
        dma("sp", gcol[0:64, 0:1], bass.AP(gqn_d, 0, [[1, 64], [1, 1]]), reads=["gcol"], writes=["gcol"])
        dma("sp", gcol[0:64, 1:2], bass.AP(gkn_d, 0, [[1, 64], [1, 1]]), reads=["gcol"], writes=["gcol"])
        posi = A("posi", [128, NT], I32)
        posf = A("posf", [128, NT], F32)
        invf = A("invf", [128, 32], F32)
        ang = A("ang", [128, NT, 16], F32)
        kk = A("kk", [128, NT, 16], F32)
        ki = A("ki", [128, NT, 16], I32)
        rr = A("rr", [128, NT, 16], F32)
        yy = A("yy", [128, NT, 16], F32)
        m_ = A("m_", [128, NT, 16], F32)
        dma("sp", posi[:, :], pos_d[:, :], writes=["posi"])
        dma("sp", invf[:, :], invf_d[:, :], writes=["invf"])
        cp("dve", posf[:, :], posi[:, :], ["posi"], ["posf"])
        for t in range(NT):
            ts("dve", ang[:, t, :], invf[:, 0:16], posf[:, t:t + 1], None, ALU.mult, None, ["invf", "posf"], ["ang"])
            stt(ang[:, t, :], invf[:, 16:32], posf[:, t:t + 1], ang[:, t, :], ALU.mult, ALU.add, ["invf", "posf", "ang"], ["ang"])
        ts("dve", kk[:, :, :], ang[:, :, :], 1.0 / (2 * PI), None, ALU.mult, None, ["ang"], ["kk"])
        cp("dve", ki[:, :, :], kk[:, :, :], ["kk"], ["ki"])
        cp("dve", kk[:, :, :], ki[:, :, :], ["ki"], ["kk"])
        C1 = 6.28125
        C2 = 2 * PI - C1
        stt(rr[:, :, :], kk[:, :, :], -C1, ang[:, :, :], ALU.mult, ALU.add, ["kk", "ang"], ["rr"])
        stt(rr[:, :, :], kk[:, :, :], -C2, rr[:, :, :], ALU.mult, ALU.add, ["kk", "rr"], ["rr"])
        for which, shift in ((1, 0.0), (0, PI / 2)):
            ts("dve", yy[:, :, :], rr[:, :, :], shift, None, ALU.add, None, ["rr"], ["yy"])
            ts("dve", m_[:, :, :], yy[:, :, :], PI, None, ALU.is_gt, None, ["yy"], ["m_"])
            stt(yy[:, :, :], m_[:, :, :], -2 * PI, yy[:, :, :], ALU.mult, ALU.add, ["m_", "yy"], ["yy"])
            ts("dve", m_[:, :, :], yy[:, :, :], -PI, None, ALU.is_lt, None, ["yy"], ["m_"])
            stt(yy[:, :, :], m_[:, :, :], 2 * PI, yy[:, :, :], ALU.mult, ALU.add, ["m_", "yy"], ["yy"])
            ts("dve", yy[:, :, :], yy[:, :, :], PI, -PI, ALU.min, ALU.max, ["yy"], ["yy"])
            if which == 0:
                act(cs[:, :, 0:16], yy[:, :, :], AF.Sin, ["yy"], ["cs"])
                act(cs[:, :, 16:32], yy[:, :, :], AF.Sin, ["yy"], ["cs"])
            else:
                act(cs[:, :, 48:64], yy[:, :, :], AF.Sin, ["yy"], ["cs"])
                act(cs[:, :, 32:48], cs[:, :, 48:64], AF.Copy, ["cs"], ["cs"], scale=-1.0)
        memset("pool", vA[:, :, :, :], 1.0, ["vA"])
        S.barrier()
        A.reset(B1m)

        sqjb1 = A("sqjb", [128, 416], BF16)
        sqjb = [sqjb1, sqjb1]
        stq = [A("stq%d" % i, [128, 32], F32) for i in range(2)]
        ab = [A("ab%d" % i, [128, 384], BF16) for i in range(2)]
        abT = [A("abT%d" % i, [128, 3, 128], BF16) for i in range(2)]
        kraw = [A("kraw%d" % i, [128, 8, 96], F32) for i in range(2)]
        sqn = A("sqn", [128, 16, 96], BF16)
        rg = [A("rg%d" % i, [128, 16, 32], F32) for i in range(2)]
        rg2 = A("rg2", [128, 16, 48], F32)
        rb = A("rb", [128, 16, 32], F32)
        qkf = [A("qkf%d" % i, [128, 16, 96], BF16) for i in range(2)]
        SQ2 = math.sqrt(2.0)

        def st_E1a(t):
            i = t % 2
            sI = "_%d" % i
            tsl = slice(t * 128, (t + 1) * 128)
            hTt = "hT_%d" % (t // 4)
            for kc in range(8):
                mm(P[0][:, 0:416], hT[:, kc, tsl], wm[:, kc, :], kc == 0, kc == 7, [hTt, "wm"], ["P0"])
            act(sqjb[i][:, 0:256], P[0][:, 0:256], AF.Square, ["P0"], ["stqA" + sI], accum_out=stq[i][:, 0:1])
            act(sqjb[i][:, 256:384], P[0][:, 256:384], AF.Square, ["P0"], ["stqA" + sI],
                accum_out=stq[i][:, 1:2], scale=SQ2)
            act(kraw[i][:, :, 64:96], bcast_ap(P[0][:, 384:416], [[0, 8], [1, 32]]), AF.Copy, ["P0"], ["krawR" + sI])
            rsqrt_act(stq[i][:, 2:4], stq[i][:, 0:2], 256, ["stqA" + sI], ["stqB" + sI])
            stt(ab[i][:, 0:256], P[0][:, 0:256], stq[i][:, 2:3], gqa[:, 0:256], ALU.mult, ALU.mult,
                ["P0", "stqB" + sI, "gqa"], ["ab" + sI])
            stt(ab[i][:, 256:384], P[0][:, 256:384], stq[i][:, 3:4], gqa[:, 256:384], ALU.mult, ALU.mult,
                ["P0", "stqB" + sI, "gqa"], ["ab" + sI])
        def st_E1b(t):
            i = t % 2
            sI = "_%d" % i
            tsl = slice(t * 128, (t + 1) * 128)
            hTt = "hT_%d" % (t // 4)
            p1v = Pb[1][:, 0:384].rearrange("p (c n) -> p c n", c=3)
            for c in range(3):
                tr(p1v[:, c, :], ab[i][:, c * 128:(c + 1) * 128], ident[:, :], ["ab" + sI, "ident"], ["P1"])
            cp("act", abT[i][:, :, :], p1v, ["P1"], ["abT" + sI])
        def st_E2(t):
            i = t % 2
            sI = "_%d" % i
            tsl = slice(t * 128, (t + 1) * 128)
            hTt = "hT_%d" % (t // 4)
            for nb in range(2):
                for kc in range(2):
                    mm(P[2 + nb][:, 0:384], abT[i][:, kc, :], wqb[:, kc, nb * 384:(nb + 1) * 384], kc == 0, kc == 1,
                       ["abT" + sI, "wqb"], [PN[2 + nb]])
                mm(P[4 + nb][:, :], abT[i][:, 2, :], wkvb[:, nb * 512:(nb + 1) * 512], True, True,
                   ["abT" + sI, "wkvb"], [PN[4 + nb]])
            for nb in range(2):
                srck = P[4 + nb][:, :].rearrange("p (h d) -> p h d", h=4)[:, :, 0:64]
                cp("act", kraw[i][:, nb * 4:nb * 4 + 4, 0:64], srck, [PN[4 + nb]], ["krawN" + sI])
                srcv = P[4 + nb][:, :].rearrange("p (a b d) -> p a b d", a=2, b=2)
                dstv = vA[:, t, nb * 4:nb * 4 + 4, :].rearrange("p (a b) d -> p a b d", b=2)
                cp("act", dstv[:, :, 0, 0:64], srcv[:, :, 0, 64:128], [PN[4 + nb]], ["vA"])
                cp("act", dstv[:, :, 1, 64:128], srcv[:, :, 1, 64:128], [PN[4 + nb]], ["vA"])
            for nb in range(2):
                act(sqn[:, nb * 4:nb * 4 + 4, :], P[2 + nb][:, 0:384].rearrange("p (h d) -> p h d", h=4), AF.Square,
                    [PN[2 + nb]], ["sqn"])
            act(sqn[:, 8:16, :], kraw[i][:, :, :], AF.Square, ["krawN" + sI, "krawR" + sI], ["sqn"])
            op("dve", lambda e, i=i: e.tensor_reduce(out=stq[i][:, 8:24], in_=sqn[:, :, :], axis=AX.X, op=ALU.add),
               reads=["sqn"], writes=["stqC" + sI])
            rsqrt_act(stq[i][:, 8:24], stq[i][:, 8:24], 96, ["stqC" + sI], ["stqC" + sI])
            for nb in range(2):
                pv = P[2 + nb][:, 0:384].rearrange("p (h d) -> p h d", h=4)
                rq = stq[i][:, 8 + nb * 4:9 + nb * 4]
                tt("dve", qkf[i][:, nb * 4:nb * 4 + 4, 0:64], pv[:, :, 0:64], bcast_ap(rq, [[1, 4], [0, 64]]), ALU.mult,
                   [PN[2 + nb], "stqC" + sI], ["qkf" + sI])
                tt("dve", rg[i][:, nb * 4:nb * 4 + 4, :], pv[:, :, 64:96], bcast_ap(rq, [[1, 4], [0, 32]]), ALU.mult,
                   [PN[2 + nb], "stqC" + sI], ["rg" + sI])
            rk = stq[i][:, 16:17]
            tt("dve", qkf[i][:, 8:16, 0:64], kraw[i][:, :, 0:64], bcast_ap(rk, [[1, 8], [0, 64]]), ALU.mult,
               ["krawN" + sI, "stqC" + sI], ["qkf" + sI])
            tt("dve", rg[i][:, 8:16, :], kraw[i][:, :, 64:96], bcast_ap(rk, [[1, 8], [0, 32]]), ALU.mult,
               ["krawR" + sI, "stqC" + sI], ["rg" + sI])
        def st_L(t):
            i = t % 2
            sI = "_%d" % i
            tsl = slice(t * 128, (t + 1) * 128)
            hTt = "hT_%d" % (t // 4)
            tt("pool", rg2[:, :, 0:32], rg[i][:, :, :], gqk[:, :, :], ALU.mult, ["rg" + sI, "gqk"], ["rg2"])
            tt("pool", rg2[:, :, 32:48], rg[i][:, :, 0:16], gqk[:, :, 0:16], ALU.mult, ["rg" + sI, "gqk"], ["rg2"])
            c1 = bcast_ap(cs[:, t, 0:32], [[0, 16], [1, 32]])
            c2 = bcast_ap(cs[:, t, 32:64], [[0, 16], [1, 32]])
            tt("pool", rg[i][:, :, :], rg2[:, :, 0:32], c1, ALU.mult, ["rg2", "cs"], ["rg" + sI])
            tt("pool", rb[:, :, :], rg2[:, :, 16:48], c2, ALU.mult, ["rg2", "cs"], ["rb"])
            tt("pool", qkf[i][:, :, 64:96], rg[i][:, :, :], rb[:, :, :], ALU.add, ["rg" + sI, "rb"], ["qkf" + sI])
            p6v = Pb[6][:, :].rearrange("p (h n) -> p h n", h=8)
            p7v = Pb[7][:, :].rearrange("p (h n) -> p h n", h=8)
            for h in range(8):
                tr(p6v[0:96, h, :], qkf[i][:, h, :], ident[:, :], ["qkf" + sI, "ident"], ["P6"])
            for h in range(8):
                tr(p7v[0:96, h, :], qkf[i][:, 8 + h, :], ident[:, :], ["qkf" + sI, "ident"], ["P7"])
            ts("dve", qhT[0:96, :, tsl], p6v[0:96, :, :], gcol[0:96, 0:1], None, ALU.mult, None, ["P6", "gcol"],
               ["qhT_%d" % (t // 4)])
            act(khT[0:96, :, tsl], p7v[0:96, :, :], AF.Identity, ["P7", "gcol"], ["khT"], scale=gcol[0:96, 1:2])

        S.noresched.add(S.seg)
        for step in range(NT + 2):
            if step < NT:
                st_E1a(step)
            if 0 <= step - 1 < NT:
                st_E2(step - 1)
            if 0 <= step - 2 < NT:
                st_L(step - 2)
            if step < NT:
                st_E1b(step)
        S.barrier()
        A.reset(B1m)
        checkpoint("B1")
        pbuf = [A("pbuf%d" % i, [128, 512], BF16, at=E1 + i * 1024) for i in range(4)]
        rcb = A("rcb", [128, 512], F32, at=E1 + 4096)
        wg = A("w_in_gla", [128, 8, 1568], BF16)
        wlr = A("wlr", [128, 8, 64], BF16)
        dma("pool", wg[:, :, :], win_d.ap()[:, 0:1568].rearrange("(c p) n -> p c n", p=128), writes=["wg"])
        memset("pool", wlr[:, :, :], 0.0, ["wlr"])
        dma("pool", wlr[:, :, 0:16], win_d.ap()[:, 1536:1552].rearrange("(c p) n -> p c n", p=128), reads=["wlr"], writes=["wlr"])
        dma("pool", wlr[:, :, 32:48], win_d.ap()[:, 1552:1568].rearrange("(c p) n -> p c n", p=128), reads=["wlr"], writes=["wlr"])
        scale = 96 ** -0.5
        it = 0
        for h in range(8):
            even = (h % 2 == 0)
            vrows = slice(0, 64) if even else slice(64, 128)
            srows = slice(64, 128) if even else slice(0, 64)
            for qg in range(4):
                qsl = slice(qg * 512, (qg + 1) * 512)
                ob = 4 + (it % 2)
                seq = []
                for kt in range(16):
                    seq.append(("s", kt))
                    if kt >= 2:
                        seq.append(("pv", kt - 2))
                seq += [("pv", 14), ("pv", 15)]
                for kind, kt in seq:
                    sb_ = kt % 3
                    pi = kt % 4
                    if kind == "s":
                        mm(P[sb_][:, :], khT[0:96, h, kt * 128:(kt + 1) * 128], qhT[0:96, h, qsl], True, True,
                           ["khT", "qhT_%d" % qg], [PN[sb_]])
                        act(pbuf[pi][:, :], P[sb_][:, :], AF.Exp, [PN[sb_]], ["pbuf%d" % pi], scale=scale)
                    else:
                        lhsT = vA[:, kt, h, :]
                        mm(P[ob][:, :], lhsT, pbuf[pi][:, :], kt == 0, kt == 15, ["vA", "pbuf%d" % pi], [PN[ob]])
                op("dve", lambda e, vrows=vrows, srows=srows, ob=ob: e.reciprocal(out=rcb[vrows, :], in_=P[ob][srows, :]),
                   reads=[PN[ob]], writes=["rcb"], n=4096)
                tt("dve", mixT[vrows, 4 + h // 2, qsl], P[ob][vrows, :], rcb[vrows, :], ALU.mult, [PN[ob], "rcb"], ["mixT_m"])
                it += 1
        S.barrier()
        A.reset(E1)

        checkpoint("C")
        A.off += 25088
        R2 = A.mark()
        qkT = A("qkT", [128, 4, S_LEN], F32)
        vtok = A("vtok", [128, NT, 512], BF16)
        sgT = A("sgT", [128, 4, S_LEN], BF16)
        lrT = A("lrT", [64, S_LEN], F32)
        lrb = A("lrb", [64, 1], F32)
        waug = A("waug", [64, 512], F32)
        masks = A("masks", [128, 256], BF16)
        gout = A("gout", [128, 1], F32)
        onesf = A("onesf", [128, 128], F32)
        R4 = A.mark()
        dma("sp", lrb[:, :], lrb_d[:, :], writes=["lrb"])
        memset("pool", waug[:, :], 0.0, ["waug"])
        dma("sp", waug[0:16, 0:256], gkf_w[:, :], reads=["waug"], writes=["waug"])
        dma("sp", waug[16:17, 0:256], gkf_b[:, :], reads=["waug"], writes=["waug"])
        dma("sp", waug[16:17, 256:512], gkb_b[:, :], reads=["waug"], writes=["waug"])
        dma("sp", waug[32:48, 256:512], gkb_w[:, :], reads=["waug"], writes=["waug"])
        dma("sp", masks[:, :], masks_d[:, :], writes=["masks"])
        dma("sp", gout[:, :], go_d[:, :], writes=["gout"])
        memset("pool", onesf[:, :], 1.0, ["onesf"])
        blk = 0
        for kind, idx in [("q", 0), ("q", 1), ("k", 0), ("k", 1), ("g", 0), ("g", 1), ("g", 2), ("g", 3), ("lr", 0)]:
            for tg in range(4):
                pbk = blk % 4
                blk += 1
                tgs = slice(tg * 512, (tg + 1) * 512)
                for kc in range(8):
                    if kind == "q":
                        lhsT = wg[:, kc, idx * 128:(idx + 1) * 128]
                    elif kind == "k":
                        lhsT = wg[:, kc, 256 + idx * 128:256 + (idx + 1) * 128]
                    elif kind == "g":
                        lhsT = wg[:, kc, 1024 + idx * 128:1024 + (idx + 1) * 128]
                    else:
                        lhsT = wlr[:, kc, :]
                    mrows = 64 if kind == "lr" else 128
                    mm(P[pbk][0:mrows, :], lhsT, hT[:, kc, tgs], kc == 0, kc == 7, ["hT_%d" % tg, "wg", "wlr"], [PN[pbk]])
                if kind == "q":
                    act(qkT[:, idx, tgs], P[pbk][:, :], AF.Copy, [PN[pbk]], ["qT"], scale=0.125)
                elif kind == "k":
                    cp("dve", qkT[:, 2 + idx, tgs], P[pbk][:, :], [PN[pbk]], ["kT"])
                elif kind == "g":
                    act(sgT[:, idx, tgs], P[pbk][:, :], AF.Silu, [PN[pbk]], ["sgT"])
                else:
                    act(lrT[:, tgs], P[pbk][0:64, :], AF.Identity, [PN[pbk], "lrb"], ["lrT"], bias=lrb[:, :])
        for t in range(NT):
            pbk = 4 + t % 2
            tsl = slice(t * 128, (t + 1) * 128)
            for kc in range(8):
                mm(P[pbk][:, :], hT[:, kc, tsl], wg[:, kc, 512:1024], kc == 0, kc == 7, ["hT_%d" % (t // 4), "wg"], [PN[pbk]])
            cp("dve" if t % 2 else "act", vtok[:, t, :], P[pbk][:, :], [PN[pbk]], ["vtok"])
        S.barrier()

        checkpoint("B2")
        HTB = L0
        QW = 512
        GL = [A("gtG%d" % i, [128, QW], F32, at=HTB + i * 2048) for i in range(2)]
        FL = [A("gtF%d" % i, [128, QW], F32, at=HTB + 4096 + i * 2048) for i in range(2)]
        DtL = [(A("gtD%d" % i, [128, QW], F32, at=HTB + 8192 + i * 2048), "Dt%d" % i) for i in range(2)]
        EbL = [(A("gtE%d" % i, [128, QW], F32, at=HTB + 12288 + i * 2048), "Eb%d" % i) for i in range(2)]
        prod = {}
        names = [(d_, hp, k_) for d_ in (0, 1) for hp in (0, 1) for k_ in ("qr", "kr", "qb")]
        slots = [HTB + 16384 + i * 4096 for i in range(4)] + [E1 + 16384 + i * 4096 for i in range(2)]
        for i, nm in enumerate(names):
            if i < 6:
                prod[nm] = A("pr", [128, S_LEN], BF16, at=slots[i])
            else:
                prod[nm] = A("pr", [128, S_LEN], BF16)
        kdTL = [A("kdT%d" % i, [128, QW], BF16) for i in range(2)]
        dec = A("dec", [128, 4, 32], F32)
        smask = A("smask", [128, QW], F32)
        EsL = [A("gtS%d" % i, [128, QW], F32) for i in range(2)]
        kd = A("kd", [128, NT, 512], BF16, at=E1)
        memset("pool", smask[:, :], 1.0, ["smask"])
        memset("pool", smask[:, :].rearrange("p (c j) -> p c j", j=64)[:, :, 0:1], 0.0, ["smask"])
        cnt = {"d": 0, "e": 0}

        def nextD():
            cnt["d"] += 1
            return DtL[cnt["d"] % 2]

        def nextE():
            cnt["e"] += 1
            return EbL[cnt["e"] % 2]

        def exp_prod(src, sname, scl, dst, base, bname, dname="prod"):
            E_, en = nextE()
            act(E_[:, :], src, AF.Exp, [sname], [en], scale=scl)
            tt("pool", dst, base, E_[:, :], ALU.mult, [bname, en], [dname])
        NCQ = QW // 64
        itd = 0
        for d_ in (0, 1):
            for hp in (0, 1):
                dh = d_ * 2 + hp
                qT = qkT[:, hp, :]
                kT = qkT[:, 2 + hp, :]
                for qd in range(S_LEN // QW):
                    ip = itd % 2
                    itd += 1
                    G, Fc, Es, kdT = GL[ip], FL[ip], EsL[ip], kdTL[ip]
                    gn, fn, esn, kn = "G%d" % ip, "Fc%d" % ip, "Es%d" % ip, "kdT%d" % ip
                    hs = slice(qd * QW, (qd + 1) * QW)
                    pbk = ip
                    mm(P[pbk][:, :], waug[0:64, dh * 128:(dh + 1) * 128], lrT[0:64, hs], True, True,
                       ["waug", "lrT"], [PN[pbk]])
                    act(Es[:, :], P[pbk][:, :], AF.Exp, [PN[pbk]], [esn], scale=-1.0)
                    act(G[:, :], Es[:, :], AF.Ln, [esn], [gn], bias=1.0)
                    op("dve", lambda e, Fc=Fc, G=G: e.tensor_tensor_scan(out=Fc[:, :], data0=smask[:, :], data1=G[:, :],
                                                                         initial=0.0, op0=ALU.mult, op1=ALU.add),
                       reads=["smask", gn], writes=[fn], n=2 * QW)
                    Fv = Fc[:, :].rearrange("p (c j) -> p c j", j=64)
                    T63 = bcast_ap(Fc[:, 63:64], [[64, NCQ], [0, 64]])
                    act(dec[:, dh, qd * NCQ:(qd + 1) * NCQ], Fv[:, :, 63], AF.Exp, [fn], ["dec"], scale=-1.0 / 16)
                    if d_ == 0:
                        ref = bcast_ap(Fc[:, 31:32], [[64, NCQ], [0, 64]])
                        D_, dn = nextD()
                        tt("dve", D_[:, :].rearrange("p (c j) -> p c j", j=64), Fv, ref, ALU.subtract, [fn], [dn])
                        exp_prod(D_[:, :], dn, -1.0 / 16, prod[(0, hp, "qr")][:, hs], qT[:, hs], "qT")
                        exp_prod(D_[:, :], dn, 1.0 / 16, prod[(0, hp, "kr")][:, hs], kT[:, hs], "kT")
                        exp_prod(Fc[:, :], fn, -1.0 / 16, prod[(0, hp, "qb")][:, hs], qT[:, hs], "qT")
                        D_, dn = nextD()
                        tt("dve", D_[:, :].rearrange("p (c j) -> p c j", j=64), Fv, T63, ALU.subtract, [fn], [dn])
                        exp_prod(D_[:, :], dn, 1.0 / 16, kdT[:, :], kT[:, hs], "kT", kn)
                    else:
                        tt("dve", G[:, :], Fc[:, :], G[:, :], ALU.subtract, [fn, gn], [gn])
                        Gv = G[:, :].rearrange("p (c j) -> p c j", j=64)
                        ref = bcast_ap(G[:, 32:33], [[64, NCQ], [0, 64]])
                        D_, dn = nextD()
                        tt("dve", D_[:, :].rearrange("p (c j) -> p c j", j=64), Gv, ref, ALU.subtract, [gn], [dn])
                        exp_prod(D_[:, :], dn, 1.0 / 16, prod[(1, hp, "qr")][:, hs], qT[:, hs], "qT")
                        exp_prod(D_[:, :], dn, -1.0 / 16, prod[(1, hp, "kr")][:, hs], kT[:, hs], "kT")
                        D_, dn = nextD()
                        tt("dve", D_[:, :].rearrange("p (c j) -> p c j", j=64), Gv, T63, ALU.subtract, [gn, fn], [dn])
                        exp_prod(D_[:, :], dn, 1.0 / 16, prod[(1, hp, "qb")][:, hs], qT[:, hs], "qT")
                        exp_prod(G[:, :], gn, -1.0 / 16, kdT[:, :], kT[:, hs], "kT", kn)
                    pbk = 2 + ip
                    pv = Pb[pbk][:, 0:512].rearrange("p (t n) -> p t n", t=4)
                    for tq in range(4):
                        tr(pv[:, tq, :], kdT[:, tq * 128:(tq + 1) * 128], ident[:, :], [kn, "ident"], [PN[pbk]])
                    t0 = qd * 4
                    cp("dve", kd[:, t0:t0 + 4, dh * 128:(dh + 1) * 128], pv, [PN[pbk]], ["kd"])
        S.barrier()

        checkpoint("D1")
        qk_off = R2
        Sst = [A("Sst%d" % i, [128, 32, 128], BF16, at=qk_off + i * 8192) for i in range(4)]
        Sf = [A("Sf%d" % i, [128, 256], F32, at=HTB + i * 1024) for i in range(4)]
        for dh in range(4):
            memset("pool", Sf[dh][:, :], 0.0, ["Sf%d" % dh])
        for step in range(32):
            for dh in range(4):
                d_, hp = divmod(dh, 2)
                n = step if d_ == 0 else 31 - step
                t, c = divmod(n, 2)
                rows = slice(c * 64, (c + 1) * 64)
                cp("pool", Sst[dh][0:64, n, :], Sf[dh][0:64, 0:128], ["Sf%d" % dh], ["SstA%d" % dh])
                cp("act", Sst[dh][64:128, n, :], Sf[dh][64:128, 128:256], ["Sf%d" % dh], ["SstB%d" % dh])
                if step == 31:
                    continue
                pbk = 4 * c + dh
                mm(P[pbk][:, 0:256], kd[rows, t, dh * 128:(dh + 1) * 128], vtok[rows, t, hp * 256:(hp + 1) * 256],
                   True, True, ["kd", "vtok"], [PN[pbk]])
                stt(Sf[dh][:, :], Sf[dh][:, :], dec[:, dh, n:n + 1], P[pbk][:, 0:256], ALU.mult, ALU.add,
                    ["Sf%d" % dh, "dec", PN[pbk]], ["Sf%d" % dh])
        S.barrier()

        checkpoint("D2")
        wo = A("wo", [128, 8, D], BF16, at=E1)
        WO_OFF = E1
        dma("pool", wo[:, :, :], wout_d.ap().rearrange("(c p) n -> p c n", p=128), reads=["kd"], writes=["wo", "kd"])
        smb = [A("smb%d" % i, [128, 2, 2, 128], BF16, at=HTB + 4096 + i * 1024) for i in range(2)]
        sqoL = [A("sqo%d" % i, [128, 256], F32, at=HTB + 6144 + i * 1024) for i in range(2)]
        rsoL = [A("rso%d" % i, [128, 256], F32, at=HTB + 8192 + i * 1024) for i in range(2)]
        t1oL = [A("t1o%d" % i, [128, 256], F32, at=HTB + 10240 + i * 1024) for i in range(2)]
        for t in range(NT):
            tsl = slice(t * 128, (t + 1) * 128)
            for par in range(2):
                rows = slice(par * 64, (par + 1) * 64)
                sbk = par
                obk = 2 + par
                scv = P[sbk][:, :].rearrange("p (a b n) -> p a b n", a=2, b=2)
                for hp in range(2):
                    for d_ in range(2):
                        mm(scv[:, hp, d_, :], prod[(d_, hp, "kr")][rows, tsl], prod[(d_, hp, "qr")][rows, tsl], True, True,
                           ["prod"], [PN[sbk]])
                mk = bcast_ap(masks[:, 0:256], [[0, 2], [1, 256]])
                tt("dve", smb[par][:, :, :, :].rearrange("p a b n -> p a (b n)"),
                   P[sbk][:, :].rearrange("p (a m) -> p a m", a=2), mk, ALU.mult, [PN[sbk], "masks"], ["smb%d" % par])
                ov = P[obk][:, 0:256].rearrange("p (a n) -> p a n", a=2)
                for hp in range(2):
                    h = hp * 2 + par
                    mm(ov[:, hp, :], vtok[:, t, h * 128:(h + 1) * 128], smb[par][:, hp, 0, :], True, False,
                       ["vtok", "smb%d" % par], [PN[obk]])
                    mm(ov[:, hp, :], vtok[:, t, h * 128:(h + 1) * 128], smb[par][:, hp, 1, :], False, False,
                       ["vtok", "smb%d" % par], [PN[obk]])
                    for d_ in range(2):
                        dh = d_ * 2 + hp
                        for c in range(2):
                            n = t * 2 + c
                            csl = slice(t * 128 + c * 64, t * 128 + (c + 1) * 64)
                            last = (d_ == 1 and c == 1)
                            mm(ov[:, hp, c * 64:(c + 1) * 64], Sst[dh][rows, n, :], prod[(d_, hp, "qb")][rows, csl],
                               False, last, ["SstA%d" % dh, "SstB%d" % dh, "prod"], [PN[obk]])
                sqo, rso, t1o = sqoL[par], rsoL[par], t1oL[par]
                sP = "%d" % par
                act(sqo[:, :], P[obk][:, 0:256], AF.Square, [PN[obk]], ["sqo" + sP])
                ebk = 4 + par
                mm(P[ebk][:, 0:256], onesf[:, :], sqo[:, :], True, True, ["onesf", "sqo" + sP], [PN[ebk]])
                rsqrt_act(rso[:, :], P[ebk][:, 0:256], 128, [PN[ebk]], ["rso" + sP])
                stt(t1o[:, :], P[obk][:, 0:256], gout[:, 0:1], rso[:, :], ALU.mult, ALU.mult, [PN[obk], "gout", "rso" + sP], ["t1o" + sP])
                for hp in range(2):
                    h = hp * 2 + par
                    tt("pool", mixT[:, h, tsl], t1o[:, hp * 128:(hp + 1) * 128], sgT[:, h, tsl], ALU.mult,
                       ["t1o" + sP, "sgT"], ["mixT_g"])
        S.barrier()
        A.reset(L0)
        if debug:
            final_ops.append(dma("sp", dbg["mix"][:, :], mixT[:, :, :].rearrange("p c n -> p (c n)"), reads=["mixT_g", "mixT_m"]))

        checkpoint("D3")
        h2T = A("h2T", [128, 8, S_LEN], BF16)
        assert A.off == E1
        A.off += 16384
        X = A("X", [128, NT, D], F32)
        g2 = A("g2", [128, D], F32)
        wr = A("wr", [128, 8, 36], BF16)
        rbias = A("rbias", [128, 36], F32)
        gTf = A("gTf", [32, S_LEN], F32)
        identf = A("identf", [128, 128], F32)
        cp("dve", identf[:, :], ident[:, :], ["ident"], ["identf"])
        gwb = [A("gwb%d" % i, [128, 512], F32) for i in range(4)]
        lgA = A("lgA", [128, NT, 36], F32)
        hb = [A("hb2_%d" % i, [128, D], BF16) for i in range(2)]
        sqj = A("sqj2", [128, D], BF16)
        st1 = A("st1_2", [128, 4], F32)
        dma("sp", g2[:, :], bcast_row(g2_d, D), writes=["g2"])
        dma("pool", wr[:, :, 0:4], wrg_d.ap().rearrange("(c p) n -> p c n", p=128), writes=["wr"])
        dma("pool", wr[:, :, 4:36], wre_d.ap().rearrange("(c p) n -> p c n", p=128), writes=["wr"])
        dma("sp", rbias[:, 0:4], bcast_row(brg_d, 4), writes=["rbias"])
        dma("sp", rbias[:, 4:36], bcast_row(bre_d, 32), writes=["rbias"])
        for t in range(NT):
            tsl = slice(t * 128, (t + 1) * 128)
            dma("sp", X[:, t, :], x_d[tsl, :], writes=["X%d" % t])
            for ch in range(2):
                pbk = (t % 2) * 2 + ch
                for kc in range(8):
                    mm(P[pbk][:, :], mixT[:, kc, tsl], wo[:, kc, ch * 512:(ch + 1) * 512], kc == 0, kc == 7,
                       ["mixT_g", "mixT_m", "wo"], [PN[pbk]])
                tt("dve", X[:, t, ch * 512:(ch + 1) * 512], X[:, t, ch * 512:(ch + 1) * 512], P[pbk][:, :], ALU.add,
                   ["X%d" % t, PN[pbk]], ["X%d" % t])
        if debug:
            for t in range(NT):
                final_ops.append(dma("sp", dbg["x1"][t * 128:(t + 1) * 128, :], X[:, t, :], reads=["X%d" % t]))
        checkpoint("E")
        for t in range(NT):
            tsl = slice(t * 128, (t + 1) * 128)
            norm_to_T(lambda t: X[:, t, :], ["X%d" % t], g2, "g2", h2T, "h2T", "F", t, 4 + t % 2)
            rbk = 6 + t % 2
            for kc in range(8):
                mm(P[rbk][:, 0:36], h2T[:, kc, tsl], wr[:, kc, :], kc == 0, kc == 7, ["h2T_%d" % (t // 4), "wr"], [PN[rbk]])
            tt("dve", lgA[:, t, :], P[rbk][:, 0:36], rbias[:, :], ALU.add, [PN[rbk], "rbias"], ["lgA"])

        def bl(ap2, k):
            return bcast_ap(ap2, [list(ap2.ap[1]), [0, k]])

        def red(out, in_, o, reads, writes):
            return op("dve", lambda e: e.tensor_reduce(out=out, in_=in_, axis=AX.X, op=o), reads=reads, writes=writes,
                      n=fsz(in_))

        r16 = lambda nm: A(nm, [128, NT], F32)
        r4 = lambda nm: A(nm, [128, NT, 4], F32)
        r8 = lambda nm: A(nm, [128, NT, 8], F32)
        mg, s4, ptop, m1, m2, dm, e2, den, w1, w2 = [r16("r16_%d" % i) for i in range(10)]
        d4, e4, oh, ohp = [r4("r4_%d" % i) for i in range(4)]
        ls, tmp8, eq1, ls2, eq2, wg8 = [r8("r8_%d" % i) for i in range(6)]
        gate = A("gate", [128, NT, 32], F32)
        red(mg[:, :], lgA[:, :, 0:4], ALU.max, ["lgA"], ["mg"])
        tt("dve", d4[:, :, :], lgA[:, :, 0:4], bl(mg[:, :], 4), ALU.subtract, ["lgA", "mg"], ["d4"])
        act(e4[:, :, :], d4[:, :, :], AF.Exp, ["d4"], ["e4"])
        red(s4[:, :], e4[:, :, :], ALU.add, ["e4"], ["s4"])
        op("dve", lambda e: e.reciprocal(out=ptop[:, :], in_=s4[:, :]), reads=["s4"], writes=["ptop"], n=128)
        ts("dve", oh[:, :, :], d4[:, :, :], 0.0, None, ALU.is_equal, None, ["d4"], ["oh"])
        tt("dve", ohp[:, :, :], oh[:, :, :], bl(ptop[:, :], 4), ALU.mult, ["oh", "ptop"], ["ohp"])
        tt("dve", ls[:, :, :], lgA[:, :, 4:12], bl(oh[:, :, 0], 8), ALU.mult, ["lgA", "oh"], ["ls"])
        for g_ in range(1, 4):
            tt("dve", tmp8[:, :, :], lgA[:, :, 4 + 8 * g_:12 + 8 * g_], bl(oh[:, :, g_], 8), ALU.mult, ["lgA", "oh"], ["tmp8"])
            tt("dve", ls[:, :, :], ls[:, :, :], tmp8[:, :, :], ALU.add, ["ls", "tmp8"], ["ls"])
        red(m1[:, :], ls[:, :, :], ALU.max, ["ls"], ["m1"])
        tt("dve", eq1[:, :, :], ls[:, :, :], bl(m1[:, :], 8), ALU.is_equal, ["ls", "m1"], ["eq1"])
        stt(ls2[:, :, :], eq1[:, :, :], -1e30, ls[:, :, :], ALU.mult, ALU.add, ["eq1", "ls"], ["ls2"])
        red(m2[:, :], ls2[:, :, :], ALU.max, ["ls2"], ["m2"])
        tt("dve", eq2[:, :, :], ls2[:, :, :], bl(m2[:, :], 8), ALU.is_equal, ["ls2", "m2"], ["eq2"])
        tt("dve", dm[:, :], m2[:, :], m1[:, :], ALU.subtract, ["m1", "m2"], ["dm"])
        act(e2[:, :], dm[:, :], AF.Exp, ["dm"], ["e2"])
        ts("dve", den[:, :], e2[:, :], 1.0, None, ALU.add, None, ["e2"], ["den"])
        op("dve", lambda e: e.reciprocal(out=w1[:, :], in_=den[:, :]), reads=["den"], writes=["w1"], n=128)
        tt("dve", w2[:, :], e2[:, :], w1[:, :], ALU.mult, ["e2", "w1"], ["w2"])
        tt("dve", wg8[:, :, :], eq1[:, :, :], bl(w1[:, :], 8), ALU.mult, ["eq1", "w1"], ["wg8"])
        tt("dve", tmp8[:, :, :], eq2[:, :, :], bl(w2[:, :], 8), ALU.mult, ["eq2", "w2"], ["tmp8"])
        tt("dve", wg8[:, :, :], wg8[:, :, :], tmp8[:, :, :], ALU.add, ["wg8", "tmp8"], ["wg8"])
        for g_ in range(4):
            tt("dve", gate[:, :, g_ * 8:(g_ + 1) * 8], wg8[:, :, :], bl(ohp[:, :, g_], 8), ALU.mult, ["wg8", "ohp"], ["gate"])
        if debug:
            for t in range(NT):
                final_ops.append(dma("sp", dbg["gate"][t * 128:(t + 1) * 128, :], gate[:, t, :], reads=["gate"]))
        for q4 in range(4):
            bk = 4 + q4
            for tq in range(4):
                t = q4 * 4 + tq
                tr(P[bk][0:32, tq * 128:(tq + 1) * 128], gate[:, t, :], identf[:, :], ["gate", "identf"], [PN[bk]])
            cp("act" if q4 % 2 else "dve", gTf[0:32, q4 * 512:(q4 + 1) * 512], P[bk][0:32, :], [PN[bk]], ["gTf"])
        dma("sp", gdr[:, :], gTf[0:32, :], reads=["gTf"], writes=["gdr"])
        checkpoint("F")

        EG = 2
        NEG = 32 // EG
        MX = SB_BASE + 256
        S.alias(["hid0", "hid1", "sil0", "sil1", "t1m0", "t1m1", "wdn0", "wdn1"], ["mixT_g", "mixT_m"])
        hid = [A("hid%d" % b, [128, EG, 2, 512], BF16, at=MX + b * 4096) for b in range(2)]
        sil = [A("sil%d" % b, [128, 512], F32, at=MX + 8192 + b * 2048) for b in range(2)]
        t1m = [A("t1m%d" % b, [128, 512], F32, at=MX + 12288 + b * 2048) for b in range(2)]
        wdn = [[A("wd%d_%d" % (b, j), [128, 2, D], BF16, at=MX + 16384 + (b * EG + j) * 4096) for j in range(EG)] for b in range(2)]
        wgt = [None, None]
        wup = [None, None]
        wgt[0] = [A("wg0_%d" % j, [128, 8, 256], BF16) for j in range(EG)]
        wup[0] = [A("wu0_%d" % j, [128, 8, 256], BF16) for j in range(EG)]
        wgt[1] = [A("wg1_%d" % j, [128, 8, 256], BF16, at=WO_OFF + j * 4096) for j in range(EG)]
        wup[1] = [A("wu1_%d" % j, [128, 8, 256], BF16, at=WO_OFF + 8192 + j * 4096) for j in range(EG)]
        itc = 0
        gcnt = [0]
        for eg in range(NEG):
            b = eg % 2
            extra = ["wo"] if b == 1 else []
            for j in range(EG):
                e_ = eg * EG + j
                dma("pool", wgt[b][j][:, :, :], weg_d.ap()[e_].rearrange("(c p) f -> p c f", p=128), writes=["wgt%d" % b] + extra)
                dma("pool", wup[b][j][:, :, :], weu_d.ap()[e_].rearrange("(c p) f -> p c f", p=128), writes=["wup%d" % b] + extra)
                dma("pool", wdn[b][j][:, :, :], wed_d.ap()[e_].rearrange("(c p) d -> p c d", p=128), writes=["wdn%d" % b])
            for tg in range(4):
                hbi = itc % 2
                itc += 1
                tgs = slice(tg * 512, (tg + 1) * 512)
                for j in range(EG):
                    e_ = eg * EG + j
                    gbk = 6 + j
                    for fh in range(2):
                        k2 = fh
                        gb_, ub_ = 0 + k2, 2 + k2
                        for kc in range(8):
                            mm(P[gb_][:, :], wgt[b][j][:, kc, fh * 128:(fh + 1) * 128], h2T[:, kc, tgs], kc == 0, kc == 7,
                               ["wgt%d" % b, "h2T_%d" % tg], [PN[gb_]])
                        for kc in range(8):
                            mm(P[ub_][:, :], wup[b][j][:, kc, fh * 128:(fh + 1) * 128], h2T[:, kc, tgs], kc == 0, kc == 7,
                               ["wup%d" % b, "h2T_%d" % tg], [PN[ub_]])
                        if fh == 0:
                            gk = gcnt[0] % 4
                            gcnt[0] += 1
                            dma("sp", gwb[gk][:, :], bass.AP(gdr, e_ * S_LEN + tg * 512, [[0, 128], [1, 512]]),
                                reads=["gdr"], writes=["gwb%d" % gk])
                        act(sil[k2][:, :], P[gb_][:, :], AF.Silu, [PN[gb_]], ["sil%d" % k2])
                        tt("dve", t1m[k2][:, :], sil[k2][:, :], P[ub_][:, :], ALU.mult, ["sil%d" % k2, PN[ub_]], ["t1m%d" % k2])
                        tt("dve", hid[hbi][:, j, fh, :], t1m[k2][:, :], gwb[gk][:, :], ALU.mult, ["t1m%d" % k2, "gwb%d" % gk],
                           ["hid%d" % hbi])
                for tt_ in range(4):
                    t = tg * 4 + tt_
                    for ch in range(2):
                        abk = 4 + (tt_ * 2 + ch) % 2
                        n_acc = EG * 2
                        a_i = 0
                        for j in range(EG):
                            for fh in range(2):
                                mm(P[abk][:, :], hid[hbi][:, j, fh, tt_ * 128:(tt_ + 1) * 128],
                                   wdn[b][j][:, fh, ch * 512:(ch + 1) * 512], a_i == 0, a_i == n_acc - 1,
                                   ["hid%d" % hbi, "wdn%d" % b], [PN[abk]])
                                a_i += 1
                        tt("dve", X[:, t, ch * 512:(ch + 1) * 512], X[:, t, ch * 512:(ch + 1) * 512], P[abk][:, :], ALU.add,
                           ["X%d" % t, PN[abk]], ["X%d" % t])
        for t in range(NT):
            final_ops.append(dma("sp", out_d[t * 128:(t + 1) * 128, :], X[:, t, :], reads=["X%d" % t]))

        S.emit(es, final_wait_ops=final_ops)
    return nc


def make_consts():
    ident = np.eye(128, dtype=np.float32).astype(ml_dtypes.bfloat16)
    j = np.arange(128)[:, None]
    i = np.arange(128)[None, :]
    same = (j // 64) == (i // 64)
    mf = (same & (j <= i)).astype(np.float32)
    mb = (same & (j > i)).astype(np.float32)
    masks = np.concatenate([mf, mb], axis=1).astype(ml_dtypes.bfloat16)
    invf64 = 10000.0 ** (-np.arange(0, 32, 2, dtype=np.float64) / 32)
    invf_hi = invf64.astype(np.float32)
    invf_lo = (invf64 - invf_hi.astype(np.float64)).astype(np.float32)
    invf = np.broadcast_to(np.concatenate([invf_hi, invf_lo])[None, :], (128, 32)).copy()
    sel = np.zeros((32, 32, 128), np.float32)
    for e in range(32):
        sel[e, e, :] = 1.0
    sel = sel.reshape(32, 32 * 128).astype(ml_dtypes.bfloat16)
    lrb = np.zeros((64, 1), np.float32)
    lrb[16, 0] = 1.0
    return {"c_ident": ident, "c_masks": masks, "c_invf": invf, "c_sel": sel, "c_lrbias": lrb}


_NC_CACHE = {}


def make_in_maps(inputs, n_cores=8):
    c = make_consts()
    f = lambda k: np.ascontiguousarray(np.asarray(inputs[k], dtype=np.float32)[0])
    shared = {
        "norm1_gain": f("norm1_gain").reshape(1, D),
        "w_in": f("w_in"),
        "gla_gk_fwd_w": f("gla_gk_fwd_w"), "gla_gk_fwd_b": f("gla_gk_fwd_b").reshape(1, 256),
        "gla_gk_bwd_w": f("gla_gk_bwd_w"), "gla_gk_bwd_b": f("gla_gk_bwd_b").reshape(1, 256),
        "gla_out_gain": f("gla_out_gain").reshape(128, 1),
        "mla_q_gain": f("mla_q_gain").reshape(1, 256), "mla_w_qb": f("mla_w_qb"),
        "mla_kv_gain": f("mla_kv_gain").reshape(1, 128), "mla_w_kvb": f("mla_w_kvb"),
        "q_norm_gain": f("q_norm_gain").reshape(1, 96), "k_norm_gain": f("k_norm_gain").reshape(1, 96),
        "w_out": f("w_out"), "norm2_gain": f("norm2_gain").reshape(1, D),
        "w_router_group": f("w_router_group"), "b_router_group": f("b_router_group").reshape(1, 4),
        "w_router_expert": f("w_router_expert"), "b_router_expert": f("b_router_expert").reshape(1, 32),
        "w_expert_gate": f("w_expert_gate").reshape(32, D, 256),
        "w_expert_up": f("w_expert_up").reshape(32, D, 256),
        "w_expert_down": f("w_expert_down").reshape(32, 256, D),
    }
    shared.update(c)
    x = np.asarray(inputs["x"], dtype=np.float32)
    pos = np.asarray(inputs["positions"]).astype(np.int32)
    maps = []
    for b in range(n_cores):
        m = dict(shared)
        m["x"] = np.ascontiguousarray(x[b])
        m["pos"] = np.ascontiguousarray(pos[b].reshape(NT, 128).T)
        maps.append(m)
    return maps


def kernel(**inputs):
    if "nc" not in _NC_CACHE:
        _NC_CACHE["nc"] = build()
    nc = _NC_CACHE["nc"]
    maps = make_in_maps(inputs, 8)
    res = run_bass_kernel_spmd(nc, maps, core_ids=list(range(8)))
    out = np.stack([np.asarray(r["out"], dtype=np.float32) for r in res.results], axis=0)
    return out
```

```python
import contextlib
import math
import numpy as np
import ml_dtypes
import concourse.bass as bass
import concourse.mybir as mybir
from concourse.bass_utils import run_bass_kernel_spmd

F32 = mybir.dt.float32
BF16 = mybir.dt.bfloat16
I32 = mybir.dt.int32
ALU = mybir.AluOpType
AF = mybir.ActivationFunctionType
AX = mybir.AxisListType

S_LEN = 2048
D = 1024
NT = 16
EPS = 1e-6
PI = math.pi


class T:
    __slots__ = ("name", "w", "r")

    def __init__(self, name):
        self.name = name
        self.w = None
        self.r = []


class Op:
    __slots__ = ("eng", "fn", "deps", "signal", "sig", "dma", "dsem", "dval", "alld", "n", "seg", "idx", "nbytes", "tag")


class Sched:
    ENGS = ("pe", "act", "dve", "pool", "sp")

    def __init__(self, nc, n_dma_sems=12):
        self.nc = nc
        self.ops = {e: [] for e in self.ENGS}
        self.n_dma_sems = n_dma_sems
        self.dma_count = {e: 0 for e in self.ENGS}
        self.tiles = {}
        self.pending = {e: [] for e in self.ENGS}
        self.dma_since_barrier = []
        self.stopped = False
        self.seg = 0
        self.nops = 0
        self.noresched = set()

    def t(self, name):
        if name not in self.tiles:
            self.tiles[name] = T(name)
        return self.tiles[name]

    def _tl(self, lst):
        out = []
        for x in lst:
            if isinstance(x, str):
                out.append(self.t(x))
            elif isinstance(x, (list, tuple)):
                out.extend(self._tl(x))
            elif x is not None:
                out.append(x)
        return out

    def alias(self, new_names, old_names):
        if self.stopped:
            return
        for nn in new_names:
            tn = self.t(nn)
            for on in old_names:
                to = self.t(on)
                if to.w is not None:
                    tn.r.append(to.w)
                tn.r.extend(to.r)

    def barrier(self):
        if self.stopped:
            return
        lasts = []
        for e in self.ENGS:
            for o in reversed(self.ops[e]):
                if not o.dma:
                    lasts.append(o)
                    break
        lasts.extend(self.dma_since_barrier)
        self.dma_since_barrier = []
        for e in self.ENGS:
            self.pending[e] = list(lasts)
        self.seg += 1

    def op(self, eng, fn, reads=(), writes=(), dma=False, n=64, nbytes=0):
        if self.stopped:
            return None
        o = Op()
        o.n = n
        o.nbytes = nbytes
        o.seg = self.seg
        o.idx = self.nops
        self.nops += 1
        o.eng = eng
        o.fn = fn
        o.dma = dma
        o.signal = False
        o.sig = 0
        deps = {}
        reads = self._tl(reads)
        writes = self._tl(writes)
        for t in reads:
            if t.w is not None:
                deps[id(t.w)] = (t.w, "raw")
            if t.name[0] == "P" and t.name[1:].isdigit():
                for r in t.r:
                    if id(r) not in deps and r.eng != eng:
                        deps[id(r)] = (r, "war")
        for t in writes:
            if t.w is not None and id(t.w) not in deps:
                deps[id(t.w)] = (t.w, "waw")
            for r in t.r:
                if id(r) not in deps:
                    deps[id(r)] = (r, "war")
        if self.pending[eng]:
            for p in self.pending[eng]:
                deps[id(p)] = (p, "raw")
            self.pending[eng] = []
        o.alld = [p for p, _k in deps.values()]
        o.tag = ("R:" + ",".join(t.name for t in reads) + " W:" + ",".join(t.name for t in writes))
        dl = []
        for p, kind in deps.values():
            if p.eng == eng and not p.dma:
                if eng == "pe":
                    continue
                if kind != "raw" and not dma and not STRICT_SAME_ENGINE:
                    continue
            dl.append(p)
        o.deps = dl
        for p in dl:
            p.signal = True
        for t in reads:
            if not dma:
                for r in t.r:
                    if not r.dma and r.eng == eng:
                        o.alld.append(r)
                t.r = [r for r in t.r if r.dma or r.eng != eng]
            t.r.append(o)
        for t in writes:
            t.w = o
            t.r = []
        if dma:
            self.dma_count[eng] += 1
            self.dma_since_barrier.append(o)
        self.ops[eng].append(o)
        return o

    @staticmethod
    def _dur(o):
        n = o.n
        if o.dma:
            return 60.0 if o.eng == "sp" else 900.0
        if o.eng == "pe":
            return 30.0 + max(n, 64) / 2.0
        if o.eng == "act":
            return 220.0 + n / 1.4
        if o.eng == "dve":
            return 120.0 + n * 1.3
        return 550.0 + n * 0.75

    def reschedule(self):
        allops = []
        for e in self.ENGS:
            allops.extend(self.ops[e])
        allops.sort(key=lambda o: o.idx)
        import heapq
        new = {e: [] for e in self.ENGS}
        segs = {}
        for o in allops:
            segs.setdefault(o.seg, []).append(o)
        LAT = 600.0
        for sg in sorted(segs):
            ops = segs[sg]
            if sg in self.noresched:
                for o in ops:
                    new[o.eng].append(o)
                continue
            inseg = set(id(o) for o in ops)
            done = {}
            users = {}
            indeg = {}
            first = {}
            for o in ops:
                if o.eng not in first:
                    first[o.eng] = o
                elif first[o.eng] not in o.alld:
                    o.alld.append(first[o.eng])
            for o in ops:
                k = 0
                for p in o.alld:
                    if id(p) in inseg:
                        k += 1
                        users.setdefault(id(p), []).append(o)
                indeg[id(o)] = k
            ready = {e: [] for e in self.ENGS}
            efree = {e: 0.0 for e in self.ENGS}
            rtime = {}
            for o in ops:
                if indeg[id(o)] == 0:
                    rtime[id(o)] = 0.0
                    ready[o.eng].append(o)
            left = len(ops)
            SLACK = 0.0
            while left:
                best = None
                for e in self.ENGS:
                    rl = ready[e]
                    if not rl:
                        continue
                    ef = efree[e]
                    oldest = None
                    fill = None
                    for o in rl:
                        st = max(rtime[id(o)], ef)
                        if oldest is None or o.idx < oldest[1].idx:
                            oldest = (st, o)
                        if fill is None or (st, o.idx) < (fill[0], fill[1].idx):
                            fill = (st, o)
                    pick = oldest if oldest[0] <= fill[0] + SLACK else fill
                    if best is None or (pick[0], pick[1].idx) < (best[0], best[1].idx):
                        best = pick
                st, o = best
                e = o.eng
                ready[e].remove(o)
                d = self._dur(o)
                efree[e] = st + d
                fin = st + d
                if o.dma:
                    fin = st + 2000.0 + o.nbytes / 150.0
                done[id(o)] = fin
                new[e].append(o)
                left -= 1
                for u in users.get(id(o), ()):
                    indeg[id(u)] -= 1
                    lat = 0.0 if (u.eng == o.eng and not o.dma) else LAT
                    rtime[id(u)] = max(rtime.get(id(u), 0.0), fin + lat)
                    if indeg[id(u)] == 0:
                        ready[u.eng].append(u)
        self.ops = new

    def emit(self, es, final_wait_ops=()):
        nc = self.nc
        if RESCHEDULE:
            self.reschedule()
        sems = {e: es.enter_context(nc.semaphore("s_" + e)) for e in self.ENGS}
        dsems = {e: [es.enter_context(nc.semaphore("d_%s_%d" % (e, i)))
                     for i in range(self.n_dma_sems)]
                 for e in self.ENGS if self.dma_count[e] > 0}
        for e in self.ENGS:
            c = 0
            i = 0
            for o in self.ops[e]:
                if o.dma:
                    o.dsem = i % self.n_dma_sems
                    o.dval = 16 * (i // self.n_dma_sems + 1)
                    i += 1
                elif o.signal:
                    c += 1
                    o.sig = c
        block = es.enter_context(nc.Block())
        eng_obj = {"pe": block.tensor, "act": block.scalar, "dve": block.vector,
                   "pool": block.gpsimd, "sp": block.sync}
        for e in self.ENGS:
            ops = self.ops[e]
            if not ops:
                continue

            def body(engine, e=e, ops=ops):
                waited = {}

                def wait(sem, key, val):
                    if waited.get(key, 0) >= val:
                        return
                    waited[key] = val
                    engine.wait_ge(sem, val)

                for o in ops:
                    for p in o.deps:
                        if p.dma:
                            wait(dsems[p.eng][p.dsem], ("d", p.eng, p.dsem), p.dval)
                        else:
                            wait(sems[p.eng], ("c", p.eng), p.sig)
                    if o.dma and o.dval > 16:
                        wait(dsems[e][o.dsem], ("d", e, o.dsem), o.dval - 16)
                    ins = o.fn(engine)
                    if o.dma:
                        ins.then_inc(dsems[e][o.dsem], 16)
                    elif o.signal:
                        ins.then_inc(sems[e], 1)
                if e == "sp":
                    for o in final_wait_ops:
                        if o is None:
                            continue
                        wait(dsems[o.eng][o.dsem], ("d", o.eng, o.dsem), o.dval)

            eng_obj[e](body)


RESCHEDULE = True
STRICT_SAME_ENGINE = True
SB_BASE = 16640
SB_END = 229376


class Alloc:
    def __init__(self, nc):
        self.nc = nc
        self.off = SB_BASE
        self.n = 0

    def mark(self):
        return self.off

    def reset(self, m):
        self.off = m

    def __call__(self, name, shape, dt, at=None):
        esz = 2 if dt == BF16 else 4
        nb = int(np.prod(shape[1:])) * esz
        nb = (nb + 63) // 64 * 64
        self.n += 1
        if at is None:
            at = self.off
            self.off += nb
        assert at + nb <= SB_END, ("SBUF overflow", name, at, nb)
        return self.nc.alloc_sbuf_tensor_at("%s_%d" % (name, self.n), list(shape), dt, offset=at)


def bcast_ap(ap, pattern):
    return bass.AP(ap.tensor, ap.offset, [list(ap.ap[0])] + [list(p) for p in pattern])


def build(debug=False, stop_after=None):
    nc = bass.Bass("TRN2", target_bir_lowering=False)
    dr = lambda n, s, dt=F32: nc.dram_tensor(n, list(s), dt, kind="ExternalInput")
    x_d = dr("x", [S_LEN, D])
    pos_d = dr("pos", [128, NT], I32)
    g1_d = dr("norm1_gain", [1, D])
    win_d = dr("w_in", [D, 1984])
    gkf_w = dr("gla_gk_fwd_w", [16, 256])
    gkf_b = dr("gla_gk_fwd_b", [1, 256])
    gkb_w = dr("gla_gk_bwd_w", [16, 256])
    gkb_b = dr("gla_gk_bwd_b", [1, 256])
    go_d = dr("gla_out_gain", [128, 1])
    gqa_d = dr("mla_q_gain", [1, 256])
    wqb_d = dr("mla_w_qb", [256, 768])
    gkva_d = dr("mla_kv_gain", [1, 128])
    wkvb_d = dr("mla_w_kvb", [128, 1024])
    gqn_d = dr("q_norm_gain", [1, 96])
    gkn_d = dr("k_norm_gain", [1, 96])
    wout_d = dr("w_out", [D, D])
    g2_d = dr("norm2_gain", [1, D])
    wrg_d = dr("w_router_group", [D, 4])
    brg_d = dr("b_router_group", [1, 4])
    wre_d = dr("w_router_expert", [D, 32])
    bre_d = dr("b_router_expert", [1, 32])
    weg_d = dr("w_expert_gate", [32, D, 256])
    weu_d = dr("w_expert_up", [32, D, 256])
    wed_d = dr("w_expert_down", [32, 256, D])
    ident_d = dr("c_ident", [128, 128], BF16)
    masks_d = dr("c_masks", [128, 256], BF16)
    invf_d = dr("c_invf", [128, 32])
    sel_d = dr("c_sel", [32, 32 * 128], BF16)
    lrb_d = dr("c_lrbias", [64, 1])
    out_d = nc.dram_tensor("out", [S_LEN, D], F32, kind="ExternalOutput")
    gdr = nc.dram_tensor("gate_scratch", [32, S_LEN], F32, kind="Internal")
    dbg = {}
    if debug:
        dbg["mix"] = nc.dram_tensor("d_mix", [128, 8 * S_LEN], BF16, kind="ExternalOutput")
        dbg["x1"] = nc.dram_tensor("d_x1", [S_LEN, D], F32, kind="ExternalOutput")
        dbg["gate"] = nc.dram_tensor("d_gate", [S_LEN, 32], F32, kind="ExternalOutput")

    if debug:
        dbg["gen"] = nc.dram_tensor("d_gen", [128, 8 * S_LEN], BF16, kind="ExternalOutput")
    S = Sched(nc)
    A = Alloc(nc)
    op = S.op
    final_ops = []

    with contextlib.ExitStack() as es:
        P = [es.enter_context(nc.psum_tensor("pb%d" % i, [128, 512], F32)) for i in range(8)]
        Pb = [p.bitcast(BF16) for p in P]
        PN = ["P%d" % i for i in range(8)]

        def fsz(ap):
            r = 1
            for d_ in list(ap.shape)[1:]:
                r *= int(d_)
            return r

        def dma(q, out, in_, reads=(), writes=(), **kw):
            return op(q, lambda e: e.dma_start(out=out, in_=in_, **kw), reads=reads, writes=writes, dma=True,
                      nbytes=fsz(out) * 4 * 128)

        def act(out, in_, func, reads, writes, **kw):
            return op("act", lambda e: e.activation(out=out, in_=in_, func=func, **kw), reads=reads, writes=writes,
                      n=fsz(out))

        def rsqrt_act(out, in_, n, reads, writes):
            act(out, in_, AF.Ln, reads, writes, scale=1.0 / n, bias=EPS)
            act(out, out, AF.Exp, writes, writes, scale=-0.5)

        def tt(eng, out, in0, in1, o, reads, writes):
            return op(eng, lambda e: e.tensor_tensor(out=out, in0=in0, in1=in1, op=o), reads=reads, writes=writes,
                      n=fsz(out))

        def ts(eng, out, in0, s1, s2, o0, o1, reads, writes):
            if o1 is None:
                return op(eng, lambda e: e.tensor_scalar(out=out, in0=in0, scalar1=s1, scalar2=None, op0=o0),
                          reads=reads, writes=writes, n=fsz(out))
            return op(eng, lambda e: e.tensor_scalar(out=out, in0=in0, scalar1=s1, scalar2=s2, op0=o0, op1=o1),
                      reads=reads, writes=writes, n=fsz(out))

        def stt(out, in0, sc, in1, o0, o1, reads, writes):
            return op("dve", lambda e: e.scalar_tensor_tensor(out=out, in0=in0, scalar=sc, in1=in1, op0=o0, op1=o1),
                      reads=reads, writes=writes, n=fsz(out))

        def mm(out, lhsT, rhs, start, stop, reads, writes):
            return op("pe", lambda e: e.matmul(out, lhsT=lhsT, rhs=rhs, start=start, stop=stop),
                      reads=reads, writes=writes, n=fsz(rhs) * (4 if rhs.dtype == F32 else 1))

        def tr(out, in_, ident, reads, writes):
            return op("pe", lambda e: e.transpose(out=out, in_=in_, identity=ident), reads=reads, writes=writes, n=128)

        def cp(eng, out, in_, reads, writes):
            if eng == "act":
                return act(out, in_, AF.Copy, reads, writes)
            return op(eng, lambda e: e.tensor_copy(out=out, in_=in_), reads=reads, writes=writes, n=fsz(out))

        def memset(eng, ap, val, writes):
            return op(eng, lambda e: e.memset(ap, val), writes=writes, n=fsz(ap))

        def bcast_row(dram, n):
            return bass.AP(dram, 0, [[0, 128], [1, n]])

        ident = A("ident", [128, 128], BF16)
        mixT = A("mixT", [128, 8, S_LEN], BF16)
        dma("sp", ident[:, :], ident_d[:, :], writes=["ident"])
        L0 = A.mark()

        hT = A("hT", [128, 8, S_LEN], BF16)
        E1 = A.mark()
        def checkpoint(name, dump=None, reads=()):
            if stop_after == name:
                if dump is not None and debug:
                    S.barrier()
                    final_ops.append(dma("sp", dbg["gen"][:, :], dump, reads=list(reads)))
                S.stopped = True

        wm = A("w_in_mla", [128, 8, 416], BF16)
        wqb = A("wqb", [128, 2, 768], BF16)
        wkvb = A("wkvb", [128, 1024], BF16)
        cs = A("cs", [128, NT, 64], F32)
        qhT = A("qhT", [128, 8, S_LEN], BF16)
        khT = A("khT", [128, 8, S_LEN], BF16)
        vA = A("vA", [128, NT, 8, 128], BF16)
        gqa = A("gqa", [128, 384], F32)
        gqk = A("gqkr", [128, 16, 32], F32)
        gcol = A("gcol", [128, 2], F32)
        B1m = A.mark()
        dma("pool", wm[:, :, :], win_d.ap()[:, 1568:1984].rearrange("(c p) n -> p c n", p=128), writes=["wm"])
        dma("pool", wqb[:, :, :], wqb_d.ap().rearrange("(c p) n -> p c n", p=128), writes=["wqb"])
        dma("pool", wkvb[:, :], wkvb_d[:, :], writes=["wkvb"])
        dma("sp", gqa[:, 0:256], bcast_row(gqa_d, 256), writes=["gqa"])
        dma("sp", gqa[:, 256:384], bcast_row(gkva_d, 128), writes=["gqa"])
        dma("sp", gqk[:, 0:8, :], bass.AP(gqn_d, 64, [[0, 128], [0, 8], [1, 32]]), writes=["gqk"])
        dma("sp", gqk[:, 8:16, :], bass.AP(gkn_d, 64, [[0, 128], [0, 8], [1, 32]]), writes=["gqk"])
        memset("pool", gcol[:, :], 1.0, ["gcol"])
        dma("sp", gcol[0:64, 0:1], bass.AP(gqn_d, 0, [[1, 64], [1, 1]]), reads=["gcol"], writes=["gcol"])
        dma("sp", gcol[0:64, 1:2], bass.AP(gkn_d, 0, [[1, 64], [1, 1]]), reads=["gcol"], writes=["gcol"])
        posi = A("posi", [128, NT], I32)
        posf = A("posf", [128, NT], F32)
        invf = A("invf", [128, 32], F32)
        ang = A("ang", [128, NT, 16], F32)
        kk = A("kk", [128, NT, 16], F32)
        ki = A("ki", [128, NT, 16], I32)
        rr = A("rr", [128, NT, 16], F32)
        yy = A("yy", [128, NT, 16], F32)
        m_ = A("m_", [128, NT, 16], F32)
        dma("sp", posi[:, :], pos_d[:, :], writes=["posi"])
        dma("sp", invf[:, :], invf_d[:, :], writes=["invf"])
        cp("dve", posf[:, :], posi[:, :], ["posi"], ["posf"])
        for t in range(NT):
            ts("dve", ang[:, t, :], invf[:, 0:16], posf[:, t:t + 1], None, ALU.mult, None, ["invf", "posf"], ["ang"])
            stt(ang[:, t, :], invf[:, 16:32], posf[:, t:t + 1], ang[:, t, :], ALU.mult, ALU.add, ["invf", "posf", "ang"], ["ang"])
        ts("dve", kk[:, :, :], ang[:, :, :], 1.0 / (2 * PI), None, ALU.mult, None, ["ang"], ["kk"])
        cp("dve", ki[:, :, :], kk[:, :, :], ["kk"], ["ki"])
        cp("dve", kk[:, :, :], ki[:, :, :], ["ki"], ["kk"])
        C1 = 6.28125
        C2 = 2 * PI - C1
        stt(rr[:, :, :], kk[:, :, :], -C1, ang[:, :, :], ALU.mult, ALU.add, ["kk", "ang"], ["rr"])
        stt(rr[:, :, :], kk[:, :, :], -C2, rr[:, :, :], ALU.mult, ALU.add, ["kk", "rr"], ["rr"])
        for which, shift in ((1, 0.0), (0, PI / 2)):
            ts("dve", yy[:, :, :], rr[:, :, :], shift, None, ALU.add, None, ["rr"], ["yy"])
            ts("dve", m_[:, :, :], yy[:, :, :], PI, None, ALU.is_gt, None, ["yy"], ["m_"])
            stt(yy[:, :, :], m_[:, :, :], -2 * PI, yy[:, :, :], ALU.mult, ALU.add, ["m_", "yy"], ["yy"])
            ts("dve", m_[:, :, :], yy[:, :, :], -PI, None, ALU.is_lt, None, ["yy"], ["m_"])
            stt(yy[:, :, :], m_[:, :, :], 2 * PI, yy[:, :, :], ALU.mult, ALU.add, ["m_", "yy"], ["yy"])
            ts("dve", yy[:, :, :], yy[:, :, :], PI, -PI, ALU.min, ALU.max, ["yy"], ["yy"])
            if which == 0:
                act(cs[:, :, 0:16], yy[:, :, :], AF.Sin, ["yy"], ["cs"])
                act(cs[:, :, 16:32], yy[:, :, :], AF.Sin, ["yy"], ["cs"])
            else:
                act(cs[:, :, 48:64], yy[:, :, :], AF.Sin, ["yy"], ["cs"])
                act(cs[:, :, 32:48], cs[:, :, 48:64], AF.Copy, ["cs"], ["cs"], scale=-1.0)
        memset("pool", vA[:, :, :, :], 1.0, ["vA"])
        g1 = A("g1", [128, D], F32)
        xt = [A("xt%d" % i, [128, D], F32) for i in range(2)]
        hb = [A("hb%d" % i, [128, D], BF16) for i in range(2)]
        sqj = A("sqj", [128, D], F32)
        st1 = A("st1", [128, 4], F32)
        dma("sp", g1[:, :], bcast_row(g1_d, D), writes=["g1"])

        def norm_to_T(src_ap_fn, src_tiles, gain, gname, dstT, dname, pfx, t, pbank):
            i = t % 2
            ssq = st1[:, 0:1]
            rs = st1[:, 1:2]
            act(sqj[:, :], src_ap_fn(t), AF.Square, src_tiles, [pfx + "sqj", pfx + "ssq"], accum_out=ssq)
            rsqrt_act(rs, ssq, D, [pfx + "ssq"], [pfx + "rs"])
            stt(hb[i][:, :], src_ap_fn(t), rs, gain[:, :], ALU.mult, ALU.mult,
                src_tiles + [pfx + "rs", gname], [pfx + "hb%d" % i])
            pbv = Pb[pbank][:, :].rearrange("p (c n) -> p c n", c=8)
            for kc in range(8):
                tr(pbv[:, kc, :], hb[i][:, kc * 128:(kc + 1) * 128], ident[:, :],
                   [pfx + "hb%d" % i, "ident"], [PN[pbank]])
            cp("dve" if t % 2 else "act", dstT[:, :, t * 128:(t + 1) * 128], pbv, [PN[pbank]], [dname + "_%d" % (t // 4)])

        for t in range(NT):
            i = t % 2
            dma("sp", xt[i][:, :], x_d[t * 128:(t + 1) * 128, :], writes=["xt%d" % i])
            norm_to_T(lambda t, i=i: xt[i][:, :], ["xt%d" % i], g1, "g1", hT, "hT", "A", t, t % 2)
        hT_tiles = ["hT_%d" % k for k in range(4)]
        S.barrier()
        checkpoint("A")
        A.reset(B1m)

        sqjb1 = A("sqjb", [128, 416], BF16)
        sqjb = [sqjb1, sqjb1]
        stq = [A("stq%d" % i, [128, 32], F32) for i in range(2)]
        ab = [A("ab%d" % i, [128, 384], BF16) for i in range(2)]
        abT = [A("abT%d" % i, [128, 3, 128], BF16) for i in range(2)]
        kraw = [A("kraw%d" % i, [128, 8, 96], F32) for i in range(2)]
        sqn = A("sqn", [128, 16, 96], BF16)
        rg = [A("rg%d" % i, [128, 16, 32], F32) for i in range(2)]
        rg2 = A("rg2", [128, 16, 48], F32)
        rb = A("rb", [128, 16, 32], F32)
        qkf = [A("qkf%d" % i, [128, 16, 96], BF16) for i in range(2)]
        SQ2 = math.sqrt(2.0)

        def st_E1a(t):
            i = t % 2
            sI = "_%d" % i
            tsl = slice(t * 128, (t + 1) * 128)
            hTt = "hT_%d" % (t // 4)
            for kc in range(8):
                mm(P[0][:, 0:416], hT[:, kc, tsl], wm[:, kc, :], kc == 0, kc == 7, [hTt, "wm"], ["P0"])
            act(sqjb[i][:, 0:256], P[0][:, 0:256], AF.Square, ["P0"], ["stqA" + sI], accum_out=stq[i][:, 0:1])
            act(sqjb[i][:, 256:384], P[0][:, 256:384], AF.Square, ["P0"], ["stqA" + sI],
                accum_out=stq[i][:, 1:2], scale=SQ2)
            act(kraw[i][:, :, 64:96], bcast_ap(P[0][:, 384:416], [[0, 8], [1, 32]]), AF.Copy, ["P0"], ["krawR" + sI])
            rsqrt_act(stq[i][:, 2:4], stq[i][:, 0:2], 256, ["stqA" + sI], ["stqB" + sI])
            stt(ab[i][:, 0:256], P[0][:, 0:256], stq[i][:, 2:3], gqa[:, 0:256], ALU.mult, ALU.mult,
                ["P0", "stqB" + sI, "gqa"], ["ab" + sI])
            stt(ab[i][:, 256:384], P[0][:, 256:384], stq[i][:, 3:4], gqa[:, 256:384], ALU.mult, ALU.mult,
                ["P0", "stqB" + sI, "gqa"], ["ab" + sI])
        def st_E1b(t):
            i = t % 2
            sI = "_%d" % i
            tsl = slice(t * 128, (t + 1) * 128)
            hTt = "hT_%d" % (t // 4)
            p1v = Pb[1][:, 0:384].rearrange("p (c n) -> p c n", c=3)
            for c in range(3):
                tr(p1v[:, c, :], ab[i][:, c * 128:(c + 1) * 128], ident[:, :], ["ab" + sI, "ident"], ["P1"])
            cp("act", abT[i][:, :, :], p1v, ["P1"], ["abT" + sI])
        def st_E2(t):
            i = t % 2
            sI = "_%d" % i
            tsl = slice(t * 128, (t + 1) * 128)
            hTt = "hT_%d" % (t // 4)
            for nb in range(2):
                for kc in range(2):
                    mm(P[2 + nb][:, 0:384], abT[i][:, kc, :], wqb[:, kc, nb * 384:(nb + 1) * 384], kc == 0, kc == 1,
                       ["abT" + sI, "wqb"], [PN[2 + nb]])
                mm(P[4 + nb][:, :], abT[i][:, 2, :], wkvb[:, nb * 512:(nb + 1) * 512], True, True,
                   ["abT" + sI, "wkvb"], [PN[4 + nb]])
            for nb in range(2):
                srck = P[4 + nb][:, :].rearrange("p (h d) -> p h d", h=4)[:, :, 0:64]
                cp("act", kraw[i][:, nb * 4:nb * 4 + 4, 0:64], srck, [PN[4 + nb]], ["krawN" + sI])
                srcv = P[4 + nb][:, :].rearrange("p (a b d) -> p a b d", a=2, b=2)
                dstv = vA[:, t, nb * 4:nb * 4 + 4, :].rearrange("p (a b) d -> p a b d", b=2)
                cp("act", dstv[:, :, 0, 0:64], srcv[:, :, 0, 64:128], [PN[4 + nb]], ["vA"])
                cp("act", dstv[:, :, 1, 64:128], srcv[:, :, 1, 64:128], [PN[4 + nb]], ["vA"])
            for nb in range(2):
                act(sqn[:, nb * 4:nb * 4 + 4, :], P[2 + nb][:, 0:384].rearrange("p (h d) -> p h d", h=4), AF.Square,
                    [PN[2 + nb]], ["sqn"])
            act(sqn[:, 8:16, :], kraw[i][:, :, :], AF.Square, ["krawN" + sI, "krawR" + sI], ["sqn"])
            op("dve", lambda e, i=i: e.tensor_reduce(out=stq[i][:, 8:24], in_=sqn[:, :, :], axis=AX.X, op=ALU.add),
               reads=["sqn"], writes=["stqC" + sI])
            rsqrt_act(stq[i][:, 8:24], stq[i][:, 8:24], 96, ["stqC" + sI], ["stqC" + sI])
            for nb in range(2):
                pv = P[2 + nb][:, 0:384].rearrange("p (h d) -> p h d", h=4)
                rq = stq[i][:, 8 + nb * 4:9 + nb * 4]
                tt("dve", qkf[i][:, nb * 4:nb * 4 + 4, 0:64], pv[:, :, 0:64], bcast_ap(rq, [[1, 4], [0, 64]]), ALU.mult,
                   [PN[2 + nb], "stqC" + sI], ["qkf" + sI])
                tt("dve", rg[i][:, nb * 4:nb * 4 + 4, :], pv[:, :, 64:96], bcast_ap(rq, [[1, 4], [0, 32]]), ALU.mult,
                   [PN[2 + nb], "stqC" + sI], ["rg" + sI])
            rk = stq[i][:, 16:17]
            tt("dve", qkf[i][:, 8:16, 0:64], kraw[i][:, :, 0:64], bcast_ap(rk, [[1, 8], [0, 64]]), ALU.mult,
               ["krawN" + sI, "stqC" + sI], ["qkf" + sI])
            tt("dve", rg[i][:, 8:16, :], kraw[i][:, :, 64:96], bcast_ap(rk, [[1, 8], [0, 32]]), ALU.mult,
               ["krawR" + sI, "stqC" + sI], ["rg" + sI])
        def st_L(t):
            i = t % 2
            sI = "_%d" % i
            tsl = slice(t * 128, (t + 1) * 128)
            hTt = "hT_%d" % (t // 4)
            tt("pool", rg2[:, :, 0:32], rg[i][:, :, :], gqk[:, :, :], ALU.mult, ["rg" + sI, "gqk"], ["rg2"])
            tt("pool", rg2[:, :, 32:48], rg[i][:, :, 0:16], gqk[:, :, 0:16], ALU.mult, ["rg" + sI, "gqk"], ["rg2"])
            c1 = bcast_ap(cs[:, t, 0:32], [[0, 16], [1, 32]])
            c2 = bcast_ap(cs[:, t, 32:64], [[0, 16], [1, 32]])
            tt("pool", rg[i][:, :, :], rg2[:, :, 0:32], c1, ALU.mult, ["rg2", "cs"], ["rg" + sI])
            tt("pool", rb[:, :, :], rg2[:, :, 16:48], c2, ALU.mult, ["rg2", "cs"], ["rb"])
            tt("pool", qkf[i][:, :, 64:96], rg[i][:, :, :], rb[:, :, :], ALU.add, ["rg" + sI, "rb"], ["qkf" + sI])
            p6v = Pb[6][:, :].rearrange("p (h n) -> p h n", h=8)
            p7v = Pb[7][:, :].rearrange("p (h n) -> p h n", h=8)
            for h in range(8):
                tr(p6v[0:96, h, :], qkf[i][:, h, :], ident[:, :], ["qkf" + sI, "ident"], ["P6"])
            for h in range(8):
                tr(p7v[0:96, h, :], qkf[i][:, 8 + h, :], ident[:, :], ["qkf" + sI, "ident"], ["P7"])
            ts("dve", qhT[0:96, :, tsl], p6v[0:96, :, :], gcol[0:96, 0:1], None, ALU.mult, None, ["P6", "gcol"],
               ["qhT_%d" % (t // 4)])
            act(khT[0:96, :, tsl], p7v[0:96, :, :], AF.Identity, ["P7", "gcol"], ["khT"], scale=gcol[0:96, 1:2])

        S.noresched.add(S.seg)
        for step in range(NT + 2):
            if step < NT:
                st_E1a(step)
            if 0 <= step - 1 < NT:
                st_E2(step - 1)
            if 0 <= step - 2 < NT:
                st_L(step - 2)
            if step < NT:
                st_E1b(step)
        S.barrier()
        A.reset(B1m)
        checkpoint("B1")
        pbuf = [A("pbuf%d" % i, [128, 512], BF16, at=E1 + i * 1024) for i in range(4)]
        rcb = A("rcb", [128, 512], F32, at=E1 + 4096)
        wg = A("w_in_gla", [128, 8, 1568], BF16)
        wlr = A("wlr", [128, 8, 64], BF16)
        dma("pool", wg[:, :, :], win_d.ap()[:, 0:1568].rearrange("(c p) n -> p c n", p=128), writes=["wg"])
        memset("pool", wlr[:, :, :], 0.0, ["wlr"])
        dma("pool", wlr[:, :, 0:16], win_d.ap()[:, 1536:1552].rearrange("(c p) n -> p c n", p=128), reads=["wlr"], writes=["wlr"])
        dma("pool", wlr[:, :, 32:48], win_d.ap()[:, 1552:1568].rearrange("(c p) n -> p c n", p=128), reads=["wlr"], writes=["wlr"])
        scale = 96 ** -0.5
        it = 0
        for h in range(8):
            even = (h % 2 == 0)
            vrows = slice(0, 64) if even else slice(64, 128)
            srows = slice(64, 128) if even else slice(0, 64)
            for qg in range(4):
                qsl = slice(qg * 512, (qg + 1) * 512)
                ob = 4 + (it % 2)
                seq = []
                for kt in range(16):
                    seq.append(("s", kt))
                    if kt >= 2:
                        seq.append(("pv", kt - 2))
                seq += [("pv", 14), ("pv", 15)]
                for kind, kt in seq:
                    sb_ = kt % 3
                    pi = kt % 4
                    if kind == "s":
                        mm(P[sb_][:, :], khT[0:96, h, kt * 128:(kt + 1) * 128], qhT[0:96, h, qsl], True, True,
                           ["khT", "qhT_%d" % qg], [PN[sb_]])
                        act(pbuf[pi][:, :], P[sb_][:, :], AF.Exp, [PN[sb_]], ["pbuf%d" % pi], scale=scale)
                    else:
                        lhsT = vA[:, kt, h, :]
                        mm(P[ob][:, :], lhsT, pbuf[pi][:, :], kt == 0, kt == 15, ["vA", "pbuf%d" % pi], [PN[ob]])
                op("dve", lambda e, vrows=vrows, srows=srows, ob=ob: e.reciprocal(out=rcb[vrows, :], in_=P[ob][srows, :]),
                   reads=[PN[ob]], writes=["rcb"], n=4096)
                tt("dve", mixT[vrows, 4 + h // 2, qsl], P[ob][vrows, :], rcb[vrows, :], ALU.mult, [PN[ob], "rcb"], ["mixT_m"])
                it += 1
        S.barrier()
        A.reset(E1)

        checkpoint("C")
        A.off += 25088
        R2 = A.mark()
        qkT = A("qkT", [128, 4, S_LEN], F32)
        vtok = A("vtok", [128, NT, 512], BF16)
        sgT = A("sgT", [128, 4, S_LEN], BF16)
        lrT = A("lrT", [64, S_LEN], F32)
        lrb = A("lrb", [64, 1], F32)
        waug = A("waug", [64, 512], F32)
        masks = A("masks", [128, 256], BF16)
        gout = A("gout", [128, 1], F32)
        onesf = A("onesf", [128, 128], F32)
        R4 = A.mark()
        dma("sp", lrb[:, :], lrb_d[:, :], writes=["lrb"])
        memset("pool", waug[:, :], 0.0, ["waug"])
        dma("sp", waug[0:16, 0:256], gkf_w[:, :], reads=["waug"], writes=["waug"])
        dma("sp", waug[16:17, 0:256], gkf_b[:, :], reads=["waug"], writes=["waug"])
        dma("sp", waug[16:17, 256:512], gkb_b[:, :], reads=["waug"], writes=["waug"])
        dma("sp", waug[32:48, 256:512], gkb_w[:, :], reads=["waug"], writes=["waug"])
        dma("sp", masks[:, :], masks_d[:, :], writes=["masks"])
        dma("sp", gout[:, :], go_d[:, :], writes=["gout"])
        memset("pool", onesf[:, :], 1.0, ["onesf"])
        blk = 0
        for kind, idx in [("q", 0), ("q", 1), ("k", 0), ("k", 1), ("g", 0), ("g", 1), ("g", 2), ("g", 3), ("lr", 0)]:
            for tg in range(4):
                pbk = blk % 4
                blk += 1
                tgs = slice(tg * 512, (tg + 1) * 512)
                for kc in range(8):
                    if kind == "q":
                        lhsT = wg[:, kc, idx * 128:(idx + 1) * 128]
                    elif kind == "k":
                        lhsT = wg[:, kc, 256 + idx * 128:256 + (idx + 1) * 128]
                    elif kind == "g":
                        lhsT = wg[:, kc, 1024 + idx * 128:1024 + (idx + 1) * 128]
                    else:
                        lhsT = wlr[:, kc, :]
                    mrows = 64 if kind == "lr" else 128
                    mm(P[pbk][0:mrows, :], lhsT, hT[:, kc, tgs], kc == 0, kc == 7, ["hT_%d" % tg, "wg", "wlr"], [PN[pbk]])
                if kind == "q":
                    act(qkT[:, idx, tgs], P[pbk][:, :], AF.Copy, [PN[pbk]], ["qT"], scale=0.125)
                elif kind == "k":
                    cp("dve", qkT[:, 2 + idx, tgs], P[pbk][:, :], [PN[pbk]], ["kT"])
                elif kind == "g":
                    act(sgT[:, idx, tgs], P[pbk][:, :], AF.Silu, [PN[pbk]], ["sgT"])
                else:
                    act(lrT[:, tgs], P[pbk][0:64, :], AF.Identity, [PN[pbk], "lrb"], ["lrT"], bias=lrb[:, :])
        for t in range(NT):
            pbk = 4 + t % 2
            tsl = slice(t * 128, (t + 1) * 128)
            for kc in range(8):
                mm(P[pbk][:, :], hT[:, kc, tsl], wg[:, kc, 512:1024], kc == 0, kc == 7, ["hT_%d" % (t // 4), "wg"], [PN[pbk]])
            cp("dve" if t % 2 else "act", vtok[:, t, :], P[pbk][:, :], [PN[pbk]], ["vtok"])
        S.barrier()

        checkpoint("B2")
        HTB = L0
        QW = 512
        GL = [A("gtG%d" % i, [128, QW], F32, at=HTB + i * 2048) for i in range(2)]
        FL = [A("gtF%d" % i, [128, QW], F32, at=HTB + 4096 + i * 2048) for i in range(2)]
        DtL = [(A("gtD%d" % i, [128, QW], F32, at=HTB + 8192 + i * 2048), "Dt%d" % i) for i in range(2)]
        EbL = [(A("gtE%d" % i, [128, QW], F32, at=HTB + 12288 + i * 2048), "Eb%d" % i) for i in range(2)]
        prod = {}
        names = [(d_, hp, k_) for d_ in (0, 1) for hp in (0, 1) for k_ in ("qr", "kr", "qb")]
        slots = [HTB + 16384 + i * 4096 for i in range(4)] + [E1 + 16384 + i * 4096 for i in range(2)]
        for i, nm in enumerate(names):
            if i < 6:
                prod[nm] = A("pr", [128, S_LEN], BF16, at=slots[i])
            else:
                prod[nm] = A("pr", [128, S_LEN], BF16)
        kdTL = [A("kdT%d" % i, [128, QW], BF16) for i in range(2)]
        dec = A("dec", [128, 4, 32], F32)
        smask = A("smask", [128, QW], F32)
        EsL = [A("gtS%d" % i, [128, QW], F32) for i in range(2)]
        kd = A("kd", [128, NT, 512], BF16, at=E1)
        memset("pool", smask[:, :], 1.0, ["smask"])
        memset("pool", smask[:, :].rearrange("p (c j) -> p c j", j=64)[:, :, 0:1], 0.0, ["smask"])
        cnt = {"d": 0, "e": 0}

        def nextD():
            cnt["d"] += 1
            return DtL[cnt["d"] % 2]

        def nextE():
            cnt["e"] += 1
            return EbL[cnt["e"] % 2]

        def exp_prod(src, sname, scl, dst, base, bname, dname="prod"):
            E_, en = nextE()
            act(E_[:, :], src, AF.Exp, [sname], [en], scale=scl)
            tt("pool", dst, base, E_[:, :], ALU.mult, [bname, en], [dname])
        NCQ = QW // 64
        itd = 0
        for d_ in (0, 1):
            for hp in (0, 1):
                dh = d_ * 2 + hp
                qT = qkT[:, hp, :]
                kT = qkT[:, 2 + hp, :]
                for qd in range(S_LEN // QW):
                    ip = itd % 2
                    itd += 1
                    G, Fc, Es, kdT = GL[ip], FL[ip], EsL[ip], kdTL[ip]
                    gn, fn, esn, kn = "G%d" % ip, "Fc%d" % ip, "Es%d" % ip, "kdT%d" % ip
                    hs = slice(qd * QW, (qd + 1) * QW)
                    pbk = ip
                    mm(P[pbk][:, :], waug[0:64, dh * 128:(dh + 1) * 128], lrT[0:64, hs], True, True,
                       ["waug", "lrT"], [PN[pbk]])
                    act(Es[:, :], P[pbk][:, :], AF.Exp, [PN[pbk]], [esn], scale=-1.0)
                    act(G[:, :], Es[:, :], AF.Ln, [esn], [gn], bias=1.0)
                    op("dve", lambda e, Fc=Fc, G=G: e.tensor_tensor_scan(out=Fc[:, :], data0=smask[:, :], data1=G[:, :],
                                                                         initial=0.0, op0=ALU.mult, op1=ALU.add),
                       reads=["smask", gn], writes=[fn], n=2 * QW)
                    Fv = Fc[:, :].rearrange("p (c j) -> p c j", j=64)
                    T63 = bcast_ap(Fc[:, 63:64], [[64, NCQ], [0, 64]])
                    act(dec[:, dh, qd * NCQ:(qd + 1) * NCQ], Fv[:, :, 63], AF.Exp, [fn], ["dec"], scale=-1.0 / 16)
                    if d_ == 0:
                        ref = bcast_ap(Fc[:, 31:32], [[64, NCQ], [0, 64]])
                        D_, dn = nextD()
                        tt("dve", D_[:, :].rearrange("p (c j) -> p c j", j=64), Fv, ref, ALU.subtract, [fn], [dn])
                        exp_prod(D_[:, :], dn, -1.0 / 16, prod[(0, hp, "qr")][:, hs], qT[:, hs], "qT")
                        exp_prod(D_[:, :], dn, 1.0 / 16, prod[(0, hp, "kr")][:, hs], kT[:, hs], "kT")
                        exp_prod(Fc[:, :], fn, -1.0 / 16, prod[(0, hp, "qb")][:, hs], qT[:, hs], "qT")
                        D_, dn = nextD()
                        tt("dve", D_[:, :].rearrange("p (c j) -> p c j", j=64), Fv, T63, ALU.subtract, [fn], [dn])
                        exp_prod(D_[:, :], dn, 1.0 / 16, kdT[:, :], kT[:, hs], "kT", kn)
                    else:
                        tt("dve", G[:, :], Fc[:, :], G[:, :], ALU.subtract, [fn, gn], [gn])
                        Gv = G[:, :].rearrange("p (c j) -> p c j", j=64)
                        ref = bcast_ap(G[:, 32:33], [[64, NCQ], [0, 64]])
                        D_, dn = nextD()
                        tt("dve", D_[:, :].rearrange("p (c j) -> p c j", j=64), Gv, ref, ALU.subtract, [gn], [dn])
                        exp_prod(D_[:, :], dn, 1.0 / 16, prod[(1, hp, "qr")][:, hs], qT[:, hs], "qT")
                        exp_prod(D_[:, :], dn, -1.0 / 16, prod[(1, hp, "kr")][:, hs], kT[:, hs], "kT")
                        D_, dn = nextD()
                        tt("dve", D_[:, :].rearrange("p (c j) -> p c j", j=64), Gv, T63, ALU.subtract, [gn, fn], [dn])
                        exp_prod(D_[:, :], dn, 1.0 / 16, prod[(1, hp, "qb")][:, hs], qT[:, hs], "qT")
                        exp_prod(G[:, :], gn, -1.0 / 16, kdT[:, :], kT[:, hs], "kT", kn)
                    pbk = 2 + ip
                    pv = Pb[pbk][:, 0:512].rearrange("p (t n) -> p t n", t=4)
                    for tq in range(4):
                        tr(pv[:, tq, :], kdT[:, tq * 128:(tq + 1) * 128], ident[:, :], [kn, "ident"], [PN[pbk]])
                    t0 = qd * 4
                    cp("dve", kd[:, t0:t0 + 4, dh * 128:(dh + 1) * 128], pv, [PN[pbk]], ["kd"])
        S.barrier()

        checkpoint("D1")
        qk_off = R2
        Sst = [A("Sst%d" % i, [128, 32, 128], BF16, at=qk_off + i * 8192) for i in range(4)]
        Sf = [A("Sf%d" % i, [128, 256], F32, at=HTB + i * 1024) for i in range(4)]
        for dh in range(4):
            memset("pool", Sf[dh][:, :], 0.0, ["Sf%d" % dh])
        for step in range(32):
            for dh in range(4):
                d_, hp = divmod(dh, 2)
                n = step if d_ == 0 else 31 - step
                t, c = divmod(n, 2)
                rows = slice(c * 64, (c + 1) * 64)
                cp("pool", Sst[dh][0:64, n, :], Sf[dh][0:64, 0:128], ["Sf%d" % dh], ["SstA%d" % dh])
                cp("act", Sst[dh][64:128, n, :], Sf[dh][64:128, 128:256], ["Sf%d" % dh], ["SstB%d" % dh])
                if step == 31:
                    continue
                pbk = 4 * c + dh
                mm(P[pbk][:, 0:256], kd[rows, t, dh * 128:(dh + 1) * 128], vtok[rows, t, hp * 256:(hp + 1) * 256],
                   True, True, ["kd", "vtok"], [PN[pbk]])
                stt(Sf[dh][:, :], Sf[dh][:, :], dec[:, dh, n:n + 1], P[pbk][:, 0:256], ALU.mult, ALU.add,
                    ["Sf%d" % dh, "dec", PN[pbk]], ["Sf%d" % dh])

        checkpoint("D2")
        wo = A("wo", [128, 8, D], BF16, at=E1)
        WO_OFF = E1
        dma("pool", wo[:, :, :], wout_d.ap().rearrange("(c p) n -> p c n", p=128), reads=["kd"], writes=["wo", "kd"])
        smb = [A("smb%d" % i, [128, 2, 2, 128], BF16, at=HTB + 4096 + i * 1024) for i in range(2)]
        sqoL = [A("sqo%d" % i, [128, 256], F32, at=HTB + 6144 + i * 1024) for i in range(2)]
        rsoL = [A("rso%d" % i, [128, 256], F32, at=HTB + 8192 + i * 1024) for i in range(2)]
        t1oL = [A("t1o%d" % i, [128, 256], F32, at=HTB + 10240 + i * 1024) for i in range(2)]
        for t in range(NT):
            tsl = slice(t * 128, (t + 1) * 128)
            for par in range(2):
                rows = slice(par * 64, (par + 1) * 64)
                sbk = par
                obk = 2 + par
                scv = P[sbk][:, :].rearrange("p (a b n) -> p a b n", a=2, b=2)
                for hp in range(2):
                    for d_ in range(2):
                        mm(scv[:, hp, d_, :], prod[(d_, hp, "kr")][rows, tsl], prod[(d_, hp, "qr")][rows, tsl], True, True,
                           ["prod"], [PN[sbk]])
                mk = bcast_ap(masks[:, 0:256], [[0, 2], [1, 256]])
                tt("dve", smb[par][:, :, :, :].rearrange("p a b n -> p a (b n)"),
                   P[sbk][:, :].rearrange("p (a m) -> p a m", a=2), mk, ALU.mult, [PN[sbk], "masks"], ["smb%d" % par])
                ov = P[obk][:, 0:256].rearrange("p (a n) -> p a n", a=2)
                for hp in range(2):
                    h = hp * 2 + par
                    mm(ov[:, hp, :], vtok[:, t, h * 128:(h + 1) * 128], smb[par][:, hp, 0, :], True, False,
                       ["vtok", "smb%d" % par], [PN[obk]])
                    mm(ov[:, hp, :], vtok[:, t, h * 128:(h + 1) * 128], smb[par][:, hp, 1, :], False, False,
                       ["vtok", "smb%d" % par], [PN[obk]])
                    for d_ in range(2):
                        dh = d_ * 2 + hp
                        for c in range(2):
                            n = t * 2 + c
                            csl = slice(t * 128 + c * 64, t * 128 + (c + 1) * 64)
                            last = (d_ == 1 and c == 1)
                            mm(ov[:, hp, c * 64:(c + 1) * 64], Sst[dh][rows, n, :], prod[(d_, hp, "qb")][rows, csl],
                               False, last, ["SstA%d" % dh, "SstB%d" % dh, "prod"], [PN[obk]])
                sqo, rso, t1o = sqoL[par], rsoL[par], t1oL[par]
                sP = "%d" % par
                act(sqo[:, :], P[obk][:, 0:256], AF.Square, [PN[obk]], ["sqo" + sP])
                ebk = 4 + par
                mm(P[ebk][:, 0:256], onesf[:, :], sqo[:, :], True, True, ["onesf", "sqo" + sP], [PN[ebk]])
                rsqrt_act(rso[:, :], P[ebk][:, 0:256], 128, [PN[ebk]], ["rso" + sP])
                stt(t1o[:, :], P[obk][:, 0:256], gout[:, 0:1], rso[:, :], ALU.mult, ALU.mult, [PN[obk], "gout", "rso" + sP], ["t1o" + sP])
                for hp in range(2):
                    h = hp * 2 + par
                    tt("pool", mixT[:, h, tsl], t1o[:, hp * 128:(hp + 1) * 128], sgT[:, h, tsl], ALU.mult,
                       ["t1o" + sP, "sgT"], ["mixT_g"])
        S.barrier()
        A.reset(L0)
        if debug:
            final_ops.append(dma("sp", dbg["mix"][:, :], mixT[:, :, :].rearrange("p c n -> p (c n)"), reads=["mixT_g", "mixT_m"]))

        checkpoint("D3")
        h2T = A("h2T", [128, 8, S_LEN], BF16)
        assert A.off == E1
        A.off += 16384
        X = A("X", [128, NT, D], F32)
        g2 = A("g2", [128, D], F32)
        wr = A("wr", [128, 8, 36], BF16)
        rbias = A("rbias", [128, 36], F32)
        gTf = A("gTf", [32, S_LEN], F32)
        identf = A("identf", [128, 128], F32)
        cp("dve", identf[:, :], ident[:, :], ["ident"], ["identf"])
        gwb = [A("gwb%d" % i, [128, 512], F32) for i in range(4)]
        lgA = A("lgA", [128, NT, 36], F32)
        hb = [A("hb2_%d" % i, [128, D], BF16) for i in range(2)]
        sqj = A("sqj2", [128, D], BF16)
        st1 = A("st1_2", [128, 4], F32)
        dma("sp", g2[:, :], bcast_row(g2_d, D), writes=["g2"])
        dma("pool", wr[:, :, 0:4], wrg_d.ap().rearrange("(c p) n -> p c n", p=128), writes=["wr"])
        dma("pool", wr[:, :, 4:36], wre_d.ap().rearrange("(c p) n -> p c n", p=128), writes=["wr"])
        dma("sp", rbias[:, 0:4], bcast_row(brg_d, 4), writes=["rbias"])
        dma("sp", rbias[:, 4:36], bcast_row(bre_d, 32), writes=["rbias"])
        for t in range(NT):
            tsl = slice(t * 128, (t + 1) * 128)
            dma("sp", X[:, t, :], x_d[tsl, :], writes=["X%d" % t])
            for ch in range(2):
                pbk = (t % 2) * 2 + ch
                for kc in range(8):
                    mm(P[pbk][:, :], mixT[:, kc, tsl], wo[:, kc, ch * 512:(ch + 1) * 512], kc == 0, kc == 7,
                       ["mixT_g", "mixT_m", "wo"], [PN[pbk]])
                tt("dve", X[:, t, ch * 512:(ch + 1) * 512], X[:, t, ch * 512:(ch + 1) * 512], P[pbk][:, :], ALU.add,
                   ["X%d" % t, PN[pbk]], ["X%d" % t])
        if debug:
            for t in range(NT):
                final_ops.append(dma("sp", dbg["x1"][t * 128:(t + 1) * 128, :], X[:, t, :], reads=["X%d" % t]))
        checkpoint("E")
        for t in range(NT):
            tsl = slice(t * 128, (t + 1) * 128)
            norm_to_T(lambda t: X[:, t, :], ["X%d" % t], g2, "g2", h2T, "h2T", "F", t, 4 + t % 2)
            rbk = 6 + t % 2
            for kc in range(8):
                mm(P[rbk][:, 0:36], h2T[:, kc, tsl], wr[:, kc, :], kc == 0, kc == 7, ["h2T_%d" % (t // 4), "wr"], [PN[rbk]])
            tt("dve", lgA[:, t, :], P[rbk][:, 0:36], rbias[:, :], ALU.add, [PN[rbk], "rbias"], ["lgA"])

        def bl(ap2, k):
            return bcast_ap(ap2, [list(ap2.ap[1]), [0, k]])

        def red(out, in_, o, reads, writes):
            return op("dve", lambda e: e.tensor_reduce(out=out, in_=in_, axis=AX.X, op=o), reads=reads, writes=writes,
                      n=fsz(in_))

        r16 = lambda nm: A(nm, [128, NT], F32)
        r4 = lambda nm: A(nm, [128, NT, 4], F32)
        r8 = lambda nm: A(nm, [128, NT, 8], F32)
        mg, s4, ptop, m1, m2, dm, e2, den, w1, w2 = [r16("r16_%d" % i) for i in range(10)]
        d4, e4, oh, ohp = [r4("r4_%d" % i) for i in range(4)]
        ls, tmp8, eq1, ls2, eq2, wg8 = [r8("r8_%d" % i) for i in range(6)]
        gate = A("gate", [128, NT, 32], F32)
        red(mg[:, :], lgA[:, :, 0:4], ALU.max, ["lgA"], ["mg"])
        tt("dve", d4[:, :, :], lgA[:, :, 0:4], bl(mg[:, :], 4), ALU.subtract, ["lgA", "mg"], ["d4"])
        act(e4[:, :, :], d4[:, :, :], AF.Exp, ["d4"], ["e4"])
        red(s4[:, :], e4[:, :, :], ALU.add, ["e4"], ["s4"])
        op("dve", lambda e: e.reciprocal(out=ptop[:, :], in_=s4[:, :]), reads=["s4"], writes=["ptop"], n=128)
        ts("dve", oh[:, :, :], d4[:, :, :], 0.0, None, ALU.is_equal, None, ["d4"], ["oh"])
        tt("dve", ohp[:, :, :], oh[:, :, :], bl(ptop[:, :], 4), ALU.mult, ["oh", "ptop"], ["ohp"])
        tt("dve", ls[:, :, :], lgA[:, :, 4:12], bl(oh[:, :, 0], 8), ALU.mult, ["lgA", "oh"], ["ls"])
        for g_ in range(1, 4):
            tt("dve", tmp8[:, :, :], lgA[:, :, 4 + 8 * g_:12 + 8 * g_], bl(oh[:, :, g_], 8), ALU.mult, ["lgA", "oh"], ["tmp8"])
            tt("dve", ls[:, :, :], ls[:, :, :], tmp8[:, :, :], ALU.add, ["ls", "tmp8"], ["ls"])
        red(m1[:, :], ls[:, :, :], ALU.max, ["ls"], ["m1"])
        tt("dve", eq1[:, :, :], ls[:, :, :], bl(m1[:, :], 8), ALU.is_equal, ["ls", "m1"], ["eq1"])
        stt(ls2[:, :, :], eq1[:, :, :], -1e30, ls[:, :, :], ALU.mult, ALU.add, ["eq1", "ls"], ["ls2"])
        red(m2[:, :], ls2[:, :, :], ALU.max, ["ls2"], ["m2"])
        tt("dve", eq2[:, :, :], ls2[:, :, :], bl(m2[:, :], 8), ALU.is_equal, ["ls2", "m2"], ["eq2"])
        tt("dve", dm[:, :], m2[:, :], m1[:, :], ALU.subtract, ["m1", "m2"], ["dm"])
        act(e2[:, :], dm[:, :], AF.Exp, ["dm"], ["e2"])
        ts("dve", den[:, :], e2[:, :], 1.0, None, ALU.add, None, ["e2"], ["den"])
        op("dve", lambda e: e.reciprocal(out=w1[:, :], in_=den[:, :]), reads=["den"], writes=["w1"], n=128)
        tt("dve", w2[:, :], e2[:, :], w1[:, :], ALU.mult, ["e2", "w1"], ["w2"])
        tt("dve", wg8[:, :, :], eq1[:, :, :], bl(w1[:, :], 8), ALU.mult, ["eq1", "w1"], ["wg8"])
        tt("dve", tmp8[:, :, :], eq2[:, :, :], bl(w2[:, :], 8), ALU.mult, ["eq2", "w2"], ["tmp8"])
        tt("dve", wg8[:, :, :], wg8[:, :, :], tmp8[:, :, :], ALU.add, ["wg8", "tmp8"], ["wg8"])
        for g_ in range(4):
            tt("dve", gate[:, :, g_ * 8:(g_ + 1) * 8], wg8[:, :, :], bl(ohp[:, :, g_], 8), ALU.mult, ["wg8", "ohp"], ["gate"])
        if debug:
            for t in range(NT):
                final_ops.append(dma("sp", dbg["gate"][t * 128:(t + 1) * 128, :], gate[:, t, :], reads=["gate"]))
        for q4 in range(4):
            bk = 4 + q4
            for tq in range(4):
                t = q4 * 4 + tq
                tr(P[bk][0:32, tq * 128:(tq + 1) * 128], gate[:, t, :], identf[:, :], ["gate", "identf"], [PN[bk]])
            cp("act" if q4 % 2 else "dve", gTf[0:32, q4 * 512:(q4 + 1) * 512], P[bk][0:32, :], [PN[bk]], ["gTf"])
        dma("sp", gdr[:, :], gTf[0:32, :], reads=["gTf"], writes=["gdr"])
        checkpoint("F")

        EG = 2
        NEG = 32 // EG
        MX = SB_BASE + 256
        S.alias(["hid0", "hid1", "sil0", "sil1", "t1m0", "t1m1", "wdn0", "wdn1"], ["mixT_g", "mixT_m"])
        hid = [A("hid%d" % b, [128, EG, 2, 512], BF16, at=MX + b * 4096) for b in range(2)]
        sil = [A("sil%d" % b, [128, 512], F32, at=MX + 8192 + b * 2048) for b in range(2)]
        t1m = [A("t1m%d" % b, [128, 512], F32, at=MX + 12288 + b * 2048) for b in range(2)]
        wdn = [[A("wd%d_%d" % (b, j), [128, 2, D], BF16, at=MX + 16384 + (b * EG + j) * 4096) for j in range(EG)] for b in range(2)]
        wgt = [None, None]
        wup = [None, None]
        wgt[0] = [A("wg0_%d" % j, [128, 8, 256], BF16) for j in range(EG)]
        wup[0] = [A("wu0_%d" % j, [128, 8, 256], BF16) for j in range(EG)]
        wgt[1] = [A("wg1_%d" % j, [128, 8, 256], BF16, at=WO_OFF + j * 4096) for j in range(EG)]
        wup[1] = [A("wu1_%d" % j, [128, 8, 256], BF16, at=WO_OFF + 8192 + j * 4096) for j in range(EG)]
        itc = 0
        gcnt = [0]
        for eg in range(NEG):
            b = eg % 2
            extra = ["wo"] if b == 1 else []
            for j in range(EG):
                e_ = eg * EG + j
                dma("pool", wgt[b][j][:, :, :], weg_d.ap()[e_].rearrange("(c p) f -> p c f", p=128), writes=["wgt%d" % b] + extra)
                dma("pool", wup[b][j][:, :, :], weu_d.ap()[e_].rearrange("(c p) f -> p c f", p=128), writes=["wup%d" % b] + extra)
                dma("pool", wdn[b][j][:, :, :], wed_d.ap()[e_].rearrange("(c p) d -> p c d", p=128), writes=["wdn%d" % b])
            for tg in range(4):
                hbi = itc % 2
                itc += 1
                tgs = slice(tg * 512, (tg + 1) * 512)
                for j in range(EG):
                    e_ = eg * EG + j
                    gbk = 6 + j
                    for fh in range(2):
                        k2 = fh
                        gb_, ub_ = 0 + k2, 2 + k2
                        for kc in range(8):
                            mm(P[gb_][:, :], wgt[b][j][:, kc, fh * 128:(fh + 1) * 128], h2T[:, kc, tgs], kc == 0, kc == 7,
                               ["wgt%d" % b, "h2T_%d" % tg], [PN[gb_]])
                        for kc in range(8):
                            mm(P[ub_][:, :], wup[b][j][:, kc, fh * 128:(fh + 1) * 128], h2T[:, kc, tgs], kc == 0, kc == 7,
                               ["wup%d" % b, "h2T_%d" % tg], [PN[ub_]])
                        if fh == 0:
                            gk = gcnt[0] % 4
                            gcnt[0] += 1
                            dma("sp", gwb[gk][:, :], bass.AP(gdr, e_ * S_LEN + tg * 512, [[0, 128], [1, 512]]),
                                reads=["gdr"], writes=["gwb%d" % gk])
                        act(sil[k2][:, :], P[gb_][:, :], AF.Silu, [PN[gb_]], ["sil%d" % k2])
                        tt("dve", t1m[k2][:, :], sil[k2][:, :], P[ub_][:, :], ALU.mult, ["sil%d" % k2, PN[ub_]], ["t1m%d" % k2])
                        tt("dve", hid[hbi][:, j, fh, :], t1m[k2][:, :], gwb[gk][:, :], ALU.mult, ["t1m%d" % k2, "gwb%d" % gk],
                           ["hid%d" % hbi])
                for tt_ in range(4):
                    t = tg * 4 + tt_
                    for ch in range(2):
                        abk = 4 + (tt_ * 2 + ch) % 2
                        n_acc = EG * 2
                        a_i = 0
                        for j in range(EG):
                            for fh in range(2):
                                mm(P[abk][:, :], hid[hbi][:, j, fh, tt_ * 128:(tt_ + 1) * 128],
                                   wdn[b][j][:, fh, ch * 512:(ch + 1) * 512], a_i == 0, a_i == n_acc - 1,
                                   ["hid%d" % hbi, "wdn%d" % b], [PN[abk]])
                                a_i += 1
                        tt("dve", X[:, t, ch * 512:(ch + 1) * 512], X[:, t, ch * 512:(ch + 1) * 512], P[abk][:, :], ALU.add,
                           ["X%d" % t, PN[abk]], ["X%d" % t])
        for t in range(NT):
            final_ops.append(dma("sp", out_d[t * 128:(t + 1) * 128, :], X[:, t, :], reads=["X%d" % t]))

        S.emit(es, final_wait_ops=final_ops)
    return nc


def make_consts():
    ident = np.eye(128, dtype=np.float32).astype(ml_dtypes.bfloat16)
    j = np.arange(128)[:, None]
    i = np.arange(128)[None, :]
    same = (j // 64) == (i // 64)
    mf = (same & (j <= i)).astype(np.float32)
    mb = (same & (j > i)).astype(np.float32)
    masks = np.concatenate([mf, mb], axis=1).astype(ml_dtypes.bfloat16)
    invf64 = 10000.0 ** (-np.arange(0, 32, 2, dtype=np.float64) / 32)
    invf_hi = invf64.astype(np.float32)
    invf_lo = (invf64 - invf_hi.astype(np.float64)).astype(np.float32)
    invf = np.broadcast_to(np.concatenate([invf_hi, invf_lo])[None, :], (128, 32)).copy()
    sel = np.zeros((32, 32, 128), np.float32)
    for e in range(32):
        sel[e, e, :] = 1.0
    sel = sel.reshape(32, 32 * 128).astype(ml_dtypes.bfloat16)
    lrb = np.zeros((64, 1), np.float32)
    lrb[16, 0] = 1.0
    return {"c_ident": ident, "c_masks": masks, "c_invf": invf, "c_sel": sel, "c_lrbias": lrb}


_NC_CACHE = {}


def make_in_maps(inputs, n_cores=8):
    c = make_consts()
    f = lambda k: np.ascontiguousarray(np.asarray(inputs[k], dtype=np.float32)[0])
    shared = {
        "norm1_gain": f("norm1_gain").reshape(1, D),
        "w_in": f("w_in"),
        "gla_gk_fwd_w": f("gla_gk_fwd_w"), "gla_gk_fwd_b": f("gla_gk_fwd_b").reshape(1, 256),
        "gla_gk_bwd_w": f("gla_gk_bwd_w"), "gla_gk_bwd_b": f("gla_gk_bwd_b").reshape(1, 256),
        "gla_out_gain": f("gla_out_gain").reshape(128, 1),
        "mla_q_gain": f("mla_q_gain").reshape(1, 256), "mla_w_qb": f("mla_w_qb"),
        "mla_kv_gain": f("mla_kv_gain").reshape(1, 128), "mla_w_kvb": f("mla_w_kvb"),
        "q_norm_gain": f("q_norm_gain").reshape(1, 96), "k_norm_gain": f("k_norm_gain").reshape(1, 96),
        "w_out": f("w_out"), "norm2_gain": f("norm2_gain").reshape(1, D),
        "w_router_group": f("w_router_group"), "b_router_group": f("b_router_group").reshape(1, 4),
        "w_router_expert": f("w_router_expert"), "b_router_expert": f("b_router_expert").reshape(1, 32),
        "w_expert_gate": f("w_expert_gate").reshape(32, D, 256),
        "w_expert_up": f("w_expert_up").reshape(32, D, 256),
        "w_expert_down": f("w_expert_down").reshape(32, 256, D),
    }
    shared.update(c)
    x = np.asarray(inputs["x"], dtype=np.float32)
    pos = np.asarray(inputs["positions"]).astype(np.int32)
    maps = []
    for b in range(n_cores):
        m = dict(shared)
        m["x"] = np.ascontiguousarray(x[b])
        m["pos"] = np.ascontiguousarray(pos[b].reshape(NT, 128).T)
        maps.append(m)
    return maps


def kernel(**inputs):
    if "nc" not in _NC_CACHE:
        _NC_CACHE["nc"] = build()
    nc = _NC_CACHE["nc"]
    maps = make_in_maps(inputs, 8)
    res = run_bass_kernel_spmd(nc, maps, core_ids=list(range(8)))
    out = np.stack([np.asarray(r["out"], dtype=np.float32) for r in res.results], axis=0)
    return out
```
